# Optimizing a Trainium2 kernel written in Bass

```python
import math
import jax, jax.numpy as jnp
from jax import lax
import numpy as np

D_MODEL = 1024
BATCH = 8
SEQ = 4096
DEPTH = 4

MLA_HEADS = 8
Q_RANK = 384
KV_RANK = 256
NOPE_DIM = 128
ROPE_DIM = 64
V_DIM = 128
ROPE_THETA = 10000.0
Q_BLOCK = 128
SSM_HEADS = 32
SSM_HEAD_DIM = 64
D_INNER = SSM_HEADS * SSM_HEAD_DIM
SSM_GROUPS = 8
D_STATE = 128
CONV_K = 4
SSD_CHUNK = 256
CONV_CH = D_INNER + 2 * SSM_GROUPS * D_STATE
XA_HEADS = 4
XA_HEAD_DIM = 256
MEM_TOKENS = 256
N_BRANCHES = 3
N_EXPERTS = 16
N_EXPERT_GROUPS = 4
EXPERTS_PER_GROUP = N_EXPERTS // N_EXPERT_GROUPS
TOP_K = 2
D_EXPERT = 512
MOE_BLOCK = 128
DN_ALPHA = (2 * DEPTH) ** 0.25
DN_BETA = (8 * DEPTH) ** -0.25
NORM_EPS = 1e-5
RMS_EPS = 1e-6
IN_SIZES = (Q_RANK, KV_RANK + ROPE_DIM, D_INNER, CONV_CH, SSM_HEADS, XA_HEADS * XA_HEAD_DIM, N_BRANCHES * D_MODEL)
N_IN = Q_RANK + KV_RANK + ROPE_DIM + D_INNER + CONV_CH + SSM_HEADS + XA_HEADS * XA_HEAD_DIM + N_BRANCHES * D_MODEL

kernel_name = "hybrid_mla_ssd_memxattn_grouped_moe_deepnorm"

F32 = jnp.float32


def layer_norm(x, g, b):
    xf = x.astype(F32)
    mu = xf.mean(-1, keepdims=True)
    var = jnp.square(xf - mu).mean(-1, keepdims=True)
    return ((xf - mu) * lax.rsqrt(var + NORM_EPS) * g + b).astype(x.dtype)


def rms_norm(x, g):
    xf = x.astype(F32)
    return (xf * lax.rsqrt(jnp.mean(xf * xf, -1, keepdims=True) + RMS_EPS) * g).astype(x.dtype)


def rope_tables(positions):
    inv = ROPE_THETA ** (-jnp.arange(0, ROPE_DIM, 2, dtype=F32) / ROPE_DIM)
    ang = positions.astype(F32)[..., None] * inv
    return jnp.cos(ang), jnp.sin(ang)


def apply_rope(t, cos, sin):
    t1, t2 = jnp.split(t.astype(F32), 2, axis=-1)
    return jnp.concatenate([t1 * cos - t2 * sin, t1 * sin + t2 * cos], axis=-1).astype(t.dtype)


def mla_branch(dq, dkv, cos, sin, q_norm, w_uq, kv_norm, w_ukv):
    bsz, seq, _ = dq.shape
    c_q = rms_norm(dq, q_norm)
    q = (c_q @ w_uq).reshape(bsz, seq, MLA_HEADS, NOPE_DIM + ROPE_DIM)
    q_nope, q_rope = q[..., :NOPE_DIM], q[..., NOPE_DIM:]
    q_rope = apply_rope(q_rope, cos[:, :, None], sin[:, :, None])
    c_kv = rms_norm(dkv[..., :KV_RANK], kv_norm)
    k_rope = apply_rope(dkv[..., KV_RANK:], cos, sin)
    w_uk, w_uv = w_ukv[..., :NOPE_DIM], w_ukv[..., NOPE_DIM:]
    q_lat = jnp.einsum('bshn,chn->bshc', q_nope, w_uk)
    nb = seq // Q_BLOCK

    def to_blocks(t):
        return jnp.moveaxis(t.reshape(bsz, nb, Q_BLOCK, *t.shape[2:]), 1, 0)

    key_pos = jnp.arange(seq)
    scale = (NOPE_DIM + ROPE_DIM) ** -0.5

    def attend_block(args):
        ql, qr, blk = args
        s = (jnp.einsum('bqhc,bkc->bhqk', ql, c_kv, preferred_element_type=F32)
             + jnp.einsum('bqhr,bkr->bhqk', qr, k_rope, preferred_element_type=F32))
        q_pos = blk * Q_BLOCK + jnp.arange(Q_BLOCK)
        causal = key_pos[None, :] <= q_pos[:, None]
        p = jax.nn.softmax(jnp.where(causal, s * scale, -jnp.inf), axis=-1).astype(c_kv.dtype)
        return jnp.einsum('bhqk,bkc->bqhc', p, c_kv)

    o_lat = lax.map(attend_block, (to_blocks(q_lat), to_blocks(q_rope), jnp.arange(nb)))
    o_lat = jnp.moveaxis(o_lat, 0, 1).reshape(bsz, seq, MLA_HEADS, KV_RANK)
    o = jnp.einsum('bshc,chv->bshv', o_lat, w_uv)
    return o.reshape(bsz, seq, MLA_HEADS * V_DIM)


def causal_depthwise_conv(u, w, b):
    out = lax.conv_general_dilated(u, w[:, None, :].astype(u.dtype), window_strides=(1,),
                                   padding=[(CONV_K - 1, 0)], dimension_numbers=('NWC', 'WIO', 'NWC'),
                                   feature_group_count=u.shape[-1])
    return out + b


def ssd_chunked_scan(xs, dt, a, bm, cm):
    bsz, seq, nh, hp = xs.shape
    rep = nh // SSM_GROUPS
    pad = (-seq) % SSD_CHUNK
    nc = (seq + pad) // SSD_CHUNK

    def chunks(t):
        t = jnp.pad(t.astype(F32), [(0, 0), (0, pad)] + [(0, 0)] * (t.ndim - 2))
        return jnp.moveaxis(t.reshape(bsz, nc, SSD_CHUNK, *t.shape[2:]), 1, 0)

    x_dt = chunks((xs.astype(F32) * dt[..., None]).reshape(bsz, seq, SSM_GROUPS, rep, hp))
    da = chunks((dt * a).reshape(bsz, seq, SSM_GROUPS, rep))
    bc, cc = chunks(bm), chunks(cm)
    causal = jnp.tril(jnp.ones((SSD_CHUNK, SSD_CHUNK), dtype=bool))[None, :, :, None, None]

    def step(state, inp):
        xq, daq, bq, cq = inp
        acum = jnp.cumsum(daq, axis=1)
        seg = acum[:, :, None] - acum[:, None, :]
        decay = jnp.exp(jnp.where(causal, seg, -jnp.inf))
        cb = jnp.einsum('blgn,bsgn->blsg', cq, bq)
        y = jnp.einsum('blsg,blsgr,bsgrp->blgrp', cb, decay, xq)
        y = y + jnp.einsum('blgn,bgrpn,blgr->blgrp', cq, state, jnp.exp(acum))
        a_end = acum[:, -1]
        to_end = jnp.exp(a_end[:, None] - acum)
        state = state * jnp.exp(a_end)[..., None, None] + jnp.einsum('bsgn,bsgr,bsgrp->bgrpn', bq, to_end, xq)
        return state, y

    state0 = jnp.zeros((bsz, SSM_GROUPS, rep, hp, D_STATE), F32)
    _, y = lax.scan(step, state0, (x_dt, da, bc, cc))
    return jnp.moveaxis(y, 0, 1).reshape(bsz, nc * SSD_CHUNK, nh, hp)[:, :seq]


def ssd_branch(z, xbc, dt_raw, conv_w, conv_b, dt_bias, a_log, d_skip, norm_g):
    bsz, seq, _ = z.shape
    xbc = jax.nn.silu(causal_depthwise_conv(xbc, conv_w, conv_b))
    gn = SSM_GROUPS * D_STATE
    xs = xbc[..., :D_INNER].reshape(bsz, seq, SSM_HEADS, SSM_HEAD_DIM)
    bm = xbc[..., D_INNER:D_INNER + gn].reshape(bsz, seq, SSM_GROUPS, D_STATE)
    cm = xbc[..., D_INNER + gn:].reshape(bsz, seq, SSM_GROUPS, D_STATE)
    dt = jax.nn.softplus(dt_raw.astype(F32) + dt_bias.astype(F32))
    a = -jnp.exp(a_log.astype(F32))
    y = ssd_chunked_scan(xs, dt, a, bm, cm)
    y = y + xs.astype(F32) * d_skip.astype(F32)[:, None]
    y = y.reshape(bsz, seq, D_INNER) * jax.nn.silu(z.astype(F32))
    yg = y.reshape(bsz, seq, SSM_GROUPS, D_INNER // SSM_GROUPS)
    yg = yg * lax.rsqrt(jnp.mean(yg * yg, -1, keepdims=True) + RMS_EPS)
    return (yg.reshape(bsz, seq, D_INNER) * norm_g).astype(z.dtype)


def memory_branch(q_in, mem, w_mem_kv):
    bsz, seq, _ = q_in.shape
    q = q_in.reshape(bsz, seq, XA_HEADS, XA_HEAD_DIM)
    kv = (mem @ w_mem_kv).reshape(bsz, mem.shape[1], 2, XA_HEADS, XA_HEAD_DIM)
    k, v = kv[:, :, 0], kv[:, :, 1]
    s = jnp.einsum('bshd,bmhd->bhsm', q, k, preferred_element_type=F32) * (XA_HEAD_DIM ** -0.5)
    p = jax.nn.softmax(s, axis=-1).astype(v.dtype)
    return jnp.einsum('bhsm,bmhd->bshd', p, v).reshape(bsz, seq, XA_HEADS * XA_HEAD_DIM)


def moe_ffn(x, router_w, router_bias, w1, w3, w2):
    bsz, seq, d = x.shape
    scores = jax.nn.sigmoid(jnp.einsum('bsd,de->bse', x, router_w, preferred_element_type=F32))
    sel = scores + router_bias.astype(F32)
    grp = sel.reshape(bsz, seq, N_EXPERT_GROUPS, EXPERTS_PER_GROUP)
    group_score = lax.top_k(grp, TOP_K)[0].sum(-1)
    best = jnp.argmax(group_score, axis=-1)
    in_group = jnp.repeat(jax.nn.one_hot(best, N_EXPERT_GROUPS, dtype=F32), EXPERTS_PER_GROUP, axis=-1) > 0
    _, idx = lax.top_k(jnp.where(in_group, sel, -jnp.inf), TOP_K)
    w = jnp.take_along_axis(scores, idx, axis=-1)
    w = w / w.sum(-1, keepdims=True)
    gates = jnp.einsum('bsk,bske->bse', w, jax.nn.one_hot(idx, N_EXPERTS, dtype=F32)).astype(x.dtype)
    nb = seq // MOE_BLOCK

    def to_blocks(t):
        return jnp.moveaxis(t.reshape(bsz, nb, MOE_BLOCK, t.shape[-1]), 1, 0)

    def expert_block(args):
        xb, gb = args
        h = jax.nn.silu(jnp.einsum('bqd,edf->bqef', xb, w1)) * jnp.einsum('bqd,edf->bqef', xb, w3)
        return jnp.einsum('bqef,efd->bqd', h * gb[..., None], w2)

    out = lax.map(expert_block, (to_blocks(x), to_blocks(gates)))
    return jnp.moveaxis(out, 0, 1).reshape(bsz, seq, d)


def setup_inputs(seed: int = 0) -> dict:
    key = jax.random.key(seed)
    ks = jax.random.split(key, 32)
    L = DEPTH

    def normal(k, shape, fan_in, gain=1.0):
        return jax.random.normal(k, shape, F32) * (gain * fan_in ** -0.5)

    def gain_vec(k, shape):
        return 1.0 + 0.05 * jax.random.normal(k, shape, F32)

    def small(k, shape):
        return 0.02 * jax.random.normal(k, shape, F32)

    x = jax.random.normal(ks[0], (BATCH, SEQ, D_MODEL), F32)
    mem = jax.random.normal(ks[1], (BATCH, MEM_TOKENS, D_MODEL), F32)
    start = jax.random.randint(ks[2], (BATCH, 1), 0, 2048, dtype=jnp.int32)
    positions = start + jnp.arange(SEQ, dtype=jnp.int32)[None, :]
    dt0 = jnp.exp(jax.random.uniform(ks[11], (L, SSM_HEADS), F32, math.log(1e-3), math.log(1e-1)))
    dt_bias = dt0 + jnp.log(-jnp.expm1(-dt0))
    a_log = jnp.log(jax.random.uniform(ks[12], (L, SSM_HEADS), F32, 1.0, 16.0))
    return {
        "x": x,
        "mem": mem,
        "positions": positions,
        "w_in": normal(ks[3], (L, D_MODEL, N_IN), D_MODEL),
        "q_norm": gain_vec(ks[4], (L, Q_RANK)),
        "w_uq": normal(ks[5], (L, Q_RANK, MLA_HEADS * (NOPE_DIM + ROPE_DIM)), Q_RANK),
        "kv_norm": gain_vec(ks[6], (L, KV_RANK)),
        "w_ukv": normal(ks[7], (L, KV_RANK, MLA_HEADS, NOPE_DIM + V_DIM), KV_RANK),
        "w_proj_a": normal(ks[8], (L, MLA_HEADS * V_DIM, D_MODEL), MLA_HEADS * V_DIM, DN_BETA),
        "conv_w": normal(ks[9], (L, CONV_K, CONV_CH), CONV_K),
        "conv_b": small(ks[10], (L, CONV_CH)),
        "dt_bias": dt_bias,
        "a_log": a_log,
        "d_skip": 1.0 + 0.1 * jax.random.normal(ks[13], (L, SSM_HEADS), F32),
        "ssm_norm": gain_vec(ks[14], (L, D_INNER)),
        "w_proj_b": normal(ks[15], (L, D_INNER, D_MODEL), D_INNER, DN_BETA),
        "w_mem_kv": normal(ks[16], (L, D_MODEL, 2 * XA_HEADS * XA_HEAD_DIM), D_MODEL),
        "w_proj_c": normal(ks[17], (L, XA_HEADS * XA_HEAD_DIM, D_MODEL), XA_HEADS * XA_HEAD_DIM, DN_BETA),
        "w_out": normal(ks[18], (L, D_MODEL, D_MODEL), D_MODEL, DN_BETA),
        "ln1_g": gain_vec(ks[19], (L, D_MODEL)),
        "ln1_b": small(ks[20], (L, D_MODEL)),
        "router_w": normal(ks[21], (D_MODEL, N_EXPERTS), D_MODEL),
        "router_bias": 0.01 * jax.random.normal(ks[22], (N_EXPERTS,), F32),
        "exp_w1": normal(ks[23], (L, N_EXPERTS, D_MODEL, D_EXPERT), D_MODEL),
        "exp_w3": normal(ks[24], (L, N_EXPERTS, D_MODEL, D_EXPERT), D_MODEL),
        "exp_w2": normal(ks[25], (L, N_EXPERTS, D_EXPERT, D_MODEL), D_EXPERT, DN_BETA),
        "ln2_g": gain_vec(ks[26], (L, D_MODEL)),
        "ln2_b": small(ks[27], (L, D_MODEL)),
    }


def reference(x, mem, positions, w_in, q_norm, w_uq, kv_norm, w_ukv, w_proj_a, conv_w, conv_b,
              dt_bias, a_log, d_skip, ssm_norm, w_proj_b, w_mem_kv, w_proj_c, w_out, ln1_g, ln1_b,
              router_w, router_bias, exp_w1, exp_w3, exp_w2, ln2_g, ln2_b):
    cos, sin = rope_tables(positions)
    split_at = [int(i) for i in np.cumsum(IN_SIZES)[:-1]]
    for l in range(DEPTH):
        h = x @ w_in[l]
        dq, dkv, z, xbc, dt_raw, q_mem, g_raw = jnp.split(h, split_at, axis=-1)
        o_a = mla_branch(dq, dkv, cos, sin, q_norm[l], w_uq[l], kv_norm[l], w_ukv[l]) @ w_proj_a[l]
        o_b = ssd_branch(z, xbc, dt_raw, conv_w[l], conv_b[l], dt_bias[l], a_log[l], d_skip[l],
                         ssm_norm[l]) @ w_proj_b[l]
        o_c = memory_branch(q_mem, mem, w_mem_kv[l]) @ w_proj_c[l]
        g = jax.nn.sigmoid(g_raw.astype(F32)).astype(x.dtype)
        merged = (g[..., :D_MODEL] * o_a + g[..., D_MODEL:2 * D_MODEL] * o_b
                  + g[..., 2 * D_MODEL:] * o_c)
        x = layer_norm(DN_ALPHA * x + merged @ w_out[l], ln1_g[l], ln1_b[l])
        x = layer_norm(DN_ALPHA * x + moe_ffn(x, router_w, router_bias, exp_w1[l], exp_w3[l], exp_w2[l]),
                       ln2_g[l], ln2_b[l])
    return x
```

```python
import math
from contextlib import ExitStack

import numpy as np
import concourse.bass as bass
import concourse.mybir as mybir
from concourse.bass_utils import run_bass_kernel_spmd

F32 = mybir.dt.float32
BF16 = mybir.dt.bfloat16
I32 = mybir.dt.int32
AF = mybir.ActivationFunctionType
ALU = mybir.AluOpType
AX = mybir.AxisListType

D = 1024
T = 4096
DEPTH = 4
NT = T // 128
TB = 512
NB = T // TB
H = 8
QR = 384
KVR = 256
NOPE = 128
ROPE = 64
VD = 128
N_IN = 10976
C_DQ = 0
C_DKV = 384
C_KR = 640
C_Z = 704
C_XBC = 2752
C_DT = 6848
C_QM = 6880
C_G = 7904
DN_ALPHA = (2 * DEPTH) ** 0.25
NORM_EPS = 1e-5
RMS_EPS = 1e-6
SCALE_A = (NOPE + ROPE) ** -0.5

EPOCH = 30000
_UID = [0]


def _uid(name):
    _UID[0] += 1
    return f"{name}_{_UID[0]}"


def _runs(ap):
    dims = [(int(st), int(n)) for st, n in ap.ap]
    total = 1
    for st, n in dims:
        total *= n
    run = 1
    exp = 1
    for st, n in reversed(dims[1:] if len(dims) > 1 else dims):
        if st == exp:
            run *= n
            exp *= n
        else:
            break
    return max(total // max(run, 1), 1)


class Prog:
    def __init__(self):
        self.nc = bass.Bass("TRN2", target_bir_lowering=False)
        nc = self.nc
        self.es = ExitStack()
        self.eng = dict(pe=nc.tensor, act=nc.scalar, dve=nc.vector, pool=nc.gpsimd, sp=nc.sync)
        self.cnt = {e: 0 for e in self.eng}
        self.sems = {e: [] for e in self.eng}
        self.waited = {e: {} for e in self.eng}
        self.lastw = {}
        self.readers = {}
        self.dsem = {}
        self.dcnt = {}
        self.nsem = 0
        self.ninst = 0

    def _new_sem(self, name):
        self.nsem += 1
        return self.es.enter_context(self.nc.semaphore(name))

    def _esem(self, e, n):
        ep = (n - 1) // EPOCH
        while len(self.sems[e]) <= ep:
            self.sems[e].append(self._new_sem(f"s_{e}_{len(self.sems[e])}"))
        return self.sems[e][ep], (n - 1) % EPOCH + 1

    def _wait(self, c, ev):
        if ev is None:
            return
        kind, p, n = ev
        if kind == 'e':
            if p == c:
                if c == 'pe' or n > self.cnt[c] or n < self.cnt[c] - 1:
                    return
            if self.waited[c].get(p, 0) >= n:
                return
            sem, val = self._esem(p, n)
            self.eng[c].wait_ge(sem, val)
            self.waited[c][p] = n
        else:
            k = ('d', p)
            if self.waited[c].get(k, 0) >= n:
                return
            self.eng[c].wait_ge(self.dsem[p], 16 * n)
            self.waited[c][k] = n

    def _deps(self, reads, writes):
        deps = []
        for r in reads:
            ev = self.lastw.get(r)
            if ev is not None:
                deps.append(ev)
        for w in writes:
            ev = self.lastw.get(w)
            if ev is not None:
                deps.append(ev)
            rd = self.readers.get(w)
            if rd:
                deps.extend(rd.values())
        return deps

    def _record(self, ev, reads, writes):
        key = (ev[0], ev[1])
        for r in reads:
            self.readers.setdefault(r, {})[key] = ev
        for w in writes:
            self.lastw[w] = ev
            self.readers[w] = {}

    def op(self, e, fn, reads=(), writes=(), inc=True):
        for ev in self._deps(reads, writes):
            self._wait(e, ev)
        inst = fn()
        self.ninst += 1
        if inc:
            self.cnt[e] += 1
            n = self.cnt[e]
            sem, _ = self._esem(e, n)
            inst.then_inc(sem, 1)
            ev = ('e', e, n)
        else:
            ev = ('e', e, self.cnt[e] + 1)
        self._record(ev, reads, writes)
        return ev

    def dma(self, q, out, in_, key, reads=(), writes=()):
        if key not in self.dsem:
            self.dsem[key] = self._new_sem(f"d_{key}")
            self.dcnt[key] = 0
        kres = ('dmakey', key)
        for ev in self._deps(reads, list(writes) + [kres]):
            self._wait(q, ev)
        inst = self.eng[q].dma_start(out=out, in_=in_)
        self.ninst += 1
        self.ndma = getattr(self, 'ndma', 0) + 1
        self.ndesc = getattr(self, 'ndesc', 0) + max(_runs(out), _runs(in_))
        self.dcnt[key] += 1
        inst.then_inc(self.dsem[key], 16)
        ev = ('d', key, self.dcnt[key])
        self._record(ev, reads, list(writes) + [kres])
        return ev

    def barrier(self):
        for c in self.eng:
            for p in self.eng:
                if p != c and self.cnt[p] > 0:
                    self._wait(c, ('e', p, self.cnt[p]))
            for k, m in self.dcnt.items():
                if m > 0:
                    self._wait(c, ('d', k, m))
        self.lastw.clear()
        self.readers.clear()

    def mm(self, out, pairs, wres, reads):
        nc = self.nc
        n = len(pairs)
        for i, (l, r) in enumerate(pairs):
            self.op('pe', lambda l=l, r=r, i=i: nc.tensor.matmul(out, l, r, start=(i == 0), stop=(i == n - 1)),
                    reads=reads if i == 0 else (), writes=[wres], inc=(i == n - 1))
        ev = ('e', 'pe', self.cnt['pe'])
        self._record(ev, reads, ())


class Net:
    def __init__(self, n_layers=DEPTH, debug=None):
        self.P = Prog()
        self.nc = self.P.nc
        self.n_layers = n_layers
        self.debug = debug or {}
        self.dbg_out = {}

    def declare(self):
        nc = self.nc
        L = DEPTH
        def inp(name, shape, dt=F32):
            return nc.dram_tensor(name, list(shape), dt, kind="ExternalInput").ap()
        self.x_in = inp("x", [T, D])
        self.mem_in = inp("mem", [256, D])
        self.pos_in = inp("positions", [1, T], I32)
        self.w_in = inp("w_in", [L, D, N_IN])
        self.q_norm = inp("q_norm", [L, QR])
        self.w_uq = inp("w_uq", [L, QR, H * 192])
        self.kv_norm = inp("kv_norm", [L, KVR])
        self.w_ukv = inp("w_ukv", [L, KVR, H, 256])
        self.w_proj_a = inp("w_proj_a", [L, D, D])
        self.conv_w = inp("conv_w", [L, 4, 4096])
        self.conv_b = inp("conv_b", [L, 4096])
        self.dt_bias = inp("dt_bias", [L, 32])
        self.a_log = inp("a_log", [L, 32])
        self.d_skip = inp("d_skip", [L, 32])
        self.ssm_norm = inp("ssm_norm", [L, 2048])
        self.w_proj_b = inp("w_proj_b", [L, 2048, D])
        self.w_mem_kv = inp("w_mem_kv", [L, D, 2048])
        self.w_proj_c = inp("w_proj_c", [L, D, D])
        self.w_out = inp("w_out", [L, D, D])
        self.ln1_g = inp("ln1_g", [L, D])
        self.ln1_b = inp("ln1_b", [L, D])
        self.router_w = inp("router_w", [D, 16])
        self.router_bias = inp("router_bias", [1, 16])
        self.exp_w1 = inp("exp_w1", [L, 16, D, 512])
        self.exp_w3 = inp("exp_w3", [L, 16, D, 512])
        self.exp_w2 = inp("exp_w2", [L, 16, 512, D])
        self.ln2_g = inp("ln2_g", [L, D])
        self.ln2_b = inp("ln2_b", [L, D])
        self.inv_freq = inp("inv_freq", [64, 1])
        self.y_out = nc.dram_tensor("y", [T, D], F32, kind="ExternalOutput").ap()

        def scr(name, shape, dt):
            return nc.dram_tensor(name, list(shape), dt, kind="Internal").ap()
        self.xT_d = scr("xT_d", [128, 8, T], BF16)
        self.x1T_d = scr("x1T_d", [128, 8, T], BF16)
        self.xa_d = scr("xa_d", [T, D], F32)
        self.x1_d = scr("x1_d", [T, D], F32)
        self.mA_d = scr("mA_d", [128, 8, T], BF16)
        self.mB_d = scr("mB_d", [128, 8, T], BF16)
        self.mC_d = scr("mC_d", [128, 8, T], BF16)
        self.xsB_d = scr("xsB_d", [T, 3072], BF16)
        self.BCT_d = scr("BCT_d", [128, 16, T], BF16)
        self.ynT_d = scr("ynT_d", [128, 16, T], BF16)
        self.gT_d = scr("gT_d", [16, T], F32)
        self.cos_d = scr("cos_d", [64, T], F32)
        self.sin_d = scr("sin_d", [64, T], F32)

    def dbg(self, name, shape, dt=F32):
        ap = self.nc.dram_tensor(name, list(shape), dt, kind="ExternalOutput").ap()
        self.dbg_out[name] = ap
        return ap


def _setup_globals(self):
    P, nc = self.P, self.nc
    es = P.es
    sb = lambda name, shape, dt: es.enter_context(nc.sbuf_tensor(_uid(name), shape, dt))
    self.ps = [es.enter_context(nc.psum_tensor(f"ps{i}", [128, 512], F32)) for i in range(7)]
    self.psb = es.enter_context(nc.psum_tensor("psb", [128, 1024], BF16))
    self.ident_bf = sb("ident_bf", [128, 128], BF16)
    self.ident_f = sb("ident_f", [128, 128], F32)
    self.ones_bf = sb("ones_bf", [128, 128], BF16)
    self.ones_f = sb("ones_f", [128, 128], F32)
    self.mask_le = sb("mask_le", [128, 128], BF16)
    self.tri_le_f = sb("tri_le_f", [128, 128], F32)
    self.tri_gt_f = sb("tri_gt_f", [128, 128], F32)
    g = nc.gpsimd
    for tl, val in ((self.ident_bf, 0.0), (self.ident_f, 0.0)):
        P.op('pool', lambda tl=tl: g.memset(tl[:], 0.0), writes=[('const', tl.name)])
        P.op('pool', lambda tl=tl: g.affine_select(out=tl[:], in_=tl[:], pattern=[[-1, 128]],
                                                   compare_op=ALU.not_equal, fill=1.0, base=0,
                                                   channel_multiplier=1), writes=[('const', tl.name)])
    P.op('pool', lambda: g.memset(self.ones_bf[:], 1.0), writes=[('const', 'ones_bf')])
    P.op('pool', lambda: g.memset(self.ones_f[:], 1.0), writes=[('const', 'ones_f')])
    for tl in (self.mask_le, self.tri_le_f):
        P.op('pool', lambda tl=tl: g.memset(tl[:], 1.0), writes=[('const', tl.name)])
        P.op('pool', lambda tl=tl: g.affine_select(out=tl[:], in_=tl[:], pattern=[[1, 128]],
                                                   compare_op=ALU.is_ge, fill=0.0, base=0,
                                                   channel_multiplier=-1), writes=[('const', tl.name)])
    P.op('pool', lambda: g.memset(self.tri_gt_f[:], 1.0), writes=[('const', 'tri_gt_f')])
    P.op('pool', lambda: g.affine_select(out=self.tri_gt_f[:], in_=self.tri_gt_f[:], pattern=[[-1, 128]],
                                         compare_op=ALU.is_gt, fill=0.0, base=0,
                                         channel_multiplier=1), writes=[('const', 'tri_gt_f')])
    self.wkey = 0
    P.barrier()


def _load_w(self, dst, src, tok, nk, q='pool'):
    P = self.P
    for kc in range(nk):
        key = f"w{self.wkey % 8}"
        self.wkey += 1
        P.dma(q, out=dst(kc), in_=src(kc), key=key, writes=[(tok, kc)])


def _toks(tok, nk):
    return [(tok, kc) for kc in range(nk)]


Net.setup_globals = _setup_globals
Net.load_w = _load_w


def _prologue(self):
    P, nc = self.P, self.nc
    with ExitStack() as es:
        sb = lambda name, shape, dt: es.enter_context(nc.sbuf_tensor(_uid(name), shape, dt))
        posi = sb("posi", [64, T], I32)
        ang = sb("ang", [64, T], F32)
        red = sb("red", [64, T], F32)
        tab = sb("tab", [64, T], F32)
        invf = sb("invf", [64, 1], F32)
        negpi = sb("negpi", [64, 1], F32)
        P.dma('sp', out=posi[:], in_=self.pos_in[0, :].partition_broadcast(64), key="ld0", writes=['posi'])
        P.dma('sp', out=invf[:], in_=self.inv_freq[:, :], key="ld1", writes=['invf'])
        P.op('dve', lambda: nc.vector.memset(negpi[:], -math.pi), writes=['negpi'])
        P.op('dve', lambda: nc.vector.tensor_copy(ang[:], posi[:]), reads=['posi'], writes=['ang'])
        P.op('dve', lambda: nc.vector.tensor_scalar(ang[:], ang[:], invf[:, 0:1], None, op0=ALU.mult),
             reads=['invf'], writes=['ang'])
        ki = sb("ki", [64, T], I32)
        kf = sb("kf", [64, T], F32)
        C1 = 6.28125
        C2 = 2 * math.pi - C1
        P.op('dve', lambda: nc.vector.tensor_scalar(red[:], ang[:], 1.0 / (2 * math.pi), None, op0=ALU.mult),
             reads=['ang'], writes=['red'])
        P.op('dve', lambda: nc.vector.tensor_copy(ki[:], red[:]), reads=['red'], writes=['ki'])
        P.op('dve', lambda: nc.vector.tensor_copy(kf[:], ki[:]), reads=['ki'], writes=['kf'])
        P.op('dve', lambda: nc.vector.scalar_tensor_tensor(out=red[:], in0=kf[:], scalar=-C1, in1=ang[:],
                                                           op0=ALU.mult, op1=ALU.add), reads=['kf', 'ang'], writes=['red'])
        P.op('dve', lambda: nc.vector.scalar_tensor_tensor(out=red[:], in0=kf[:], scalar=-C2, in1=red[:],
                                                           op0=ALU.mult, op1=ALU.add), reads=['kf', 'red'], writes=['red'])

        def wrap_sin(shift):
            P.op('dve', lambda: nc.vector.tensor_scalar(ang[:], red[:], shift, None, op0=ALU.add),
                 reads=['red'], writes=['ang'])
            P.op('dve', lambda: nc.vector.tensor_scalar(kf[:], ang[:], math.pi, 2 * math.pi, op0=ALU.is_gt, op1=ALU.mult),
                 reads=['ang'], writes=['kf'])
            P.op('dve', lambda: nc.vector.tensor_tensor(ang[:], ang[:], kf[:], op=ALU.subtract),
                 reads=['ang', 'kf'], writes=['ang'])
            P.op('dve', lambda: nc.vector.tensor_scalar(ang[:], ang[:], -math.pi, math.pi, op0=ALU.max, op1=ALU.min),
                 reads=['ang'], writes=['ang'])
            P.op('act', lambda: nc.scalar.activation(out=tab[:], in_=ang[:], func=AF.Sin),
                 reads=['ang'], writes=['tab'])
        wrap_sin(0.0)
        P.op('dve', lambda: nc.vector.tensor_scalar(tab[0:32, :], tab[0:32, :], -1.0, None, op0=ALU.mult),
             reads=['tab'], writes=['tab'])
        P.dma('sp', out=self.sin_d[:, :], in_=tab[:], key="st0", reads=['tab'], writes=['sin_d'])
        wrap_sin(0.5 * math.pi)
        P.dma('sp', out=self.cos_d[:, :], in_=tab[:], key="st1", reads=['tab'], writes=['cos_d'])
        xin = [sb(f"xin{i}", [128, D], F32) for i in range(2)]
        xbf = [sb(f"xbf{i}", [128, D], BF16) for i in range(2)]
        stg = [sb(f"stg{i}", [128, 8, TB], BF16) for i in range(2)]
        for t in range(NT):
            i = t % 2
            b, tt = divmod(t, 4)
            P.dma('sp', out=xin[i][:], in_=self.x_in[t * 128:(t + 1) * 128, :], key=f"ld{2 + i}", writes=[('xin', i)])
            P.op('dve', lambda i=i: nc.vector.tensor_copy(xbf[i][:], xin[i][:]), reads=[('xin', i)], writes=[('xbf', i)])
            for kc in range(8):
                P.op('pe', lambda i=i, kc=kc: nc.tensor.transpose(self.psb[:, kc * 128:(kc + 1) * 128],
                                                                   xbf[i][:, kc * 128:(kc + 1) * 128], self.ident_bf[:]),
                     reads=[('xbf', i)], writes=['psb'], inc=(kc == 7))
            P.op('act', lambda b=b, tt=tt: nc.scalar.copy(stg[b % 2][:, :, tt * 128:(tt + 1) * 128],
                                                         self.psb[:].rearrange("p (k n) -> p k n", k=8)),
                 reads=['psb'], writes=[('stg', b % 2)])
            if tt == 3:
                P.dma('sp', out=self.xT_d[:, :, b * TB:(b + 1) * TB], in_=stg[b % 2][:], key=f"st{2 + b % 2}",
                      reads=[('stg', b % 2)], writes=[('xT_d', b)])
    P.barrier()


Net.prologue = _prologue


def _pass_mla(self, l):
    P, nc = self.P, self.nc
    ps = self.ps
    with ExitStack() as es:
        sb = lambda name, shape, dt: es.enter_context(nc.sbuf_tensor(_uid(name), shape, dt))
        Wmla = sb("Wmla", [128, 8, 640], BF16)
        Wkr = sb("Wkr", [128, 8, 128], BF16)
        WgA = sb("WgA", [128, 8, D], BF16)
        wuq = sb("wuq", [128, 3, H * 192], BF16)
        wuqsw = sb("wuqsw", [128, 3, H, 64], BF16)
        wukv = sb("wukv", [128, 2, H, 256], BF16)
        wukT = sb("wukT", [128, H, 256], BF16)
        wpa = sb("wpa", [128, 8, D], BF16)
        gq = sb("gq", [128, QR], F32)
        gkv = sb("gkv", [128, KVR], F32)
        w_in = self.w_in
        rows = lambda kc: slice(kc * 128, (kc + 1) * 128)
        self.load_w(lambda kc: Wmla[:, kc, :], lambda kc: w_in[l, rows(kc), 0:640], 'Wmla', 8)
        self.load_w(lambda kc: Wkr[:, kc, 0:64], lambda kc: w_in[l, rows(kc), C_KR:C_KR + 64], 'Wkr', 8)
        self.load_w(lambda kc: wuq[:, kc, :], lambda kc: self.w_uq[l, rows(kc), :], 'wuq', 3)
        self.load_w(lambda kc: wukv[:, kc, :, :], lambda kc: self.w_ukv[l, rows(kc), :, :], 'wukv', 2)
        P.dma('sp', out=gq[:], in_=self.q_norm[l, :].partition_broadcast(128), key="ld0", writes=['gq'])
        P.dma('sp', out=gkv[:], in_=self.kv_norm[l, :].partition_broadcast(128), key="ld1", writes=['gkv'])
        self.load_w(lambda kc: wpa[:, kc, :], lambda kc: self.w_proj_a[l, rows(kc), :], 'wpa', 8)
        self.load_w(lambda kc: WgA[:, kc, :], lambda kc: w_in[l, rows(kc), C_G:C_G + D], 'WgA', 8)
        for kc in range(8):
            P.op('dve', lambda kc=kc: nc.vector.tensor_copy(Wkr[:, kc, 64:96], Wkr[:, kc, 32:64]),
                 reads=[('Wkr', kc)], writes=[('Wkrs', kc)])
            P.op('dve', lambda kc=kc: nc.vector.tensor_copy(Wkr[:, kc, 96:128], Wkr[:, kc, 0:32]),
                 reads=[('Wkr', kc)], writes=[('Wkrs', kc)])
        for kc in range(3):
            v = wuq[:, kc, :].rearrange("p (h c) -> p h c", h=H)
            P.op('dve', lambda kc=kc, v=v: nc.vector.tensor_copy(wuqsw[:, kc, :, 0:32], v[:, :, 160:192]),
                 reads=[('wuq', kc)], writes=[('wuqsw', kc)])
            P.op('dve', lambda kc=kc, v=v: nc.vector.tensor_copy(wuqsw[:, kc, :, 32:64], v[:, :, 128:160]),
                 reads=[('wuq', kc)], writes=[('wuqsw', kc)])
        for h in range(H):
            for cc in range(2):
                P.op('pe', lambda h=h, cc=cc: nc.tensor.transpose(self.psb[:, cc * 128:(cc + 1) * 128],
                                                                   wukv[:, cc, h, 0:128], self.ident_bf[:]),
                     reads=[('wukv', cc)], writes=['psb'], inc=(cc == 1))
            P.op('act', lambda h=h: nc.scalar.copy(wukT[:, h, :], self.psb[:, 0:256]), reads=['psb'], writes=[('wukT', h)])

        ckvT = sb("ckvT", [128, 2, T], BF16)
        krT = sb("krT", [64, T], BF16)
        ckvTM = sb("ckvTM", [128, NT, KVR], BF16)
        xblk = [sb(f"xblk{i}", [128, 8, TB], BF16) for i in range(2)]
        cosb = [sb(f"cosb{i}", [64, TB], F32) for i in range(2)]
        sinb = [sb(f"sinb{i}", [64, TB], F32) for i in range(2)]
        cqTM = sb("cqTM", [128, QR], BF16)
        cqT = sb("cqT", [128, 3, TB], BF16)
        junk = sb("junk", [128, 512], F32)
        ss = sb("ss", [128, 8], F32)
        qn = sb("qn", [128, TB], BF16)
        qlat = sb("qlat", [128, H, 2, TB], BF16)
        qrope = sb("qrope", [64, H, TB], BF16)
        rtmp = sb("rtmp", [64, TB], F32)
        rtmp2 = sb("rtmp2", [64, TB], F32)
        PT = [sb(f"PT{i}", [128, TB], BF16) for i in range(3)]
        rs = sb("rs", [128, TB], F32)
        olat = sb("olat", [128, 2, TB], BF16)
        oT = sb("oT", [128, H, TB], BF16)
        sig = sb("sig", [128, TB], F32)
        mT = sb("mT", [128, 8, TB], BF16)
        W = lambda name, n: _toks(name, n)

        for b in range(NB):
            i2 = b % 2
            t0 = b * TB
            P.dma('sp', out=xblk[i2][:], in_=self.xT_d[:, :, t0:t0 + TB], key=f"ld{2 + i2}", writes=[('xblk', i2)])
            P.dma('sp', out=cosb[i2][:], in_=self.cos_d[:, t0:t0 + TB], key=f"ld{4 + i2}", writes=[('cosb', i2)])
            P.dma('sp', out=sinb[i2][:], in_=self.sin_d[:, t0:t0 + TB], key=f"ld{6 + i2}", writes=[('sinb', i2)])
            xb = xblk[i2]
            for tt in range(4):
                t = b * 4 + tt
                cols = slice(tt * 128, (tt + 1) * 128)
                P.mm(ps[5][:, 0:512], [(xb[:, kc, cols], Wmla[:, kc, 0:512]) for kc in range(8)], 'ps5',
                     [('xblk', i2)] + W('Wmla', 8))
                P.mm(ps[6][:, 0:128], [(xb[:, kc, cols], Wmla[:, kc, 512:640]) for kc in range(8)], 'ps6',
                     [('xblk', i2)] + W('Wmla', 8))
                P.op('dve', lambda: nc.vector.memset(ss[:, 0:3], 0.0), writes=['ss0', 'ss1', 'ss2'])
                P.op('act', lambda: nc.scalar.activation(out=junk[:, 0:384], in_=ps[5][:, 0:384], func=AF.Square,
                                                         accum_out=ss[:, 0:1]), reads=['ps5'], writes=['junk', 'ss0'])
                P.op('act', lambda: nc.scalar.activation(out=junk[:, 384:512], in_=ps[5][:, 384:512], func=AF.Square,
                                                         accum_out=ss[:, 1:2]), reads=['ps5'], writes=['junk', 'ss1'])
                P.op('act', lambda: nc.scalar.activation(out=junk[:, 0:128], in_=ps[6][:, 0:128], func=AF.Square,
                                                         accum_out=ss[:, 2:3]), reads=['ps6'], writes=['junk', 'ss2'])
                P.op('dve', lambda: nc.vector.tensor_scalar(ss[:, 4:5], ss[:, 0:1], 1.0 / QR, RMS_EPS, op0=ALU.mult, op1=ALU.add),
                     reads=['ss0'], writes=['ss4'])
                P.op('act', lambda: nc.scalar.activation(out=ss[:, 6:7], in_=ss[:, 4:5], func=AF.Sqrt), reads=['ss4'], writes=['ss6'])
                P.op('dve', lambda: nc.vector.reciprocal(ss[:, 4:5], ss[:, 6:7]), reads=['ss6'], writes=['ss4'])
                P.op('dve', lambda: nc.vector.tensor_tensor(ss[:, 5:6], ss[:, 1:2], ss[:, 2:3], op=ALU.add),
                     reads=['ss1', 'ss2'], writes=['ss5'])
                P.op('dve', lambda: nc.vector.tensor_scalar(ss[:, 5:6], ss[:, 5:6], 1.0 / KVR, RMS_EPS, op0=ALU.mult, op1=ALU.add),
                     reads=['ss5'], writes=['ss5'])
                P.op('act', lambda: nc.scalar.activation(out=ss[:, 7:8], in_=ss[:, 5:6], func=AF.Sqrt), reads=['ss5'], writes=['ss7'])
                P.op('dve', lambda: nc.vector.reciprocal(ss[:, 5:6], ss[:, 7:8]), reads=['ss7'], writes=['ss5'])
                P.op('dve', lambda: nc.vector.scalar_tensor_tensor(out=cqTM[:], in0=ps[5][:, 0:384], scalar=ss[:, 4:5],
                                                                   in1=gq[:], op0=ALU.mult, op1=ALU.mult),
                     reads=['ps5', 'ss4', 'gq'], writes=['cqTM'])
                P.op('dve', lambda t=t: nc.vector.scalar_tensor_tensor(out=ckvTM[:, t, 0:128], in0=ps[5][:, 384:512],
                                                                       scalar=ss[:, 5:6], in1=gkv[:, 0:128],
                                                                       op0=ALU.mult, op1=ALU.mult),
                     reads=['ps5', 'ss5', 'gkv'], writes=[('ckvTM', t)])
                P.op('dve', lambda t=t: nc.vector.scalar_tensor_tensor(out=ckvTM[:, t, 128:256], in0=ps[6][:, 0:128],
                                                                       scalar=ss[:, 5:6], in1=gkv[:, 128:256],
                                                                       op0=ALU.mult, op1=ALU.mult),
                     reads=['ps6', 'ss5', 'gkv'], writes=[('ckvTM', t)])
                for j in range(3):
                    P.op('pe', lambda j=j: nc.tensor.transpose(self.psb[:, j * 128:(j + 1) * 128],
                                                               cqTM[:, j * 128:(j + 1) * 128], self.ident_bf[:]),
                         reads=['cqTM'], writes=['psb'], inc=False)
                for j in range(2):
                    P.op('pe', lambda j=j, t=t: nc.tensor.transpose(self.psb[:, (3 + j) * 128:(4 + j) * 128],
                                                                    ckvTM[:, t, j * 128:(j + 1) * 128], self.ident_bf[:]),
                         reads=[('ckvTM', t)], writes=['psb'], inc=(j == 1))
                P.op('pool' if False else 'act', lambda cols=cols: nc.scalar.copy(
                    cqT[:, :, cols], self.psb[:, 0:384].rearrange("p (k n) -> p k n", k=3)),
                     reads=['psb'], writes=['cqT'])
                P.op('act', lambda t=t: nc.scalar.copy(
                    ckvT[:, :, t * 128:(t + 1) * 128], self.psb[:, 384:640].rearrange("p (k n) -> p k n", k=2)),
                     reads=['psb'], writes=[('ckvT', t)])
            P.mm(ps[5][0:64, :], [(Wkr[:, kc, 0:64], xb[:, kc, :]) for kc in range(8)], 'ps5',
                 [('xblk', i2)] + W('Wkr', 8))
            P.mm(ps[6][0:64, :], [(Wkr[:, kc, 64:128], xb[:, kc, :]) for kc in range(8)], 'ps6',
                 [('xblk', i2)] + W('Wkrs', 8))

            def rope(psA, psB, outap, rA, rB, wtok):
                P.op('dve', lambda: nc.vector.tensor_tensor(rtmp[:], psB, sinb[i2][:], op=ALU.mult),
                     reads=[rB, ('sinb', i2)], writes=['rtmp'])
                P.op('dve', lambda: nc.vector.tensor_tensor(rtmp2[:], psA, cosb[i2][:], op=ALU.mult),
                     reads=[rA, ('cosb', i2)], writes=['rtmp2'])
                P.op('dve', lambda: nc.vector.tensor_tensor(outap, rtmp[:], rtmp2[:], op=ALU.add),
                     reads=['rtmp', 'rtmp2'], writes=[wtok])
            rope(ps[5][0:64, :], ps[6][0:64, :], krT[:, t0:t0 + TB], 'ps5', 'ps6', ('krT', b))

            for h in range(H):
                c0 = h * 192
                P.mm(ps[5][:, :], [(wuq[:, kc, c0:c0 + 128], cqT[:, kc, :]) for kc in range(3)], 'ps5',
                     ['cqT'] + W('wuq', 3))
                P.op('act', lambda: nc.scalar.copy(qn[:], ps[5][:, :]), reads=['ps5'], writes=['qn'])
                for cc in range(2):
                    pb = ps[6] if cc == 0 else ps[5]
                    pt = 'ps6' if cc == 0 else 'ps5'
                    P.mm(pb[:, :], [(wukT[:, h, cc * 128:(cc + 1) * 128], qn[:])], pt, ['qn', ('wukT', h)])
                    P.op('dve', lambda h=h, cc=cc, pb=pb: nc.vector.tensor_copy(qlat[:, h, cc, :], pb[:, :]),
                         reads=[pt], writes=[('qlat', h)])
                P.mm(ps[6][0:64, :], [(wuq[:, kc, c0 + 128:c0 + 192], cqT[:, kc, :]) for kc in range(3)], 'ps6',
                     ['cqT'] + W('wuq', 3))
                P.mm(ps[5][0:64, :], [(wuqsw[:, kc, h, :], cqT[:, kc, :]) for kc in range(3)], 'ps5',
                     ['cqT'] + W('wuqsw', 3))
                rope(ps[6][0:64, :], ps[5][0:64, :], qrope[:, h, :], 'ps6', 'ps5', ('qrope', h))

            nkt = 4 * b + 4
            pcount = 0
            for h in range(H):
                for kt in range(nkt):
                    d = kt - 4 * b
                    q0 = max(d, 0) * 128
                    qs = slice(q0, TB)
                    si = pcount % 2
                    pi = pcount % 3
                    pcount += 1
                    st = ps[si]
                    kk = slice(kt * 128, (kt + 1) * 128)
                    P.mm(st[:, qs], [(ckvT[:, 0, kk], qlat[:, h, 0, qs]), (ckvT[:, 1, kk], qlat[:, h, 1, qs]),
                                     (krT[:, kk], qrope[:, h, qs])], ('ST', si),
                         [('ckvT', kt), ('krT', kt // 4), ('qlat', h), ('qrope', h)])
                    P.op('act', lambda st=st, qs=qs, pi=pi: nc.scalar.activation(out=PT[pi][:, qs], in_=st[:, qs], func=AF.Exp,
                                                                               scale=SCALE_A),
                         reads=[('ST', si)], writes=[('PT', pi)])
                    if d >= 0:
                        dq = slice(q0, q0 + 128)
                        P.op('pool', lambda pi=pi, dq=dq: nc.gpsimd.tensor_tensor(PT[pi][:, dq], PT[pi][:, dq], self.mask_le[:],
                                                                                op=ALU.mult),
                             reads=[('PT', pi)], writes=[('PT', pi)])
                    first, last = (kt == 0), (kt == nkt - 1)
                    for cc in range(2):
                        P.op('pe', lambda cc=cc, kt=kt, pi=pi, qs=qs, first=first, last=last: nc.tensor.matmul(
                            ps[2 + cc][:, qs], ckvTM[:, kt, cc * 128:(cc + 1) * 128], PT[pi][:, qs], start=first, stop=last),
                             reads=[('PT', pi), ('ckvTM', kt)], writes=[('oacc', cc)], inc=False)
                    P.op('pe', lambda pi=pi, qs=qs, first=first, last=last: nc.tensor.matmul(
                        ps[4][:, qs], self.ones_bf[:], PT[pi][:, qs], start=first, stop=last),
                         reads=[('PT', pi)], writes=['osum'], inc=True)
                    P._record(('e', 'pe', P.cnt['pe']), [('PT', pi), ('ckvTM', kt)], [('oacc', 0), ('oacc', 1)])
                P.op('dve', lambda: nc.vector.reciprocal(rs[:], ps[4][:, :]), reads=['osum'], writes=['rs'])
                for cc in range(2):
                    P.op('dve', lambda cc=cc: nc.vector.tensor_tensor(olat[:, cc, :], ps[2 + cc][:, :], rs[:], op=ALU.mult),
                         reads=[('oacc', cc), 'rs'], writes=['olat'])
                P.mm(ps[5][:, :], [(wukv[:, cc, h, 128:256], olat[:, cc, :]) for cc in range(2)], 'ps5',
                     ['olat'] + W('wukv', 2))
                P.op('act', lambda h=h: nc.scalar.copy(oT[:, h, :], ps[5][:, :]), reads=['ps5'], writes=[('oT', h)])

            for dc in range(8):
                dd = slice(dc * 128, (dc + 1) * 128)
                P.mm(ps[5][:, :], [(WgA[:, kc, dd], xb[:, kc, :]) for kc in range(8)], 'ps5',
                     [('xblk', i2)] + W('WgA', 8))
                P.op('act', lambda: nc.scalar.activation(out=sig[:], in_=ps[5][:, :], func=AF.Sigmoid),
                     reads=['ps5'], writes=['sig'])
                P.mm(ps[6][:, :], [(wpa[:, hh, dd], oT[:, hh, :]) for hh in range(H)], 'ps6',
                     [('oT', hh) for hh in range(H)] + W('wpa', 8))
                P.op('dve', lambda dc=dc: nc.vector.tensor_tensor(mT[:, dc, :], ps[6][:, :], sig[:], op=ALU.mult),
                     reads=['ps6', 'sig'], writes=[('mT', dc)])
            P.dma('sp', out=self.mA_d[:, :, t0:t0 + TB], in_=mT[:], key="st0",
                  reads=[('mT', dc) for dc in range(8)], writes=[('mA_d', b)])
    P.barrier()


Net.pass_mla = _pass_mla


def _pass_mem(self, l):
    P, nc = self.P, self.nc
    ps = self.ps
    SC = 256 ** -0.5
    with ExitStack() as es:
        sb = lambda name, shape, dt: es.enter_context(nc.sbuf_tensor(_uid(name), shape, dt))
        Wqm = sb("Wqm", [128, 8, D], BF16)
        WgC = sb("WgC", [128, 8, D], BF16)
        wpc = sb("wpc", [128, 8, D], BF16)
        KT = sb("KT", [128, 4, 2, 256], BF16)
        V = sb("V", [128, 2, D], BF16)
        rows = lambda kc: slice(kc * 128, (kc + 1) * 128)
        W = lambda name, n: _toks(name, n)
        self.load_w(lambda kc: Wqm[:, kc, :], lambda kc: self.w_in[l, rows(kc), C_QM:C_QM + D], 'Wqm', 8)
        self.load_w(lambda kc: WgC[:, kc, :], lambda kc: self.w_in[l, rows(kc), C_G + 2 * D:C_G + 3 * D], 'WgC', 8)
        self.load_w(lambda kc: wpc[:, kc, :], lambda kc: self.w_proj_c[l, rows(kc), :], 'wpc', 8)
        with ExitStack() as es2:
            sb2 = lambda name, shape, dt: es2.enter_context(nc.sbuf_tensor(_uid(name), shape, dt))
            wmkv = sb2("wmkv", [128, 8, 2048], BF16)
            memT = sb2("memT", [128, 8, 256], BF16)
            mtile = sb2("mtile", [128, D], BF16)
            self.load_w(lambda kc: wmkv[:, kc, :], lambda kc: self.w_mem_kv[l, rows(kc), :], 'wmkv', 8)
            for mt in range(2):
                P.dma('pool', out=mtile[:], in_=self.mem_in[mt * 128:(mt + 1) * 128, :], key="ld0", writes=['mtile'])
                for kc in range(8):
                    P.op('pe', lambda kc=kc: nc.tensor.transpose(self.psb[:, kc * 128:(kc + 1) * 128],
                                                                 mtile[:, kc * 128:(kc + 1) * 128], self.ident_bf[:]),
                         reads=['mtile'], writes=['psb'], inc=(kc == 7))
                P.op('act', lambda mt=mt: nc.scalar.copy(memT[:, :, mt * 128:(mt + 1) * 128],
                                                         self.psb[:].rearrange("p (k n) -> p k n", k=8)),
                     reads=['psb'], writes=['memT'])
            for h in range(4):
                for dc in range(2):
                    c0 = h * 256 + dc * 128
                    P.mm(ps[5][:, 0:256], [(wmkv[:, kc, c0:c0 + 128], memT[:, kc, :]) for kc in range(8)], 'ps5',
                         ['memT'] + W('wmkv', 8))
                    P.op('act', lambda h=h, dc=dc: nc.scalar.copy(KT[:, h, dc, :], ps[5][:, 0:256]), reads=['ps5'], writes=['KT'])
            for mt in range(2):
                for half in range(2):
                    c0 = 1024 + half * 512
                    P.mm(ps[6][:, :], [(memT[:, kc, mt * 128:(mt + 1) * 128], wmkv[:, kc, c0:c0 + 512]) for kc in range(8)],
                         'ps6', ['memT'] + W('wmkv', 8))
                    P.op('dve', lambda mt=mt, half=half: nc.vector.tensor_copy(V[:, mt, half * 512:(half + 1) * 512], ps[6][:, :]),
                         reads=['ps6'], writes=['V'])
            P.barrier()
        xblk = [sb(f"xblk{i}", [128, 8, TB], BF16) for i in range(2)]
        qm = sb("qm", [128, 2, TB], BF16)
        PT = [sb(f"PT{i}", [128, 2, TB], BF16) for i in range(2)]
        rs = sb("rs", [128, TB], F32)
        ocT = sb("ocT", [128, 8, TB], BF16)
        sig = sb("sig", [128, TB], F32)
        mT = sb("mT", [128, 8, TB], BF16)
        for b in range(NB):
            i2 = b % 2
            t0 = b * TB
            P.dma('sp', out=xblk[i2][:], in_=self.xT_d[:, :, t0:t0 + TB], key=f"ld{2 + i2}", writes=[('xblk', i2)])
            xb = xblk[i2]
            for h in range(4):
                pi = h % 2
                for dc in range(2):
                    c0 = h * 256 + dc * 128
                    pb, pt = (ps[5], 'ps5') if dc == 0 else (ps[6], 'ps6')
                    P.mm(pb[:, :], [(Wqm[:, kc, c0:c0 + 128], xb[:, kc, :]) for kc in range(8)], pt,
                         [('xblk', i2)] + W('Wqm', 8))
                    if dc == 0:
                        P.op('act', lambda pb=pb: nc.scalar.copy(qm[:, 0, :], pb[:, :]), reads=[pt], writes=[('qm', 0)])
                    else:
                        P.op('dve', lambda pb=pb: nc.vector.tensor_copy(qm[:, 1, :], pb[:, :]), reads=[pt], writes=[('qm', 1)])
                for mt in range(2):
                    P.mm(ps[mt][:, :], [(KT[:, h, dc, mt * 128:(mt + 1) * 128], qm[:, dc, :]) for dc in range(2)], ('ST', mt),
                         ['KT', ('qm', 0), ('qm', 1)])
                    P.op('act', lambda mt=mt, pi=pi: nc.scalar.activation(out=PT[pi][:, mt, :], in_=ps[mt][:, :], func=AF.Exp, scale=SC),
                         reads=[('ST', mt)], writes=[('PT', pi, mt)])
                for vc in range(2):
                    c0 = h * 256 + vc * 128
                    P.mm(ps[2 + vc][:, :], [(V[:, mt, c0:c0 + 128], PT[pi][:, mt, :]) for mt in range(2)], ('oacc', vc),
                         ['V', ('PT', pi, 0), ('PT', pi, 1)])
                P.mm(ps[4][:, :], [(self.ones_bf[:], PT[pi][:, mt, :]) for mt in range(2)], 'osum',
                     [('PT', pi, 0), ('PT', pi, 1)])
                P.op('dve', lambda: nc.vector.reciprocal(rs[:], ps[4][:, :]), reads=['osum'], writes=['rs'])
                for vc in range(2):
                    P.op('dve', lambda h=h, vc=vc: nc.vector.tensor_tensor(ocT[:, h * 2 + vc, :], ps[2 + vc][:, :], rs[:], op=ALU.mult),
                         reads=[('oacc', vc), 'rs'], writes=[('ocT', h * 2 + vc)])
            for dc in range(8):
                dd = slice(dc * 128, (dc + 1) * 128)
                P.mm(ps[5][:, :], [(WgC[:, kc, dd], xb[:, kc, :]) for kc in range(8)], 'ps5',
                     [('xblk', i2)] + W('WgC', 8))
                P.op('act', lambda: nc.scalar.activation(out=sig[:], in_=ps[5][:, :], func=AF.Sigmoid),
                     reads=['ps5'], writes=['sig'])
                P.mm(ps[6][:, :], [(wpc[:, j, dd], ocT[:, j, :]) for j in range(8)], 'ps6',
                     [('ocT', j) for j in range(8)] + W('wpc', 8))
                P.op('dve', lambda dc=dc: nc.vector.tensor_tensor(mT[:, dc, :], ps[6][:, :], sig[:], op=ALU.mult),
                     reads=['ps6', 'sig'], writes=[('mT', dc)])
            P.dma('sp', out=self.mC_d[:, :, t0:t0 + TB], in_=mT[:], key="st0",
                  reads=[('mT', dc) for dc in range(8)], writes=[('mC_d', b)])
    P.barrier()


Net.pass_mem = _pass_mem


def _pass_ssd_a(self, l):
    P, nc = self.P, self.nc
    ps = self.ps
    with ExitStack() as es:
        sb = lambda name, shape, dt: es.enter_context(nc.sbuf_tensor(_uid(name), shape, dt))
        Wx = sb("Wx", [128, 8, 4096], BF16)
        cw5 = sb("cw5", [5, 4096], F32)
        cwb = sb("cwb", [128, 32, 5], F32)
        rows = lambda kc: slice(kc * 128, (kc + 1) * 128)
        W = lambda name, n: _toks(name, n)
        self.load_w(lambda kc: Wx[:, kc, :], lambda kc: self.w_in[l, rows(kc), C_XBC:C_XBC + 4096], 'Wx', 8)
        P.dma('sp', out=cw5[0:4, :], in_=self.conv_w[l, :, :], key="ld0", writes=['cw5a'])
        P.dma('sp', out=cw5[4:5, :], in_=self.conv_b[l:l + 1, :], key="ld1", writes=['cw5b'])
        for cc in range(32):
            P.op('pe', lambda cc=cc: nc.tensor.transpose(ps[5][:, cc * 5:(cc + 1) * 5], cw5[0:5, cc * 128:(cc + 1) * 128],
                                                         self.ident_f[0:5, 0:5]),
                 reads=['cw5a', 'cw5b'], writes=['ps5'], inc=(cc == 31))
        P.op('act', lambda: nc.scalar.copy(cwb[:].rearrange("p c k -> p (c k)"), ps[5][:, 0:160]), reads=['ps5'], writes=['cwb'])
        xblk = [sb(f"xblk{i}", [128, 8, TB], BF16) for i in range(2)]
        pre = [sb(f"pre{i}", [128, TB + 3], F32) for i in range(2)]
        acc = [sb(f"acc{i}", [128, TB], F32) for i in range(2)]
        carry = sb("carry", [128, 32, 3], F32)
        xbcT = sb("xbcT", [128, 32, TB], BF16)
        tmst = [sb(f"tmst{i}", [128, 3072], BF16) for i in range(2)]
        P.op('dve', lambda: nc.vector.memset(carry[:], 0.0), writes=[('carry', cc) for cc in range(32)])
        n = 0
        for b in range(NB):
            i2 = b % 2
            t0 = b * TB
            P.dma('sp', out=xblk[i2][:], in_=self.xT_d[:, :, t0:t0 + TB], key=f"ld{2 + i2}", writes=[('xblk', i2)])
            xb = xblk[i2]
            for cc in range(32):
                j = n % 2
                n += 1
                pb, pt = (ps[5], 'ps5') if j == 0 else (ps[6], 'ps6')
                pr, ac = pre[j], acc[j]
                P.mm(pb[:, :], [(Wx[:, kc, cc * 128:(cc + 1) * 128], xb[:, kc, :]) for kc in range(8)], pt,
                     [('xblk', i2)] + W('Wx', 8))
                P.op('pool', lambda pr=pr, cc=cc: nc.gpsimd.tensor_copy(pr[:, 0:3], carry[:, cc, :]),
                     reads=[('carry', cc)], writes=[('pre', j)])
                P.op('act', lambda pr=pr, pb=pb: nc.scalar.copy(pr[:, 3:TB + 3], pb[:, :]), reads=[pt], writes=[('pre', j)])
                P.op('pool', lambda pr=pr, cc=cc: nc.gpsimd.tensor_copy(carry[:, cc, :], pr[:, TB:TB + 3]),
                     reads=[('pre', j)], writes=[('carry', cc)])
                P.op('dve', lambda pr=pr, ac=ac, cc=cc: nc.vector.tensor_scalar(ac[:], pr[:, 0:TB], cwb[:, cc, 0:1], cwb[:, cc, 4:5],
                                                                               op0=ALU.mult, op1=ALU.add),
                     reads=[('pre', j), 'cwb'], writes=[('acc', j)])
                for k in range(1, 4):
                    P.op('dve', lambda pr=pr, ac=ac, cc=cc, k=k: nc.vector.scalar_tensor_tensor(
                        out=ac[:], in0=pr[:, k:k + TB], scalar=cwb[:, cc, k:k + 1], in1=ac[:], op0=ALU.mult, op1=ALU.add),
                         reads=[('pre', j), 'cwb'], writes=[('acc', j)])
                P.op('act', lambda ac=ac, cc=cc: nc.scalar.activation(out=xbcT[:, cc, :], in_=ac[:], func=AF.Silu),
                     reads=[('acc', j)], writes=[('xbcT', cc)])
            P.dma('sp', out=self.BCT_d[:, :, t0:t0 + TB], in_=xbcT[:, 16:32, :], key="st0",
                  reads=[('xbcT', cc) for cc in range(16, 32)], writes=[('BCT_d', b)])
            for tt in range(4):
                t = b * 4 + tt
                ti = t % 2
                for r in range(3):
                    for q in range(8):
                        cc = r * 8 + q
                        P.op('pe', lambda cc=cc, q=q, tt=tt: nc.tensor.transpose(self.psb[:, q * 128:(q + 1) * 128],
                                                                                 xbcT[:, cc, tt * 128:(tt + 1) * 128], self.ident_bf[:]),
                             reads=[('xbcT', cc)], writes=['psb'], inc=(q == 7))
                    if r % 2 == 0:
                        P.op('act', lambda ti=ti, r=r: nc.scalar.copy(tmst[ti][:, r * 1024:(r + 1) * 1024], self.psb[:, :]),
                             reads=['psb'], writes=[('tmst', ti)])
                    else:
                        P.op('dve', lambda ti=ti, r=r: nc.vector.tensor_copy(tmst[ti][:, r * 1024:(r + 1) * 1024], self.psb[:, :]),
                             reads=['psb'], writes=[('tmst', ti)])
                P.dma('sp', out=self.xsB_d[t * 128:(t + 1) * 128, :], in_=tmst[ti][:], key=f"st{1 + ti}",
                      reads=[('tmst', ti)], writes=[('xsB_d', t)])
    P.barrier()


def _pass_ssd_b(self, l):
    P, nc = self.P, self.nc
    ps = self.ps
    with ExitStack() as es:
        sb = lambda name, shape, dt: es.enter_context(nc.sbuf_tensor(_uid(name), shape, dt))
        rows = lambda kc: slice(kc * 128, (kc + 1) * 128)
        W = lambda name, n: _toks(name, n)
        Wz = sb("Wz", [128, 8, 2048], BF16)
        Wdt = sb("Wdt", [128, 8, 32], BF16)
        dtb = sb("dtb", [128, 32], F32)
        abc = sb("abc", [128, 32], F32)
        dsk = sb("dsk", [128, 32], F32)
        ngb = sb("ngb", [128, 2048], F32)
        self.load_w(lambda kc: Wz[:, kc, :], lambda kc: self.w_in[l, rows(kc), C_Z:C_Z + 2048], 'Wz', 8)
        self.load_w(lambda kc: Wdt[:, kc, :], lambda kc: self.w_in[l, rows(kc), C_DT:C_DT + 32], 'Wdt', 8)
        P.dma('sp', out=dtb[:], in_=self.dt_bias[l, :].partition_broadcast(128), key="ld0", writes=['dtb'])
        P.dma('sp', out=abc[:], in_=self.a_log[l, :].partition_broadcast(128), key="ld1", writes=['abc'])
        P.dma('sp', out=dsk[:], in_=self.d_skip[l, :].partition_broadcast(128), key="ld2", writes=['dsk'])
        P.dma('sp', out=ngb[:], in_=self.ssm_norm[l, :].partition_broadcast(128), key="ld3", writes=['ngb'])
        P.op('act', lambda: nc.scalar.activation(out=abc[:], in_=abc[:], func=AF.Exp), reads=['abc'], writes=['abc'])
        P.op('dve', lambda: nc.vector.tensor_scalar(abc[:], abc[:], -1.0, None, op0=ALU.mult), reads=['abc'], writes=['abc'])
        ST = sb("ST", [128, 8, 256], F32)
        STb = sb("STb", [128, 8, 256], BF16)
        P.op('dve', lambda: nc.vector.memset(ST[:], 0.0), writes=[('ST', g) for g in range(8)])
        P.op('pool', lambda: nc.gpsimd.memset(STb[:], 0.0), writes=[('STb', g) for g in range(8)])
        xblk = [sb(f"xblk{i}", [128, 8, TB], BF16) for i in range(2)]
        bct = [sb(f"bct{i}", [128, 16, TB], BF16) for i in range(2)]
        xsB = [sb(f"xsB{i}", [128, 3072], BF16) for i in range(2)]
        sm = sb("sm", [128, 8, 32], F32)
        xdt = sb("xdt", [128, 2048], BF16)
        xdt2 = sb("xdt2", [128, 2048], BF16)
        dam = [sb(f"dam{i}", [128, 4, 128], F32) for i in range(2)]
        dec = [sb(f"dec{i}", [128, 4, 128], F32) for i in range(2)]
        cbm = [sb(f"cbm{i}", [128, 128], F32) for i in range(2)]
        G = [sb(f"G{i}", [128, 4, 128], BF16) for i in range(2)]
        tmp = [sb(f"tmp{i}", [128, 256], F32) for i in range(2)]
        y = sb("y", [128, 2048], F32)
        zs = [sb(f"zs{i}", [128, 512], F32) for i in range(2)]
        junk = sb("junk", [128, 256], F32)
        ssq = sb("ssq", [128, 16], F32)
        yn = sb("yn", [128, 2048], BF16)
        ynT = [sb(f"ynT{i}", [128, 16, TB], BF16) for i in range(2)]
        v3 = lambda ap, h: ap.rearrange("p (h c) -> p h c", h=h)
        bc3 = lambda ap, n: ap.unsqueeze(2).to_broadcast([128, ap.shape[1], n])
        gcount = 0
        for b in range(NB):
            i2 = b % 2
            t0 = b * TB
            P.dma('sp', out=xblk[i2][:], in_=self.xT_d[:, :, t0:t0 + TB], key=f"ld{4 + i2}", writes=[('xblk', i2)])
            P.dma('sp', out=bct[i2][:], in_=self.BCT_d[:, :, t0:t0 + TB], key=f"ld{6 + i2}", writes=[('bct', i2)])
            xb = xblk[i2]
            for tt in range(4):
                t = b * 4 + tt
                ti = t % 2
                cols = slice(tt * 128, (tt + 1) * 128)
                xs = xsB[ti]
                P.dma('sp', out=xs[:], in_=self.xsB_d[t * 128:(t + 1) * 128, :], key=f"ld{8 + ti}", writes=[('xsB', ti)])
                P.mm(ps[5][:, 0:32], [(xb[:, kc, cols], Wdt[:, kc, :]) for kc in range(8)], 'ps5', [('xblk', i2)] + W('Wdt', 8))
                P.op('dve', lambda: nc.vector.tensor_tensor(sm[:, 0, :], ps[5][:, 0:32], dtb[:], op=ALU.add),
                     reads=['ps5', 'dtb'], writes=['sm0'])
                P.op('dve', lambda: nc.vector.tensor_scalar(sm[:, 1, :], sm[:, 0, :], -1.0, None, op0=ALU.mult),
                     reads=['sm0'], writes=['sm1'])
                P.op('dve', lambda: nc.vector.tensor_tensor(sm[:, 1, :], sm[:, 1, :], sm[:, 0, :], op=ALU.min),
                     reads=['sm0', 'sm1'], writes=['sm1'])
                P.op('act', lambda: nc.scalar.activation(out=sm[:, 2, :], in_=sm[:, 1, :], func=AF.Exp),
                     reads=['sm1'], writes=['sm2'])
                P.op('act', lambda: nc.scalar.activation(out=sm[:, 2, :], in_=sm[:, 2, :], func=AF.Ln, bias=1.0),
                     reads=['sm2'], writes=['sm2'])
                P.op('dve', lambda: nc.vector.scalar_tensor_tensor(out=sm[:, 3, :], in0=sm[:, 0, :], scalar=0.0, in1=sm[:, 2, :],
                                                                   op0=ALU.max, op1=ALU.add), reads=['sm0', 'sm2'], writes=['sm3'])
                P.op('dve', lambda: nc.vector.tensor_tensor(sm[:, 4, :], sm[:, 3, :], abc[:], op=ALU.mult),
                     reads=['sm3', 'abc'], writes=['sm4'])
                P.mm(ps[5][:, 64:96], [(self.tri_le_f[:], sm[:, 4, :])], 'ps5', ['sm4'])
                P.mm(ps[5][:, 128:160], [(self.tri_gt_f[:], sm[:, 4, :])], 'ps5', ['sm4'])
                P.mm(ps[5][:, 192:224], [(self.ones_f[:], sm[:, 4, :])], 'ps5', ['sm4'])
                P.op('act', lambda: nc.scalar.activation(out=sm[:, 5, :], in_=ps[5][:, 64:96], func=AF.Exp), reads=['ps5'], writes=['sm5'])
                P.op('act', lambda: nc.scalar.activation(out=sm[:, 6, :], in_=ps[5][:, 128:160], func=AF.Exp), reads=['ps5'], writes=['sm6'])
                P.op('act', lambda: nc.scalar.activation(out=sm[:, 7, :], in_=ps[5][:, 192:224], func=AF.Exp), reads=['ps5'], writes=['sm7'])
                P.op('dve', lambda xs=xs: nc.vector.tensor_tensor(v3(xdt[:], 32), v3(xs[:, 0:2048], 32), bc3(sm[:, 3, :], 64), op=ALU.mult),
                     reads=[('xsB', ti), 'sm3'], writes=['xdt'])
                P.op('pool', lambda: nc.gpsimd.tensor_tensor(v3(xdt2[:], 32), v3(xdt[:], 32), bc3(sm[:, 6, :], 64), op=ALU.mult),
                     reads=['xdt', 'sm6'], writes=['xdt2'])
                for g in range(8):
                    gi = gcount % 2
                    gcount += 1
                    hs = slice(g * 4, (g + 1) * 4)
                    P.op('pool', lambda gi=gi, hs=hs: nc.gpsimd.tensor_tensor(
                        dam[gi][:], self.tri_gt_f[:, :].unsqueeze(1).to_broadcast([128, 4, 128]), bc3(sm[:, 4, hs], 128), op=ALU.mult),
                         reads=['sm4'], writes=[('dam', gi)])
                    sp_, spt = (ps[0], ('SEG', 0)) if gi == 0 else (ps[1], ('SEG', 1))
                    for r in range(4):
                        P.mm(sp_[:, r * 128:(r + 1) * 128], [(dam[gi][:, r, :], self.tri_le_f[:])], spt, [('dam', gi)])
                    P.op('act', lambda gi=gi, sp_=sp_: nc.scalar.activation(out=dec[gi][:].rearrange("p r s -> p (r s)"), in_=sp_[:, :], func=AF.Exp),
                         reads=[spt], writes=[('dec', gi)])
                    cp_, cpt = (ps[2], ('CB', 0)) if gi == 0 else (ps[3], ('CB', 1))
                    P.mm(cp_[:, 0:128], [(bct[i2][:, g, cols], bct[i2][:, 8 + g, cols])], cpt, [('bct', i2)])
                    P.op('dve', lambda gi=gi, cp_=cp_: nc.vector.tensor_tensor(cbm[gi][:], cp_[:, 0:128], self.tri_le_f[:], op=ALU.mult),
                         reads=[cpt], writes=[('cbm', gi)])
                    P.op('dve', lambda gi=gi: nc.vector.tensor_tensor(G[gi][:], dec[gi][:], cbm[gi][:, :].unsqueeze(1).to_broadcast([128, 4, 128]),
                                                                      op=ALU.mult),
                         reads=[('dec', gi), ('cbm', gi)], writes=[('G', gi)])
                    for r in range(4):
                        hh = g * 4 + r
                        P.mm(cp_[:, 128 + r * 64 - 128 + 128:128 + (r + 1) * 64] if False else ps[4][:, (gi * 256) + r * 64:(gi * 256) + (r + 1) * 64],
                             [(G[gi][:, r, :], xdt[:, hh * 64:(hh + 1) * 64])], ('YI', gi), [('G', gi), 'xdt'])
                    P.mm(cp_[:, 256:512], [(bct[i2][:, 8 + g, cols], STb[:, g, :])], ('YS', gi), [('bct', i2), ('STb', g)])
                    P.op('dve', lambda gi=gi, cp_=cp_, hs=hs: nc.vector.tensor_tensor(v3(tmp[gi][:], 4), v3(cp_[:, 256:512], 4), bc3(sm[:, 5, hs], 64), op=ALU.mult),
                         reads=[('YS', gi), 'sm5'], writes=[('tmp', gi)])
                    P.op('dve', lambda gi=gi, g=g: nc.vector.tensor_tensor(y[:, g * 256:(g + 1) * 256], tmp[gi][:], ps[4][:, gi * 256:(gi + 1) * 256], op=ALU.add),
                         reads=[('tmp', gi), ('YI', gi)], writes=[('y', g)])
                    P.mm(sp_[:, 0:256] if False else ps[6][:, gi * 256:(gi + 1) * 256], [(xs[:, 2048 + g * 128:2048 + (g + 1) * 128], xdt2[:, g * 256:(g + 1) * 256])],
                         ('SU', gi), [('xsB', ti), 'xdt2'])
                    P.op('pool', lambda g=g, hs=hs: nc.gpsimd.tensor_tensor(v3(ST[:, g, :], 4), v3(ST[:, g, :], 4), bc3(sm[:, 7, hs], 64), op=ALU.mult),
                         reads=['sm7'], writes=[('ST', g)])
                    P.op('dve', lambda g=g, gi=gi: nc.vector.tensor_tensor(ST[:, g, :], ST[:, g, :], ps[6][:, gi * 256:(gi + 1) * 256], op=ALU.add),
                         reads=[('SU', gi)], writes=[('ST', g)])
                    P.op('act', lambda g=g: nc.scalar.copy(STb[:, g, :], ST[:, g, :]), reads=[('ST', g)], writes=[('STb', g)])
                ally = [('y', g) for g in range(8)]
                P.op('pool', lambda xs=xs: nc.gpsimd.tensor_tensor(v3(xdt2[:], 32), v3(xs[:, 0:2048], 32), bc3(dsk[:, :], 64), op=ALU.mult),
                     reads=[('xsB', ti), 'dsk'], writes=['xdt2'])
                P.op('dve', lambda: nc.vector.tensor_tensor(y[:], y[:], xdt2[:], op=ALU.add), reads=ally + ['xdt2'], writes=ally)
                for q in range(4):
                    zi = q % 2
                    qs = slice(q * 512, (q + 1) * 512)
                    zp, zpt = (ps[0], ('SEG', 0)) if zi == 0 else (ps[1], ('SEG', 1))
                    P.mm(zp[:, :], [(xb[:, kc, cols], Wz[:, kc, qs]) for kc in range(8)], zpt, [('xblk', i2)] + W('Wz', 8))
                    P.op('act', lambda zi=zi, zp=zp: nc.scalar.activation(out=zs[zi][:], in_=zp[:, :], func=AF.Silu), reads=[zpt], writes=[('zs', zi)])
                    P.op('dve', lambda zi=zi, qs=qs: nc.vector.tensor_tensor(y[:, qs], y[:, qs], zs[zi][:], op=ALU.mult),
                         reads=[('zs', zi)] + ally, writes=ally)
                for g in range(8):
                    P.op('act', lambda g=g: nc.scalar.activation(out=junk[:], in_=y[:, g * 256:(g + 1) * 256], func=AF.Square, accum_out=ssq[:, g:g + 1]),
                         reads=ally, writes=['junk', ('ssq', g)])
                allq = [('ssq', g) for g in range(8)]
                P.op('dve', lambda: nc.vector.tensor_scalar(ssq[:, 8:16], ssq[:, 0:8], 1.0 / 256, RMS_EPS, op0=ALU.mult, op1=ALU.add),
                     reads=allq, writes=['ssq8'])
                P.op('act', lambda: nc.scalar.activation(out=ssq[:, 8:16], in_=ssq[:, 8:16], func=AF.Sqrt), reads=['ssq8'], writes=['ssq8'])
                P.op('dve', lambda: nc.vector.reciprocal(ssq[:, 8:16], ssq[:, 8:16]), reads=['ssq8'], writes=['ssq8'])
                for g in range(8):
                    gs = slice(g * 256, (g + 1) * 256)
                    P.op('dve', lambda g=g, gs=gs: nc.vector.scalar_tensor_tensor(
                        out=yn[:, gs], in0=y[:, gs], scalar=ssq[:, 8 + g:9 + g], in1=ngb[:, gs], op0=ALU.mult, op1=ALU.mult),
                         reads=ally + ['ssq8', 'ngb'], writes=[('yn', g)])
                for r in range(2):
                    for q in range(8):
                        P.op('pe', lambda r=r, q=q: nc.tensor.transpose(self.psb[:, q * 128:(q + 1) * 128],
                                                                        yn[:, (r * 8 + q) * 128:(r * 8 + q + 1) * 128], self.ident_bf[:]),
                             reads=[('yn', g) for g in range(8)], writes=['psb'], inc=(q == 7))
                    P.op('act', lambda r=r, cols=cols: nc.scalar.copy(ynT[i2][:, r * 8:(r + 1) * 8, cols],
                                                                      self.psb[:].rearrange("p (k n) -> p k n", k=8)),
                         reads=['psb'], writes=[('ynT', i2)])
            P.dma('sp', out=self.ynT_d[:, :, t0:t0 + TB], in_=ynT[i2][:], key=f"st{i2}", reads=[('ynT', i2)], writes=[('ynT_d', b)])
    P.barrier()


def _pass_ssd_c(self, l):
    P, nc = self.P, self.nc
    ps = self.ps
    with ExitStack() as es:
        sb = lambda name, shape, dt: es.enter_context(nc.sbuf_tensor(_uid(name), shape, dt))
        rows = lambda kc: slice(kc * 128, (kc + 1) * 128)
        W = lambda name, n: _toks(name, n)
        WgB = sb("WgB", [128, 8, D], BF16)
        wpb = sb("wpb", [128, 16, D], BF16)
        self.load_w(lambda kc: WgB[:, kc, :], lambda kc: self.w_in[l, rows(kc), C_G + D:C_G + 2 * D], 'WgB', 8)
        self.load_w(lambda kc: wpb[:, kc, :], lambda kc: self.w_proj_b[l, rows(kc), :], 'wpb', 16)
        xblk = [sb(f"xblk{i}", [128, 8, TB], BF16) for i in range(2)]
        ynb = [sb(f"ynb{i}", [128, 16, TB], BF16) for i in range(2)]
        sig = [sb(f"sig{i}", [128, TB], F32) for i in range(2)]
        mT = [sb(f"mT{i}", [128, 8, TB], BF16) for i in range(2)]
        for b in range(NB):
            i2 = b % 2
            t0 = b * TB
            P.dma('sp', out=xblk[i2][:], in_=self.xT_d[:, :, t0:t0 + TB], key=f"ld{2 + i2}", writes=[('xblk', i2)])
            P.dma('sp', out=ynb[i2][:], in_=self.ynT_d[:, :, t0:t0 + TB], key=f"ld{4 + i2}", writes=[('ynb', i2)])
            xb = xblk[i2]
            for dc in range(8):
                dd = slice(dc * 128, (dc + 1) * 128)
                si = dc % 2
                pa, pat = (ps[5], 'ps5') if si == 0 else (ps[3], 'ps3')
                pb, pbt = (ps[6], 'ps6') if si == 0 else (ps[4], 'ps4')
                P.mm(pa[:, :], [(WgB[:, kc, dd], xb[:, kc, :]) for kc in range(8)], pat, [('xblk', i2)] + W('WgB', 8))
                P.op('act', lambda si=si, pa=pa: nc.scalar.activation(out=sig[si][:], in_=pa[:, :], func=AF.Sigmoid),
                     reads=[pat], writes=[('sig', si)])
                P.mm(pb[:, :], [(wpb[:, j, dd], ynb[i2][:, j, :]) for j in range(16)], pbt, [('ynb', i2)] + W('wpb', 16))
                P.op('dve', lambda dc=dc, si=si, pb=pb: nc.vector.tensor_tensor(mT[i2][:, dc, :], pb[:, :], sig[si][:], op=ALU.mult),
                     reads=[pbt, ('sig', si)], writes=[('mT', i2)])
            P.dma('sp', out=self.mB_d[:, :, t0:t0 + TB], in_=mT[i2][:], key=f"st{i2}", reads=[('mT', i2)], writes=[('mB_d', b)])
    P.barrier()


Net.pass_ssd_a = _pass_ssd_a
Net.pass_ssd_b = _pass_ssd_b
Net.pass_ssd_c = _pass_ssd_c


def _layer_norm_tile(self, u, gbt, bbt, out, st, tag):
    P, nc = self.P, self.nc
    junk = self.ln_junk
    P.op('act', lambda: nc.scalar.activation(out=junk[:], in_=u, func=AF.Identity, accum_out=st[:, 0:1]),
         reads=[tag + 'u'], writes=['lnjunk', tag + 's0'])
    P.op('act', lambda: nc.scalar.activation(out=junk[:], in_=u, func=AF.Square, accum_out=st[:, 1:2]),
         reads=[tag + 'u'], writes=['lnjunk', tag + 's1'])
    P.op('dve', lambda: nc.vector.tensor_scalar(st[:, 2:4], st[:, 0:2], 1.0 / D, None, op0=ALU.mult),
         reads=[tag + 's0', tag + 's1'], writes=[tag + 's2'])
    P.op('dve', lambda: nc.vector.tensor_tensor(st[:, 4:5], st[:, 2:3], st[:, 2:3], op=ALU.mult), reads=[tag + 's2'], writes=[tag + 's4'])
    P.op('dve', lambda: nc.vector.tensor_tensor(st[:, 5:6], st[:, 3:4], st[:, 4:5], op=ALU.subtract), reads=[tag + 's2', tag + 's4'], writes=[tag + 's5'])
    P.op('dve', lambda: nc.vector.tensor_scalar(st[:, 5:6], st[:, 5:6], NORM_EPS, None, op0=ALU.add), reads=[tag + 's5'], writes=[tag + 's5'])
    P.op('act', lambda: nc.scalar.activation(out=st[:, 6:7], in_=st[:, 5:6], func=AF.Sqrt), reads=[tag + 's5'], writes=[tag + 's6'])
    P.op('dve', lambda: nc.vector.reciprocal(st[:, 7:8], st[:, 6:7]), reads=[tag + 's6'], writes=[tag + 's7'])
    P.op('dve', lambda: nc.vector.tensor_scalar(out, u, st[:, 2:3], st[:, 7:8], op0=ALU.subtract, op1=ALU.mult),
         reads=[tag + 'u', tag + 's2', tag + 's7'], writes=[tag + 'o'])
    P.op('pool', lambda: nc.gpsimd.tensor_tensor(out, out, gbt[:], op=ALU.mult), reads=[tag + 'g'], writes=[tag + 'o'])
    P.op('pool', lambda: nc.gpsimd.tensor_tensor(out, out, bbt[:], op=ALU.add), reads=[tag + 'g'], writes=[tag + 'o'])


def _pass_ln1(self, l):
    P, nc = self.P, self.nc
    ps = self.ps
    xa_src = self.x_in if l == 0 else self.xa_d
    with ExitStack() as es:
        sb = lambda name, shape, dt: es.enter_context(nc.sbuf_tensor(_uid(name), shape, dt))
        rows = lambda kc: slice(kc * 128, (kc + 1) * 128)
        W = lambda name, n: _toks(name, n)
        wout = sb("wout", [128, 8, D], BF16)
        rw = sb("rw", [128, 8, 16], F32)
        rb = sb("rb", [128, 16], F32)
        gbt = sb("gbt", [128, D], F32)
        bbt = sb("bbt", [128, D], F32)
        self.ln_junk = sb("lnjunk", [128, D], F32)
        self.load_w(lambda kc: wout[:, kc, :], lambda kc: self.w_out[l, rows(kc), :], 'wout', 8)
        for kc in range(8):
            P.dma('sp', out=rw[:, kc, :], in_=self.router_w[rows(kc), :], key="ld0", writes=[('rw', kc)])
        P.dma('sp', out=rb[:], in_=self.router_bias[0, :].partition_broadcast(128), key="ld1", writes=['rb'])
        P.dma('sp', out=gbt[:], in_=self.ln1_g[l, :].partition_broadcast(128), key="ld2", writes=['lg'])
        P.dma('sp', out=bbt[:], in_=self.ln1_b[l, :].partition_broadcast(128), key="ld3", writes=['lg'])
        mblk = [[sb(f"m{n}{i}", [128, 8, TB], BF16) for n in "ABC"] for i in range(2)]
        xa = [sb(f"xa{i}", [128, D], F32) for i in range(2)]
        u = [sb(f"u{i}", [128, D], F32) for i in range(2)]
        x1 = [sb(f"x1{i}", [128, D], F32) for i in range(2)]
        st = sb("st", [128, 8], F32)
        x1Tf = sb("x1Tf", [128, 8, 128], F32)
        stg = [sb(f"stg{i}", [128, 8, TB], BF16) for i in range(2)]
        r = sb("r", [128, 12, 16], F32)
        gts = [sb(f"gts{i}", [16, TB], F32) for i in range(2)]
        srcs = [self.mA_d, self.mB_d, self.mC_d]
        for b in range(NB):
            i2 = b % 2
            t0 = b * TB
            for n in range(3):
                P.dma('sp', out=mblk[i2][n][:], in_=srcs[n][:, :, t0:t0 + TB], key=f"ld{4 + 3 * i2 + n}", writes=[('mblk', i2, n)])
            for tt in range(4):
                t = b * 4 + tt
                ti = t % 2
                cols = slice(tt * 128, (tt + 1) * 128)
                P.dma('sp', out=xa[ti][:], in_=xa_src[t * 128:(t + 1) * 128, :], key=f"ld{10 + ti}", writes=[('xa', ti)])
                for half in range(2):
                    pb, pt = (ps[5], 'ps5') if half == 0 else (ps[6], 'ps6')
                    P.mm(pb[:, :], [(mblk[i2][n][:, dc, cols], wout[:, dc, half * 512:(half + 1) * 512]) for n in range(3) for dc in range(8)],
                         pt, [('mblk', i2, n) for n in range(3)] + W('wout', 8))
                    P.op('dve', lambda ti=ti, half=half, pb=pb: nc.vector.scalar_tensor_tensor(
                        out=u[ti][:, half * 512:(half + 1) * 512], in0=xa[ti][:, half * 512:(half + 1) * 512], scalar=DN_ALPHA, in1=pb[:, :],
                        op0=ALU.mult, op1=ALU.add), reads=[('xa', ti), pt], writes=['L1u'])
                self.layer_norm_tile(u[ti][:], gbt, bbt, x1[ti][:], st, 'L1')
                P._record(('e', 'pool', P.cnt['pool']), ['lg'], [])
                P.dma('sp', out=self.x1_d[t * 128:(t + 1) * 128, :], in_=x1[ti][:], key=f"st{ti}", reads=['L1o'], writes=[('x1_d', t)])
                import os
                CUT = int(os.environ.get("K_CUT", "99"))
                if CUT < 1:
                    continue
                for kc in range(8):
                    pb = ps[0] if kc < 4 else ps[1]
                    P.op('pe', lambda kc=kc, pb=pb, ti=ti: nc.tensor.matmul(pb[:, (kc % 4) * 128:(kc % 4 + 1) * 128],
                                                                           x1[ti][:, kc * 128:(kc + 1) * 128], self.ident_f[:],
                                                                           start=True, stop=True),
                         reads=['L1o'], writes=[('TP', kc // 4)], inc=(kc % 4 == 3))
                for hf in range(2):
                    P.op('act', lambda hf=hf: nc.scalar.copy(x1Tf[:, hf * 4:(hf + 1) * 4, :], ps[hf][:].rearrange("p (k n) -> p k n", k=4)),
                         reads=[('TP', hf)], writes=['x1Tf'])
                    P.op('dve', lambda hf=hf, cols=cols: nc.vector.tensor_copy(stg[i2][:, hf * 4:(hf + 1) * 4, cols], x1Tf[:, hf * 4:(hf + 1) * 4, :]),
                         reads=['x1Tf'], writes=[('stg', i2)])
                if CUT < 2:
                    continue
                P.mm(ps[2][:, 0:16], [(x1Tf[:, kc, :], rw[:, kc, :]) for kc in range(8)], 'ps2', ['x1Tf'] + W('rw', 8))
                R = lambda i: r[:, i, :]
                R4 = lambda i: r[:, i, :].rearrange("p (g e) -> p g e", g=4)
                P.op('act', lambda: nc.scalar.activation(out=R(0), in_=ps[2][:, 0:16], func=AF.Sigmoid), reads=['ps2'], writes=['r0'])
                P.op('dve', lambda: nc.vector.tensor_tensor(R(1), R(0), rb[:], op=ALU.add), reads=['r0', 'rb'], writes=['r1'])
                if CUT < 3:
                    continue
                pairs = [(0, 1), (0, 2), (0, 3), (1, 2), (1, 3), (2, 3)]
                for pi_, (a_, b_) in enumerate(pairs):
                    P.op('dve', lambda pi_=pi_, a_=a_, b_=b_: nc.vector.tensor_tensor(r[:, 2 + pi_ // 4, (pi_ % 4) * 4:(pi_ % 4) * 4 + 4],
                                                                                    R4(1)[:, :, a_], R4(1)[:, :, b_], op=ALU.add),
                         reads=['r1'], writes=['r2'])
                P.op('dve', lambda: nc.vector.tensor_tensor(r[:, 4, 0:4], r[:, 2, 0:4], r[:, 2, 4:8], op=ALU.max), reads=['r2'], writes=['r4'])
                P.op('dve', lambda: nc.vector.tensor_tensor(r[:, 4, 4:8], r[:, 2, 8:12], r[:, 2, 12:16], op=ALU.max), reads=['r2'], writes=['r4'])
                P.op('dve', lambda: nc.vector.tensor_tensor(r[:, 4, 8:12], r[:, 3, 0:4], r[:, 3, 4:8], op=ALU.max), reads=['r2'], writes=['r4'])
                P.op('dve', lambda: nc.vector.tensor_tensor(r[:, 4, 0:4], r[:, 4, 0:4], r[:, 4, 4:8], op=ALU.max), reads=['r4'], writes=['r4'])
                P.op('dve', lambda: nc.vector.tensor_tensor(r[:, 4, 0:4], r[:, 4, 0:4], r[:, 4, 8:12], op=ALU.max), reads=['r4'], writes=['r4'])
                P.op('dve', lambda: nc.vector.tensor_reduce(out=r[:, 5, 0:1], in_=r[:, 4, 0:4], axis=AX.X, op=ALU.max), reads=['r4'], writes=['r5'])
                P.op('dve', lambda: nc.vector.tensor_scalar(r[:, 5, 4:8], r[:, 4, 0:4], r[:, 5, 0:1], None, op0=ALU.is_equal), reads=['r4', 'r5'], writes=['r5m'])
                P.op('dve', lambda: nc.vector.tensor_scalar(r[:, 5, 8:12], r[:, 5, 4:8], 1.0, 1e30, op0=ALU.subtract, op1=ALU.mult), reads=['r5m'], writes=['r5p'])
                P.op('dve', lambda: nc.vector.tensor_tensor(R4(6), R4(1), r[:, 5, 4:8].unsqueeze(2).to_broadcast([128, 4, 4]), op=ALU.mult),
                     reads=['r1', 'r5m'], writes=['r6'])
                P.op('dve', lambda: nc.vector.tensor_tensor(R4(6), R4(6), r[:, 5, 8:12].unsqueeze(2).to_broadcast([128, 4, 4]), op=ALU.add),
                     reads=['r6', 'r5p'], writes=['r6'])
                P.op('dve', lambda: nc.vector.tensor_reduce(out=r[:, 7, 0:1], in_=R(6), axis=AX.X, op=ALU.max), reads=['r6'], writes=['r7'])
                P.op('dve', lambda: nc.vector.tensor_scalar(R(8), R(6), r[:, 7, 0:1], None, op0=ALU.is_equal), reads=['r6', 'r7'], writes=['r8'])
                P.op('dve', lambda: nc.vector.scalar_tensor_tensor(out=R(9), in0=R(8), scalar=-1e30, in1=R(6), op0=ALU.mult, op1=ALU.add),
                     reads=['r8', 'r6'], writes=['r9'])
                P.op('dve', lambda: nc.vector.tensor_reduce(out=r[:, 7, 1:2], in_=R(9), axis=AX.X, op=ALU.max), reads=['r9'], writes=['r7b'])
                P.op('dve', lambda: nc.vector.tensor_scalar(R(10), R(9), r[:, 7, 1:2], None, op0=ALU.is_equal), reads=['r9', 'r7b'], writes=['r10'])
                P.op('dve', lambda: nc.vector.tensor_tensor(R(10), R(10), R(8), op=ALU.add), reads=['r10', 'r8'], writes=['r10'])
                P.op('dve', lambda: nc.vector.tensor_tensor(R(11), R(10), R(0), op=ALU.mult), reads=['r10', 'r0'], writes=['r11'])
                P.op('dve', lambda: nc.vector.tensor_reduce(out=r[:, 7, 2:3], in_=R(11), axis=AX.X, op=ALU.add), reads=['r11'], writes=['r7c'])
                P.op('dve', lambda: nc.vector.reciprocal(r[:, 7, 3:4], r[:, 7, 2:3]), reads=['r7c'], writes=['r7d'])
                P.op('dve', lambda: nc.vector.tensor_scalar(R(11), R(11), r[:, 7, 3:4], None, op0=ALU.mult), reads=['r11', 'r7d'], writes=['r11'])
                if CUT < 4:
                    continue
                P.op('pe', lambda: nc.tensor.matmul(ps[3][0:16, 0:128], R(11), self.ident_f[:], start=True, stop=True), reads=['r11'], writes=['ps3'])
                P.op('act', lambda cols=cols: nc.scalar.copy(gts[i2][:, cols], ps[3][0:16, 0:128]), reads=['ps3'], writes=[('gts', i2)])
            P.dma('sp', out=self.x1T_d[:, :, t0:t0 + TB], in_=stg[i2][:], key=f"st{2 + i2}", reads=[('stg', i2)], writes=[('x1T_d', b)])
            P.dma('sp', out=self.gT_d[:, t0:t0 + TB], in_=gts[i2][:], key=f"st{4 + i2}", reads=[('gts', i2)], writes=[('gT_d', b)])
    P.barrier()


def _pass_moe(self, l, last):
    P, nc = self.P, self.nc
    ps = self.ps
    BB = 1024
    with ExitStack() as es:
        sb = lambda name, shape, dt: es.enter_context(nc.sbuf_tensor(_uid(name), shape, dt))
        rows = lambda kc: slice(kc * 128, (kc + 1) * 128)
        W = lambda name, n: _toks(name, n)
        gbt = sb("gbt", [128, D], F32)
        bbt = sb("bbt", [128, D], F32)
        self.ln_junk = sb("lnjunk", [128, D], F32)
        E16 = sb("E16", [16, 16, 128], F32)
        P.dma('sp', out=gbt[:], in_=self.ln2_g[l, :].partition_broadcast(128), key="ld2", writes=['lg'])
        P.dma('sp', out=bbt[:], in_=self.ln2_b[l, :].partition_broadcast(128), key="ld3", writes=['lg'])
        P.op('pool', lambda: nc.gpsimd.memset(E16[:], 0.0), writes=['E16'])
        P.op('pool', lambda: nc.gpsimd.affine_select(out=E16[:], in_=E16[:], pattern=[[-1, 16], [0, 128]], compare_op=ALU.not_equal,
                                                     fill=1.0, base=0, channel_multiplier=1), writes=['E16'])
        x1T = sb("x1T", [128, 8, BB], BF16)
        gT = sb("gT", [16, BB], F32)
        acc = sb("acc", [128, 8, D], F32)
        w1 = [sb(f"w1{i}", [128, 8, 512], BF16) for i in range(2)]
        w3 = [sb(f"w3{i}", [128, 8, 512], BF16) for i in range(2)]
        w2 = [sb(f"w2{i}", [128, 4, D], BF16) for i in range(2)]
        gb = [sb(f"gb{i}", [128, 512], F32) for i in range(2)]
        s1 = [sb(f"s1{i}", [128, 512], F32) for i in range(2)]
        tq = [sb(f"tq{i}", [128, 512], F32) for i in range(2)]
        hT = [sb(f"hT{i}", [128, 4, 512], BF16) for i in range(2)]
        x1t = [sb(f"x1t{i}", [128, D], F32) for i in range(2)]
        u = [sb(f"u{i}", [128, D], F32) for i in range(2)]
        x2 = [sb(f"x2{i}", [128, D], F32) for i in range(2)]
        x2b = [sb(f"x2b{i}", [128, D], BF16) for i in range(2)]
        st = sb("st", [128, 8], F32)
        stg = sb("stg", [128, 8, BB], BF16)
        dst = self.y_out if last else self.xa_d
        ecount = 0
        hcount = 0
        for bb in range(T // BB):
            t0 = bb * BB
            P.dma('sp', out=x1T[:], in_=self.x1T_d[:, :, t0:t0 + BB], key="ld4", writes=['x1T'])
            P.dma('sp', out=gT[:], in_=self.gT_d[:, t0:t0 + BB], key="ld5", writes=['gT'])
            for e in range(16):
                ei = ecount % 2
                ecount += 1
                self.load_w(lambda kc: w1[ei][:, kc, :], lambda kc: self.exp_w1[l, e, rows(kc), :], ('w1', ei), 8)
                self.load_w(lambda kc: w3[ei][:, kc, :], lambda kc: self.exp_w3[l, e, rows(kc), :], ('w3', ei), 8)
                self.load_w(lambda kc: w2[ei][:, kc, :], lambda kc: self.exp_w2[l, e, rows(kc), :], ('w2', ei), 4)
                for sub in range(BB // 512):
                    hi = hcount % 2
                    hcount += 1
                    sc = slice(sub * 512, (sub + 1) * 512)
                    P.mm(ps[4][:, :], [(E16[:, e, :], gT[:, sc])], 'ps4', ['E16', 'gT'])
                    P.op('act', lambda hi=hi: nc.scalar.copy(gb[hi][:], ps[4][:, :]), reads=['ps4'], writes=[('gb', hi)])
                    for fc in range(4):
                        fi = fc % 2
                        fs = slice(fc * 128, (fc + 1) * 128)
                        pa, pat = (ps[0], 'ps0') if fi == 0 else (ps[2], 'ps2')
                        pb, pbt = (ps[1], 'ps1') if fi == 0 else (ps[3], 'ps3')
                        P.mm(pa[:, :], [(w1[ei][:, kc, fs], x1T[:, kc, sc]) for kc in range(8)], pat, ['x1T'] + W(('w1', ei), 8))
                        P.mm(pb[:, :], [(w3[ei][:, kc, fs], x1T[:, kc, sc]) for kc in range(8)], pbt, ['x1T'] + W(('w3', ei), 8))
                        P.op('act', lambda fi=fi, pa=pa: nc.scalar.activation(out=s1[fi][:], in_=pa[:, :], func=AF.Silu), reads=[pat], writes=[('s1', fi)])
                        P.op('dve', lambda fi=fi, pb=pb: nc.vector.tensor_tensor(tq[fi][:], pb[:, :], s1[fi][:], op=ALU.mult),
                             reads=[pbt, ('s1', fi)], writes=[('tq', fi)])
                        P.op('pool', lambda fi=fi, hi=hi, fc=fc: nc.gpsimd.tensor_tensor(hT[hi][:, fc, :], tq[fi][:], gb[hi][:], op=ALU.mult),
                             reads=[('tq', fi), ('gb', hi)], writes=[('hT', hi, fc)])
                    for tt in range(4):
                        tl = sub * 4 + tt
                        cols = slice(tt * 128, (tt + 1) * 128)
                        for half in range(2):
                            pb, pt = (ps[5], 'ps5') if half == 0 else (ps[6], 'ps6')
                            hs = slice(half * 512, (half + 1) * 512)
                            P.mm(pb[:, :], [(hT[hi][:, fc, cols], w2[ei][:, fc, hs]) for fc in range(4)], pt,
                                 [('hT', hi, fc) for fc in range(4)] + W(('w2', ei), 4))
                            if e == 0:
                                P.op('dve', lambda tl=tl, hs=hs, pb=pb: nc.vector.tensor_copy(acc[:, tl, hs], pb[:, :]),
                                     reads=[pt], writes=[('acc', tl, half)])
                            else:
                                P.op('dve', lambda tl=tl, hs=hs, pb=pb: nc.vector.tensor_tensor(acc[:, tl, hs], acc[:, tl, hs], pb[:, :], op=ALU.add),
                                     reads=[pt], writes=[('acc', tl, half)])
            for tl in range(BB // 128):
                t = bb * (BB // 128) + tl
                ti = t % 2
                P.dma('sp', out=x1t[ti][:], in_=self.x1_d[t * 128:(t + 1) * 128, :], key=f"ld{6 + ti}", writes=[('x1t', ti)])
                P.op('dve', lambda ti=ti, tl=tl: nc.vector.scalar_tensor_tensor(
                    out=u[ti][:], in0=x1t[ti][:], scalar=DN_ALPHA, in1=acc[:, tl, :], op0=ALU.mult, op1=ALU.add),
                     reads=[('x1t', ti), ('acc', tl, 0), ('acc', tl, 1)], writes=['L2u'])
                self.layer_norm_tile(u[ti][:], gbt, bbt, x2[ti][:], st, 'L2')
                P._record(('e', 'pool', P.cnt['pool']), ['lg'], [])
                P.dma('sp', out=dst[t * 128:(t + 1) * 128, :], in_=x2[ti][:], key=f"st{ti}", reads=['L2o'], writes=[('dst', t)])
                if not last:
                    P.op('act', lambda ti=ti: nc.scalar.copy(x2b[ti][:], x2[ti][:]), reads=['L2o'], writes=[('x2b', ti)])
                    for kc in range(8):
                        P.op('pe', lambda kc=kc, ti=ti: nc.tensor.transpose(self.psb[:, kc * 128:(kc + 1) * 128],
                                                                           x2b[ti][:, kc * 128:(kc + 1) * 128], self.ident_bf[:]),
                             reads=[('x2b', ti)], writes=['psb'], inc=(kc == 7))
                    P.op('act', lambda tl=tl: nc.scalar.copy(stg[:, :, tl * 128:(tl + 1) * 128],
                                                             self.psb[:].rearrange("p (k n) -> p k n", k=8)),
                         reads=['psb'], writes=['stg'])
            if not last:
                P.dma('sp', out=self.xT_d[:, :, t0:t0 + BB], in_=stg[:], key="st2", reads=['stg'], writes=[('xT_d', bb)])
    P.barrier()


Net.layer_norm_tile = _layer_norm_tile
Net.pass_ln1 = _pass_ln1
Net.pass_moe = _pass_moe


def build(n_layers=DEPTH, stages=("mla", "ssd", "mem", "moe"), dbg=()):
    net = Net(n_layers)
    net.declare()
    P = net.P
    net.setup_globals()
    net.prologue()
    for l in range(n_layers):
        if "mla" in stages:
            net.pass_mla(l)
        if "mem" in stages:
            net.pass_mem(l)
        if "ssd" in stages:
            net.pass_ssd_a(l)
            net.pass_ssd_b(l)
            net.pass_ssd_c(l)
        if "moe" in stages or "ln1" in stages:
            net.pass_ln1(l)
        if "moe" in stages:
            net.pass_moe(l, last=(l == n_layers - 1))
    for name in dbg:
        src = getattr(net, name)
        dst = net.dbg("dbg_" + name, src.shape, src.dtype)
        P.dma('sp', out=dst, in_=src, key="st0", reads=[], writes=[('dbg', name)])
    P.barrier()
    print("instructions:", P.ninst, "sems:", P.nsem, {e: P.cnt[e] for e in P.cnt}, "dmas:", getattr(P, 'ndma', 0), "descs~", getattr(P, 'ndesc', 0))
    return net


def make_inputs(inputs, b):
    m = {}
    for k, v in inputs.items():
        v = np.asarray(v)
        if k in ("x", "mem"):
            m[k] = np.ascontiguousarray(v[b])
        elif k == "positions":
            m[k] = np.ascontiguousarray(v[b:b + 1]).astype(np.int32)
        elif k == "router_bias":
            m[k] = np.ascontiguousarray(v.reshape(1, 16))
        else:
            m[k] = np.ascontiguousarray(v)
    inv = (10000.0 ** (-np.arange(0, 64, 2, dtype=np.float32) / 64)).astype(np.float32)
    m["inv_freq"] = np.concatenate([inv, inv]).reshape(64, 1).astype(np.float32)
    return m


def kernel(**inputs):
    net = build()
    in_maps = [make_inputs(inputs, b) for b in range(8)]
    res = run_bass_kernel_spmd(net.nc, in_maps, core_ids=list(range(8)))
    return np.stack([np.asarray(res.results[b]["y"]) for b in range(8)], axis=0).astype(np.float32)
```

```python
import math
from contextlib import ExitStack

import numpy as np
import concourse.bass as bass
import concourse.mybir as mybir
from concourse.bass_utils import run_bass_kernel_spmd

F32 = mybir.dt.float32
BF16 = mybir.dt.bfloat16
I32 = mybir.dt.int32
AF = mybir.ActivationFunctionType
ALU = mybir.AluOpType
AX = mybir.AxisListType

D = 1024
T = 4096
DEPTH = 4
NT = T // 128
TB = 512
NB = T // TB
H = 8
QR = 384
KVR = 256
NOPE = 128
ROPE = 64
VD = 128
N_IN = 10976
C_DQ = 0
C_DKV = 384
C_KR = 640
C_Z = 704
C_XBC = 2752
C_DT = 6848
C_QM = 6880
C_G = 7904
DN_ALPHA = (2 * DEPTH) ** 0.25
NORM_EPS = 1e-5
RMS_EPS = 1e-6
SCALE_A = (NOPE + ROPE) ** -0.5

EPOCH = 30000
import os
SSD_PIPE = int(os.environ.get('SSD_PIPE', '1'))
SSD_HOIST = int(os.environ.get('SSD_HOIST', '1'))
_UID = [0]


def _uid(name):
    _UID[0] += 1
    return f"{name}_{_UID[0]}"


def _runs(ap):
    dims = [(int(st), int(n)) for st, n in ap.ap]
    total = 1
    for st, n in dims:
        total *= n
    run = 1
    exp = 1
    for st, n in reversed(dims[1:] if len(dims) > 1 else dims):
        if st == exp:
            run *= n
            exp *= n
        else:
            break
    return max(total // max(run, 1), 1)


_BANK_OF = {'ST': lambda i: i, 'SEG': lambda i: i, 'TP': lambda i: i, 'oacc': lambda i: 2 + i, 'CB': lambda i: 2 + i,
            'YS': lambda i: 2 + i, 'YI': lambda i: 4 if i == 0 else 6, 'SU': lambda i: 4 if i == 0 else 6}
_BANK_NAMES = {'ps0': 0, 'ps1': 1, 'ps2': 2, 'ps3': 3, 'ps4': 4, 'ps5': 5, 'ps6': 6, 'psb': 7, 'osum': 4}


def _banks(tokens):
    out = []
    for t in tokens:
        if isinstance(t, str):
            b = _BANK_NAMES.get(t)
        elif isinstance(t, tuple) and t and t[0] in _BANK_OF and len(t) == 2:
            b = _BANK_OF[t[0]](t[1])
        else:
            b = None
        if b is not None:
            out.append(('bank', b))
    return out


class Prog:
    def __init__(self):
        self.nc = bass.Bass("TRN2", target_bir_lowering=False)
        nc = self.nc
        self.es = ExitStack()
        self.eng = dict(pe=nc.tensor, act=nc.scalar, dve=nc.vector, pool=nc.gpsimd, sp=nc.sync)
        self.cnt = {e: 0 for e in self.eng}
        self.sems = {e: [] for e in self.eng}
        self.waited = {e: {} for e in self.eng}
        self.lastw = {}
        self.readers = {}
        self.dsem = {}
        self.dcnt = {}
        self.nsem = 0
        self.ninst = 0

    def _new_sem(self, name):
        self.nsem += 1
        return self.es.enter_context(self.nc.semaphore(name))

    def _esem(self, e, n):
        ep = (n - 1) // EPOCH
        while len(self.sems[e]) <= ep:
            self.sems[e].append(self._new_sem(f"s_{e}_{len(self.sems[e])}"))
        return self.sems[e][ep], (n - 1) % EPOCH + 1

    def _wait(self, c, ev):
        if ev is None:
            return
        kind, p, n = ev
        if kind == 'e':
            if p == c:
                if c == 'pe' or n > self.cnt[c] or n < self.cnt[c] - 1:
                    return
            if self.waited[c].get(p, 0) >= n:
                return
            sem, val = self._esem(p, n)
            self.eng[c].wait_ge(sem, val)
            self.waited[c][p] = n
        else:
            k = ('d', p)
            if self.waited[c].get(k, 0) >= n:
                return
            self.eng[c].wait_ge(self.dsem[p], 16 * n)
            self.waited[c][k] = n

    def _deps(self, reads, writes):
        deps = []
        for r in reads:
            ev = self.lastw.get(r)
            if ev is not None:
                deps.append(ev)
        for w in writes:
            ev = self.lastw.get(w)
            if ev is not None:
                deps.append(ev)
            rd = self.readers.get(w)
            if rd:
                deps.extend(rd.values())
        return deps

    def _record(self, ev, reads, writes):
        key = (ev[0], ev[1])
        for r in reads:
            self.readers.setdefault(r, {})[key] = ev
        for w in writes:
            self.lastw[w] = ev
            self.readers[w] = {}

    def op(self, e, fn, reads=(), writes=(), inc=True):
        bk = _banks(reads) + _banks(writes)
        if bk:
            writes = list(writes) + bk
        for ev in self._deps(reads, writes):
            self._wait(e, ev)
        inst = fn()
        self.ninst += 1
        if inc:
            self.cnt[e] += 1
            n = self.cnt[e]
            sem, _ = self._esem(e, n)
            inst.then_inc(sem, 1)
            ev = ('e', e, n)
        else:
            ev = ('e', e, self.cnt[e] + 1)
        self._record(ev, reads, writes)
        return ev

    def dma(self, q, out, in_, key, reads=(), writes=()):
        if key not in self.dsem:
            self.dsem[key] = self._new_sem(f"d_{key}")
            self.dcnt[key] = 0
        kres = ('dmakey', key)
        for ev in self._deps(reads, list(writes) + [kres]):
            self._wait(q, ev)
        inst = self.eng[q].dma_start(out=out, in_=in_)
        self.ninst += 1
        self.ndma = getattr(self, 'ndma', 0) + 1
        self.ndesc = getattr(self, 'ndesc', 0) + max(_runs(out), _runs(in_))
        self.dcnt[key] += 1
        inst.then_inc(self.dsem[key], 16)
        ev = ('d', key, self.dcnt[key])
        self._record(ev, reads, list(writes) + [kres])
        return ev

    def barrier(self):
        for c in self.eng:
            for p in self.eng:
                if p != c and self.cnt[p] > 0:
                    self._wait(c, ('e', p, self.cnt[p]))
            for k, m in self.dcnt.items():
                if m > 0:
                    self._wait(c, ('d', k, m))
        self.lastw.clear()
        self.readers.clear()

    def mm(self, out, pairs, wres, reads):
        nc = self.nc
        n = len(pairs)
        for i, (l, r) in enumerate(pairs):
            self.op('pe', lambda l=l, r=r, i=i: nc.tensor.matmul(out, l, r, start=(i == 0), stop=(i == n - 1)),
                    reads=reads if i == 0 else (), writes=[wres], inc=(i == n - 1))
        ev = ('e', 'pe', self.cnt['pe'])
        self._record(ev, reads, ())


class Net:
    def __init__(self, n_layers=DEPTH, debug=None):
        self.P = Prog()
        self.nc = self.P.nc
        self.n_layers = n_layers
        self.debug = debug or {}
        self.dbg_out = {}

    def declare(self):
        nc = self.nc
        L = DEPTH
        def inp(name, shape, dt=F32):
            return nc.dram_tensor(name, list(shape), dt, kind="ExternalInput").ap()
        self.x_in = inp("x", [T, D])
        self.mem_in = inp("mem", [256, D])
        self.pos_in = inp("positions", [1, T], I32)
        self.w_in = inp("w_in", [L, D, N_IN])
        self.q_norm = inp("q_norm", [L, QR])
        self.w_uq = inp("w_uq", [L, QR, H * 192])
        self.kv_norm = inp("kv_norm", [L, KVR])
        self.w_ukv = inp("w_ukv", [L, KVR, H, 256])
        self.w_proj_a = inp("w_proj_a", [L, D, D])
        self.conv_w = inp("conv_w", [L, 4, 4096])
        self.conv_b = inp("conv_b", [L, 4096])
        self.dt_bias = inp("dt_bias", [L, 32])
        self.a_log = inp("a_log", [L, 32])
        self.d_skip = inp("d_skip", [L, 32])
        self.ssm_norm = inp("ssm_norm", [L, 2048])
        self.w_proj_b = inp("w_proj_b", [L, 2048, D])
        self.w_mem_kv = inp("w_mem_kv", [L, D, 2048])
        self.w_proj_c = inp("w_proj_c", [L, D, D])
        self.w_out = inp("w_out", [L, D, D])
        self.ln1_g = inp("ln1_g", [L, D])
        self.ln1_b = inp("ln1_b", [L, D])
        self.router_w = inp("router_w", [D, 16])
        self.router_bias = inp("router_bias", [1, 16])
        self.exp_w1 = inp("exp_w1", [L, 16, D, 512])
        self.exp_w3 = inp("exp_w3", [L, 16, D, 512])
        self.exp_w2 = inp("exp_w2", [L, 16, 512, D])
        self.ln2_g = inp("ln2_g", [L, D])
        self.ln2_b = inp("ln2_b", [L, D])
        self.inv_freq = inp("inv_freq", [64, 1])
        self.y_out = nc.dram_tensor("y", [T, D], F32, kind="ExternalOutput").ap()

        def scr(name, shape, dt):
            return nc.dram_tensor(name, list(shape), dt, kind="Internal").ap()
        self.xT_d = scr("xT_d", [128, 8, T], BF16)
        self.x1T_d = scr("x1T_d", [128, 8, T], BF16)
        self.xa_d = scr("xa_d", [T, D], F32)
        self.x1_d = scr("x1_d", [T, D], F32)
        self.mA_d = scr("mA_d", [128, 8, T], BF16)
        self.mB_d = scr("mB_d", [128, 8, T], BF16)
        self.mC_d = scr("mC_d", [128, 8, T], BF16)
        self.xsB_d = scr("xsB_d", [T, 3072], BF16)
        self.BCT_d = scr("BCT_d", [128, 16, T], BF16)
        self.ynT_d = scr("ynT_d", [128, 16, T], BF16)
        self.gT_d = scr("gT_d", [16, T], F32)
        self.cos_d = scr("cos_d", [64, T], F32)
        self.sin_d = scr("sin_d", [64, T], F32)

    def dbg(self, name, shape, dt=F32):
        ap = self.nc.dram_tensor(name, list(shape), dt, kind="ExternalOutput").ap()
        self.dbg_out[name] = ap
        return ap


def _setup_globals(self):
    P, nc = self.P, self.nc
    es = P.es
    sb = lambda name, shape, dt: es.enter_context(nc.sbuf_tensor(_uid(name), shape, dt))
    self.ps = [es.enter_context(nc.psum_tensor(f"ps{i}", [128, 512], F32)) for i in range(7)]
    self.psb = es.enter_context(nc.psum_tensor("psb", [128, 1024], BF16))
    self.ident_bf = sb("ident_bf", [128, 128], BF16)
    self.ident_f = sb("ident_f", [128, 128], F32)
    self.ones_bf = sb("ones_bf", [128, 128], BF16)
    self.ones_f = sb("ones_f", [128, 128], F32)
    self.mask_le = sb("mask_le", [128, 128], BF16)
    self.tri_le_f = sb("tri_le_f", [128, 128], F32)
    self.tri_gt_f = sb("tri_gt_f", [128, 128], F32)
    g = nc.gpsimd
    for tl, val in ((self.ident_bf, 0.0), (self.ident_f, 0.0)):
        P.op('pool', lambda tl=tl: g.memset(tl[:], 0.0), writes=[('const', tl.name)])
        P.op('pool', lambda tl=tl: g.affine_select(out=tl[:], in_=tl[:], pattern=[[-1, 128]],
                                                   compare_op=ALU.not_equal, fill=1.0, base=0,
                                                   channel_multiplier=1), writes=[('const', tl.name)])
    P.op('pool', lambda: g.memset(self.ones_bf[:], 1.0), writes=[('const', 'ones_bf')])
    P.op('pool', lambda: g.memset(self.ones_f[:], 1.0), writes=[('const', 'ones_f')])
    for tl in (self.mask_le, self.tri_le_f):
        P.op('pool', lambda tl=tl: g.memset(tl[:], 1.0), writes=[('const', tl.name)])
        P.op('pool', lambda tl=tl: g.affine_select(out=tl[:], in_=tl[:], pattern=[[1, 128]],
                                                   compare_op=ALU.is_ge, fill=0.0, base=0,
                                                   channel_multiplier=-1), writes=[('const', tl.name)])
    P.op('pool', lambda: g.memset(self.tri_gt_f[:], 1.0), writes=[('const', 'tri_gt_f')])
    P.op('pool', lambda: g.affine_select(out=self.tri_gt_f[:], in_=self.tri_gt_f[:], pattern=[[-1, 128]],
                                         compare_op=ALU.is_gt, fill=0.0, base=0,
                                         channel_multiplier=1), writes=[('const', 'tri_gt_f')])
    self.wkey = 0
    P.barrier()


def _load_w(self, dst, src, tok, nk, q='pool'):
    P = self.P
    for kc in range(nk):
        key = f"w{self.wkey % 8}"
        self.wkey += 1
        P.dma(q, out=dst(kc), in_=src(kc), key=key, writes=[(tok, kc)])


def _toks(tok, nk):
    return [(tok, kc) for kc in range(nk)]


Net.setup_globals = _setup_globals
Net.load_w = _load_w


def _prologue(self):
    P, nc = self.P, self.nc
    with ExitStack() as es:
        sb = lambda name, shape, dt: es.enter_context(nc.sbuf_tensor(_uid(name), shape, dt))
        posi = sb("posi", [64, T], I32)
        ang = sb("ang", [64, T], F32)
        red = sb("red", [64, T], F32)
        tab = sb("tab", [64, T], F32)
        invf = sb("invf", [64, 1], F32)
        negpi = sb("negpi", [64, 1], F32)
        P.dma('sp', out=posi[:], in_=self.pos_in[0, :].partition_broadcast(64), key="ld0", writes=['posi'])
        P.dma('sp', out=invf[:], in_=self.inv_freq[:, :], key="ld1", writes=['invf'])
        P.op('dve', lambda: nc.vector.memset(negpi[:], -math.pi), writes=['negpi'])
        P.op('dve', lambda: nc.vector.tensor_copy(ang[:], posi[:]), reads=['posi'], writes=['ang'])
        P.op('dve', lambda: nc.vector.tensor_scalar(ang[:], ang[:], invf[:, 0:1], None, op0=ALU.mult),
             reads=['invf'], writes=['ang'])
        ki = sb("ki", [64, T], I32)
        kf = sb("kf", [64, T], F32)
        C1 = 6.28125
        C2 = 2 * math.pi - C1
        P.op('dve', lambda: nc.vector.tensor_scalar(red[:], ang[:], 1.0 / (2 * math.pi), None, op0=ALU.mult),
             reads=['ang'], writes=['red'])
        P.op('dve', lambda: nc.vector.tensor_copy(ki[:], red[:]), reads=['red'], writes=['ki'])
        P.op('dve', lambda: nc.vector.tensor_copy(kf[:], ki[:]), reads=['ki'], writes=['kf'])
        P.op('dve', lambda: nc.vector.scalar_tensor_tensor(out=red[:], in0=kf[:], scalar=-C1, in1=ang[:],
                                                           op0=ALU.mult, op1=ALU.add), reads=['kf', 'ang'], writes=['red'])
        P.op('dve', lambda: nc.vector.scalar_tensor_tensor(out=red[:], in0=kf[:], scalar=-C2, in1=red[:],
                                                           op0=ALU.mult, op1=ALU.add), reads=['kf', 'red'], writes=['red'])

        def wrap_sin(shift):
            P.op('dve', lambda: nc.vector.tensor_scalar(ang[:], red[:], shift, None, op0=ALU.add),
                 reads=['red'], writes=['ang'])
            P.op('dve', lambda: nc.vector.tensor_scalar(kf[:], ang[:], math.pi, 2 * math.pi, op0=ALU.is_gt, op1=ALU.mult),
                 reads=['ang'], writes=['kf'])
            P.op('dve', lambda: nc.vector.tensor_tensor(ang[:], ang[:], kf[:], op=ALU.subtract),
                 reads=['ang', 'kf'], writes=['ang'])
            P.op('dve', lambda: nc.vector.tensor_scalar(ang[:], ang[:], -math.pi, math.pi, op0=ALU.max, op1=ALU.min),
                 reads=['ang'], writes=['ang'])
            P.op('act', lambda: nc.scalar.activation(out=tab[:], in_=ang[:], func=AF.Sin),
                 reads=['ang'], writes=['tab'])
        wrap_sin(0.0)
        P.op('dve', lambda: nc.vector.tensor_scalar(tab[0:32, :], tab[0:32, :], -1.0, None, op0=ALU.mult),
             reads=['tab'], writes=['tab'])
        P.dma('sp', out=self.sin_d[:, :], in_=tab[:], key="st0", reads=['tab'], writes=['sin_d'])
        wrap_sin(0.5 * math.pi)
        P.dma('sp', out=self.cos_d[:, :], in_=tab[:], key="st1", reads=['tab'], writes=['cos_d'])
        xin = [sb(f"xin{i}", [128, D], F32) for i in range(2)]
        xbf = [sb(f"xbf{i}", [128, D], BF16) for i in range(2)]
        stg = [sb(f"stg{i}", [128, 8, TB], BF16) for i in range(2)]
        for t in range(NT):
            i = t % 2
            b, tt = divmod(t, 4)
            P.dma('sp', out=xin[i][:], in_=self.x_in[t * 128:(t + 1) * 128, :], key=f"ld{2 + i}", writes=[('xin', i)])
            P.op('dve', lambda i=i: nc.vector.tensor_copy(xbf[i][:], xin[i][:]), reads=[('xin', i)], writes=[('xbf', i)])
            for kc in range(8):
                P.op('pe', lambda i=i, kc=kc: nc.tensor.transpose(self.psb[:, kc * 128:(kc + 1) * 128],
                                                                   xbf[i][:, kc * 128:(kc + 1) * 128], self.ident_bf[:]),
                     reads=[('xbf', i)], writes=['psb'], inc=(kc == 7))
            P.op('act', lambda b=b, tt=tt: nc.scalar.copy(stg[b % 2][:, :, tt * 128:(tt + 1) * 128],
                                                         self.psb[:].rearrange("p (k n) -> p k n", k=8)),
                 reads=['psb'], writes=[('stg', b % 2)])
            if tt == 3:
                P.dma('sp', out=self.xT_d[:, :, b * TB:(b + 1) * TB], in_=stg[b % 2][:], key=f"st{2 + b % 2}",
                      reads=[('stg', b % 2)], writes=[('xT_d', b)])
    P.barrier()


Net.prologue = _prologue


def _pass_mla(self, l):
    P, nc = self.P, self.nc
    ps = self.ps
    with ExitStack() as es:
        sb = lambda name, shape, dt: es.enter_context(nc.sbuf_tensor(_uid(name), shape, dt))
        Wmla = sb("Wmla", [128, 8, 640], BF16)
        Wkr = sb("Wkr", [128, 8, 128], BF16)
        WgA = sb("WgA", [128, 8, D], BF16)
        wuq = sb("wuq", [128, 3, H * 192], BF16)
        wuqsw = sb("wuqsw", [128, 3, H, 64], BF16)
        wukv = sb("wukv", [128, 2, H, 256], BF16)
        wukT = sb("wukT", [128, H, 256], BF16)
        wpa = sb("wpa", [128, 8, D], BF16)
        gq = sb("gq", [128, QR], F32)
        gkv = sb("gkv", [128, KVR], F32)
        w_in = self.w_in
        rows = lambda kc: slice(kc * 128, (kc + 1) * 128)
        self.load_w(lambda kc: Wmla[:, kc, :], lambda kc: w_in[l, rows(kc), 0:640], 'Wmla', 8)
        self.load_w(lambda kc: Wkr[:, kc, 0:64], lambda kc: w_in[l, rows(kc), C_KR:C_KR + 64], 'Wkr', 8)
        self.load_w(lambda kc: wuq[:, kc, :], lambda kc: self.w_uq[l, rows(kc), :], 'wuq', 3)
        self.load_w(lambda kc: wukv[:, kc, :, :], lambda kc: self.w_ukv[l, rows(kc), :, :], 'wukv', 2)
        P.dma('sp', out=gq[:], in_=self.q_norm[l, :].partition_broadcast(128), key="ld0", writes=['gq'])
        P.dma('sp', out=gkv[:], in_=self.kv_norm[l, :].partition_broadcast(128), key="ld1", writes=['gkv'])
        self.load_w(lambda kc: wpa[:, kc, :], lambda kc: self.w_proj_a[l, rows(kc), :], 'wpa', 8)
        self.load_w(lambda kc: WgA[:, kc, :], lambda kc: w_in[l, rows(kc), C_G:C_G + D], 'WgA', 8)
        for kc in range(8):
            P.op('dve', lambda kc=kc: nc.vector.tensor_copy(Wkr[:, kc, 64:96], Wkr[:, kc, 32:64]),
                 reads=[('Wkr', kc)], writes=[('Wkrs', kc)])
            P.op('dve', lambda kc=kc: nc.vector.tensor_copy(Wkr[:, kc, 96:128], Wkr[:, kc, 0:32]),
                 reads=[('Wkr', kc)], writes=[('Wkrs', kc)])
        for kc in range(3):
            v = wuq[:, kc, :].rearrange("p (h c) -> p h c", h=H)
            P.op('dve', lambda kc=kc, v=v: nc.vector.tensor_copy(wuqsw[:, kc, :, 0:32], v[:, :, 160:192]),
                 reads=[('wuq', kc)], writes=[('wuqsw', kc)])
            P.op('dve', lambda kc=kc, v=v: nc.vector.tensor_copy(wuqsw[:, kc, :, 32:64], v[:, :, 128:160]),
                 reads=[('wuq', kc)], writes=[('wuqsw', kc)])
        for h in range(H):
            for cc in range(2):
                P.op('pe', lambda h=h, cc=cc: nc.tensor.transpose(self.psb[:, cc * 128:(cc + 1) * 128],
                                                                   wukv[:, cc, h, 0:128], self.ident_bf[:]),
                     reads=[('wukv', cc)], writes=['psb'], inc=(cc == 1))
            P.op('act', lambda h=h: nc.scalar.copy(wukT[:, h, :], self.psb[:, 0:256]), reads=['psb'], writes=[('wukT', h)])

        ckvT = sb("ckvT", [128, 2, T], BF16)
        krT = sb("krT", [64, T], BF16)
        ckvTM = sb("ckvTM", [128, NT, KVR], BF16)
        xblk = [sb(f"xblk{i}", [128, 8, TB], BF16) for i in range(2)]
        cosb = [sb(f"cosb{i}", [64, TB], F32) for i in range(2)]
        sinb = [sb(f"sinb{i}", [64, TB], F32) for i in range(2)]
        cqTM = sb("cqTM", [128, QR], BF16)
        cqT = sb("cqT", [128, 3, TB], BF16)
        junk = sb("junk", [128, 512], F32)
        ss = sb("ss", [128, 8], F32)
        qn = sb("qn", [128, TB], BF16)
        qlat = sb("qlat", [128, H, 2, TB], BF16)
        qrope = sb("qrope", [64, H, TB], BF16)
        rtmp = sb("rtmp", [64, TB], F32)
        rtmp2 = sb("rtmp2", [64, TB], F32)
        PT = [sb(f"PT{i}", [128, TB], BF16) for i in range(3)]
        rs = sb("rs", [128, TB], F32)
        olat = [sb(f"olat{i}", [128, 2, TB], BF16) for i in range(2)]
        oT = sb("oT", [128, H, TB], BF16)
        sig = sb("sig", [128, TB], F32)
        mT = sb("mT", [128, 8, TB], BF16)
        W = lambda name, n: _toks(name, n)

        for b in range(NB):
            i2 = b % 2
            t0 = b * TB
            P.dma('sp', out=xblk[i2][:], in_=self.xT_d[:, :, t0:t0 + TB], key=f"ld{2 + i2}", writes=[('xblk', i2)])
            P.dma('sp', out=cosb[i2][:], in_=self.cos_d[:, t0:t0 + TB], key=f"ld{4 + i2}", writes=[('cosb', i2)])
            P.dma('sp', out=sinb[i2][:], in_=self.sin_d[:, t0:t0 + TB], key=f"ld{6 + i2}", writes=[('sinb', i2)])
            xb = xblk[i2]
            for tt in range(4):
                t = b * 4 + tt
                cols = slice(tt * 128, (tt + 1) * 128)
                P.mm(ps[5][:, 0:512], [(xb[:, kc, cols], Wmla[:, kc, 0:512]) for kc in range(8)], 'ps5',
                     [('xblk', i2)] + W('Wmla', 8))
                P.mm(ps[6][:, 0:128], [(xb[:, kc, cols], Wmla[:, kc, 512:640]) for kc in range(8)], 'ps6',
                     [('xblk', i2)] + W('Wmla', 8))
                P.op('dve', lambda: nc.vector.memset(ss[:, 0:3], 0.0), writes=['ss0', 'ss1', 'ss2'])
                P.op('act', lambda: nc.scalar.activation(out=junk[:, 0:384], in_=ps[5][:, 0:384], func=AF.Square,
                                                         accum_out=ss[:, 0:1]), reads=['ps5'], writes=['junk', 'ss0'])
                P.op('act', lambda: nc.scalar.activation(out=junk[:, 384:512], in_=ps[5][:, 384:512], func=AF.Square,
                                                         accum_out=ss[:, 1:2]), reads=['ps5'], writes=['junk', 'ss1'])
                P.op('act', lambda: nc.scalar.activation(out=junk[:, 0:128], in_=ps[6][:, 0:128], func=AF.Square,
                                                         accum_out=ss[:, 2:3]), reads=['ps6'], writes=['junk', 'ss2'])
                P.op('dve', lambda: nc.vector.tensor_scalar(ss[:, 4:5], ss[:, 0:1], 1.0 / QR, RMS_EPS, op0=ALU.mult, op1=ALU.add),
                     reads=['ss0'], writes=['ss4'])
                P.op('act', lambda: nc.scalar.activation(out=ss[:, 6:7], in_=ss[:, 4:5], func=AF.Sqrt), reads=['ss4'], writes=['ss6'])
                P.op('dve', lambda: nc.vector.reciprocal(ss[:, 4:5], ss[:, 6:7]), reads=['ss6'], writes=['ss4'])
                P.op('dve', lambda: nc.vector.tensor_tensor(ss[:, 5:6], ss[:, 1:2], ss[:, 2:3], op=ALU.add),
                     reads=['ss1', 'ss2'], writes=['ss5'])
                P.op('dve', lambda: nc.vector.tensor_scalar(ss[:, 5:6], ss[:, 5:6], 1.0 / KVR, RMS_EPS, op0=ALU.mult, op1=ALU.add),
                     reads=['ss5'], writes=['ss5'])
                P.op('act', lambda: nc.scalar.activation(out=ss[:, 7:8], in_=ss[:, 5:6], func=AF.Sqrt), reads=['ss5'], writes=['ss7'])
                P.op('dve', lambda: nc.vector.reciprocal(ss[:, 5:6], ss[:, 7:8]), reads=['ss7'], writes=['ss5'])
                P.op('dve', lambda: nc.vector.scalar_tensor_tensor(out=cqTM[:], in0=ps[5][:, 0:384], scalar=ss[:, 4:5],
                                                                   in1=gq[:], op0=ALU.mult, op1=ALU.mult),
                     reads=['ps5', 'ss4', 'gq'], writes=['cqTM'])
                P.op('dve', lambda t=t: nc.vector.scalar_tensor_tensor(out=ckvTM[:, t, 0:128], in0=ps[5][:, 384:512],
                                                                       scalar=ss[:, 5:6], in1=gkv[:, 0:128],
                                                                       op0=ALU.mult, op1=ALU.mult),
                     reads=['ps5', 'ss5', 'gkv'], writes=[('ckvTM', t)])
                P.op('dve', lambda t=t: nc.vector.scalar_tensor_tensor(out=ckvTM[:, t, 128:256], in0=ps[6][:, 0:128],
                                                                       scalar=ss[:, 5:6], in1=gkv[:, 128:256],
                                                                       op0=ALU.mult, op1=ALU.mult),
                     reads=['ps6', 'ss5', 'gkv'], writes=[('ckvTM', t)])
                for j in range(3):
                    P.op('pe', lambda j=j: nc.tensor.transpose(self.psb[:, j * 128:(j + 1) * 128],
                                                               cqTM[:, j * 128:(j + 1) * 128], self.ident_bf[:]),
                         reads=['cqTM'], writes=['psb'], inc=False)
                for j in range(2):
                    P.op('pe', lambda j=j, t=t: nc.tensor.transpose(self.psb[:, (3 + j) * 128:(4 + j) * 128],
                                                                    ckvTM[:, t, j * 128:(j + 1) * 128], self.ident_bf[:]),
                         reads=[('ckvTM', t)], writes=['psb'], inc=(j == 1))
                P.op('pool' if False else 'act', lambda cols=cols: nc.scalar.copy(
                    cqT[:, :, cols], self.psb[:, 0:384].rearrange("p (k n) -> p k n", k=3)),
                     reads=['psb'], writes=['cqT'])
                P.op('act', lambda t=t: nc.scalar.copy(
                    ckvT[:, :, t * 128:(t + 1) * 128], self.psb[:, 384:640].rearrange("p (k n) -> p k n", k=2)),
                     reads=['psb'], writes=[('ckvT', t)])
            P.mm(ps[5][0:64, :], [(Wkr[:, kc, 0:64], xb[:, kc, :]) for kc in range(8)], 'ps5',
                 [('xblk', i2)] + W('Wkr', 8))
            P.mm(ps[6][0:64, :], [(Wkr[:, kc, 64:128], xb[:, kc, :]) for kc in range(8)], 'ps6',
                 [('xblk', i2)] + W('Wkrs', 8))

            def rope(psA, psB, outap, rA, rB, wtok):
                P.op('dve', lambda: nc.vector.tensor_tensor(rtmp[:], psB, sinb[i2][:], op=ALU.mult),
                     reads=[rB, ('sinb', i2)], writes=['rtmp'])
                P.op('dve', lambda: nc.vector.tensor_tensor(rtmp2[:], psA, cosb[i2][:], op=ALU.mult),
                     reads=[rA, ('cosb', i2)], writes=['rtmp2'])
                P.op('dve', lambda: nc.vector.tensor_tensor(outap, rtmp[:], rtmp2[:], op=ALU.add),
                     reads=['rtmp', 'rtmp2'], writes=[wtok])
            rope(ps[5][0:64, :], ps[6][0:64, :], krT[:, t0:t0 + TB], 'ps5', 'ps6', ('krT', b))

            for h in range(H):
                c0 = h * 192
                P.mm(ps[5][:, :], [(wuq[:, kc, c0:c0 + 128], cqT[:, kc, :]) for kc in range(3)], 'ps5',
                     ['cqT'] + W('wuq', 3))
                P.op('act', lambda: nc.scalar.copy(qn[:], ps[5][:, :]), reads=['ps5'], writes=['qn'])
                for cc in range(2):
                    pb = ps[6] if cc == 0 else ps[5]
                    pt = 'ps6' if cc == 0 else 'ps5'
                    P.mm(pb[:, :], [(wukT[:, h, cc * 128:(cc + 1) * 128], qn[:])], pt, ['qn', ('wukT', h)])
                    P.op('dve', lambda h=h, cc=cc, pb=pb: nc.vector.tensor_copy(qlat[:, h, cc, :], pb[:, :]),
                         reads=[pt], writes=[('qlat', h)])
                P.mm(ps[6][0:64, :], [(wuq[:, kc, c0 + 128:c0 + 192], cqT[:, kc, :]) for kc in range(3)], 'ps6',
                     ['cqT'] + W('wuq', 3))
                P.mm(ps[5][0:64, :], [(wuqsw[:, kc, h, :], cqT[:, kc, :]) for kc in range(3)], 'ps5',
                     ['cqT'] + W('wuqsw', 3))
                rope(ps[6][0:64, :], ps[5][0:64, :], qrope[:, h, :], 'ps6', 'ps5', ('qrope', h))

            nkt = 4 * b + 4
            items = [(h, kt) for h in range(H) for kt in range(nkt)]

            def stage_S(i):
                h, kt = items[i]
                d = kt - 4 * b
                q0 = max(d, 0) * 128
                qs = slice(q0, TB)
                si, pi = i % 2, i % 3
                st = ps[si]
                kk = slice(kt * 128, (kt + 1) * 128)
                P.mm(st[:, qs], [(ckvT[:, 0, kk], qlat[:, h, 0, qs]), (ckvT[:, 1, kk], qlat[:, h, 1, qs]),
                                 (krT[:, kk], qrope[:, h, qs])], ('ST', si),
                     [('ckvT', kt), ('krT', kt // 4), ('qlat', h), ('qrope', h)])
                P.op('act', lambda: nc.scalar.activation(out=PT[pi][:, qs], in_=st[:, qs], func=AF.Exp, scale=SCALE_A),
                     reads=[('ST', si)], writes=[('PT', pi)])
                if d >= 0:
                    dq = slice(q0, q0 + 128)
                    P.op('pool', lambda: nc.gpsimd.tensor_tensor(PT[pi][:, dq], PT[pi][:, dq], self.mask_le[:], op=ALU.mult),
                         reads=[('PT', pi)], writes=[('PT', pi)])

            def stage_V(i):
                h, kt = items[i]
                d = kt - 4 * b
                q0 = max(d, 0) * 128
                qs = slice(q0, TB)
                pi = i % 3
                first, last = (kt == 0), (kt == nkt - 1)
                for cc in range(2):
                    P.op('pe', lambda cc=cc: nc.tensor.matmul(
                        ps[2 + cc][:, qs], ckvTM[:, kt, cc * 128:(cc + 1) * 128], PT[pi][:, qs], start=first, stop=last),
                         reads=[('PT', pi), ('ckvTM', kt)], writes=[('oacc', cc)], inc=False)
                P.op('pe', lambda: nc.tensor.matmul(ps[4][:, qs], self.ones_bf[:], PT[pi][:, qs], start=first, stop=last),
                     reads=[('PT', pi)], writes=['osum'], inc=True)
                P._record(('e', 'pe', P.cnt['pe']), [('PT', pi), ('ckvTM', kt)], [('oacc', 0), ('oacc', 1)])
                if last:
                    ol = olat[h % 2]
                    P.op('dve', lambda: nc.vector.reciprocal(rs[:], ps[4][:, :]), reads=['osum'], writes=['rs'])
                    for cc in range(2):
                        P.op('dve', lambda cc=cc: nc.vector.tensor_tensor(ol[:, cc, :], ps[2 + cc][:, :], rs[:], op=ALU.mult),
                             reads=[('oacc', cc), 'rs'], writes=[('olat', h % 2)])

            def proj_o(h):
                ol = olat[h % 2]
                P.mm(ps[5][:, :], [(wukv[:, cc, h, 128:256], ol[:, cc, :]) for cc in range(2)], 'ps5',
                     [('olat', h % 2)] + W('wukv', 2))
                P.op('act', lambda: nc.scalar.copy(oT[:, h, :], ps[5][:, :]), reads=['ps5'], writes=[('oT', h)])

            pending = []
            stage_S(0)
            for i in range(len(items)):
                if i + 1 < len(items):
                    stage_S(i + 1)
                stage_V(i)
                h, kt = items[i]
                if kt == nkt - 1:
                    pending.append((i + 2, h))
                while pending and pending[0][0] <= i:
                    proj_o(pending.pop(0)[1])
            for _, h in pending:
                proj_o(h)

            for dc in range(8):
                dd = slice(dc * 128, (dc + 1) * 128)
                P.mm(ps[5][:, :], [(WgA[:, kc, dd], xb[:, kc, :]) for kc in range(8)], 'ps5',
                     [('xblk', i2)] + W('WgA', 8))
                P.op('act', lambda: nc.scalar.activation(out=sig[:], in_=ps[5][:, :], func=AF.Sigmoid),
                     reads=['ps5'], writes=['sig'])
                P.mm(ps[6][:, :], [(wpa[:, hh, dd], oT[:, hh, :]) for hh in range(H)], 'ps6',
                     [('oT', hh) for hh in range(H)] + W('wpa', 8))
                P.op('dve', lambda dc=dc: nc.vector.tensor_tensor(mT[:, dc, :], ps[6][:, :], sig[:], op=ALU.mult),
                     reads=['ps6', 'sig'], writes=[('mT', dc)])
            P.dma('sp', out=self.mA_d[:, :, t0:t0 + TB], in_=mT[:], key="st0",
                  reads=[('mT', dc) for dc in range(8)], writes=[('mA_d', b)])
    P.barrier()


Net.pass_mla = _pass_mla


def _pass_mem(self, l):
    P, nc = self.P, self.nc
    ps = self.ps
    SC = 256 ** -0.5
    with ExitStack() as es:
        sb = lambda name, shape, dt: es.enter_context(nc.sbuf_tensor(_uid(name), shape, dt))
        Wqm = sb("Wqm", [128, 8, D], BF16)
        WgC = sb("WgC", [128, 8, D], BF16)
        wpc = sb("wpc", [128, 8, D], BF16)
        KT = sb("KT", [128, 4, 2, 256], BF16)
        V = sb("V", [128, 2, D], BF16)
        rows = lambda kc: slice(kc * 128, (kc + 1) * 128)
        W = lambda name, n: _toks(name, n)
        self.load_w(lambda kc: Wqm[:, kc, :], lambda kc: self.w_in[l, rows(kc), C_QM:C_QM + D], 'Wqm', 8)
        self.load_w(lambda kc: WgC[:, kc, :], lambda kc: self.w_in[l, rows(kc), C_G + 2 * D:C_G + 3 * D], 'WgC', 8)
        self.load_w(lambda kc: wpc[:, kc, :], lambda kc: self.w_proj_c[l, rows(kc), :], 'wpc', 8)
        with ExitStack() as es2:
            sb2 = lambda name, shape, dt: es2.enter_context(nc.sbuf_tensor(_uid(name), shape, dt))
            wmkv = sb2("wmkv", [128, 8, 2048], BF16)
            memT = sb2("memT", [128, 8, 256], BF16)
            mtile = sb2("mtile", [128, D], BF16)
            self.load_w(lambda kc: wmkv[:, kc, :], lambda kc: self.w_mem_kv[l, rows(kc), :], 'wmkv', 8)
            for mt in range(2):
                P.dma('pool', out=mtile[:], in_=self.mem_in[mt * 128:(mt + 1) * 128, :], key="ld0", writes=['mtile'])
                for kc in range(8):
                    P.op('pe', lambda kc=kc: nc.tensor.transpose(self.psb[:, kc * 128:(kc + 1) * 128],
                                                                 mtile[:, kc * 128:(kc + 1) * 128], self.ident_bf[:]),
                         reads=['mtile'], writes=['psb'], inc=(kc == 7))
                P.op('act', lambda mt=mt: nc.scalar.copy(memT[:, :, mt * 128:(mt + 1) * 128],
                                                         self.psb[:].rearrange("p (k n) -> p k n", k=8)),
                     reads=['psb'], writes=['memT'])
            for h in range(4):
                for dc in range(2):
                    c0 = h * 256 + dc * 128
                    P.mm(ps[5][:, 0:256], [(wmkv[:, kc, c0:c0 + 128], memT[:, kc, :]) for kc in range(8)], 'ps5',
                         ['memT'] + W('wmkv', 8))
                    P.op('act', lambda h=h, dc=dc: nc.scalar.copy(KT[:, h, dc, :], ps[5][:, 0:256]), reads=['ps5'], writes=['KT'])
            for mt in range(2):
                for half in range(2):
                    c0 = 1024 + half * 512
                    P.mm(ps[6][:, :], [(memT[:, kc, mt * 128:(mt + 1) * 128], wmkv[:, kc, c0:c0 + 512]) for kc in range(8)],
                         'ps6', ['memT'] + W('wmkv', 8))
                    P.op('dve', lambda mt=mt, half=half: nc.vector.tensor_copy(V[:, mt, half * 512:(half + 1) * 512], ps[6][:, :]),
                         reads=['ps6'], writes=['V'])
            P.barrier()
        xblk = [sb(f"xblk{i}", [128, 8, TB], BF16) for i in range(2)]
        qm = sb("qm", [128, 2, TB], BF16)
        PT = [sb(f"PT{i}", [128, 2, TB], BF16) for i in range(2)]
        rs = sb("rs", [128, TB], F32)
        ocT = sb("ocT", [128, 8, TB], BF16)
        sig = sb("sig", [128, TB], F32)
        mT = sb("mT", [128, 8, TB], BF16)
        for b in range(NB):
            i2 = b % 2
            t0 = b * TB
            P.dma('sp', out=xblk[i2][:], in_=self.xT_d[:, :, t0:t0 + TB], key=f"ld{2 + i2}", writes=[('xblk', i2)])
            xb = xblk[i2]
            for h in range(4):
                pi = h % 2
                for dc in range(2):
                    c0 = h * 256 + dc * 128
                    pb, pt = (ps[5], 'ps5') if dc == 0 else (ps[6], 'ps6')
                    P.mm(pb[:, :], [(Wqm[:, kc, c0:c0 + 128], xb[:, kc, :]) for kc in range(8)], pt,
                         [('xblk', i2)] + W('Wqm', 8))
                    if dc == 0:
                        P.op('act', lambda pb=pb: nc.scalar.copy(qm[:, 0, :], pb[:, :]), reads=[pt], writes=[('qm', 0)])
                    else:
                        P.op('dve', lambda pb=pb: nc.vector.tensor_copy(qm[:, 1, :], pb[:, :]), reads=[pt], writes=[('qm', 1)])
                for mt in range(2):
                    P.mm(ps[mt][:, :], [(KT[:, h, dc, mt * 128:(mt + 1) * 128], qm[:, dc, :]) for dc in range(2)], ('ST', mt),
                         ['KT', ('qm', 0), ('qm', 1)])
                    P.op('act', lambda mt=mt, pi=pi: nc.scalar.activation(out=PT[pi][:, mt, :], in_=ps[mt][:, :], func=AF.Exp, scale=SC),
                         reads=[('ST', mt)], writes=[('PT', pi, mt)])
                for vc in range(2):
                    c0 = h * 256 + vc * 128
                    P.mm(ps[2 + vc][:, :], [(V[:, mt, c0:c0 + 128], PT[pi][:, mt, :]) for mt in range(2)], ('oacc', vc),
                         ['V', ('PT', pi, 0), ('PT', pi, 1)])
                P.mm(ps[4][:, :], [(self.ones_bf[:], PT[pi][:, mt, :]) for mt in range(2)], 'osum',
                     [('PT', pi, 0), ('PT', pi, 1)])
                P.op('dve', lambda: nc.vector.reciprocal(rs[:], ps[4][:, :]), reads=['osum'], writes=['rs'])
                for vc in range(2):
                    P.op('dve', lambda h=h, vc=vc: nc.vector.tensor_tensor(ocT[:, h * 2 + vc, :], ps[2 + vc][:, :], rs[:], op=ALU.mult),
                         reads=[('oacc', vc), 'rs'], writes=[('ocT', h * 2 + vc)])
            for dc in range(8):
                dd = slice(dc * 128, (dc + 1) * 128)
                P.mm(ps[5][:, :], [(WgC[:, kc, dd], xb[:, kc, :]) for kc in range(8)], 'ps5',
                     [('xblk', i2)] + W('WgC', 8))
                P.op('act', lambda: nc.scalar.activation(out=sig[:], in_=ps[5][:, :], func=AF.Sigmoid),
                     reads=['ps5'], writes=['sig'])
                P.mm(ps[6][:, :], [(wpc[:, j, dd], ocT[:, j, :]) for j in range(8)], 'ps6',
                     [('ocT', j) for j in range(8)] + W('wpc', 8))
                P.op('dve', lambda dc=dc: nc.vector.tensor_tensor(mT[:, dc, :], ps[6][:, :], sig[:], op=ALU.mult),
                     reads=['ps6', 'sig'], writes=[('mT', dc)])
            P.dma('sp', out=self.mC_d[:, :, t0:t0 + TB], in_=mT[:], key="st0",
                  reads=[('mT', dc) for dc in range(8)], writes=[('mC_d', b)])
    P.barrier()


Net.pass_mem = _pass_mem


def _pass_ssd_a(self, l):
    P, nc = self.P, self.nc
    ps = self.ps
    with ExitStack() as es:
        sb = lambda name, shape, dt: es.enter_context(nc.sbuf_tensor(_uid(name), shape, dt))
        Wx = sb("Wx", [128, 8, 4096], BF16)
        cw5 = sb("cw5", [5, 4096], F32)
        cwb = sb("cwb", [128, 32, 5], F32)
        rows = lambda kc: slice(kc * 128, (kc + 1) * 128)
        W = lambda name, n: _toks(name, n)
        self.load_w(lambda kc: Wx[:, kc, :], lambda kc: self.w_in[l, rows(kc), C_XBC:C_XBC + 4096], 'Wx', 8)
        P.dma('sp', out=cw5[0:4, :], in_=self.conv_w[l, :, :], key="ld0", writes=['cw5a'])
        P.dma('sp', out=cw5[4:5, :], in_=self.conv_b[l:l + 1, :], key="ld1", writes=['cw5b'])
        for cc in range(32):
            P.op('pe', lambda cc=cc: nc.tensor.transpose(ps[5][:, cc * 5:(cc + 1) * 5], cw5[0:5, cc * 128:(cc + 1) * 128],
                                                         self.ident_f[0:5, 0:5]),
                 reads=['cw5a', 'cw5b'], writes=['ps5'], inc=(cc == 31))
        P.op('act', lambda: nc.scalar.copy(cwb[:].rearrange("p c k -> p (c k)"), ps[5][:, 0:160]), reads=['ps5'], writes=['cwb'])
        xblk = [sb(f"xblk{i}", [128, 8, TB], BF16) for i in range(2)]
        pre = [sb(f"pre{i}", [128, TB + 3], F32) for i in range(2)]
        acc = [sb(f"acc{i}", [128, TB], F32) for i in range(2)]
        carry = sb("carry", [128, 32, 3], F32)
        xbcT = sb("xbcT", [128, 32, TB], BF16)
        tmst = [sb(f"tmst{i}", [128, 3072], BF16) for i in range(2)]
        P.op('dve', lambda: nc.vector.memset(carry[:], 0.0), writes=[('carry', cc) for cc in range(32)])
        n = 0
        for b in range(NB):
            i2 = b % 2
            t0 = b * TB
            P.dma('sp', out=xblk[i2][:], in_=self.xT_d[:, :, t0:t0 + TB], key=f"ld{2 + i2}", writes=[('xblk', i2)])
            xb = xblk[i2]
            for cc in range(32):
                j = n % 2
                n += 1
                pb, pt = (ps[5], 'ps5') if j == 0 else (ps[6], 'ps6')
                pr, ac = pre[j], acc[j]
                P.mm(pb[:, :], [(Wx[:, kc, cc * 128:(cc + 1) * 128], xb[:, kc, :]) for kc in range(8)], pt,
                     [('xblk', i2)] + W('Wx', 8))
                P.op('pool', lambda pr=pr, cc=cc: nc.gpsimd.tensor_copy(pr[:, 0:3], carry[:, cc, :]),
                     reads=[('carry', cc)], writes=[('pre', j)])
                P.op('act', lambda pr=pr, pb=pb: nc.scalar.copy(pr[:, 3:TB + 3], pb[:, :]), reads=[pt], writes=[('pre', j)])
                P.op('pool', lambda pr=pr, cc=cc: nc.gpsimd.tensor_copy(carry[:, cc, :], pr[:, TB:TB + 3]),
                     reads=[('pre', j)], writes=[('carry', cc)])
                P.op('dve', lambda pr=pr, ac=ac, cc=cc: nc.vector.tensor_scalar(ac[:], pr[:, 0:TB], cwb[:, cc, 0:1], cwb[:, cc, 4:5],
                                                                               op0=ALU.mult, op1=ALU.add),
                     reads=[('pre', j), 'cwb'], writes=[('acc', j)])
                for k in range(1, 4):
                    P.op('dve', lambda pr=pr, ac=ac, cc=cc, k=k: nc.vector.scalar_tensor_tensor(
                        out=ac[:], in0=pr[:, k:k + TB], scalar=cwb[:, cc, k:k + 1], in1=ac[:], op0=ALU.mult, op1=ALU.add),
                         reads=[('pre', j), 'cwb'], writes=[('acc', j)])
                P.op('act', lambda ac=ac, cc=cc: nc.scalar.activation(out=xbcT[:, cc, :], in_=ac[:], func=AF.Silu),
                     reads=[('acc', j)], writes=[('xbcT', cc)])
            P.dma('sp', out=self.BCT_d[:, :, t0:t0 + TB], in_=xbcT[:, 16:32, :], key="st0",
                  reads=[('xbcT', cc) for cc in range(16, 32)], writes=[('BCT_d', b)])
            for tt in range(4):
                t = b * 4 + tt
                ti = t % 2
                for r in range(3):
                    for q in range(8):
                        cc = r * 8 + q
                        P.op('pe', lambda cc=cc, q=q, tt=tt: nc.tensor.transpose(self.psb[:, q * 128:(q + 1) * 128],
                                                                                 xbcT[:, cc, tt * 128:(tt + 1) * 128], self.ident_bf[:]),
                             reads=[('xbcT', cc)], writes=['psb'], inc=(q == 7))
                    if r % 2 == 0:
                        P.op('act', lambda ti=ti, r=r: nc.scalar.copy(tmst[ti][:, r * 1024:(r + 1) * 1024], self.psb[:, :]),
                             reads=['psb'], writes=[('tmst', ti)])
                    else:
                        P.op('dve', lambda ti=ti, r=r: nc.vector.tensor_copy(tmst[ti][:, r * 1024:(r + 1) * 1024], self.psb[:, :]),
                             reads=['psb'], writes=[('tmst', ti)])
                P.dma('sp', out=self.xsB_d[t * 128:(t + 1) * 128, :], in_=tmst[ti][:], key=f"st{1 + ti}",
                      reads=[('tmst', ti)], writes=[('xsB_d', t)])
    P.barrier()


def _pass_ssd_b(self, l):
    P, nc = self.P, self.nc
    ps = self.ps
    with ExitStack() as es:
        sb = lambda name, shape, dt: es.enter_context(nc.sbuf_tensor(_uid(name), shape, dt))
        rows = lambda kc: slice(kc * 128, (kc + 1) * 128)
        W = lambda name, n: _toks(name, n)
        Wz = sb("Wz", [128, 8, 2048], BF16)
        Wdt = sb("Wdt", [128, 8, 32], BF16)
        dtb = sb("dtb", [128, 32], F32)
        abc = sb("abc", [128, 32], F32)
        dsk = sb("dsk", [128, 32], F32)
        ngb = sb("ngb", [128, 2048], F32)
        self.load_w(lambda kc: Wz[:, kc, :], lambda kc: self.w_in[l, rows(kc), C_Z:C_Z + 2048], 'Wz', 8)
        self.load_w(lambda kc: Wdt[:, kc, :], lambda kc: self.w_in[l, rows(kc), C_DT:C_DT + 32], 'Wdt', 8)
        P.dma('sp', out=dtb[:], in_=self.dt_bias[l, :].partition_broadcast(128), key="ld0", writes=['dtb'])
        P.dma('sp', out=abc[:], in_=self.a_log[l, :].partition_broadcast(128), key="ld1", writes=['abc'])
        P.dma('sp', out=dsk[:], in_=self.d_skip[l, :].partition_broadcast(128), key="ld2", writes=['dsk'])
        P.dma('sp', out=ngb[:], in_=self.ssm_norm[l, :].partition_broadcast(128), key="ld3", writes=['ngb'])
        P.op('act', lambda: nc.scalar.activation(out=abc[:], in_=abc[:], func=AF.Exp), reads=['abc'], writes=['abc'])
        P.op('dve', lambda: nc.vector.tensor_scalar(abc[:], abc[:], -1.0, None, op0=ALU.mult), reads=['abc'], writes=['abc'])
        ST = sb("ST", [128, 8, 256], F32)
        STb = sb("STb", [128, 8, 256], BF16)
        P.op('dve', lambda: nc.vector.memset(ST[:], 0.0), writes=[('STATE', g) for g in range(8)])
        P.op('pool', lambda: nc.gpsimd.memset(STb[:], 0.0), writes=[('STb', g) for g in range(8)])
        xblk = [sb(f"xblk{i}", [128, 8, TB], BF16) for i in range(2)]
        bct = [sb(f"bct{i}", [128, 16, TB], BF16) for i in range(2)]
        xsB = [sb(f"xsB{i}", [128, 3072], BF16) for i in range(2)]
        sm = sb("sm", [128, 8, 32], F32)
        xdt = sb("xdt", [128, 2048], BF16)
        xdt2 = sb("xdt2", [128, 2048], BF16)
        dam = [sb(f"dam{i}", [128, 4, 128], F32) for i in range(2)]
        dec = [sb(f"dec{i}", [128, 4, 128], F32) for i in range(2)]
        cbm = [sb(f"cbm{i}", [128, 128], F32) for i in range(2)]
        G = [sb(f"G{i}", [128, 4, 128], BF16) for i in range(2)]
        tmp = [sb(f"tmp{i}", [128, 256], F32) for i in range(2)]
        y = sb("y", [128, 2048], F32)
        zs = [sb(f"zs{i}", [128, 512], F32) for i in range(2)]
        junk = sb("junk", [128, 256], F32)
        ssq = sb("ssq", [128, 16], F32)
        yn = sb("yn", [128, 2048], BF16)
        ynT = [sb(f"ynT{i}", [128, 16, TB], BF16) for i in range(2)]
        v3 = lambda ap, h: ap.rearrange("p (h c) -> p h c", h=h)
        bc3 = lambda ap, n: ap.unsqueeze(2).to_broadcast([128, ap.shape[1], n])
        sm2 = [sm, sb("smB", [128, 8, 32], F32)]
        xdtA = [xdt, sb("xdtB", [128, 2048], BF16)]
        xdt2A = [xdt2, sb("xdt2B", [128, 2048], BF16)]
        yA = [y, sb("yB", [128, 2048], F32)]

        def load_blk(b):
            i2 = b % 2
            t0 = b * TB
            P.dma('sp', out=xblk[i2][:], in_=self.xT_d[:, :, t0:t0 + TB], key=f"ld{4 + i2}", writes=[('xblk', i2)])
            P.dma('sp', out=bct[i2][:], in_=self.BCT_d[:, :, t0:t0 + TB], key=f"ld{6 + i2}", writes=[('bct', i2)])

        def pre(t):
            b, tt = divmod(t, 4)
            i2 = b % 2
            ti = t % 2
            if tt == 0:
                load_blk(b)
            xb = xblk[i2]
            cols = slice(tt * 128, (tt + 1) * 128)
            xs = xsB[ti]
            sm = sm2[ti]
            S = lambda k: ('sm', ti, k)
            P.dma('sp', out=xs[:], in_=self.xsB_d[t * 128:(t + 1) * 128, :], key=f"ld{8 + ti}", writes=[('xsB', ti)])
            P.mm(ps[5][:, 0:32], [(xb[:, kc, cols], Wdt[:, kc, :]) for kc in range(8)], 'ps5', [('xblk', i2)] + W('Wdt', 8))
            P.op('dve', lambda: nc.vector.tensor_tensor(sm[:, 0, :], ps[5][:, 0:32], dtb[:], op=ALU.add),
                 reads=['ps5', 'dtb'], writes=[S(0)])
            P.op('dve', lambda: nc.vector.tensor_scalar(sm[:, 1, :], sm[:, 0, :], -1.0, None, op0=ALU.mult),
                 reads=[S(0)], writes=[S(1)])
            P.op('dve', lambda: nc.vector.tensor_tensor(sm[:, 1, :], sm[:, 1, :], sm[:, 0, :], op=ALU.min),
                 reads=[S(0), S(1)], writes=[S(1)])
            P.op('act', lambda: nc.scalar.activation(out=sm[:, 2, :], in_=sm[:, 1, :], func=AF.Exp),
                 reads=[S(1)], writes=[S(2)])
            P.op('act', lambda: nc.scalar.activation(out=sm[:, 2, :], in_=sm[:, 2, :], func=AF.Ln, bias=1.0),
                 reads=[S(2)], writes=[S(2)])
            P.op('dve', lambda: nc.vector.scalar_tensor_tensor(out=sm[:, 3, :], in0=sm[:, 0, :], scalar=0.0, in1=sm[:, 2, :],
                                                               op0=ALU.max, op1=ALU.add), reads=[S(0), S(2)], writes=[S(3)])
            P.op('dve', lambda: nc.vector.tensor_tensor(sm[:, 4, :], sm[:, 3, :], abc[:], op=ALU.mult),
                 reads=[S(3), 'abc'], writes=[S(4)])
            P.mm(ps[5][:, 64:96], [(self.tri_le_f[:], sm[:, 4, :])], 'ps5', [S(4)])
            P.mm(ps[5][:, 128:160], [(self.tri_gt_f[:], sm[:, 4, :])], 'ps5', [S(4)])
            P.mm(ps[5][:, 192:224], [(self.ones_f[:], sm[:, 4, :])], 'ps5', [S(4)])
            P.op('act', lambda: nc.scalar.activation(out=sm[:, 5, :], in_=ps[5][:, 64:96], func=AF.Exp), reads=['ps5'], writes=[S(5)])
            P.op('act', lambda: nc.scalar.activation(out=sm[:, 6, :], in_=ps[5][:, 128:160], func=AF.Exp), reads=['ps5'], writes=[S(6)])
            P.op('act', lambda: nc.scalar.activation(out=sm[:, 7, :], in_=ps[5][:, 192:224], func=AF.Exp), reads=['ps5'], writes=[S(7)])
            P.op('dve', lambda: nc.vector.tensor_tensor(v3(xdtA[ti][:], 32), v3(xs[:, 0:2048], 32), bc3(sm[:, 3, :], 64), op=ALU.mult),
                 reads=[('xsB', ti), S(3)], writes=[('xdt', ti)])
            P.op('pool', lambda: nc.gpsimd.tensor_tensor(v3(xdt2A[ti][:], 32), v3(xdtA[ti][:], 32), bc3(sm[:, 6, :], 64), op=ALU.mult),
                 reads=[('xdt', ti), S(6)], writes=[('xdt2', ti)])

        def grp(t):
            b, tt = divmod(t, 4)
            i2 = b % 2
            ti = t % 2
            cols = slice(tt * 128, (tt + 1) * 128)
            xs = xsB[ti]
            sm = sm2[ti]
            S = lambda k: ('sm', ti, k)
            xdt_, xdt2_, y_ = xdtA[ti], xdt2A[ti], yA[ti]

            def stage1(g):
                gi = g % 2
                hs = slice(g * 4, (g + 1) * 4)
                P.op('pool', lambda: nc.gpsimd.tensor_tensor(
                    dam[gi][:], self.tri_gt_f[:, :].unsqueeze(1).to_broadcast([128, 4, 128]), bc3(sm[:, 4, hs], 128), op=ALU.mult),
                     reads=[S(4)], writes=[('dam', gi)])
                sp_, spt = (ps[0], ('SEG', 0)) if gi == 0 else (ps[1], ('SEG', 1))
                for r in range(4):
                    P.mm(sp_[:, r * 128:(r + 1) * 128], [(dam[gi][:, r, :], self.tri_le_f[:])], spt, [('dam', gi)])
                P.op('act', lambda: nc.scalar.activation(out=dec[gi][:].rearrange("p r s -> p (r s)"), in_=sp_[:, :], func=AF.Exp),
                     reads=[spt], writes=[('dec', gi)])
                cp_, cpt = (ps[2], ('CB', 0)) if gi == 0 else (ps[3], ('CB', 1))
                P.mm(cp_[:, 0:128], [(bct[i2][:, g, cols], bct[i2][:, 8 + g, cols])], cpt, [('bct', i2)])
                P.op('dve', lambda: nc.vector.tensor_tensor(cbm[gi][:], cp_[:, 0:128], self.tri_le_f[:], op=ALU.mult),
                     reads=[cpt], writes=[('cbm', gi)])
                P.op('dve', lambda: nc.vector.tensor_tensor(G[gi][:], dec[gi][:], cbm[gi][:, :].unsqueeze(1).to_broadcast([128, 4, 128]),
                                                            op=ALU.mult),
                     reads=[('dec', gi), ('cbm', gi)], writes=[('G', gi)])

            def stage2(g):
                gi = g % 2
                hs = slice(g * 4, (g + 1) * 4)
                cp_ = ps[2] if gi == 0 else ps[3]
                bp_ = ps[4] if gi == 0 else ps[6]
                for r in range(4):
                    hh = g * 4 + r
                    P.mm(bp_[:, r * 64:(r + 1) * 64],
                         [(G[gi][:, r, :], xdt_[:, hh * 64:(hh + 1) * 64])], ('YI', gi), [('G', gi), ('xdt', ti)])
                P.mm(bp_[:, 256:512], [(xs[:, 2048 + g * 128:2048 + (g + 1) * 128], xdt2_[:, g * 256:(g + 1) * 256])],
                     ('SU', gi), [('xsB', ti), ('xdt2', ti)])
                P.mm(cp_[:, 256:512], [(bct[i2][:, 8 + g, cols], STb[:, g, :])], ('YS', gi), [('bct', i2), ('STb', g)])
                P.op('dve', lambda: nc.vector.tensor_tensor(v3(tmp[gi][:], 4), v3(cp_[:, 256:512], 4), bc3(sm[:, 5, hs], 64), op=ALU.mult),
                     reads=[('YS', gi), S(5)], writes=[('tmp', gi)])
                P.op('dve', lambda: nc.vector.tensor_tensor(y_[:, g * 256:(g + 1) * 256], tmp[gi][:], bp_[:, 0:256], op=ALU.add),
                     reads=[('tmp', gi), ('YI', gi)], writes=[('y', ti, g)])
                P.op('pool', lambda: nc.gpsimd.tensor_tensor(v3(ST[:, g, :], 4), v3(ST[:, g, :], 4), bc3(sm[:, 7, hs], 64), op=ALU.mult),
                     reads=[S(7)], writes=[('STATE', g)])
                P.op('dve', lambda: nc.vector.tensor_tensor(ST[:, g, :], ST[:, g, :], bp_[:, 256:512], op=ALU.add),
                     reads=[('SU', gi)], writes=[('STATE', g)])
                P.op('act', lambda: nc.scalar.copy(STb[:, g, :], ST[:, g, :]), reads=[('STATE', g)], writes=[('STb', g)])

            if SSD_PIPE:
                stage1(0)
                for g in range(8):
                    if g + 1 < 8:
                        stage1(g + 1)
                    stage2(g)
            else:
                for g in range(8):
                    stage1(g)
                    stage2(g)

        def post(t):
            b, tt = divmod(t, 4)
            i2 = b % 2
            ti = t % 2
            t0 = b * TB
            cols = slice(tt * 128, (tt + 1) * 128)
            xb = xblk[i2]
            xs = xsB[ti]
            xdt2_, y_ = xdt2A[ti], yA[ti]
            ally = [('y', ti, g) for g in range(8)]
            P.op('pool', lambda: nc.gpsimd.tensor_tensor(v3(xdt2_[:], 32), v3(xs[:, 0:2048], 32), bc3(dsk[:, :], 64), op=ALU.mult),
                 reads=[('xsB', ti), 'dsk'], writes=[('xdt2', ti)])
            P.op('dve', lambda: nc.vector.tensor_tensor(y_[:], y_[:], xdt2_[:], op=ALU.add), reads=ally + [('xdt2', ti)], writes=ally)
            for q in range(4):
                zi = q % 2
                qs = slice(q * 512, (q + 1) * 512)
                zp, zpt = (ps[0], ('SEG', 0)) if zi == 0 else (ps[1], ('SEG', 1))
                P.mm(zp[:, :], [(xb[:, kc, cols], Wz[:, kc, qs]) for kc in range(8)], zpt, [('xblk', i2)] + W('Wz', 8))
                P.op('act', lambda zi=zi, zp=zp: nc.scalar.activation(out=zs[zi][:], in_=zp[:, :], func=AF.Silu), reads=[zpt], writes=[('zs', zi)])
                P.op('dve', lambda zi=zi, qs=qs: nc.vector.tensor_tensor(y_[:, qs], y_[:, qs], zs[zi][:], op=ALU.mult),
                     reads=[('zs', zi)] + ally, writes=ally)
            for g in range(8):
                P.op('act', lambda g=g: nc.scalar.activation(out=junk[:], in_=y_[:, g * 256:(g + 1) * 256], func=AF.Square, accum_out=ssq[:, g:g + 1]),
                     reads=ally, writes=['junk', ('ssq', g)])
            allq = [('ssq', g) for g in range(8)]
            P.op('dve', lambda: nc.vector.tensor_scalar(ssq[:, 8:16], ssq[:, 0:8], 1.0 / 256, RMS_EPS, op0=ALU.mult, op1=ALU.add),
                 reads=allq, writes=['ssq8'])
            P.op('act', lambda: nc.scalar.activation(out=ssq[:, 8:16], in_=ssq[:, 8:16], func=AF.Sqrt), reads=['ssq8'], writes=['ssq8'])
            P.op('dve', lambda: nc.vector.reciprocal(ssq[:, 8:16], ssq[:, 8:16]), reads=['ssq8'], writes=['ssq8'])
            for g in range(8):
                gs = slice(g * 256, (g + 1) * 256)
                P.op('dve', lambda g=g, gs=gs: nc.vector.scalar_tensor_tensor(
                    out=yn[:, gs], in0=y_[:, gs], scalar=ssq[:, 8 + g:9 + g], in1=ngb[:, gs], op0=ALU.mult, op1=ALU.mult),
                     reads=ally + ['ssq8', 'ngb'], writes=[('yn', g)])
            for r in range(2):
                for q in range(8):
                    P.op('pe', lambda r=r, q=q: nc.tensor.transpose(self.psb[:, q * 128:(q + 1) * 128],
                                                                    yn[:, (r * 8 + q) * 128:(r * 8 + q + 1) * 128], self.ident_bf[:]),
                         reads=[('yn', g) for g in range(8)], writes=['psb'], inc=(q == 7))
                P.op('act', lambda r=r: nc.scalar.copy(ynT[i2][:, r * 8:(r + 1) * 8, cols],
                                                       self.psb[:].rearrange("p (k n) -> p k n", k=8)),
                     reads=['psb'], writes=[('ynT', i2)])
            if tt == 3:
                P.dma('sp', out=self.ynT_d[:, :, t0:t0 + TB], in_=ynT[i2][:], key=f"st{i2}", reads=[('ynT', i2)], writes=[('ynT_d', b)])

        if SSD_HOIST:
            pre(0)
            for t in range(NT):
                if t + 1 < NT:
                    pre(t + 1)
                grp(t)
                post(t)
        else:
            for t in range(NT):
                pre(t)
                grp(t)
                post(t)
    P.barrier()


def _pass_ssd_c(self, l):
    P, nc = self.P, self.nc
    ps = self.ps
    with ExitStack() as es:
        sb = lambda name, shape, dt: es.enter_context(nc.sbuf_tensor(_uid(name), shape, dt))
        rows = lambda kc: slice(kc * 128, (kc + 1) * 128)
        W = lambda name, n: _toks(name, n)
        WgB = sb("WgB", [128, 8, D], BF16)
        wpb = sb("wpb", [128, 16, D], BF16)
        self.load_w(lambda kc: WgB[:, kc, :], lambda kc: self.w_in[l, rows(kc), C_G + D:C_G + 2 * D], 'WgB', 8)
        self.load_w(lambda kc: wpb[:, kc, :], lambda kc: self.w_proj_b[l, rows(kc), :], 'wpb', 16)
        xblk = [sb(f"xblk{i}", [128, 8, TB], BF16) for i in range(2)]
        ynb = [sb(f"ynb{i}", [128, 16, TB], BF16) for i in range(2)]
        sig = [sb(f"sig{i}", [128, TB], F32) for i in range(2)]
        mT = [sb(f"mT{i}", [128, 8, TB], BF16) for i in range(2)]
        for b in range(NB):
            i2 = b % 2
            t0 = b * TB
            P.dma('sp', out=xblk[i2][:], in_=self.xT_d[:, :, t0:t0 + TB], key=f"ld{2 + i2}", writes=[('xblk', i2)])
            P.dma('sp', out=ynb[i2][:], in_=self.ynT_d[:, :, t0:t0 + TB], key=f"ld{4 + i2}", writes=[('ynb', i2)])
            xb = xblk[i2]
            for dc in range(8):
                dd = slice(dc * 128, (dc + 1) * 128)
                si = dc % 2
                pa, pat = (ps[5], 'ps5') if si == 0 else (ps[3], 'ps3')
                pb, pbt = (ps[6], 'ps6') if si == 0 else (ps[4], 'ps4')
                P.mm(pa[:, :], [(WgB[:, kc, dd], xb[:, kc, :]) for kc in range(8)], pat, [('xblk', i2)] + W('WgB', 8))
                P.op('act', lambda si=si, pa=pa: nc.scalar.activation(out=sig[si][:], in_=pa[:, :], func=AF.Sigmoid),
                     reads=[pat], writes=[('sig', si)])
                P.mm(pb[:, :], [(wpb[:, j, dd], ynb[i2][:, j, :]) for j in range(16)], pbt, [('ynb', i2)] + W('wpb', 16))
                P.op('dve', lambda dc=dc, si=si, pb=pb: nc.vector.tensor_tensor(mT[i2][:, dc, :], pb[:, :], sig[si][:], op=ALU.mult),
                     reads=[pbt, ('sig', si)], writes=[('mT', i2)])
            P.dma('sp', out=self.mB_d[:, :, t0:t0 + TB], in_=mT[i2][:], key=f"st{i2}", reads=[('mT', i2)], writes=[('mB_d', b)])
    P.barrier()


Net.pass_ssd_a = _pass_ssd_a
Net.pass_ssd_b = _pass_ssd_b
Net.pass_ssd_c = _pass_ssd_c


def _layer_norm_tile(self, u, gbt, bbt, out, st, tag):
    P, nc = self.P, self.nc
    junk = self.ln_junk
    P.op('act', lambda: nc.scalar.activation(out=junk[:], in_=u, func=AF.Identity, accum_out=st[:, 0:1]),
         reads=[tag + 'u'], writes=['lnjunk', tag + 's0'])
    P.op('act', lambda: nc.scalar.activation(out=junk[:], in_=u, func=AF.Square, accum_out=st[:, 1:2]),
         reads=[tag + 'u'], writes=['lnjunk', tag + 's1'])
    P.op('dve', lambda: nc.vector.tensor_scalar(st[:, 2:4], st[:, 0:2], 1.0 / D, None, op0=ALU.mult),
         reads=[tag + 's0', tag + 's1'], writes=[tag + 's2'])
    P.op('dve', lambda: nc.vector.tensor_tensor(st[:, 4:5], st[:, 2:3], st[:, 2:3], op=ALU.mult), reads=[tag + 's2'], writes=[tag + 's4'])
    P.op('dve', lambda: nc.vector.tensor_tensor(st[:, 5:6], st[:, 3:4], st[:, 4:5], op=ALU.subtract), reads=[tag + 's2', tag + 's4'], writes=[tag + 's5'])
    P.op('dve', lambda: nc.vector.tensor_scalar(st[:, 5:6], st[:, 5:6], NORM_EPS, None, op0=ALU.add), reads=[tag + 's5'], writes=[tag + 's5'])
    P.op('act', lambda: nc.scalar.activation(out=st[:, 6:7], in_=st[:, 5:6], func=AF.Sqrt), reads=[tag + 's5'], writes=[tag + 's6'])
    P.op('dve', lambda: nc.vector.reciprocal(st[:, 7:8], st[:, 6:7]), reads=[tag + 's6'], writes=[tag + 's7'])
    P.op('dve', lambda: nc.vector.tensor_scalar(out, u, st[:, 2:3], st[:, 7:8], op0=ALU.subtract, op1=ALU.mult),
         reads=[tag + 'u', tag + 's2', tag + 's7'], writes=[tag + 'o'])
    P.op('pool', lambda: nc.gpsimd.tensor_tensor(out, out, gbt[:], op=ALU.mult), reads=[tag + 'g'], writes=[tag + 'o'])
    P.op('pool', lambda: nc.gpsimd.tensor_tensor(out, out, bbt[:], op=ALU.add), reads=[tag + 'g'], writes=[tag + 'o'])


def _pass_ln1(self, l):
    P, nc = self.P, self.nc
    ps = self.ps
    xa_src = self.x_in if l == 0 else self.xa_d
    with ExitStack() as es:
        sb = lambda name, shape, dt: es.enter_context(nc.sbuf_tensor(_uid(name), shape, dt))
        rows = lambda kc: slice(kc * 128, (kc + 1) * 128)
        W = lambda name, n: _toks(name, n)
        wout = sb("wout", [128, 8, D], BF16)
        rw = sb("rw", [128, 8, 16], F32)
        rb = sb("rb", [128, 16], F32)
        gbt = sb("gbt", [128, D], F32)
        bbt = sb("bbt", [128, D], F32)
        self.ln_junk = sb("lnjunk", [128, D], F32)
        self.load_w(lambda kc: wout[:, kc, :], lambda kc: self.w_out[l, rows(kc), :], 'wout', 8)
        for kc in range(8):
            P.dma('sp', out=rw[:, kc, :], in_=self.router_w[rows(kc), :], key="ld0", writes=[('rw', kc)])
        P.dma('sp', out=rb[:], in_=self.router_bias[0, :].partition_broadcast(128), key="ld1", writes=['rb'])
        P.dma('sp', out=gbt[:], in_=self.ln1_g[l, :].partition_broadcast(128), key="ld2", writes=['lg'])
        P.dma('sp', out=bbt[:], in_=self.ln1_b[l, :].partition_broadcast(128), key="ld3", writes=['lg'])
        mblk = [[sb(f"m{n}{i}", [128, 8, TB], BF16) for n in "ABC"] for i in range(2)]
        xa = [sb(f"xa{i}", [128, D], F32) for i in range(2)]
        u = [sb(f"u{i}", [128, D], F32) for i in range(2)]
        x1 = [sb(f"x1{i}", [128, D], F32) for i in range(2)]
        st = sb("st", [128, 8], F32)
        x1Tf = sb("x1Tf", [128, 8, 128], F32)
        stg = [sb(f"stg{i}", [128, 8, TB], BF16) for i in range(2)]
        r = sb("r", [128, 12, 16], F32)
        gts = [sb(f"gts{i}", [16, TB], F32) for i in range(2)]
        srcs = [self.mA_d, self.mB_d, self.mC_d]
        for b in range(NB):
            i2 = b % 2
            t0 = b * TB
            for n in range(3):
                P.dma('sp', out=mblk[i2][n][:], in_=srcs[n][:, :, t0:t0 + TB], key=f"ld{4 + 3 * i2 + n}", writes=[('mblk', i2, n)])
            for tt in range(4):
                t = b * 4 + tt
                ti = t % 2
                cols = slice(tt * 128, (tt + 1) * 128)
                P.dma('sp', out=xa[ti][:], in_=xa_src[t * 128:(t + 1) * 128, :], key=f"ld{10 + ti}", writes=[('xa', ti)])
                for half in range(2):
                    pb, pt = (ps[5], 'ps5') if half == 0 else (ps[6], 'ps6')
                    P.mm(pb[:, :], [(mblk[i2][n][:, dc, cols], wout[:, dc, half * 512:(half + 1) * 512]) for n in range(3) for dc in range(8)],
                         pt, [('mblk', i2, n) for n in range(3)] + W('wout', 8))
                    P.op('dve', lambda ti=ti, half=half, pb=pb: nc.vector.scalar_tensor_tensor(
                        out=u[ti][:, half * 512:(half + 1) * 512], in0=xa[ti][:, half * 512:(half + 1) * 512], scalar=DN_ALPHA, in1=pb[:, :],
                        op0=ALU.mult, op1=ALU.add), reads=[('xa', ti), pt], writes=['L1u'])
                self.layer_norm_tile(u[ti][:], gbt, bbt, x1[ti][:], st, 'L1')
                P._record(('e', 'pool', P.cnt['pool']), ['lg'], [])
                P.dma('sp', out=self.x1_d[t * 128:(t + 1) * 128, :], in_=x1[ti][:], key=f"st{ti}", reads=['L1o'], writes=[('x1_d', t)])
                import os
                CUT = int(os.environ.get("K_CUT", "99"))
                if CUT < 1:
                    continue
                for kc in range(8):
                    pb = ps[0] if kc < 4 else ps[1]
                    P.op('pe', lambda kc=kc, pb=pb, ti=ti: nc.tensor.matmul(pb[:, (kc % 4) * 128:(kc % 4 + 1) * 128],
                                                                           x1[ti][:, kc * 128:(kc + 1) * 128], self.ident_f[:],
                                                                           start=True, stop=True),
                         reads=['L1o'], writes=[('TP', kc // 4)], inc=(kc % 4 == 3))
                for hf in range(2):
                    P.op('act', lambda hf=hf: nc.scalar.copy(x1Tf[:, hf * 4:(hf + 1) * 4, :], ps[hf][:].rearrange("p (k n) -> p k n", k=4)),
                         reads=[('TP', hf)], writes=['x1Tf'])
                    P.op('dve', lambda hf=hf, cols=cols: nc.vector.tensor_copy(stg[i2][:, hf * 4:(hf + 1) * 4, cols], x1Tf[:, hf * 4:(hf + 1) * 4, :]),
                         reads=['x1Tf'], writes=[('stg', i2)])
                if CUT < 2:
                    continue
                P.mm(ps[2][:, 0:16], [(x1Tf[:, kc, :], rw[:, kc, :]) for kc in range(8)], 'ps2', ['x1Tf'] + W('rw', 8))
                R = lambda i: r[:, i, :]
                R4 = lambda i: r[:, i, :].rearrange("p (g e) -> p g e", g=4)
                P.op('act', lambda: nc.scalar.activation(out=R(0), in_=ps[2][:, 0:16], func=AF.Sigmoid), reads=['ps2'], writes=['r0'])
                P.op('dve', lambda: nc.vector.tensor_tensor(R(1), R(0), rb[:], op=ALU.add), reads=['r0', 'rb'], writes=['r1'])
                if CUT < 3:
                    continue
                pairs = [(0, 1), (0, 2), (0, 3), (1, 2), (1, 3), (2, 3)]
                for pi_, (a_, b_) in enumerate(pairs):
                    P.op('dve', lambda pi_=pi_, a_=a_, b_=b_: nc.vector.tensor_tensor(r[:, 2 + pi_ // 4, (pi_ % 4) * 4:(pi_ % 4) * 4 + 4],
                                                                                    R4(1)[:, :, a_], R4(1)[:, :, b_], op=ALU.add),
                         reads=['r1'], writes=['r2'])
                P.op('dve', lambda: nc.vector.tensor_tensor(r[:, 4, 0:4], r[:, 2, 0:4], r[:, 2, 4:8], op=ALU.max), reads=['r2'], writes=['r4'])
                P.op('dve', lambda: nc.vector.tensor_tensor(r[:, 4, 4:8], r[:, 2, 8:12], r[:, 2, 12:16], op=ALU.max), reads=['r2'], writes=['r4'])
                P.op('dve', lambda: nc.vector.tensor_tensor(r[:, 4, 8:12], r[:, 3, 0:4], r[:, 3, 4:8], op=ALU.max), reads=['r2'], writes=['r4'])
                P.op('dve', lambda: nc.vector.tensor_tensor(r[:, 4, 0:4], r[:, 4, 0:4], r[:, 4, 4:8], op=ALU.max), reads=['r4'], writes=['r4'])
                P.op('dve', lambda: nc.vector.tensor_tensor(r[:, 4, 0:4], r[:, 4, 0:4], r[:, 4, 8:12], op=ALU.max), reads=['r4'], writes=['r4'])
                P.op('dve', lambda: nc.vector.tensor_reduce(out=r[:, 5, 0:1], in_=r[:, 4, 0:4], axis=AX.X, op=ALU.max), reads=['r4'], writes=['r5'])
                P.op('dve', lambda: nc.vector.tensor_scalar(r[:, 5, 4:8], r[:, 4, 0:4], r[:, 5, 0:1], None, op0=ALU.is_equal), reads=['r4', 'r5'], writes=['r5m'])
                P.op('dve', lambda: nc.vector.tensor_scalar(r[:, 5, 8:12], r[:, 5, 4:8], 1.0, 1e30, op0=ALU.subtract, op1=ALU.mult), reads=['r5m'], writes=['r5p'])
                P.op('dve', lambda: nc.vector.tensor_tensor(R4(6), R4(1), r[:, 5, 4:8].unsqueeze(2).to_broadcast([128, 4, 4]), op=ALU.mult),
                     reads=['r1', 'r5m'], writes=['r6'])
                P.op('dve', lambda: nc.vector.tensor_tensor(R4(6), R4(6), r[:, 5, 8:12].unsqueeze(2).to_broadcast([128, 4, 4]), op=ALU.add),
                     reads=['r6', 'r5p'], writes=['r6'])
                P.op('dve', lambda: nc.vector.tensor_reduce(out=r[:, 7, 0:1], in_=R(6), axis=AX.X, op=ALU.max), reads=['r6'], writes=['r7'])
                P.op('dve', lambda: nc.vector.tensor_scalar(R(8), R(6), r[:, 7, 0:1], None, op0=ALU.is_equal), reads=['r6', 'r7'], writes=['r8'])
                P.op('dve', lambda: nc.vector.scalar_tensor_tensor(out=R(9), in0=R(8), scalar=-1e30, in1=R(6), op0=ALU.mult, op1=ALU.add),
                     reads=['r8', 'r6'], writes=['r9'])
                P.op('dve', lambda: nc.vector.tensor_reduce(out=r[:, 7, 1:2], in_=R(9), axis=AX.X, op=ALU.max), reads=['r9'], writes=['r7b'])
                P.op('dve', lambda: nc.vector.tensor_scalar(R(10), R(9), r[:, 7, 1:2], None, op0=ALU.is_equal), reads=['r9', 'r7b'], writes=['r10'])
                P.op('dve', lambda: nc.vector.tensor_tensor(R(10), R(10), R(8), op=ALU.add), reads=['r10', 'r8'], writes=['r10'])
                P.op('dve', lambda: nc.vector.tensor_tensor(R(11), R(10), R(0), op=ALU.mult), reads=['r10', 'r0'], writes=['r11'])
                P.op('dve', lambda: nc.vector.tensor_reduce(out=r[:, 7, 2:3], in_=R(11), axis=AX.X, op=ALU.add), reads=['r11'], writes=['r7c'])
                P.op('dve', lambda: nc.vector.reciprocal(r[:, 7, 3:4], r[:, 7, 2:3]), reads=['r7c'], writes=['r7d'])
                P.op('dve', lambda: nc.vector.tensor_scalar(R(11), R(11), r[:, 7, 3:4], None, op0=ALU.mult), reads=['r11', 'r7d'], writes=['r11'])
                if CUT < 4:
                    continue
                P.op('pe', lambda: nc.tensor.matmul(ps[3][0:16, 0:128], R(11), self.ident_f[:], start=True, stop=True), reads=['r11'], writes=['ps3'])
                P.op('act', lambda cols=cols: nc.scalar.copy(gts[i2][:, cols], ps[3][0:16, 0:128]), reads=['ps3'], writes=[('gts', i2)])
            P.dma('sp', out=self.x1T_d[:, :, t0:t0 + TB], in_=stg[i2][:], key=f"st{2 + i2}", reads=[('stg', i2)], writes=[('x1T_d', b)])
            P.dma('sp', out=self.gT_d[:, t0:t0 + TB], in_=gts[i2][:], key=f"st{4 + i2}", reads=[('gts', i2)], writes=[('gT_d', b)])
    P.barrier()


def _pass_moe(self, l, last):
    P, nc = self.P, self.nc
    ps = self.ps
    BB = 1024
    with ExitStack() as es:
        sb = lambda name, shape, dt: es.enter_context(nc.sbuf_tensor(_uid(name), shape, dt))
        rows = lambda kc: slice(kc * 128, (kc + 1) * 128)
        W = lambda name, n: _toks(name, n)
        gbt = sb("gbt", [128, D], F32)
        bbt = sb("bbt", [128, D], F32)
        self.ln_junk = sb("lnjunk", [128, D], F32)
        E16 = sb("E16", [16, 16, 128], F32)
        P.dma('sp', out=gbt[:], in_=self.ln2_g[l, :].partition_broadcast(128), key="ld2", writes=['lg'])
        P.dma('sp', out=bbt[:], in_=self.ln2_b[l, :].partition_broadcast(128), key="ld3", writes=['lg'])
        P.op('pool', lambda: nc.gpsimd.memset(E16[:], 0.0), writes=['E16'])
        P.op('pool', lambda: nc.gpsimd.affine_select(out=E16[:], in_=E16[:], pattern=[[-1, 16], [0, 128]], compare_op=ALU.not_equal,
                                                     fill=1.0, base=0, channel_multiplier=1), writes=['E16'])
        x1T = sb("x1T", [128, 8, BB], BF16)
        gT = sb("gT", [16, BB], F32)
        acc = sb("acc", [128, 8, D], F32)
        w1 = [sb(f"w1{i}", [128, 8, 512], BF16) for i in range(2)]
        w3 = [sb(f"w3{i}", [128, 8, 512], BF16) for i in range(2)]
        w2 = [sb(f"w2{i}", [128, 4, D], BF16) for i in range(2)]
        gb = [sb(f"gb{i}", [128, 512], F32) for i in range(2)]
        s1 = [sb(f"s1{i}", [128, 512], F32) for i in range(2)]
        tq = [sb(f"tq{i}", [128, 512], F32) for i in range(2)]
        hT = [sb(f"hT{i}", [128, 4, 512], BF16) for i in range(2)]
        x1t = [sb(f"x1t{i}", [128, D], F32) for i in range(2)]
        u = [sb(f"u{i}", [128, D], F32) for i in range(2)]
        x2 = [sb(f"x2{i}", [128, D], F32) for i in range(2)]
        x2b = [sb(f"x2b{i}", [128, D], BF16) for i in range(2)]
        st = sb("st", [128, 8], F32)
        stg = sb("stg", [128, 8, BB], BF16)
        dst = self.y_out if last else self.xa_d
        for bb in range(T // BB):
            t0 = bb * BB
            P.dma('sp', out=x1T[:], in_=self.x1T_d[:, :, t0:t0 + BB], key="ld4", writes=['x1T'])
            P.dma('sp', out=gT[:], in_=self.gT_d[:, t0:t0 + BB], key="ld5", writes=['gT'])
            units = [(e, sub) for e in range(16) for sub in range(BB // 512)]

            def stage_F(ui):
                e, sub = units[ui]
                ei = (ebase + e) % 2
                hi = (ubase + ui) % 2
                if sub == 0:
                    self.load_w(lambda kc: w1[ei][:, kc, :], lambda kc: self.exp_w1[l, e, rows(kc), :], ('w1', ei), 8)
                    self.load_w(lambda kc: w3[ei][:, kc, :], lambda kc: self.exp_w3[l, e, rows(kc), :], ('w3', ei), 8)
                    self.load_w(lambda kc: w2[ei][:, kc, :], lambda kc: self.exp_w2[l, e, rows(kc), :], ('w2', ei), 4)
                sc = slice(sub * 512, (sub + 1) * 512)
                P.mm(ps[4][:, :], [(E16[:, e, :], gT[:, sc])], 'ps4', ['E16', 'gT'])
                P.op('act', lambda: nc.scalar.copy(gb[hi][:], ps[4][:, :]), reads=['ps4'], writes=[('gb', hi)])
                for fc in range(4):
                    fi = fc % 2
                    fs = slice(fc * 128, (fc + 1) * 128)
                    pa, pat = (ps[0], 'ps0') if fi == 0 else (ps[2], 'ps2')
                    pb, pbt = (ps[1], 'ps1') if fi == 0 else (ps[3], 'ps3')
                    P.mm(pa[:, :], [(w1[ei][:, kc, fs], x1T[:, kc, sc]) for kc in range(8)], pat, ['x1T'] + W(('w1', ei), 8))
                    P.mm(pb[:, :], [(w3[ei][:, kc, fs], x1T[:, kc, sc]) for kc in range(8)], pbt, ['x1T'] + W(('w3', ei), 8))
                    P.op('act', lambda fi=fi, pa=pa: nc.scalar.activation(out=s1[fi][:], in_=pa[:, :], func=AF.Silu), reads=[pat], writes=[('s1', fi)])
                    P.op('dve', lambda fi=fi, pb=pb: nc.vector.tensor_tensor(tq[fi][:], pb[:, :], gb[hi][:], op=ALU.mult),
                         reads=[pbt, ('gb', hi)], writes=[('tq', fi)])
                    P.op('dve', lambda fi=fi, fc=fc: nc.vector.tensor_tensor(hT[hi][:, fc, :], tq[fi][:], s1[fi][:], op=ALU.mult),
                         reads=[('tq', fi), ('s1', fi)], writes=[('hT', hi, fc)])

            def stage_S(ui):
                e, sub = units[ui]
                ei = (ebase + e) % 2
                hi = (ubase + ui) % 2
                for tt in range(4):
                    tl = sub * 4 + tt
                    cols = slice(tt * 128, (tt + 1) * 128)
                    for half in range(2):
                        pb, pt = (ps[5], 'ps5') if half == 0 else (ps[6], 'ps6')
                        hs = slice(half * 512, (half + 1) * 512)
                        P.mm(pb[:, :], [(hT[hi][:, fc, cols], w2[ei][:, fc, hs]) for fc in range(4)], pt,
                             [('hT', hi, fc) for fc in range(4)] + W(('w2', ei), 4))
                        if e == 0:
                            P.op('dve', lambda tl=tl, hs=hs, pb=pb: nc.vector.tensor_copy(acc[:, tl, hs], pb[:, :]),
                                 reads=[pt], writes=[('acc', tl, half)])
                        else:
                            P.op('dve', lambda tl=tl, hs=hs, pb=pb: nc.vector.tensor_tensor(acc[:, tl, hs], acc[:, tl, hs], pb[:, :], op=ALU.add),
                                 reads=[pt], writes=[('acc', tl, half)])

            ebase = bb * 16
            ubase = bb * len(units)
            stage_F(0)
            for ui in range(len(units)):
                if ui + 1 < len(units):
                    stage_F(ui + 1)
                stage_S(ui)
            for tl in range(BB // 128):
                t = bb * (BB // 128) + tl
                ti = t % 2
                P.dma('sp', out=x1t[ti][:], in_=self.x1_d[t * 128:(t + 1) * 128, :], key=f"ld{6 + ti}", writes=[('x1t', ti)])
                P.op('dve', lambda ti=ti, tl=tl: nc.vector.scalar_tensor_tensor(
                    out=u[ti][:], in0=x1t[ti][:], scalar=DN_ALPHA, in1=acc[:, tl, :], op0=ALU.mult, op1=ALU.add),
                     reads=[('x1t', ti), ('acc', tl, 0), ('acc', tl, 1)], writes=['L2u'])
                self.layer_norm_tile(u[ti][:], gbt, bbt, x2[ti][:], st, 'L2')
                P._record(('e', 'pool', P.cnt['pool']), ['lg'], [])
                P.dma('sp', out=dst[t * 128:(t + 1) * 128, :], in_=x2[ti][:], key=f"st{ti}", reads=['L2o'], writes=[('dst', t)])
                if not last:
                    P.op('act', lambda ti=ti: nc.scalar.copy(x2b[ti][:], x2[ti][:]), reads=['L2o'], writes=[('x2b', ti)])
                    for kc in range(8):
                        P.op('pe', lambda kc=kc, ti=ti: nc.tensor.transpose(self.psb[:, kc * 128:(kc + 1) * 128],
                                                                           x2b[ti][:, kc * 128:(kc + 1) * 128], self.ident_bf[:]),
                             reads=[('x2b', ti)], writes=['psb'], inc=(kc == 7))
                    P.op('act', lambda tl=tl: nc.scalar.copy(stg[:, :, tl * 128:(tl + 1) * 128],
                                                             self.psb[:].rearrange("p (k n) -> p k n", k=8)),
                         reads=['psb'], writes=['stg'])
            if not last:
                P.dma('sp', out=self.xT_d[:, :, t0:t0 + BB], in_=stg[:], key="st2", reads=['stg'], writes=[('xT_d', bb)])
    P.barrier()


Net.layer_norm_tile = _layer_norm_tile
Net.pass_ln1 = _pass_ln1
Net.pass_moe = _pass_moe


def build(n_layers=DEPTH, stages=("mla", "ssd", "mem", "moe"), dbg=()):
    net = Net(n_layers)
    net.declare()
    P = net.P
    net.setup_globals()
    net.prologue()
    for l in range(n_layers):
        if "mla" in stages:
            net.pass_mla(l)
        if "mem" in stages:
            net.pass_mem(l)
        if "ssd" in stages or "ssda" in stages:
            net.pass_ssd_a(l)
        if "ssd" in stages or "ssdb" in stages:
            net.pass_ssd_b(l)
        if "ssd" in stages or "ssdc" in stages:
            net.pass_ssd_c(l)
        if "moe" in stages or "ln1" in stages:
            net.pass_ln1(l)
        if "moe" in stages:
            net.pass_moe(l, last=(l == n_layers - 1))
    for name in dbg:
        src = getattr(net, name)
        dst = net.dbg("dbg_" + name, src.shape, src.dtype)
        P.dma('sp', out=dst, in_=src, key="st0", reads=[], writes=[('dbg', name)])
    P.barrier()
    print("instructions:", P.ninst, "sems:", P.nsem, {e: P.cnt[e] for e in P.cnt}, "dmas:", getattr(P, 'ndma', 0), "descs~", getattr(P, 'ndesc', 0))
    return net


def make_inputs(inputs, b):
    m = {}
    for k, v in inputs.items():
        v = np.asarray(v)
        if k in ("x", "mem"):
            m[k] = np.ascontiguousarray(v[b])
        elif k == "positions":
            m[k] = np.ascontiguousarray(v[b:b + 1]).astype(np.int32)
        elif k == "router_bias":
            m[k] = np.ascontiguousarray(v.reshape(1, 16))
        else:
            m[k] = np.ascontiguousarray(v)
    inv = (10000.0 ** (-np.arange(0, 64, 2, dtype=np.float32) / 64)).astype(np.float32)
    m["inv_freq"] = np.concatenate([inv, inv]).reshape(64, 1).astype(np.float32)
    return m


def kernel(**inputs):
    net = build()
    in_maps = [make_inputs(inputs, b) for b in range(8)]
    res = run_bass_kernel_spmd(net.nc, in_maps, core_ids=list(range(8)))
    return np.stack([np.asarray(res.results[b]["y"]) for b in range(8)], axis=0).astype(np.float32)
```

```python
import math
from contextlib import ExitStack

import numpy as np
import concourse.bass as bass
import concourse.mybir as mybir
from concourse.bass_utils import run_bass_kernel_spmd

F32 = mybir.dt.float32
BF16 = mybir.dt.bfloat16
I32 = mybir.dt.int32
AF = mybir.ActivationFunctionType
ALU = mybir.AluOpType
AX = mybir.AxisListType

D = 1024
T = 4096
DEPTH = 4
NT = T // 128
TB = 512
NB = T // TB
H = 8
QR = 384
KVR = 256
NOPE = 128
ROPE = 64
VD = 128
N_IN = 10976
C_DQ = 0
C_DKV = 384
C_KR = 640
C_Z = 704
C_XBC = 2752
C_DT = 6848
C_QM = 6880
C_G = 7904
DN_ALPHA = (2 * DEPTH) ** 0.25
NORM_EPS = 1e-5
RMS_EPS = 1e-6
SCALE_A = (NOPE + ROPE) ** -0.5

EPOCH = 30000
import os
SSD_PIPE = int(os.environ.get('SSD_PIPE', '1'))
SSD_HOIST = int(os.environ.get('SSD_HOIST', '1'))
_UID = [0]


def _uid(name):
    _UID[0] += 1
    return f"{name}_{_UID[0]}"


def _runs(ap):
    dims = [(int(st), int(n)) for st, n in ap.ap]
    total = 1
    for st, n in dims:
        total *= n
    run = 1
    exp = 1
    for st, n in reversed(dims[1:] if len(dims) > 1 else dims):
        if st == exp:
            run *= n
            exp *= n
        else:
            break
    return max(total // max(run, 1), 1)


_BANK_OF = {'ST': lambda i: i, 'SEG': lambda i: i, 'TP': lambda i: i, 'oacc': lambda i: 2 + i, 'CB': lambda i: 2 + i,
            'YS': lambda i: 2 + i, 'YI': lambda i: 4 if i == 0 else 6, 'SU': lambda i: 4 if i == 0 else 6}
_BANK_NAMES = {'ps0': 0, 'ps1': 1, 'ps2': 2, 'ps3': 3, 'ps4': 4, 'ps5': 5, 'ps6': 6, 'psb': 7, 'osum': 4}


def _banks(tokens):
    out = []
    for t in tokens:
        if isinstance(t, str):
            b = _BANK_NAMES.get(t)
        elif isinstance(t, tuple) and t and t[0] in _BANK_OF and len(t) == 2:
            b = _BANK_OF[t[0]](t[1])
        else:
            b = None
        if b is not None:
            out.append(('bank', b))
    return out


class Prog:
    def __init__(self):
        self.nc = bass.Bass("TRN2", target_bir_lowering=False)
        nc = self.nc
        self.es = ExitStack()
        self.eng = dict(pe=nc.tensor, act=nc.scalar, dve=nc.vector, pool=nc.gpsimd, sp=nc.sync)
        self.cnt = {e: 0 for e in self.eng}
        self.sems = {e: [] for e in self.eng}
        self.waited = {e: {} for e in self.eng}
        self.lastw = {}
        self.readers = {}
        self.dsem = {}
        self.dcnt = {}
        self.nsem = 0
        self.ninst = 0

    def _new_sem(self, name):
        self.nsem += 1
        return self.es.enter_context(self.nc.semaphore(name))

    def _esem(self, e, n):
        ep = (n - 1) // EPOCH
        while len(self.sems[e]) <= ep:
            self.sems[e].append(self._new_sem(f"s_{e}_{len(self.sems[e])}"))
        return self.sems[e][ep], (n - 1) % EPOCH + 1

    def _wait(self, c, ev):
        if ev is None:
            return
        kind, p, n = ev
        if kind == 'e':
            if p == c:
                if c == 'pe' or n > self.cnt[c] or n < self.cnt[c] - 1:
                    return
            if self.waited[c].get(p, 0) >= n:
                return
            sem, val = self._esem(p, n)
            self.eng[c].wait_ge(sem, val)
            self.waited[c][p] = n
        else:
            k = ('d', p)
            if self.waited[c].get(k, 0) >= n:
                return
            self.eng[c].wait_ge(self.dsem[p], 16 * n)
            self.waited[c][k] = n

    def _deps(self, reads, writes):
        deps = []
        for r in reads:
            ev = self.lastw.get(r)
            if ev is not None:
                deps.append(ev)
        for w in writes:
            ev = self.lastw.get(w)
            if ev is not None:
                deps.append(ev)
            rd = self.readers.get(w)
            if rd:
                deps.extend(rd.values())
        return deps

    def _record(self, ev, reads, writes):
        key = (ev[0], ev[1])
        for r in reads:
            self.readers.setdefault(r, {})[key] = ev
        for w in writes:
            self.lastw[w] = ev
            self.readers[w] = {}

    def op(self, e, fn, reads=(), writes=(), inc=True):
        bk = _banks(reads) + _banks(writes)
        if bk:
            writes = list(writes) + bk
        for ev in self._deps(reads, writes):
            self._wait(e, ev)
        inst = fn()
        self.ninst += 1
        if inc:
            self.cnt[e] += 1
            n = self.cnt[e]
            sem, _ = self._esem(e, n)
            inst.then_inc(sem, 1)
            ev = ('e', e, n)
        else:
            ev = ('e', e, self.cnt[e] + 1)
        self._record(ev, reads, writes)
        return ev

    def dma(self, q, out, in_, key, reads=(), writes=()):
        if key not in self.dsem:
            self.dsem[key] = self._new_sem(f"d_{key}")
            self.dcnt[key] = 0
        kres = ('dmakey', key)
        for ev in self._deps(reads, list(writes) + [kres]):
            self._wait(q, ev)
        inst = self.eng[q].dma_start(out=out, in_=in_)
        self.ninst += 1
        self.ndma = getattr(self, 'ndma', 0) + 1
        self.ndesc = getattr(self, 'ndesc', 0) + max(_runs(out), _runs(in_))
        self.dcnt[key] += 1
        inst.then_inc(self.dsem[key], 16)
        ev = ('d', key, self.dcnt[key])
        self._record(ev, reads, list(writes) + [kres])
        return ev

    def barrier(self):
        for c in self.eng:
            for p in self.eng:
                if p != c and self.cnt[p] > 0:
                    self._wait(c, ('e', p, self.cnt[p]))
            for k, m in self.dcnt.items():
                if m > 0:
                    self._wait(c, ('d', k, m))
        self.lastw.clear()
        self.readers.clear()

    def mm(self, out, pairs, wres, reads):
        nc = self.nc
        n = len(pairs)
        for i, (l, r) in enumerate(pairs):
            self.op('pe', lambda l=l, r=r, i=i: nc.tensor.matmul(out, l, r, start=(i == 0), stop=(i == n - 1)),
                    reads=reads if i == 0 else (), writes=[wres], inc=(i == n - 1))
        ev = ('e', 'pe', self.cnt['pe'])
        self._record(ev, reads, ())


class Net:
    def __init__(self, n_layers=DEPTH, debug=None):
        self.P = Prog()
        self.nc = self.P.nc
        self.n_layers = n_layers
        self.debug = debug or {}
        self.dbg_out = {}

    def declare(self):
        nc = self.nc
        L = DEPTH
        def inp(name, shape, dt=F32):
            return nc.dram_tensor(name, list(shape), dt, kind="ExternalInput").ap()
        self.x_in = inp("x", [T, D])
        self.mem_in = inp("mem", [256, D])
        self.pos_in = inp("positions", [1, T], I32)
        self.w_in = inp("w_in", [L, D, N_IN])
        self.q_norm = inp("q_norm", [L, QR])
        self.w_uq = inp("w_uq", [L, QR, H * 192])
        self.kv_norm = inp("kv_norm", [L, KVR])
        self.w_ukv = inp("w_ukv", [L, KVR, H, 256])
        self.w_proj_a = inp("w_proj_a", [L, D, D])
        self.conv_w = inp("conv_w", [L, 4, 4096])
        self.conv_b = inp("conv_b", [L, 4096])
        self.dt_bias = inp("dt_bias", [L, 32])
        self.a_log = inp("a_log", [L, 32])
        self.d_skip = inp("d_skip", [L, 32])
        self.ssm_norm = inp("ssm_norm", [L, 2048])
        self.w_proj_b = inp("w_proj_b", [L, 2048, D])
        self.w_mem_kv = inp("w_mem_kv", [L, D, 2048])
        self.w_proj_c = inp("w_proj_c", [L, D, D])
        self.w_out = inp("w_out", [L, D, D])
        self.ln1_g = inp("ln1_g", [L, D])
        self.ln1_b = inp("ln1_b", [L, D])
        self.router_w = inp("router_w", [D, 16])
        self.router_bias = inp("router_bias", [1, 16])
        self.exp_w1 = inp("exp_w1", [L, 16, D, 512])
        self.exp_w3 = inp("exp_w3", [L, 16, D, 512])
        self.exp_w2 = inp("exp_w2", [L, 16, 512, D])
        self.ln2_g = inp("ln2_g", [L, D])
        self.ln2_b = inp("ln2_b", [L, D])
        self.inv_freq = inp("inv_freq", [64, 1])
        self.y_out = nc.dram_tensor("y", [T, D], F32, kind="ExternalOutput").ap()

        def scr(name, shape, dt):
            return nc.dram_tensor(name, list(shape), dt, kind="Internal").ap()
        self.xT_d = scr("xT_d", [128, 8, T], BF16)
        self.x1T_d = scr("x1T_d", [128, 8, T], BF16)
        self.xa_d = scr("xa_d", [T, D], F32)
        self.x1_d = scr("x1_d", [T, D], F32)
        self.mA_d = scr("mA_d", [128, 8, T], BF16)
        self.mB_d = scr("mB_d", [128, 8, T], BF16)
        self.mC_d = scr("mC_d", [128, 8, T], BF16)
        self.xsB_d = scr("xsB_d", [T, 3072], BF16)
        self.BCT_d = scr("BCT_d", [128, 16, T], BF16)
        self.ynT_d = scr("ynT_d", [128, 16, T], BF16)
        self.gT_d = scr("gT_d", [16, T], F32)
        self.cos_d = scr("cos_d", [64, T], F32)
        self.sin_d = scr("sin_d", [64, T], F32)

    def dbg(self, name, shape, dt=F32):
        ap = self.nc.dram_tensor(name, list(shape), dt, kind="ExternalOutput").ap()
        self.dbg_out[name] = ap
        return ap


def _setup_globals(self):
    P, nc = self.P, self.nc
    es = P.es
    sb = lambda name, shape, dt: es.enter_context(nc.sbuf_tensor(_uid(name), shape, dt))
    self.ps = [es.enter_context(nc.psum_tensor(f"ps{i}", [128, 512], F32)) for i in range(7)]
    self.psb = es.enter_context(nc.psum_tensor("psb", [128, 1024], BF16))
    self.ident_bf = sb("ident_bf", [128, 128], BF16)
    self.ident_f = sb("ident_f", [128, 128], F32)
    self.ones_bf = sb("ones_bf", [128, 128], BF16)
    self.ones_f = sb("ones_f", [128, 128], F32)
    self.mask_le = sb("mask_le", [128, 128], BF16)
    self.tri_le_f = sb("tri_le_f", [128, 128], F32)
    self.tri_gt_f = sb("tri_gt_f", [128, 128], F32)
    g = nc.gpsimd
    for tl, val in ((self.ident_bf, 0.0), (self.ident_f, 0.0)):
        P.op('pool', lambda tl=tl: g.memset(tl[:], 0.0), writes=[('const', tl.name)])
        P.op('pool', lambda tl=tl: g.affine_select(out=tl[:], in_=tl[:], pattern=[[-1, 128]],
                                                   compare_op=ALU.not_equal, fill=1.0, base=0,
                                                   channel_multiplier=1), writes=[('const', tl.name)])
    P.op('pool', lambda: g.memset(self.ones_bf[:], 1.0), writes=[('const', 'ones_bf')])
    P.op('pool', lambda: g.memset(self.ones_f[:], 1.0), writes=[('const', 'ones_f')])
    for tl in (self.mask_le, self.tri_le_f):
        P.op('pool', lambda tl=tl: g.memset(tl[:], 1.0), writes=[('const', tl.name)])
        P.op('pool', lambda tl=tl: g.affine_select(out=tl[:], in_=tl[:], pattern=[[1, 128]],
                                                   compare_op=ALU.is_ge, fill=0.0, base=0,
                                                   channel_multiplier=-1), writes=[('const', tl.name)])
    P.op('pool', lambda: g.memset(self.tri_gt_f[:], 1.0), writes=[('const', 'tri_gt_f')])
    P.op('pool', lambda: g.affine_select(out=self.tri_gt_f[:], in_=self.tri_gt_f[:], pattern=[[-1, 128]],
                                         compare_op=ALU.is_gt, fill=0.0, base=0,
                                         channel_multiplier=1), writes=[('const', 'tri_gt_f')])
    self.wkey = 0
    P.barrier()


def _load_w(self, dst, src, tok, nk, q='pool'):
    P = self.P
    for kc in range(nk):
        key = f"w{self.wkey % 8}"
        self.wkey += 1
        P.dma(q, out=dst(kc), in_=src(kc), key=key, writes=[(tok, kc)])


def _toks(tok, nk):
    return [(tok, kc) for kc in range(nk)]


Net.setup_globals = _setup_globals
Net.load_w = _load_w


def _prologue(self):
    P, nc = self.P, self.nc
    with ExitStack() as es:
        sb = lambda name, shape, dt: es.enter_context(nc.sbuf_tensor(_uid(name), shape, dt))
        posi = sb("posi", [64, T], I32)
        ang = sb("ang", [64, T], F32)
        red = sb("red", [64, T], F32)
        tab = sb("tab", [64, T], F32)
        invf = sb("invf", [64, 1], F32)
        negpi = sb("negpi", [64, 1], F32)
        P.dma('sp', out=posi[:], in_=self.pos_in[0, :].partition_broadcast(64), key="ld0", writes=['posi'])
        P.dma('sp', out=invf[:], in_=self.inv_freq[:, :], key="ld1", writes=['invf'])
        P.op('dve', lambda: nc.vector.memset(negpi[:], -math.pi), writes=['negpi'])
        P.op('dve', lambda: nc.vector.tensor_copy(ang[:], posi[:]), reads=['posi'], writes=['ang'])
        P.op('dve', lambda: nc.vector.tensor_scalar(ang[:], ang[:], invf[:, 0:1], None, op0=ALU.mult),
             reads=['invf'], writes=['ang'])
        ki = sb("ki", [64, T], I32)
        kf = sb("kf", [64, T], F32)
        C1 = 6.28125
        C2 = 2 * math.pi - C1
        P.op('dve', lambda: nc.vector.tensor_scalar(red[:], ang[:], 1.0 / (2 * math.pi), None, op0=ALU.mult),
             reads=['ang'], writes=['red'])
        P.op('dve', lambda: nc.vector.tensor_copy(ki[:], red[:]), reads=['red'], writes=['ki'])
        P.op('dve', lambda: nc.vector.tensor_copy(kf[:], ki[:]), reads=['ki'], writes=['kf'])
        P.op('dve', lambda: nc.vector.scalar_tensor_tensor(out=red[:], in0=kf[:], scalar=-C1, in1=ang[:],
                                                           op0=ALU.mult, op1=ALU.add), reads=['kf', 'ang'], writes=['red'])
        P.op('dve', lambda: nc.vector.scalar_tensor_tensor(out=red[:], in0=kf[:], scalar=-C2, in1=red[:],
                                                           op0=ALU.mult, op1=ALU.add), reads=['kf', 'red'], writes=['red'])

        def wrap_sin(shift):
            P.op('dve', lambda: nc.vector.tensor_scalar(ang[:], red[:], shift, None, op0=ALU.add),
                 reads=['red'], writes=['ang'])
            P.op('dve', lambda: nc.vector.tensor_scalar(kf[:], ang[:], math.pi, 2 * math.pi, op0=ALU.is_gt, op1=ALU.mult),
                 reads=['ang'], writes=['kf'])
            P.op('dve', lambda: nc.vector.tensor_tensor(ang[:], ang[:], kf[:], op=ALU.subtract),
                 reads=['ang', 'kf'], writes=['ang'])
            P.op('dve', lambda: nc.vector.tensor_scalar(ang[:], ang[:], -math.pi, math.pi, op0=ALU.max, op1=ALU.min),
                 reads=['ang'], writes=['ang'])
            P.op('act', lambda: nc.scalar.activation(out=tab[:], in_=ang[:], func=AF.Sin),
                 reads=['ang'], writes=['tab'])
        wrap_sin(0.0)
        P.op('dve', lambda: nc.vector.tensor_scalar(tab[0:32, :], tab[0:32, :], -1.0, None, op0=ALU.mult),
             reads=['tab'], writes=['tab'])
        P.dma('sp', out=self.sin_d[:, :], in_=tab[:], key="st0", reads=['tab'], writes=['sin_d'])
        wrap_sin(0.5 * math.pi)
        P.dma('sp', out=self.cos_d[:, :], in_=tab[:], key="st1", reads=['tab'], writes=['cos_d'])
        xin = [sb(f"xin{i}", [128, D], F32) for i in range(2)]
        xbf = [sb(f"xbf{i}", [128, D], BF16) for i in range(2)]
        stg = [sb(f"stg{i}", [128, 8, TB], BF16) for i in range(2)]
        for t in range(NT):
            i = t % 2
            b, tt = divmod(t, 4)
            P.dma('sp', out=xin[i][:], in_=self.x_in[t * 128:(t + 1) * 128, :], key=f"ld{2 + i}", writes=[('xin', i)])
            P.op('dve', lambda i=i: nc.vector.tensor_copy(xbf[i][:], xin[i][:]), reads=[('xin', i)], writes=[('xbf', i)])
            for kc in range(8):
                P.op('pe', lambda i=i, kc=kc: nc.tensor.transpose(self.psb[:, kc * 128:(kc + 1) * 128],
                                                                   xbf[i][:, kc * 128:(kc + 1) * 128], self.ident_bf[:]),
                     reads=[('xbf', i)], writes=['psb'], inc=(kc == 7))
            P.op('act', lambda b=b, tt=tt: nc.scalar.copy(stg[b % 2][:, :, tt * 128:(tt + 1) * 128],
                                                         self.psb[:].rearrange("p (k n) -> p k n", k=8)),
                 reads=['psb'], writes=[('stg', b % 2)])
            if tt == 3:
                P.dma('sp', out=self.xT_d[:, :, b * TB:(b + 1) * TB], in_=stg[b % 2][:], key=f"st{2 + b % 2}",
                      reads=[('stg', b % 2)], writes=[('xT_d', b)])
    P.barrier()


Net.prologue = _prologue


def _pass_mla(self, l):
    P, nc = self.P, self.nc
    ps = self.ps
    with ExitStack() as es:
        sb = lambda name, shape, dt: es.enter_context(nc.sbuf_tensor(_uid(name), shape, dt))
        Wmla = sb("Wmla", [128, 8, 640], BF16)
        Wkr = sb("Wkr", [128, 8, 128], BF16)
        WgA = sb("WgA", [128, 8, D], BF16)
        wuq = sb("wuq", [128, 3, H * 192], BF16)
        wuqsw = sb("wuqsw", [128, 3, H, 64], BF16)
        wukv = sb("wukv", [128, 2, H, 256], BF16)
        wukT = sb("wukT", [128, H, 256], BF16)
        wpa = sb("wpa", [128, 8, D], BF16)
        gq = sb("gq", [128, QR], F32)
        gkv = sb("gkv", [128, KVR], F32)
        w_in = self.w_in
        rows = lambda kc: slice(kc * 128, (kc + 1) * 128)
        self.load_w(lambda kc: Wmla[:, kc, :], lambda kc: w_in[l, rows(kc), 0:640], 'Wmla', 8)
        self.load_w(lambda kc: Wkr[:, kc, 0:64], lambda kc: w_in[l, rows(kc), C_KR:C_KR + 64], 'Wkr', 8)
        self.load_w(lambda kc: wuq[:, kc, :], lambda kc: self.w_uq[l, rows(kc), :], 'wuq', 3)
        self.load_w(lambda kc: wukv[:, kc, :, :], lambda kc: self.w_ukv[l, rows(kc), :, :], 'wukv', 2)
        P.dma('sp', out=gq[:], in_=self.q_norm[l, :].partition_broadcast(128), key="ld0", writes=['gq'])
        P.dma('sp', out=gkv[:], in_=self.kv_norm[l, :].partition_broadcast(128), key="ld1", writes=['gkv'])
        self.load_w(lambda kc: wpa[:, kc, :], lambda kc: self.w_proj_a[l, rows(kc), :], 'wpa', 8)
        self.load_w(lambda kc: WgA[:, kc, :], lambda kc: w_in[l, rows(kc), C_G:C_G + D], 'WgA', 8)
        for kc in range(8):
            P.op('dve', lambda kc=kc: nc.vector.tensor_copy(Wkr[:, kc, 64:96], Wkr[:, kc, 32:64]),
                 reads=[('Wkr', kc)], writes=[('Wkrs', kc)])
            P.op('dve', lambda kc=kc: nc.vector.tensor_copy(Wkr[:, kc, 96:128], Wkr[:, kc, 0:32]),
                 reads=[('Wkr', kc)], writes=[('Wkrs', kc)])
        for kc in range(3):
            v = wuq[:, kc, :].rearrange("p (h c) -> p h c", h=H)
            P.op('dve', lambda kc=kc, v=v: nc.vector.tensor_copy(wuqsw[:, kc, :, 0:32], v[:, :, 160:192]),
                 reads=[('wuq', kc)], writes=[('wuqsw', kc)])
            P.op('dve', lambda kc=kc, v=v: nc.vector.tensor_copy(wuqsw[:, kc, :, 32:64], v[:, :, 128:160]),
                 reads=[('wuq', kc)], writes=[('wuqsw', kc)])
        for h in range(H):
            for cc in range(2):
                P.op('pe', lambda h=h, cc=cc: nc.tensor.transpose(self.psb[:, cc * 128:(cc + 1) * 128],
                                                                   wukv[:, cc, h, 0:128], self.ident_bf[:]),
                     reads=[('wukv', cc)], writes=['psb'], inc=(cc == 1))
            P.op('act', lambda h=h: nc.scalar.copy(wukT[:, h, :], self.psb[:, 0:256]), reads=['psb'], writes=[('wukT', h)])

        ckvT = sb("ckvT", [128, 2, T], BF16)
        krT = sb("krT", [64, T], BF16)
        ckvTM = sb("ckvTM", [128, NT, KVR], BF16)
        xblk = [sb(f"xblk{i}", [128, 8, TB], BF16) for i in range(2)]
        cosb = [sb(f"cosb{i}", [64, TB], F32) for i in range(2)]
        sinb = [sb(f"sinb{i}", [64, TB], F32) for i in range(2)]
        cqTM = sb("cqTM", [128, QR], BF16)
        cqT = sb("cqT", [128, 3, TB], BF16)
        junk = sb("junk", [128, 512], F32)
        ss = sb("ss", [128, 8], F32)
        qn = [sb(f"qn{i}", [128, TB], BF16) for i in range(2)]
        qlat = sb("qlat", [128, H, 2, TB], BF16)
        qrope = sb("qrope", [64, H, TB], BF16)
        rtmp = sb("rtmp", [64, TB], F32)
        rtmp2 = sb("rtmp2", [64, TB], F32)
        PT = [sb(f"PT{i}", [128, TB], BF16) for i in range(3)]
        rs = sb("rs", [128, TB], F32)
        olat = [sb(f"olat{i}", [128, 2, TB], BF16) for i in range(2)]
        oT = sb("oT", [128, H, TB], BF16)
        sig2 = [sb(f"sig{i}", [128, TB], F32) for i in range(2)]
        mT = sb("mT", [128, 8, TB], BF16)
        W = lambda name, n: _toks(name, n)
        rot = [0]

        def nb():
            i = rot[0] % 7
            rot[0] += 1
            return ps[i], f'ps{i}'

        for b in range(NB):
            i2 = b % 2
            t0 = b * TB
            P.dma('sp', out=xblk[i2][:], in_=self.xT_d[:, :, t0:t0 + TB], key=f"ld{2 + i2}", writes=[('xblk', i2)])
            P.dma('sp', out=cosb[i2][:], in_=self.cos_d[:, t0:t0 + TB], key=f"ld{4 + i2}", writes=[('cosb', i2)])
            P.dma('sp', out=sinb[i2][:], in_=self.sin_d[:, t0:t0 + TB], key=f"ld{6 + i2}", writes=[('sinb', i2)])
            xb = xblk[i2]
            for tt in range(4):
                t = b * 4 + tt
                cols = slice(tt * 128, (tt + 1) * 128)
                pA, tA = nb()
                pB, tB = nb()
                P.mm(pA[:, 0:512], [(xb[:, kc, cols], Wmla[:, kc, 0:512]) for kc in range(8)], tA,
                     [('xblk', i2)] + W('Wmla', 8))
                P.mm(pB[:, 0:128], [(xb[:, kc, cols], Wmla[:, kc, 512:640]) for kc in range(8)], tB,
                     [('xblk', i2)] + W('Wmla', 8))
                P.op('dve', lambda pA=pA, pB=pB: nc.vector.memset(ss[:, 0:3], 0.0), writes=['ss0', 'ss1', 'ss2'])
                P.op('act', lambda pA=pA, pB=pB: nc.scalar.activation(out=junk[:, 0:384], in_=pA[:, 0:384], func=AF.Square,
                                                         accum_out=ss[:, 0:1]), reads=[tA], writes=['junk', 'ss0'])
                P.op('act', lambda pA=pA, pB=pB: nc.scalar.activation(out=junk[:, 384:512], in_=pA[:, 384:512], func=AF.Square,
                                                         accum_out=ss[:, 1:2]), reads=[tA], writes=['junk', 'ss1'])
                P.op('act', lambda pA=pA, pB=pB: nc.scalar.activation(out=junk[:, 0:128], in_=pB[:, 0:128], func=AF.Square,
                                                         accum_out=ss[:, 2:3]), reads=[tB], writes=['junk', 'ss2'])
                P.op('dve', lambda pA=pA, pB=pB: nc.vector.tensor_scalar(ss[:, 4:5], ss[:, 0:1], 1.0 / QR, RMS_EPS, op0=ALU.mult, op1=ALU.add),
                     reads=['ss0'], writes=['ss4'])
                P.op('act', lambda pA=pA, pB=pB: nc.scalar.activation(out=ss[:, 6:7], in_=ss[:, 4:5], func=AF.Sqrt), reads=['ss4'], writes=['ss6'])
                P.op('dve', lambda pA=pA, pB=pB: nc.vector.reciprocal(ss[:, 4:5], ss[:, 6:7]), reads=['ss6'], writes=['ss4'])
                P.op('dve', lambda pA=pA, pB=pB: nc.vector.tensor_tensor(ss[:, 5:6], ss[:, 1:2], ss[:, 2:3], op=ALU.add),
                     reads=['ss1', 'ss2'], writes=['ss5'])
                P.op('dve', lambda pA=pA, pB=pB: nc.vector.tensor_scalar(ss[:, 5:6], ss[:, 5:6], 1.0 / KVR, RMS_EPS, op0=ALU.mult, op1=ALU.add),
                     reads=['ss5'], writes=['ss5'])
                P.op('act', lambda pA=pA, pB=pB: nc.scalar.activation(out=ss[:, 7:8], in_=ss[:, 5:6], func=AF.Sqrt), reads=['ss5'], writes=['ss7'])
                P.op('dve', lambda pA=pA, pB=pB: nc.vector.reciprocal(ss[:, 5:6], ss[:, 7:8]), reads=['ss7'], writes=['ss5'])
                P.op('dve', lambda pA=pA, pB=pB: nc.vector.scalar_tensor_tensor(out=cqTM[:], in0=pA[:, 0:384], scalar=ss[:, 4:5],
                                                                   in1=gq[:], op0=ALU.mult, op1=ALU.mult),
                     reads=[tA, 'ss4', 'gq'], writes=['cqTM'])
                P.op('dve', lambda t=t, pA=pA, pB=pB: nc.vector.scalar_tensor_tensor(out=ckvTM[:, t, 0:128], in0=pA[:, 384:512],
                                                                       scalar=ss[:, 5:6], in1=gkv[:, 0:128],
                                                                       op0=ALU.mult, op1=ALU.mult),
                     reads=[tA, 'ss5', 'gkv'], writes=[('ckvTM', t)])
                P.op('dve', lambda t=t, pA=pA, pB=pB: nc.vector.scalar_tensor_tensor(out=ckvTM[:, t, 128:256], in0=pB[:, 0:128],
                                                                       scalar=ss[:, 5:6], in1=gkv[:, 128:256],
                                                                       op0=ALU.mult, op1=ALU.mult),
                     reads=[tB, 'ss5', 'gkv'], writes=[('ckvTM', t)])
                for j in range(3):
                    P.op('pe', lambda j=j: nc.tensor.transpose(self.psb[:, j * 128:(j + 1) * 128],
                                                               cqTM[:, j * 128:(j + 1) * 128], self.ident_bf[:]),
                         reads=['cqTM'], writes=['psb'], inc=False)
                for j in range(2):
                    P.op('pe', lambda j=j, t=t: nc.tensor.transpose(self.psb[:, (3 + j) * 128:(4 + j) * 128],
                                                                    ckvTM[:, t, j * 128:(j + 1) * 128], self.ident_bf[:]),
                         reads=[('ckvTM', t)], writes=['psb'], inc=(j == 1))
                P.op('pool' if False else 'act', lambda cols=cols: nc.scalar.copy(
                    cqT[:, :, cols], self.psb[:, 0:384].rearrange("p (k n) -> p k n", k=3)),
                     reads=['psb'], writes=['cqT'])
                P.op('act', lambda t=t: nc.scalar.copy(
                    ckvT[:, :, t * 128:(t + 1) * 128], self.psb[:, 384:640].rearrange("p (k n) -> p k n", k=2)),
                     reads=['psb'], writes=[('ckvT', t)])
            pA, tA = nb()
            pB, tB = nb()
            P.mm(pA[0:64, :], [(Wkr[:, kc, 0:64], xb[:, kc, :]) for kc in range(8)], tA,
                 [('xblk', i2)] + W('Wkr', 8))
            P.mm(pB[0:64, :], [(Wkr[:, kc, 64:128], xb[:, kc, :]) for kc in range(8)], tB,
                 [('xblk', i2)] + W('Wkrs', 8))

            def rope(psA, psB, outap, rA, rB, wtok):
                P.op('dve', lambda: nc.vector.tensor_tensor(rtmp[:], psB, sinb[i2][:], op=ALU.mult),
                     reads=[rB, ('sinb', i2)], writes=['rtmp'])
                P.op('dve', lambda: nc.vector.tensor_tensor(rtmp2[:], psA, cosb[i2][:], op=ALU.mult),
                     reads=[rA, ('cosb', i2)], writes=['rtmp2'])
                P.op('dve', lambda: nc.vector.tensor_tensor(outap, rtmp[:], rtmp2[:], op=ALU.add),
                     reads=['rtmp', 'rtmp2'], writes=[wtok])
            rope(pA[0:64, :], pB[0:64, :], krT[:, t0:t0 + TB], tA, tB, ('krT', b))

            for h in range(H):
                c0 = h * 192
                pq, tq_ = nb()
                P.mm(pq[:, :], [(wuq[:, kc, c0:c0 + 128], cqT[:, kc, :]) for kc in range(3)], tq_,
                     ['cqT'] + W('wuq', 3))
                P.op('act', lambda pq=pq, h=h: nc.scalar.copy(qn[h % 2][:], pq[:, :]), reads=[tq_], writes=[('qn', h % 2)])
                pa_, ta_ = nb()
                pb_, tb_ = nb()
                P.mm(pa_[0:64, :], [(wuq[:, kc, c0 + 128:c0 + 192], cqT[:, kc, :]) for kc in range(3)], ta_,
                     ['cqT'] + W('wuq', 3))
                P.mm(pb_[0:64, :], [(wuqsw[:, kc, h, :], cqT[:, kc, :]) for kc in range(3)], tb_,
                     ['cqT'] + W('wuqsw', 3))
                for cc in range(2):
                    pl, tl_ = nb()
                    P.mm(pl[:, :], [(wukT[:, h, cc * 128:(cc + 1) * 128], qn[h % 2][:])], tl_, [('qn', h % 2), ('wukT', h)])
                    P.op('dve' if cc == 0 else 'act',
                         (lambda h=h, cc=cc, pl=pl: nc.vector.tensor_copy(qlat[:, h, cc, :], pl[:, :])) if cc == 0 else
                         (lambda h=h, cc=cc, pl=pl: nc.scalar.copy(qlat[:, h, cc, :], pl[:, :])),
                         reads=[tl_], writes=[('qlat', h)])
                rope(pa_[0:64, :], pb_[0:64, :], qrope[:, h, :], ta_, tb_, ('qrope', h))

            nkt = 4 * b + 4
            items = [(h, kt) for h in range(H) for kt in range(nkt)]

            def stage_S(i):
                h, kt = items[i]
                d = kt - 4 * b
                q0 = max(d, 0) * 128
                qs = slice(q0, TB)
                si, pi = i % 2, i % 3
                st = ps[si]
                kk = slice(kt * 128, (kt + 1) * 128)
                P.mm(st[:, qs], [(ckvT[:, 0, kk], qlat[:, h, 0, qs]), (ckvT[:, 1, kk], qlat[:, h, 1, qs]),
                                 (krT[:, kk], qrope[:, h, qs])], ('ST', si),
                     [('ckvT', kt), ('krT', kt // 4), ('qlat', h), ('qrope', h)])
                P.op('act', lambda: nc.scalar.activation(out=PT[pi][:, qs], in_=st[:, qs], func=AF.Exp, scale=SCALE_A),
                     reads=[('ST', si)], writes=[('PT', pi)])
                if d >= 0:
                    dq = slice(q0, q0 + 128)
                    P.op('pool', lambda: nc.gpsimd.tensor_tensor(PT[pi][:, dq], PT[pi][:, dq], self.mask_le[:], op=ALU.mult),
                         reads=[('PT', pi)], writes=[('PT', pi)])

            def stage_V(i):
                h, kt = items[i]
                d = kt - 4 * b
                q0 = max(d, 0) * 128
                qs = slice(q0, TB)
                pi = i % 3
                first, last = (kt == 0), (kt == nkt - 1)
                for cc in range(2):
                    P.op('pe', lambda cc=cc: nc.tensor.matmul(
                        ps[2 + cc][:, qs], ckvTM[:, kt, cc * 128:(cc + 1) * 128], PT[pi][:, qs], start=first, stop=last),
                         reads=[('PT', pi), ('ckvTM', kt)], writes=[('oacc', cc)], inc=False)
                P.op('pe', lambda: nc.tensor.matmul(ps[4][:, qs], self.ones_bf[:], PT[pi][:, qs], start=first, stop=last),
                     reads=[('PT', pi)], writes=['osum'], inc=True)
                P._record(('e', 'pe', P.cnt['pe']), [('PT', pi), ('ckvTM', kt)], [('oacc', 0), ('oacc', 1)])
                if last:
                    ol = olat[h % 2]
                    P.op('dve', lambda: nc.vector.reciprocal(rs[:], ps[4][:, :]), reads=['osum'], writes=['rs'])
                    for cc in range(2):
                        P.op('dve', lambda cc=cc: nc.vector.tensor_tensor(ol[:, cc, :], ps[2 + cc][:, :], rs[:], op=ALU.mult),
                             reads=[('oacc', cc), 'rs'], writes=[('olat', h % 2)])

            def proj_o(h):
                ol = olat[h % 2]
                P.mm(ps[5][:, :], [(wukv[:, cc, h, 128:256], ol[:, cc, :]) for cc in range(2)], 'ps5',
                     [('olat', h % 2)] + W('wukv', 2))
                P.op('act', lambda: nc.scalar.copy(oT[:, h, :], ps[5][:, :]), reads=['ps5'], writes=[('oT', h)])

            pending = []
            stage_S(0)
            for i in range(len(items)):
                if i + 1 < len(items):
                    stage_S(i + 1)
                stage_V(i)
                h, kt = items[i]
                if kt == nkt - 1:
                    pending.append((i + 2, h))
                while pending and pending[0][0] <= i:
                    proj_o(pending.pop(0)[1])
            for _, h in pending:
                proj_o(h)

            for dc in range(8):
                dd = slice(dc * 128, (dc + 1) * 128)
                pA, tA = nb()
                pB, tB = nb()
                sg = sig2[dc % 2]
                P.mm(pA[:, :], [(WgA[:, kc, dd], xb[:, kc, :]) for kc in range(8)], tA,
                     [('xblk', i2)] + W('WgA', 8))
                P.op('act', lambda pA=pA, sg=sg: nc.scalar.activation(out=sg[:], in_=pA[:, :], func=AF.Sigmoid),
                     reads=[tA], writes=[('sig', dc % 2)])
                P.mm(pB[:, :], [(wpa[:, hh, dd], oT[:, hh, :]) for hh in range(H)], tB,
                     [('oT', hh) for hh in range(H)] + W('wpa', 8))
                P.op('dve', lambda dc=dc, pB=pB, sg=sg: nc.vector.tensor_tensor(mT[:, dc, :], pB[:, :], sg[:], op=ALU.mult),
                     reads=[tB, ('sig', dc % 2)], writes=[('mT', dc)])
            P.dma('sp', out=self.mA_d[:, :, t0:t0 + TB], in_=mT[:], key="st0",
                  reads=[('mT', dc) for dc in range(8)], writes=[('mA_d', b)])
    P.barrier()


Net.pass_mla = _pass_mla


def _pass_mem(self, l):
    P, nc = self.P, self.nc
    ps = self.ps
    SC = 256 ** -0.5
    with ExitStack() as es:
        sb = lambda name, shape, dt: es.enter_context(nc.sbuf_tensor(_uid(name), shape, dt))
        Wqm = sb("Wqm", [128, 8, D], BF16)
        WgC = sb("WgC", [128, 8, D], BF16)
        wpc = sb("wpc", [128, 8, D], BF16)
        KT = sb("KT", [128, 4, 2, 256], BF16)
        V = sb("V", [128, 2, D], BF16)
        rows = lambda kc: slice(kc * 128, (kc + 1) * 128)
        W = lambda name, n: _toks(name, n)
        self.load_w(lambda kc: Wqm[:, kc, :], lambda kc: self.w_in[l, rows(kc), C_QM:C_QM + D], 'Wqm', 8)
        self.load_w(lambda kc: WgC[:, kc, :], lambda kc: self.w_in[l, rows(kc), C_G + 2 * D:C_G + 3 * D], 'WgC', 8)
        self.load_w(lambda kc: wpc[:, kc, :], lambda kc: self.w_proj_c[l, rows(kc), :], 'wpc', 8)
        with ExitStack() as es2:
            sb2 = lambda name, shape, dt: es2.enter_context(nc.sbuf_tensor(_uid(name), shape, dt))
            wmkv = sb2("wmkv", [128, 8, 2048], BF16)
            memT = sb2("memT", [128, 8, 256], BF16)
            mtile = sb2("mtile", [128, D], BF16)
            self.load_w(lambda kc: wmkv[:, kc, :], lambda kc: self.w_mem_kv[l, rows(kc), :], 'wmkv', 8)
            for mt in range(2):
                P.dma('pool', out=mtile[:], in_=self.mem_in[mt * 128:(mt + 1) * 128, :], key="ld0", writes=['mtile'])
                for kc in range(8):
                    P.op('pe', lambda kc=kc: nc.tensor.transpose(self.psb[:, kc * 128:(kc + 1) * 128],
                                                                 mtile[:, kc * 128:(kc + 1) * 128], self.ident_bf[:]),
                         reads=['mtile'], writes=['psb'], inc=(kc == 7))
                P.op('act', lambda mt=mt: nc.scalar.copy(memT[:, :, mt * 128:(mt + 1) * 128],
                                                         self.psb[:].rearrange("p (k n) -> p k n", k=8)),
                     reads=['psb'], writes=['memT'])
            for h in range(4):
                for dc in range(2):
                    c0 = h * 256 + dc * 128
                    P.mm(ps[5][:, 0:256], [(wmkv[:, kc, c0:c0 + 128], memT[:, kc, :]) for kc in range(8)], 'ps5',
                         ['memT'] + W('wmkv', 8))
                    P.op('act', lambda h=h, dc=dc: nc.scalar.copy(KT[:, h, dc, :], ps[5][:, 0:256]), reads=['ps5'], writes=['KT'])
            for mt in range(2):
                for half in range(2):
                    c0 = 1024 + half * 512
                    P.mm(ps[6][:, :], [(memT[:, kc, mt * 128:(mt + 1) * 128], wmkv[:, kc, c0:c0 + 512]) for kc in range(8)],
                         'ps6', ['memT'] + W('wmkv', 8))
                    P.op('dve', lambda mt=mt, half=half: nc.vector.tensor_copy(V[:, mt, half * 512:(half + 1) * 512], ps[6][:, :]),
                         reads=['ps6'], writes=['V'])
            P.barrier()
        xblk = [sb(f"xblk{i}", [128, 8, TB], BF16) for i in range(2)]
        qm = sb("qm", [128, 2, TB], BF16)
        PT = [sb(f"PT{i}", [128, 2, TB], BF16) for i in range(2)]
        rs = sb("rs", [128, TB], F32)
        ocT = sb("ocT", [128, 8, TB], BF16)
        sig = sb("sig", [128, TB], F32)
        mT = sb("mT", [128, 8, TB], BF16)
        for b in range(NB):
            i2 = b % 2
            t0 = b * TB
            P.dma('sp', out=xblk[i2][:], in_=self.xT_d[:, :, t0:t0 + TB], key=f"ld{2 + i2}", writes=[('xblk', i2)])
            xb = xblk[i2]
            for h in range(4):
                pi = h % 2
                for dc in range(2):
                    c0 = h * 256 + dc * 128
                    pb, pt = (ps[5], 'ps5') if dc == 0 else (ps[6], 'ps6')
                    P.mm(pb[:, :], [(Wqm[:, kc, c0:c0 + 128], xb[:, kc, :]) for kc in range(8)], pt,
                         [('xblk', i2)] + W('Wqm', 8))
                    if dc == 0:
                        P.op('act', lambda pb=pb: nc.scalar.copy(qm[:, 0, :], pb[:, :]), reads=[pt], writes=[('qm', 0)])
                    else:
                        P.op('dve', lambda pb=pb: nc.vector.tensor_copy(qm[:, 1, :], pb[:, :]), reads=[pt], writes=[('qm', 1)])
                for mt in range(2):
                    P.mm(ps[mt][:, :], [(KT[:, h, dc, mt * 128:(mt + 1) * 128], qm[:, dc, :]) for dc in range(2)], ('ST', mt),
                         ['KT', ('qm', 0), ('qm', 1)])
                    P.op('act', lambda mt=mt, pi=pi: nc.scalar.activation(out=PT[pi][:, mt, :], in_=ps[mt][:, :], func=AF.Exp, scale=SC),
                         reads=[('ST', mt)], writes=[('PT', pi, mt)])
                for vc in range(2):
                    c0 = h * 256 + vc * 128
                    P.mm(ps[2 + vc][:, :], [(V[:, mt, c0:c0 + 128], PT[pi][:, mt, :]) for mt in range(2)], ('oacc', vc),
                         ['V', ('PT', pi, 0), ('PT', pi, 1)])
                P.mm(ps[4][:, :], [(self.ones_bf[:], PT[pi][:, mt, :]) for mt in range(2)], 'osum',
                     [('PT', pi, 0), ('PT', pi, 1)])
                P.op('dve', lambda: nc.vector.reciprocal(rs[:], ps[4][:, :]), reads=['osum'], writes=['rs'])
                for vc in range(2):
                    P.op('dve', lambda h=h, vc=vc: nc.vector.tensor_tensor(ocT[:, h * 2 + vc, :], ps[2 + vc][:, :], rs[:], op=ALU.mult),
                         reads=[('oacc', vc), 'rs'], writes=[('ocT', h * 2 + vc)])
            for dc in range(8):
                dd = slice(dc * 128, (dc + 1) * 128)
                P.mm(ps[5][:, :], [(WgC[:, kc, dd], xb[:, kc, :]) for kc in range(8)], 'ps5',
                     [('xblk', i2)] + W('WgC', 8))
                P.op('act', lambda: nc.scalar.activation(out=sig[:], in_=ps[5][:, :], func=AF.Sigmoid),
                     reads=['ps5'], writes=['sig'])
                P.mm(ps[6][:, :], [(wpc[:, j, dd], ocT[:, j, :]) for j in range(8)], 'ps6',
                     [('ocT', j) for j in range(8)] + W('wpc', 8))
                P.op('dve', lambda dc=dc: nc.vector.tensor_tensor(mT[:, dc, :], ps[6][:, :], sig[:], op=ALU.mult),
                     reads=['ps6', 'sig'], writes=[('mT', dc)])
            P.dma('sp', out=self.mC_d[:, :, t0:t0 + TB], in_=mT[:], key="st0",
                  reads=[('mT', dc) for dc in range(8)], writes=[('mC_d', b)])
    P.barrier()


Net.pass_mem = _pass_mem


def _pass_ssd_a(self, l):
    P, nc = self.P, self.nc
    ps = self.ps
    with ExitStack() as es:
        sb = lambda name, shape, dt: es.enter_context(nc.sbuf_tensor(_uid(name), shape, dt))
        Wx = sb("Wx", [128, 8, 4096], BF16)
        cw5 = sb("cw5", [5, 4096], F32)
        cwb = sb("cwb", [128, 32, 5], F32)
        rows = lambda kc: slice(kc * 128, (kc + 1) * 128)
        W = lambda name, n: _toks(name, n)
        self.load_w(lambda kc: Wx[:, kc, :], lambda kc: self.w_in[l, rows(kc), C_XBC:C_XBC + 4096], 'Wx', 8)
        P.dma('sp', out=cw5[0:4, :], in_=self.conv_w[l, :, :], key="ld0", writes=['cw5a'])
        P.dma('sp', out=cw5[4:5, :], in_=self.conv_b[l:l + 1, :], key="ld1", writes=['cw5b'])
        for cc in range(32):
            P.op('pe', lambda cc=cc: nc.tensor.transpose(ps[5][:, cc * 5:(cc + 1) * 5], cw5[0:5, cc * 128:(cc + 1) * 128],
                                                         self.ident_f[0:5, 0:5]),
                 reads=['cw5a', 'cw5b'], writes=['ps5'], inc=(cc == 31))
        P.op('act', lambda: nc.scalar.copy(cwb[:].rearrange("p c k -> p (c k)"), ps[5][:, 0:160]), reads=['ps5'], writes=['cwb'])
        xblk = [sb(f"xblk{i}", [128, 8, TB], BF16) for i in range(2)]
        pre = [sb(f"pre{i}", [128, TB + 3], BF16) for i in range(3)]
        carry = sb("carry", [128, 32, 3], BF16)
        xbcT = sb("xbcT", [128, 32, TB], BF16)
        tmst = [sb(f"tmst{i}", [128, 3072], BF16) for i in range(2)]
        dg = sb("dg", [128, 32, 4, 128], BF16)
        for cc in range(32):
            for k in range(4):
                P.op('dve', lambda cc=cc, k=k: nc.vector.tensor_scalar(dg[:, cc, k, :], self.ident_bf[:], cwb[:, cc, k:k + 1], None, op0=ALU.mult),
                     reads=['cwb'], writes=[('dg', cc)])
        P.op('dve', lambda: nc.vector.memset(carry[:], 0.0), writes=[('carry', cc) for cc in range(32)])
        n = 0
        rot = [0]

        def nb():
            i = rot[0] % 7
            rot[0] += 1
            return ps[i], f'ps{i}'

        for b in range(NB):
            i2 = b % 2
            t0 = b * TB
            P.dma('sp', out=xblk[i2][:], in_=self.xT_d[:, :, t0:t0 + TB], key=f"ld{2 + i2}", writes=[('xblk', i2)])
            xb = xblk[i2]
            for cc in range(32):
                j = n % 3
                n += 1
                pb, pt = nb()
                pr = pre[j]
                P.mm(pb[:, :], [(Wx[:, kc, cc * 128:(cc + 1) * 128], xb[:, kc, :]) for kc in range(8)], pt,
                     [('xblk', i2)] + W('Wx', 8))
                P.op('act', lambda pr=pr, cc=cc: nc.scalar.copy(pr[:, 0:3], carry[:, cc, :]),
                     reads=[('carry', cc)], writes=[('pre', j)])
                P.op('act', lambda pr=pr, pb=pb: nc.scalar.copy(pr[:, 3:TB + 3], pb[:, :]), reads=[pt], writes=[('pre', j)])
                P.op('act', lambda pr=pr, cc=cc: nc.scalar.copy(carry[:, cc, :], pr[:, TB:TB + 3]),
                     reads=[('pre', j)], writes=[('carry', cc)])
                pc, pct = nb()
                P.mm(pc[:, :], [(dg[:, cc, k, :], pr[:, k:k + TB]) for k in range(4)], pct, [('pre', j), ('dg', cc)])
                P.op('act', lambda pc=pc, cc=cc: nc.scalar.activation(out=xbcT[:, cc, :], in_=pc[:, :], func=AF.Silu, bias=cwb[:, cc, 4:5]),
                     reads=[pct, 'cwb'], writes=[('xbcT', cc)])
            P.dma('sp', out=self.BCT_d[:, :, t0:t0 + TB], in_=xbcT[:, 16:32, :], key="st0",
                  reads=[('xbcT', cc) for cc in range(16, 32)], writes=[('BCT_d', b)])
            for tt in range(4):
                t = b * 4 + tt
                ti = t % 2
                for r in range(3):
                    for q in range(8):
                        cc = r * 8 + q
                        P.op('pe', lambda cc=cc, q=q, tt=tt: nc.tensor.transpose(self.psb[:, q * 128:(q + 1) * 128],
                                                                                 xbcT[:, cc, tt * 128:(tt + 1) * 128], self.ident_bf[:]),
                             reads=[('xbcT', cc)], writes=['psb'], inc=(q == 7))
                    P.op('dve', lambda ti=ti, r=r: nc.vector.tensor_copy(tmst[ti][:, r * 1024:(r + 1) * 1024], self.psb[:, :]),
                         reads=['psb'], writes=[('tmst', ti)])
                P.dma('sp', out=self.xsB_d[t * 128:(t + 1) * 128, :], in_=tmst[ti][:], key=f"st{1 + ti}",
                      reads=[('tmst', ti)], writes=[('xsB_d', t)])
    P.barrier()


def _pass_ssd_b(self, l):
    P, nc = self.P, self.nc
    ps = self.ps
    with ExitStack() as es:
        sb = lambda name, shape, dt: es.enter_context(nc.sbuf_tensor(_uid(name), shape, dt))
        rows = lambda kc: slice(kc * 128, (kc + 1) * 128)
        W = lambda name, n: _toks(name, n)
        Wz = sb("Wz", [128, 8, 2048], BF16)
        Wdt = sb("Wdt", [128, 8, 32], BF16)
        dtb = sb("dtb", [128, 32], F32)
        abc = sb("abc", [128, 32], F32)
        dsk = sb("dsk", [128, 32], F32)
        ngb = sb("ngb", [128, 2048], F32)
        self.load_w(lambda kc: Wz[:, kc, :], lambda kc: self.w_in[l, rows(kc), C_Z:C_Z + 2048], 'Wz', 8)
        self.load_w(lambda kc: Wdt[:, kc, :], lambda kc: self.w_in[l, rows(kc), C_DT:C_DT + 32], 'Wdt', 8)
        P.dma('sp', out=dtb[:], in_=self.dt_bias[l, :].partition_broadcast(128), key="ld0", writes=['dtb'])
        P.dma('sp', out=abc[:], in_=self.a_log[l, :].partition_broadcast(128), key="ld1", writes=['abc'])
        P.dma('sp', out=dsk[:], in_=self.d_skip[l, :].partition_broadcast(128), key="ld2", writes=['dsk'])
        P.dma('sp', out=ngb[:], in_=self.ssm_norm[l, :].partition_broadcast(128), key="ld3", writes=['ngb'])
        P.op('act', lambda: nc.scalar.activation(out=abc[:], in_=abc[:], func=AF.Exp), reads=['abc'], writes=['abc'])
        P.op('dve', lambda: nc.vector.tensor_scalar(abc[:], abc[:], -1.0, None, op0=ALU.mult), reads=['abc'], writes=['abc'])
        ST = sb("ST", [128, 8, 256], F32)
        STb = sb("STb", [128, 8, 256], BF16)
        P.op('dve', lambda: nc.vector.memset(ST[:], 0.0), writes=[('STATE', g) for g in range(8)])
        P.op('pool', lambda: nc.gpsimd.memset(STb[:], 0.0), writes=[('STb', g) for g in range(8)])
        xblk = [sb(f"xblk{i}", [128, 8, TB], BF16) for i in range(2)]
        bct = [sb(f"bct{i}", [128, 16, TB], BF16) for i in range(2)]
        xsB = [sb(f"xsB{i}", [128, 3072], BF16) for i in range(2)]
        sm = sb("sm", [128, 8, 32], F32)
        xdt = sb("xdt", [128, 2048], BF16)
        xdt2 = sb("xdt2", [128, 2048], BF16)
        dam = [sb(f"dam{i}", [128, 4, 128], BF16) for i in range(2)]
        dec = [sb(f"dec{i}", [128, 4, 128], F32) for i in range(2)]
        cbm = [sb(f"cbm{i}", [128, 128], F32) for i in range(2)]
        G = [sb(f"G{i}", [128, 4, 128], BF16) for i in range(2)]
        tmp = [sb(f"tmp{i}", [128, 256], F32) for i in range(2)]
        y = sb("y", [128, 2048], F32)
        zs = [sb(f"zs{i}", [128, 512], F32) for i in range(2)]
        junk = sb("junk", [128, 256], F32)
        ssq = sb("ssq", [128, 16], F32)
        yn = sb("yn", [128, 2048], BF16)
        ynT = [sb(f"ynT{i}", [128, 16, TB], BF16) for i in range(2)]
        v3 = lambda ap, h: ap.rearrange("p (h c) -> p h c", h=h)
        bc3 = lambda ap, n: ap.unsqueeze(2).to_broadcast([128, ap.shape[1], n])
        sm2 = [sm, sb("smB", [128, 8, 32], F32)]
        xdtA = [xdt, sb("xdtB", [128, 2048], BF16)]
        xdt2A = [xdt2, sb("xdt2B", [128, 2048], BF16)]
        yA = [y, sb("yB", [128, 2048], F32)]

        def load_blk(b):
            i2 = b % 2
            t0 = b * TB
            P.dma('sp', out=xblk[i2][:], in_=self.xT_d[:, :, t0:t0 + TB], key=f"ld{4 + i2}", writes=[('xblk', i2)])
            P.dma('sp', out=bct[i2][:], in_=self.BCT_d[:, :, t0:t0 + TB], key=f"ld{6 + i2}", writes=[('bct', i2)])

        def pre(t):
            b, tt = divmod(t, 4)
            i2 = b % 2
            ti = t % 2
            if tt == 0:
                load_blk(b)
            xb = xblk[i2]
            cols = slice(tt * 128, (tt + 1) * 128)
            xs = xsB[ti]
            sm = sm2[ti]
            S = lambda k: ('sm', ti, k)
            P.dma('sp', out=xs[:], in_=self.xsB_d[t * 128:(t + 1) * 128, :], key=f"ld{8 + ti}", writes=[('xsB', ti)])
            P.mm(ps[5][:, 0:32], [(xb[:, kc, cols], Wdt[:, kc, :]) for kc in range(8)], 'ps5', [('xblk', i2)] + W('Wdt', 8))
            P.op('dve', lambda: nc.vector.tensor_tensor(sm[:, 0, :], ps[5][:, 0:32], dtb[:], op=ALU.add),
                 reads=['ps5', 'dtb'], writes=[S(0)])
            P.op('dve', lambda: nc.vector.tensor_scalar(sm[:, 1, :], sm[:, 0, :], -1.0, None, op0=ALU.mult),
                 reads=[S(0)], writes=[S(1)])
            P.op('dve', lambda: nc.vector.tensor_tensor(sm[:, 1, :], sm[:, 1, :], sm[:, 0, :], op=ALU.min),
                 reads=[S(0), S(1)], writes=[S(1)])
            P.op('act', lambda: nc.scalar.activation(out=sm[:, 2, :], in_=sm[:, 1, :], func=AF.Exp),
                 reads=[S(1)], writes=[S(2)])
            P.op('act', lambda: nc.scalar.activation(out=sm[:, 2, :], in_=sm[:, 2, :], func=AF.Ln, bias=1.0),
                 reads=[S(2)], writes=[S(2)])
            P.op('dve', lambda: nc.vector.scalar_tensor_tensor(out=sm[:, 3, :], in0=sm[:, 0, :], scalar=0.0, in1=sm[:, 2, :],
                                                               op0=ALU.max, op1=ALU.add), reads=[S(0), S(2)], writes=[S(3)])
            P.op('dve', lambda: nc.vector.tensor_tensor(sm[:, 4, :], sm[:, 3, :], abc[:], op=ALU.mult),
                 reads=[S(3), 'abc'], writes=[S(4)])
            P.mm(ps[5][:, 64:96], [(self.tri_le_f[:], sm[:, 4, :])], 'ps5', [S(4)])
            P.mm(ps[5][:, 128:160], [(self.tri_gt_f[:], sm[:, 4, :])], 'ps5', [S(4)])
            P.mm(ps[5][:, 192:224], [(self.ones_f[:], sm[:, 4, :])], 'ps5', [S(4)])
            P.op('act', lambda: nc.scalar.activation(out=sm[:, 5, :], in_=ps[5][:, 64:96], func=AF.Exp), reads=['ps5'], writes=[S(5)])
            P.op('act', lambda: nc.scalar.activation(out=sm[:, 6, :], in_=ps[5][:, 128:160], func=AF.Exp), reads=['ps5'], writes=[S(6)])
            P.op('act', lambda: nc.scalar.activation(out=sm[:, 7, :], in_=ps[5][:, 192:224], func=AF.Exp), reads=['ps5'], writes=[S(7)])
            P.op('dve', lambda: nc.vector.tensor_tensor(v3(xdtA[ti][:], 32), v3(xs[:, 0:2048], 32), bc3(sm[:, 3, :], 64), op=ALU.mult),
                 reads=[('xsB', ti), S(3)], writes=[('xdt', ti)])
            P.op('pool', lambda: nc.gpsimd.tensor_tensor(v3(xdt2A[ti][:], 32), v3(xdtA[ti][:], 32), bc3(sm[:, 6, :], 64), op=ALU.mult),
                 reads=[('xdt', ti), S(6)], writes=[('xdt2', ti)])

        def grp(t):
            b, tt = divmod(t, 4)
            i2 = b % 2
            ti = t % 2
            cols = slice(tt * 128, (tt + 1) * 128)
            xs = xsB[ti]
            sm = sm2[ti]
            S = lambda k: ('sm', ti, k)
            xdt_, xdt2_, y_ = xdtA[ti], xdt2A[ti], yA[ti]

            def stage1(g):
                gi = g % 2
                hs = slice(g * 4, (g + 1) * 4)
                P.op('pool', lambda: nc.gpsimd.tensor_tensor(
                    dam[gi][:], self.tri_gt_f[:, :].unsqueeze(1).to_broadcast([128, 4, 128]), bc3(sm[:, 4, hs], 128), op=ALU.mult),
                     reads=[S(4)], writes=[('dam', gi)])
                sp_, spt = (ps[0], ('SEG', 0)) if gi == 0 else (ps[1], ('SEG', 1))
                for r in range(4):
                    P.mm(sp_[:, r * 128:(r + 1) * 128], [(dam[gi][:, r, :], self.mask_le[:])], spt, [('dam', gi)])
                P.op('act', lambda: nc.scalar.activation(out=dec[gi][:].rearrange("p r s -> p (r s)"), in_=sp_[:, :], func=AF.Exp),
                     reads=[spt], writes=[('dec', gi)])
                cp_, cpt = (ps[2], ('CB', 0)) if gi == 0 else (ps[3], ('CB', 1))
                P.mm(cp_[:, 0:128], [(bct[i2][:, g, cols], bct[i2][:, 8 + g, cols])], cpt, [('bct', i2)])
                P.op('dve', lambda: nc.vector.tensor_tensor(cbm[gi][:], cp_[:, 0:128], self.tri_le_f[:], op=ALU.mult),
                     reads=[cpt], writes=[('cbm', gi)])
                P.op('dve', lambda: nc.vector.tensor_tensor(G[gi][:], dec[gi][:], cbm[gi][:, :].unsqueeze(1).to_broadcast([128, 4, 128]),
                                                            op=ALU.mult),
                     reads=[('dec', gi), ('cbm', gi)], writes=[('G', gi)])

            def stage2(g):
                gi = g % 2
                hs = slice(g * 4, (g + 1) * 4)
                cp_ = ps[2] if gi == 0 else ps[3]
                bp_ = ps[4] if gi == 0 else ps[6]
                for r in range(4):
                    hh = g * 4 + r
                    P.mm(bp_[:, r * 64:(r + 1) * 64],
                         [(G[gi][:, r, :], xdt_[:, hh * 64:(hh + 1) * 64])], ('YI', gi), [('G', gi), ('xdt', ti)])
                P.mm(bp_[:, 256:512], [(xs[:, 2048 + g * 128:2048 + (g + 1) * 128], xdt2_[:, g * 256:(g + 1) * 256])],
                     ('SU', gi), [('xsB', ti), ('xdt2', ti)])
                P.mm(cp_[:, 256:512], [(bct[i2][:, 8 + g, cols], STb[:, g, :])], ('YS', gi), [('bct', i2), ('STb', g)])
                P.op('dve', lambda: nc.vector.tensor_tensor(v3(tmp[gi][:], 4), v3(cp_[:, 256:512], 4), bc3(sm[:, 5, hs], 64), op=ALU.mult),
                     reads=[('YS', gi), S(5)], writes=[('tmp', gi)])
                P.op('dve', lambda: nc.vector.tensor_tensor(y_[:, g * 256:(g + 1) * 256], tmp[gi][:], bp_[:, 0:256], op=ALU.add),
                     reads=[('tmp', gi), ('YI', gi)], writes=[('y', ti, g)])
                P.op('pool', lambda: nc.gpsimd.tensor_tensor(v3(ST[:, g, :], 4), v3(ST[:, g, :], 4), bc3(sm[:, 7, hs], 64), op=ALU.mult),
                     reads=[S(7)], writes=[('STATE', g)])
                P.op('dve', lambda: nc.vector.tensor_tensor(ST[:, g, :], ST[:, g, :], bp_[:, 256:512], op=ALU.add),
                     reads=[('SU', gi)], writes=[('STATE', g)])
                P.op('act', lambda: nc.scalar.copy(STb[:, g, :], ST[:, g, :]), reads=[('STATE', g)], writes=[('STb', g)])

            if SSD_PIPE:
                stage1(0)
                for g in range(8):
                    if g + 1 < 8:
                        stage1(g + 1)
                    stage2(g)
            else:
                for g in range(8):
                    stage1(g)
                    stage2(g)

        def post(t):
            b, tt = divmod(t, 4)
            i2 = b % 2
            ti = t % 2
            t0 = b * TB
            cols = slice(tt * 128, (tt + 1) * 128)
            xb = xblk[i2]
            xs = xsB[ti]
            xdt2_, y_ = xdt2A[ti], yA[ti]
            ally = [('y', ti, g) for g in range(8)]
            P.op('pool', lambda: nc.gpsimd.tensor_tensor(v3(xdt2_[:], 32), v3(xs[:, 0:2048], 32), bc3(dsk[:, :], 64), op=ALU.mult),
                 reads=[('xsB', ti), 'dsk'], writes=[('xdt2', ti)])
            P.op('dve', lambda: nc.vector.tensor_tensor(y_[:], y_[:], xdt2_[:], op=ALU.add), reads=ally + [('xdt2', ti)], writes=ally)
            for q in range(4):
                zi = q % 2
                qs = slice(q * 512, (q + 1) * 512)
                zp, zpt = (ps[0], ('SEG', 0)) if zi == 0 else (ps[1], ('SEG', 1))
                P.mm(zp[:, :], [(xb[:, kc, cols], Wz[:, kc, qs]) for kc in range(8)], zpt, [('xblk', i2)] + W('Wz', 8))
                P.op('act', lambda zi=zi, zp=zp: nc.scalar.activation(out=zs[zi][:], in_=zp[:, :], func=AF.Silu), reads=[zpt], writes=[('zs', zi)])
                P.op('dve', lambda zi=zi, qs=qs: nc.vector.tensor_tensor(y_[:, qs], y_[:, qs], zs[zi][:], op=ALU.mult),
                     reads=[('zs', zi)] + ally, writes=ally)
            for g in range(8):
                P.op('act', lambda g=g: nc.scalar.activation(out=junk[:], in_=y_[:, g * 256:(g + 1) * 256], func=AF.Square, accum_out=ssq[:, g:g + 1]),
                     reads=ally, writes=['junk', ('ssq', g)])
            allq = [('ssq', g) for g in range(8)]
            P.op('dve', lambda: nc.vector.tensor_scalar(ssq[:, 8:16], ssq[:, 0:8], 1.0 / 256, RMS_EPS, op0=ALU.mult, op1=ALU.add),
                 reads=allq, writes=['ssq8'])
            P.op('act', lambda: nc.scalar.activation(out=ssq[:, 8:16], in_=ssq[:, 8:16], func=AF.Sqrt), reads=['ssq8'], writes=['ssq8'])
            P.op('dve', lambda: nc.vector.reciprocal(ssq[:, 8:16], ssq[:, 8:16]), reads=['ssq8'], writes=['ssq8'])
            for g in range(8):
                gs = slice(g * 256, (g + 1) * 256)
                P.op('dve', lambda g=g, gs=gs: nc.vector.scalar_tensor_tensor(
                    out=yn[:, gs], in0=y_[:, gs], scalar=ssq[:, 8 + g:9 + g], in1=ngb[:, gs], op0=ALU.mult, op1=ALU.mult),
                     reads=ally + ['ssq8', 'ngb'], writes=[('yn', g)])
            for r in range(2):
                for q in range(8):
                    P.op('pe', lambda r=r, q=q: nc.tensor.transpose(self.psb[:, q * 128:(q + 1) * 128],
                                                                    yn[:, (r * 8 + q) * 128:(r * 8 + q + 1) * 128], self.ident_bf[:]),
                         reads=[('yn', g) for g in range(8)], writes=['psb'], inc=(q == 7))
                P.op('act', lambda r=r: nc.scalar.copy(ynT[i2][:, r * 8:(r + 1) * 8, cols],
                                                       self.psb[:].rearrange("p (k n) -> p k n", k=8)),
                     reads=['psb'], writes=[('ynT', i2)])
            if tt == 3:
                P.dma('sp', out=self.ynT_d[:, :, t0:t0 + TB], in_=ynT[i2][:], key=f"st{i2}", reads=[('ynT', i2)], writes=[('ynT_d', b)])

        if SSD_HOIST:
            pre(0)
            for t in range(NT):
                if t + 1 < NT:
                    pre(t + 1)
                grp(t)
                post(t)
        else:
            for t in range(NT):
                pre(t)
                grp(t)
                post(t)
    P.barrier()


def _pass_ssd_c(self, l):
    P, nc = self.P, self.nc
    ps = self.ps
    with ExitStack() as es:
        sb = lambda name, shape, dt: es.enter_context(nc.sbuf_tensor(_uid(name), shape, dt))
        rows = lambda kc: slice(kc * 128, (kc + 1) * 128)
        W = lambda name, n: _toks(name, n)
        WgB = sb("WgB", [128, 8, D], BF16)
        wpb = sb("wpb", [128, 16, D], BF16)
        self.load_w(lambda kc: WgB[:, kc, :], lambda kc: self.w_in[l, rows(kc), C_G + D:C_G + 2 * D], 'WgB', 8)
        self.load_w(lambda kc: wpb[:, kc, :], lambda kc: self.w_proj_b[l, rows(kc), :], 'wpb', 16)
        xblk = [sb(f"xblk{i}", [128, 8, TB], BF16) for i in range(2)]
        ynb = [sb(f"ynb{i}", [128, 16, TB], BF16) for i in range(2)]
        sig = [sb(f"sig{i}", [128, TB], F32) for i in range(2)]
        mT = [sb(f"mT{i}", [128, 8, TB], BF16) for i in range(2)]
        for b in range(NB):
            i2 = b % 2
            t0 = b * TB
            P.dma('sp', out=xblk[i2][:], in_=self.xT_d[:, :, t0:t0 + TB], key=f"ld{2 + i2}", writes=[('xblk', i2)])
            P.dma('sp', out=ynb[i2][:], in_=self.ynT_d[:, :, t0:t0 + TB], key=f"ld{4 + i2}", writes=[('ynb', i2)])
            xb = xblk[i2]
            for dc in range(8):
                dd = slice(dc * 128, (dc + 1) * 128)
                si = dc % 2
                pa, pat = (ps[5], 'ps5') if si == 0 else (ps[3], 'ps3')
                pb, pbt = (ps[6], 'ps6') if si == 0 else (ps[4], 'ps4')
                P.mm(pa[:, :], [(WgB[:, kc, dd], xb[:, kc, :]) for kc in range(8)], pat, [('xblk', i2)] + W('WgB', 8))
                P.op('act', lambda si=si, pa=pa: nc.scalar.activation(out=sig[si][:], in_=pa[:, :], func=AF.Sigmoid),
                     reads=[pat], writes=[('sig', si)])
                P.mm(pb[:, :], [(wpb[:, j, dd], ynb[i2][:, j, :]) for j in range(16)], pbt, [('ynb', i2)] + W('wpb', 16))
                P.op('dve', lambda dc=dc, si=si, pb=pb: nc.vector.tensor_tensor(mT[i2][:, dc, :], pb[:, :], sig[si][:], op=ALU.mult),
                     reads=[pbt, ('sig', si)], writes=[('mT', i2)])
            P.dma('sp', out=self.mB_d[:, :, t0:t0 + TB], in_=mT[i2][:], key=f"st{i2}", reads=[('mT', i2)], writes=[('mB_d', b)])
    P.barrier()


Net.pass_ssd_a = _pass_ssd_a
Net.pass_ssd_b = _pass_ssd_b
Net.pass_ssd_c = _pass_ssd_c


def _layer_norm_tile(self, u, gbt, bbt, out, st, tag):
    P, nc = self.P, self.nc
    junk = self.ln_junk
    P.op('act', lambda: nc.scalar.activation(out=junk[:], in_=u, func=AF.Identity, accum_out=st[:, 0:1]),
         reads=[tag + 'u'], writes=['lnjunk', tag + 's0'])
    P.op('act', lambda: nc.scalar.activation(out=junk[:], in_=u, func=AF.Square, accum_out=st[:, 1:2]),
         reads=[tag + 'u'], writes=['lnjunk', tag + 's1'])
    P.op('dve', lambda: nc.vector.tensor_scalar(st[:, 2:4], st[:, 0:2], 1.0 / D, None, op0=ALU.mult),
         reads=[tag + 's0', tag + 's1'], writes=[tag + 's2'])
    P.op('dve', lambda: nc.vector.tensor_tensor(st[:, 4:5], st[:, 2:3], st[:, 2:3], op=ALU.mult), reads=[tag + 's2'], writes=[tag + 's4'])
    P.op('dve', lambda: nc.vector.tensor_tensor(st[:, 5:6], st[:, 3:4], st[:, 4:5], op=ALU.subtract), reads=[tag + 's2', tag + 's4'], writes=[tag + 's5'])
    P.op('dve', lambda: nc.vector.tensor_scalar(st[:, 5:6], st[:, 5:6], NORM_EPS, None, op0=ALU.add), reads=[tag + 's5'], writes=[tag + 's5'])
    P.op('act', lambda: nc.scalar.activation(out=st[:, 6:7], in_=st[:, 5:6], func=AF.Sqrt), reads=[tag + 's5'], writes=[tag + 's6'])
    P.op('dve', lambda: nc.vector.reciprocal(st[:, 7:8], st[:, 6:7]), reads=[tag + 's6'], writes=[tag + 's7'])
    P.op('dve', lambda: nc.vector.tensor_scalar(out, u, st[:, 2:3], st[:, 7:8], op0=ALU.subtract, op1=ALU.mult),
         reads=[tag + 'u', tag + 's2', tag + 's7'], writes=[tag + 'o'])
    P.op('pool', lambda: nc.gpsimd.tensor_tensor(out, out, gbt[:], op=ALU.mult), reads=[tag + 'g'], writes=[tag + 'o'])
    P.op('pool', lambda: nc.gpsimd.tensor_tensor(out, out, bbt[:], op=ALU.add), reads=[tag + 'g'], writes=[tag + 'o'])


def _pass_ln1(self, l):
    P, nc = self.P, self.nc
    ps = self.ps
    xa_src = self.x_in if l == 0 else self.xa_d
    with ExitStack() as es:
        sb = lambda name, shape, dt: es.enter_context(nc.sbuf_tensor(_uid(name), shape, dt))
        rows = lambda kc: slice(kc * 128, (kc + 1) * 128)
        W = lambda name, n: _toks(name, n)
        wout = sb("wout", [128, 8, D], BF16)
        rw = sb("rw", [128, 8, 16], F32)
        rb = sb("rb", [128, 16], F32)
        gbt = sb("gbt", [128, D], F32)
        bbt = sb("bbt", [128, D], F32)
        self.ln_junk = sb("lnjunk", [128, D], F32)
        self.load_w(lambda kc: wout[:, kc, :], lambda kc: self.w_out[l, rows(kc), :], 'wout', 8)
        for kc in range(8):
            P.dma('sp', out=rw[:, kc, :], in_=self.router_w[rows(kc), :], key="ld0", writes=[('rw', kc)])
        P.dma('sp', out=rb[:], in_=self.router_bias[0, :].partition_broadcast(128), key="ld1", writes=['rb'])
        P.dma('sp', out=gbt[:], in_=self.ln1_g[l, :].partition_broadcast(128), key="ld2", writes=['lg'])
        P.dma('sp', out=bbt[:], in_=self.ln1_b[l, :].partition_broadcast(128), key="ld3", writes=['lg'])
        mblk = [[sb(f"m{n}{i}", [128, 8, TB], BF16) for n in "ABC"] for i in range(2)]
        xa = [sb(f"xa{i}", [128, D], F32) for i in range(2)]
        u = [sb(f"u{i}", [128, D], F32) for i in range(2)]
        x1 = [sb(f"x1{i}", [128, D], F32) for i in range(2)]
        st = sb("st", [128, 8], F32)
        x1Tf = sb("x1Tf", [128, 8, 128], F32)
        stg = [sb(f"stg{i}", [128, 8, TB], BF16) for i in range(2)]
        r = sb("r", [128, 12, 16], F32)
        gts = [sb(f"gts{i}", [16, TB], F32) for i in range(2)]
        srcs = [self.mA_d, self.mB_d, self.mC_d]
        st2 = [st, sb("stB", [128, 8], F32)]

        def stA(t):
            b, tt = divmod(t, 4)
            i2, ti = b % 2, t % 2
            t0 = b * TB
            cols = slice(tt * 128, (tt + 1) * 128)
            tag = f'L1{ti}'
            if tt == 0:
                for n in range(3):
                    P.dma('sp', out=mblk[i2][n][:], in_=srcs[n][:, :, t0:t0 + TB], key=f"ld{4 + 3 * i2 + n}", writes=[('mblk', i2, n)])
            P.dma('sp', out=xa[ti][:], in_=xa_src[t * 128:(t + 1) * 128, :], key=f"ld{10 + ti}", writes=[('xa', ti)])
            for half in range(2):
                pb, pt = (ps[5], 'ps5') if half == 0 else (ps[6], 'ps6')
                P.mm(pb[:, :], [(mblk[i2][n][:, dc, cols], wout[:, dc, half * 512:(half + 1) * 512]) for n in range(3) for dc in range(8)],
                     pt, [('mblk', i2, n) for n in range(3)] + W('wout', 8))
                P.op('dve', lambda half=half, pb=pb: nc.vector.scalar_tensor_tensor(
                    out=u[ti][:, half * 512:(half + 1) * 512], in0=xa[ti][:, half * 512:(half + 1) * 512], scalar=DN_ALPHA, in1=pb[:, :],
                    op0=ALU.mult, op1=ALU.add), reads=[('xa', ti), pt], writes=[tag + 'u'])

        def stB(t):
            ti = t % 2
            tag = f'L1{ti}'
            self.layer_norm_tile(u[ti][:], gbt, bbt, x1[ti][:], st2[ti], tag)
            P._record(('e', 'pool', P.cnt['pool']), ['lg'], [])
            P.dma('sp', out=self.x1_d[t * 128:(t + 1) * 128, :], in_=x1[ti][:], key=f"st{ti}", reads=[tag + 'o'], writes=[('x1_d', t)])

        def stC(t):
            b, tt = divmod(t, 4)
            i2, ti = b % 2, t % 2
            t0 = b * TB
            cols = slice(tt * 128, (tt + 1) * 128)
            tag = f'L1{ti}'
            for kc in range(8):
                pb = ps[0] if kc < 4 else ps[1]
                P.op('pe', lambda kc=kc, pb=pb: nc.tensor.matmul(pb[:, (kc % 4) * 128:(kc % 4 + 1) * 128],
                                                                 x1[ti][:, kc * 128:(kc + 1) * 128], self.ident_f[:],
                                                                 start=True, stop=True),
                     reads=[tag + 'o'], writes=[('TP', kc // 4)], inc=(kc % 4 == 3))
            for hf in range(2):
                P.op('act', lambda hf=hf: nc.scalar.copy(x1Tf[:, hf * 4:(hf + 1) * 4, :], ps[hf][:].rearrange("p (k n) -> p k n", k=4)),
                     reads=[('TP', hf)], writes=['x1Tf'])
                P.op('dve', lambda hf=hf: nc.vector.tensor_copy(stg[i2][:, hf * 4:(hf + 1) * 4, cols], x1Tf[:, hf * 4:(hf + 1) * 4, :]),
                     reads=['x1Tf'], writes=[('stg', i2)])
            P.mm(ps[2][:, 0:16], [(x1Tf[:, kc, :], rw[:, kc, :]) for kc in range(8)], 'ps2', ['x1Tf'] + W('rw', 8))
            R = lambda i: r[:, i, :]
            R4 = lambda i: r[:, i, :].rearrange("p (g e) -> p g e", g=4)
            P.op('act', lambda: nc.scalar.activation(out=R(0), in_=ps[2][:, 0:16], func=AF.Sigmoid), reads=['ps2'], writes=['r0'])
            P.op('dve', lambda: nc.vector.tensor_tensor(R(1), R(0), rb[:], op=ALU.add), reads=['r0', 'rb'], writes=['r1'])
            pairs = [(0, 1), (0, 2), (0, 3), (1, 2), (1, 3), (2, 3)]
            for pi_, (a_, b_) in enumerate(pairs):
                P.op('dve', lambda pi_=pi_, a_=a_, b_=b_: nc.vector.tensor_tensor(r[:, 2 + pi_ // 4, (pi_ % 4) * 4:(pi_ % 4) * 4 + 4],
                                                                                R4(1)[:, :, a_], R4(1)[:, :, b_], op=ALU.add),
                     reads=['r1'], writes=['r2'])
            P.op('dve', lambda: nc.vector.tensor_tensor(r[:, 4, 0:4], r[:, 2, 0:4], r[:, 2, 4:8], op=ALU.max), reads=['r2'], writes=['r4'])
            P.op('dve', lambda: nc.vector.tensor_tensor(r[:, 4, 4:8], r[:, 2, 8:12], r[:, 2, 12:16], op=ALU.max), reads=['r2'], writes=['r4'])
            P.op('dve', lambda: nc.vector.tensor_tensor(r[:, 4, 8:12], r[:, 3, 0:4], r[:, 3, 4:8], op=ALU.max), reads=['r2'], writes=['r4'])
            P.op('dve', lambda: nc.vector.tensor_tensor(r[:, 4, 0:4], r[:, 4, 0:4], r[:, 4, 4:8], op=ALU.max), reads=['r4'], writes=['r4'])
            P.op('dve', lambda: nc.vector.tensor_tensor(r[:, 4, 0:4], r[:, 4, 0:4], r[:, 4, 8:12], op=ALU.max), reads=['r4'], writes=['r4'])
            P.op('dve', lambda: nc.vector.tensor_reduce(out=r[:, 5, 0:1], in_=r[:, 4, 0:4], axis=AX.X, op=ALU.max), reads=['r4'], writes=['r5'])
            P.op('dve', lambda: nc.vector.tensor_scalar(r[:, 5, 4:8], r[:, 4, 0:4], r[:, 5, 0:1], None, op0=ALU.is_equal), reads=['r4', 'r5'], writes=['r5m'])
            P.op('dve', lambda: nc.vector.tensor_scalar(r[:, 5, 8:12], r[:, 5, 4:8], 1.0, 1e30, op0=ALU.subtract, op1=ALU.mult), reads=['r5m'], writes=['r5p'])
            P.op('dve', lambda: nc.vector.tensor_tensor(R4(6), R4(1), r[:, 5, 4:8].unsqueeze(2).to_broadcast([128, 4, 4]), op=ALU.mult),
                 reads=['r1', 'r5m'], writes=['r6'])
            P.op('dve', lambda: nc.vector.tensor_tensor(R4(6), R4(6), r[:, 5, 8:12].unsqueeze(2).to_broadcast([128, 4, 4]), op=ALU.add),
                 reads=['r6', 'r5p'], writes=['r6'])
            P.op('dve', lambda: nc.vector.tensor_reduce(out=r[:, 7, 0:1], in_=R(6), axis=AX.X, op=ALU.max), reads=['r6'], writes=['r7'])
            P.op('dve', lambda: nc.vector.tensor_scalar(R(8), R(6), r[:, 7, 0:1], None, op0=ALU.is_equal), reads=['r6', 'r7'], writes=['r8'])
            P.op('dve', lambda: nc.vector.scalar_tensor_tensor(out=R(9), in0=R(8), scalar=-1e30, in1=R(6), op0=ALU.mult, op1=ALU.add),
                 reads=['r8', 'r6'], writes=['r9'])
            P.op('dve', lambda: nc.vector.tensor_reduce(out=r[:, 7, 1:2], in_=R(9), axis=AX.X, op=ALU.max), reads=['r9'], writes=['r7b'])
            P.op('dve', lambda: nc.vector.tensor_scalar(R(10), R(9), r[:, 7, 1:2], None, op0=ALU.is_equal), reads=['r9', 'r7b'], writes=['r10'])
            P.op('dve', lambda: nc.vector.tensor_tensor(R(10), R(10), R(8), op=ALU.add), reads=['r10', 'r8'], writes=['r10'])
            P.op('dve', lambda: nc.vector.tensor_tensor(R(11), R(10), R(0), op=ALU.mult), reads=['r10', 'r0'], writes=['r11'])
            P.op('dve', lambda: nc.vector.tensor_reduce(out=r[:, 7, 2:3], in_=R(11), axis=AX.X, op=ALU.add), reads=['r11'], writes=['r7c'])
            P.op('dve', lambda: nc.vector.reciprocal(r[:, 7, 3:4], r[:, 7, 2:3]), reads=['r7c'], writes=['r7d'])
            P.op('dve', lambda: nc.vector.tensor_scalar(R(11), R(11), r[:, 7, 3:4], None, op0=ALU.mult), reads=['r11', 'r7d'], writes=['r11'])
            P.op('pe', lambda: nc.tensor.matmul(ps[3][0:16, 0:128], R(11), self.ident_f[:], start=True, stop=True), reads=['r11'], writes=['ps3'])
            P.op('act', lambda: nc.scalar.copy(gts[i2][:, cols], ps[3][0:16, 0:128]), reads=['ps3'], writes=[('gts', i2)])
            if tt == 3:
                P.dma('sp', out=self.x1T_d[:, :, t0:t0 + TB], in_=stg[i2][:], key=f"st{2 + i2}", reads=[('stg', i2)], writes=[('x1T_d', b)])
                P.dma('sp', out=self.gT_d[:, t0:t0 + TB], in_=gts[i2][:], key=f"st{4 + i2}", reads=[('gts', i2)], writes=[('gT_d', b)])

        for step in range(NT + 2):
            if step < NT:
                stA(step)
            if 0 <= step - 1 < NT:
                stB(step - 1)
            if 0 <= step - 2 < NT:
                stC(step - 2)
    P.barrier()


def _pass_moe(self, l, last):
    P, nc = self.P, self.nc
    ps = self.ps
    BB = 1024
    with ExitStack() as es:
        sb = lambda name, shape, dt: es.enter_context(nc.sbuf_tensor(_uid(name), shape, dt))
        rows = lambda kc: slice(kc * 128, (kc + 1) * 128)
        W = lambda name, n: _toks(name, n)
        gbt = sb("gbt", [128, D], F32)
        bbt = sb("bbt", [128, D], F32)
        self.ln_junk = sb("lnjunk", [128, D], F32)
        E16 = sb("E16", [16, 16, 128], F32)
        P.dma('sp', out=gbt[:], in_=self.ln2_g[l, :].partition_broadcast(128), key="ld2", writes=['lg'])
        P.dma('sp', out=bbt[:], in_=self.ln2_b[l, :].partition_broadcast(128), key="ld3", writes=['lg'])
        P.op('pool', lambda: nc.gpsimd.memset(E16[:], 0.0), writes=['E16'])
        P.op('pool', lambda: nc.gpsimd.affine_select(out=E16[:], in_=E16[:], pattern=[[-1, 16], [0, 128]], compare_op=ALU.not_equal,
                                                     fill=1.0, base=0, channel_multiplier=1), writes=['E16'])
        x1T = sb("x1T", [128, 8, BB], BF16)
        gT = sb("gT", [16, BB], F32)
        acc = sb("acc", [128, 8, D], F32)
        w1 = [sb(f"w1{i}", [128, 8, 512], BF16) for i in range(2)]
        w3 = [sb(f"w3{i}", [128, 8, 512], BF16) for i in range(2)]
        w2 = [sb(f"w2{i}", [128, 4, D], BF16) for i in range(2)]
        gb = [sb(f"gb{i}", [128, 512], F32) for i in range(2)]
        s1 = [sb(f"s1{i}", [128, 512], F32) for i in range(2)]
        tq = [sb(f"tq{i}", [128, 512], F32) for i in range(2)]
        hT = [sb(f"hT{i}", [128, 4, 512], BF16) for i in range(2)]
        x1t = [sb(f"x1t{i}", [128, D], F32) for i in range(2)]
        u = [sb(f"u{i}", [128, D], F32) for i in range(2)]
        x2 = [sb(f"x2{i}", [128, D], F32) for i in range(2)]
        x2b = [sb(f"x2b{i}", [128, D], BF16) for i in range(2)]
        st = sb("st", [128, 8], F32)
        stg = sb("stg", [128, 8, BB], BF16)
        dst = self.y_out if last else self.xa_d
        for bb in range(T // BB):
            t0 = bb * BB
            P.dma('sp', out=x1T[:], in_=self.x1T_d[:, :, t0:t0 + BB], key="ld4", writes=['x1T'])
            P.dma('sp', out=gT[:], in_=self.gT_d[:, t0:t0 + BB], key="ld5", writes=['gT'])
            units = [(e, sub) for e in range(16) for sub in range(BB // 512)]

            def stage_F(ui):
                e, sub = units[ui]
                ei = (ebase + e) % 2
                hi = (ubase + ui) % 2
                if sub == 0:
                    self.load_w(lambda kc: w1[ei][:, kc, :], lambda kc: self.exp_w1[l, e, rows(kc), :], ('w1', ei), 8)
                    self.load_w(lambda kc: w3[ei][:, kc, :], lambda kc: self.exp_w3[l, e, rows(kc), :], ('w3', ei), 8)
                    self.load_w(lambda kc: w2[ei][:, kc, :], lambda kc: self.exp_w2[l, e, rows(kc), :], ('w2', ei), 4)
                sc = slice(sub * 512, (sub + 1) * 512)
                P.mm(ps[4][:, :], [(E16[:, e, :], gT[:, sc])], 'ps4', ['E16', 'gT'])
                P.op('act', lambda: nc.scalar.copy(gb[hi][:], ps[4][:, :]), reads=['ps4'], writes=[('gb', hi)])
                for fc in range(4):
                    fi = fc % 2
                    fs = slice(fc * 128, (fc + 1) * 128)
                    pa, pat = (ps[0], 'ps0') if fi == 0 else (ps[2], 'ps2')
                    pb, pbt = (ps[1], 'ps1') if fi == 0 else (ps[3], 'ps3')
                    P.mm(pa[:, :], [(w1[ei][:, kc, fs], x1T[:, kc, sc]) for kc in range(8)], pat, ['x1T'] + W(('w1', ei), 8))
                    P.mm(pb[:, :], [(w3[ei][:, kc, fs], x1T[:, kc, sc]) for kc in range(8)], pbt, ['x1T'] + W(('w3', ei), 8))
                    P.op('act', lambda fi=fi, pa=pa: nc.scalar.activation(out=s1[fi][:], in_=pa[:, :], func=AF.Silu), reads=[pat], writes=[('s1', fi)])
                    P.op('dve', lambda fi=fi, pb=pb: nc.vector.tensor_tensor(tq[fi][:], pb[:, :], gb[hi][:], op=ALU.mult),
                         reads=[pbt, ('gb', hi)], writes=[('tq', fi)])
                    P.op('dve', lambda fi=fi, fc=fc: nc.vector.tensor_tensor(hT[hi][:, fc, :], tq[fi][:], s1[fi][:], op=ALU.mult),
                         reads=[('tq', fi), ('s1', fi)], writes=[('hT', hi, fc)])

            def stage_S(ui):
                e, sub = units[ui]
                ei = (ebase + e) % 2
                hi = (ubase + ui) % 2
                for tt in range(4):
                    tl = sub * 4 + tt
                    cols = slice(tt * 128, (tt + 1) * 128)
                    for half in range(2):
                        pb, pt = (ps[5], 'ps5') if half == 0 else (ps[6], 'ps6')
                        hs = slice(half * 512, (half + 1) * 512)
                        P.mm(pb[:, :], [(hT[hi][:, fc, cols], w2[ei][:, fc, hs]) for fc in range(4)], pt,
                             [('hT', hi, fc) for fc in range(4)] + W(('w2', ei), 4))
                        if e == 0:
                            P.op('dve', lambda tl=tl, hs=hs, pb=pb: nc.vector.tensor_copy(acc[:, tl, hs], pb[:, :]),
                                 reads=[pt], writes=[('acc', tl, half)])
                        else:
                            P.op('dve', lambda tl=tl, hs=hs, pb=pb: nc.vector.tensor_tensor(acc[:, tl, hs], acc[:, tl, hs], pb[:, :], op=ALU.add),
                                 reads=[pt], writes=[('acc', tl, half)])

            ebase = bb * 16
            ubase = bb * len(units)
            stage_F(0)
            for ui in range(len(units)):
                if ui + 1 < len(units):
                    stage_F(ui + 1)
                stage_S(ui)
            for tl in range(BB // 128):
                t = bb * (BB // 128) + tl
                ti = t % 2
                P.dma('sp', out=x1t[ti][:], in_=self.x1_d[t * 128:(t + 1) * 128, :], key=f"ld{6 + ti}", writes=[('x1t', ti)])
                P.op('dve', lambda ti=ti, tl=tl: nc.vector.scalar_tensor_tensor(
                    out=u[ti][:], in0=x1t[ti][:], scalar=DN_ALPHA, in1=acc[:, tl, :], op0=ALU.mult, op1=ALU.add),
                     reads=[('x1t', ti), ('acc', tl, 0), ('acc', tl, 1)], writes=['L2u'])
                self.layer_norm_tile(u[ti][:], gbt, bbt, x2[ti][:], st, 'L2')
                P._record(('e', 'pool', P.cnt['pool']), ['lg'], [])
                P.dma('sp', out=dst[t * 128:(t + 1) * 128, :], in_=x2[ti][:], key=f"st{ti}", reads=['L2o'], writes=[('dst', t)])
                if not last:
                    P.op('act', lambda ti=ti: nc.scalar.copy(x2b[ti][:], x2[ti][:]), reads=['L2o'], writes=[('x2b', ti)])
                    for kc in range(8):
                        P.op('pe', lambda kc=kc, ti=ti: nc.tensor.transpose(self.psb[:, kc * 128:(kc + 1) * 128],
                                                                           x2b[ti][:, kc * 128:(kc + 1) * 128], self.ident_bf[:]),
                             reads=[('x2b', ti)], writes=['psb'], inc=(kc == 7))
                    P.op('act', lambda tl=tl: nc.scalar.copy(stg[:, :, tl * 128:(tl + 1) * 128],
                                                             self.psb[:].rearrange("p (k n) -> p k n", k=8)),
                         reads=['psb'], writes=['stg'])
            if not last:
                P.dma('sp', out=self.xT_d[:, :, t0:t0 + BB], in_=stg[:], key="st2", reads=['stg'], writes=[('xT_d', bb)])
    P.barrier()


Net.layer_norm_tile = _layer_norm_tile
Net.pass_ln1 = _pass_ln1
Net.pass_moe = _pass_moe


def build(n_layers=DEPTH, stages=("mla", "ssd", "mem", "moe"), dbg=()):
    net = Net(n_layers)
    net.declare()
    P = net.P
    net.setup_globals()
    net.prologue()
    for l in range(n_layers):
        if "mla" in stages:
            net.pass_mla(l)
        if "mem" in stages:
            net.pass_mem(l)
        if "ssd" in stages or "ssda" in stages:
            net.pass_ssd_a(l)
        if "ssd" in stages or "ssdb" in stages:
            net.pass_ssd_b(l)
        if "ssd" in stages or "ssdc" in stages:
            net.pass_ssd_c(l)
        if "moe" in stages or "ln1" in stages:
            net.pass_ln1(l)
        if "moe" in stages:
            net.pass_moe(l, last=(l == n_layers - 1))
    for name in dbg:
        src = getattr(net, name)
        dst = net.dbg("dbg_" + name, src.shape, src.dtype)
        P.dma('sp', out=dst, in_=src, key="st0", reads=[], writes=[('dbg', name)])
    P.barrier()
    print("instructions:", P.ninst, "sems:", P.nsem, {e: P.cnt[e] for e in P.cnt}, "dmas:", getattr(P, 'ndma', 0), "descs~", getattr(P, 'ndesc', 0))
    return net


def make_inputs(inputs, b):
    m = {}
    for k, v in inputs.items():
        v = np.asarray(v)
        if k in ("x", "mem"):
            m[k] = np.ascontiguousarray(v[b])
        elif k == "positions":
            m[k] = np.ascontiguousarray(v[b:b + 1]).astype(np.int32)
        elif k == "router_bias":
            m[k] = np.ascontiguousarray(v.reshape(1, 16))
        else:
            m[k] = np.ascontiguousarray(v)
    inv = (10000.0 ** (-np.arange(0, 64, 2, dtype=np.float32) / 64)).astype(np.float32)
    m["inv_freq"] = np.concatenate([inv, inv]).reshape(64, 1).astype(np.float32)
    return m


def kernel(**inputs):
    net = build()
    in_maps = [make_inputs(inputs, b) for b in range(8)]
    res = run_bass_kernel_spmd(net.nc, in_maps, core_ids=list(range(8)))
    return np.stack([np.asarray(res.results[b]["y"]) for b in range(8)], axis=0).astype(np.float32)
```

```python
import math
from contextlib import ExitStack

import numpy as np
import concourse.bass as bass
import concourse.mybir as mybir
from concourse.bass_utils import run_bass_kernel_spmd

F32 = mybir.dt.float32
BF16 = mybir.dt.bfloat16
I32 = mybir.dt.int32
AF = mybir.ActivationFunctionType
ALU = mybir.AluOpType
AX = mybir.AxisListType

D = 1024
T = 4096
DEPTH = 4
NT = T // 128
TB = 512
NB = T // TB
H = 8
QR = 384
KVR = 256
NOPE = 128
ROPE = 64
VD = 128
N_IN = 10976
C_DQ = 0
C_DKV = 384
C_KR = 640
C_Z = 704
C_XBC = 2752
C_DT = 6848
C_QM = 6880
C_G = 7904
DN_ALPHA = (2 * DEPTH) ** 0.25
NORM_EPS = 1e-5
RMS_EPS = 1e-6
SCALE_A = (NOPE + ROPE) ** -0.5

EPOCH = 30000
import os
SSD_PIPE = int(os.environ.get('SSD_PIPE', '1'))
SSD_HOIST = int(os.environ.get('SSD_HOIST', '1'))
_UID = [0]


def _uid(name):
    _UID[0] += 1
    return f"{name}_{_UID[0]}"


def _runs(ap):
    dims = [(int(st), int(n)) for st, n in ap.ap]
    total = 1
    for st, n in dims:
        total *= n
    run = 1
    exp = 1
    for st, n in reversed(dims[1:] if len(dims) > 1 else dims):
        if st == exp:
            run *= n
            exp *= n
        else:
            break
    return max(total // max(run, 1), 1)


_BANK_OF = {'ST': lambda i: i, 'SEG': lambda i: i, 'TP': lambda i: i, 'oacc': lambda i: 2 + i, 'CB': lambda i: 2 + i,
            'YS': lambda i: 2 + i, 'YI': lambda i: 4 if i == 0 else 6, 'SU': lambda i: 4 if i == 0 else 6}
_BANK_NAMES = {'ps0': 0, 'ps1': 1, 'ps2': 2, 'ps3': 3, 'ps4': 4, 'ps5': 5, 'ps6': 6, 'psb': 7, 'osum': 4}


def _banks(tokens):
    out = []
    for t in tokens:
        if isinstance(t, str):
            b = _BANK_NAMES.get(t)
        elif isinstance(t, tuple) and t and t[0] in _BANK_OF and len(t) == 2:
            b = _BANK_OF[t[0]](t[1])
        else:
            b = None
        if b is not None:
            out.append(('bank', b))
    return out


class Prog:
    def __init__(self):
        self.nc = bass.Bass("TRN2", target_bir_lowering=False)
        nc = self.nc
        self.es = ExitStack()
        self.eng = dict(pe=nc.tensor, act=nc.scalar, dve=nc.vector, pool=nc.gpsimd, sp=nc.sync)
        self.cnt = {e: 0 for e in self.eng}
        self.sems = {e: [] for e in self.eng}
        self.waited = {e: {} for e in self.eng}
        self.lastw = {}
        self.readers = {}
        self.dsem = {}
        self.dcnt = {}
        self.nsem = 0
        self.ninst = 0

    def _new_sem(self, name):
        self.nsem += 1
        return self.es.enter_context(self.nc.semaphore(name))

    def _esem(self, e, n):
        ep = (n - 1) // EPOCH
        while len(self.sems[e]) <= ep:
            self.sems[e].append(self._new_sem(f"s_{e}_{len(self.sems[e])}"))
        return self.sems[e][ep], (n - 1) % EPOCH + 1

    def _wait(self, c, ev):
        if ev is None:
            return
        kind, p, n = ev
        if kind == 'e':
            if p == c:
                if c == 'pe' or n > self.cnt[c] or n < self.cnt[c] - 1:
                    return
            if self.waited[c].get(p, 0) >= n:
                return
            sem, val = self._esem(p, n)
            self.eng[c].wait_ge(sem, val)
            self.waited[c][p] = n
        else:
            k = ('d', p)
            if self.waited[c].get(k, 0) >= n:
                return
            self.eng[c].wait_ge(self.dsem[p], 16 * n)
            self.waited[c][k] = n

    def _deps(self, reads, writes):
        deps = []
        for r in reads:
            ev = self.lastw.get(r)
            if ev is not None:
                deps.append(ev)
        for w in writes:
            ev = self.lastw.get(w)
            if ev is not None:
                deps.append(ev)
            rd = self.readers.get(w)
            if rd:
                deps.extend(rd.values())
        return deps

    def _record(self, ev, reads, writes):
        key = (ev[0], ev[1])
        for r in reads:
            self.readers.setdefault(r, {})[key] = ev
        for w in writes:
            self.lastw[w] = ev
            self.readers[w] = {}

    def op(self, e, fn, reads=(), writes=(), inc=True):
        bk = _banks(reads) + _banks(writes)
        if bk:
            writes = list(writes) + bk
        for ev in self._deps(reads, writes):
            self._wait(e, ev)
        inst = fn()
        self.ninst += 1
        if inc:
            self.cnt[e] += 1
            n = self.cnt[e]
            sem, _ = self._esem(e, n)
            inst.then_inc(sem, 1)
            ev = ('e', e, n)
        else:
            ev = ('e', e, self.cnt[e] + 1)
        self._record(ev, reads, writes)
        return ev

    def dma(self, q, out, in_, key, reads=(), writes=()):
        if key not in self.dsem:
            self.dsem[key] = self._new_sem(f"d_{key}")
            self.dcnt[key] = 0
        kres = ('dmakey', key)
        for ev in self._deps(reads, list(writes) + [kres]):
            self._wait(q, ev)
        inst = self.eng[q].dma_start(out=out, in_=in_)
        self.ninst += 1
        self.ndma = getattr(self, 'ndma', 0) + 1
        self.ndesc = getattr(self, 'ndesc', 0) + max(_runs(out), _runs(in_))
        self.dcnt[key] += 1
        inst.then_inc(self.dsem[key], 16)
        ev = ('d', key, self.dcnt[key])
        self._record(ev, reads, list(writes) + [kres])
        return ev

    def barrier(self):
        for c in self.eng:
            for p in self.eng:
                if p != c and self.cnt[p] > 0:
                    self._wait(c, ('e', p, self.cnt[p]))
            for k, m in self.dcnt.items():
                if m > 0:
                    self._wait(c, ('d', k, m))
        self.lastw.clear()
        self.readers.clear()

    def mm(self, out, pairs, wres, reads):
        nc = self.nc
        n = len(pairs)
        for i, (l, r) in enumerate(pairs):
            self.op('pe', lambda l=l, r=r, i=i: nc.tensor.matmul(out, l, r, start=(i == 0), stop=(i == n - 1)),
                    reads=reads if i == 0 else (), writes=[wres], inc=(i == n - 1))
        ev = ('e', 'pe', self.cnt['pe'])
        self._record(ev, reads, ())


class Net:
    def __init__(self, n_layers=DEPTH, debug=None):
        self.P = Prog()
        self.nc = self.P.nc
        self.n_layers = n_layers
        self.debug = debug or {}
        self.dbg_out = {}

    def declare(self):
        nc = self.nc
        L = DEPTH
        def inp(name, shape, dt=F32):
            return nc.dram_tensor(name, list(shape), dt, kind="ExternalInput").ap()
        self.x_in = inp("x", [T, D])
        self.mem_in = inp("mem", [256, D])
        self.pos_in = inp("positions", [1, T], I32)
        self.w_in = inp("w_in", [L, D, N_IN])
        self.q_norm = inp("q_norm", [L, QR])
        self.w_uq = inp("w_uq", [L, QR, H * 192])
        self.kv_norm = inp("kv_norm", [L, KVR])
        self.w_ukv = inp("w_ukv", [L, KVR, H, 256])
        self.w_proj_a = inp("w_proj_a", [L, D, D])
        self.conv_w = inp("conv_w", [L, 4, 4096])
        self.conv_b = inp("conv_b", [L, 4096])
        self.dt_bias = inp("dt_bias", [L, 32])
        self.a_log = inp("a_log", [L, 32])
        self.d_skip = inp("d_skip", [L, 32])
        self.ssm_norm = inp("ssm_norm", [L, 2048])
        self.w_proj_b = inp("w_proj_b", [L, 2048, D])
        self.w_mem_kv = inp("w_mem_kv", [L, D, 2048])
        self.w_proj_c = inp("w_proj_c", [L, D, D])
        self.w_out = inp("w_out", [L, D, D])
        self.ln1_g = inp("ln1_g", [L, D])
        self.ln1_b = inp("ln1_b", [L, D])
        self.router_w = inp("router_w", [D, 16])
        self.router_bias = inp("router_bias", [1, 16])
        self.exp_w1 = inp("exp_w1", [L, 16, D, 512])
        self.exp_w3 = inp("exp_w3", [L, 16, D, 512])
        self.exp_w2 = inp("exp_w2", [L, 16, 512, D])
        self.ln2_g = inp("ln2_g", [L, D])
        self.ln2_b = inp("ln2_b", [L, D])
        self.inv_freq = inp("inv_freq", [64, 1])
        self.y_out = nc.dram_tensor("y", [T, D], F32, kind="ExternalOutput").ap()

        def scr(name, shape, dt):
            return nc.dram_tensor(name, list(shape), dt, kind="Internal").ap()
        self.xT_d = scr("xT_d", [128, 8, T], BF16)
        self.x1T_d = scr("x1T_d", [128, 8, T], BF16)
        self.xa_d = scr("xa_d", [T, D], F32)
        self.x1_d = scr("x1_d", [T, D], F32)
        self.mA_d = scr("mA_d", [128, 8, T], BF16)
        self.mB_d = scr("mB_d", [128, 8, T], BF16)
        self.mC_d = scr("mC_d", [128, 8, T], BF16)
        self.xsB_d = scr("xsB_d", [T, 3072], BF16)
        self.BCT_d = scr("BCT_d", [128, 16, T], BF16)
        self.ynT_d = scr("ynT_d", [128, 16, T], BF16)
        self.gT_d = scr("gT_d", [16, T], F32)
        self.cos_d = scr("cos_d", [64, T], F32)
        self.sin_d = scr("sin_d", [64, T], F32)

    def dbg(self, name, shape, dt=F32):
        ap = self.nc.dram_tensor(name, list(shape), dt, kind="ExternalOutput").ap()
        self.dbg_out[name] = ap
        return ap


def _setup_globals(self):
    P, nc = self.P, self.nc
    es = P.es
    sb = lambda name, shape, dt: es.enter_context(nc.sbuf_tensor(_uid(name), shape, dt))
    self.ps = [es.enter_context(nc.psum_tensor(f"ps{i}", [128, 512], F32)) for i in range(7)]
    self.psb = es.enter_context(nc.psum_tensor("psb", [128, 1024], BF16))
    self.ident_bf = sb("ident_bf", [128, 128], BF16)
    self.ident_f = sb("ident_f", [128, 128], F32)
    self.ones_bf = sb("ones_bf", [128, 128], BF16)
    self.ones_f = sb("ones_f", [128, 128], F32)
    self.mask_le = sb("mask_le", [128, 128], BF16)
    self.tri_le_f = sb("tri_le_f", [128, 128], F32)
    self.tri_gt_f = sb("tri_gt_f", [128, 128], F32)
    g = nc.gpsimd
    for tl, val in ((self.ident_bf, 0.0), (self.ident_f, 0.0)):
        P.op('pool', lambda tl=tl: g.memset(tl[:], 0.0), writes=[('const', tl.name)])
        P.op('pool', lambda tl=tl: g.affine_select(out=tl[:], in_=tl[:], pattern=[[-1, 128]],
                                                   compare_op=ALU.not_equal, fill=1.0, base=0,
                                                   channel_multiplier=1), writes=[('const', tl.name)])
    P.op('pool', lambda: g.memset(self.ones_bf[:], 1.0), writes=[('const', 'ones_bf')])
    P.op('pool', lambda: g.memset(self.ones_f[:], 1.0), writes=[('const', 'ones_f')])
    for tl in (self.mask_le, self.tri_le_f):
        P.op('pool', lambda tl=tl: g.memset(tl[:], 1.0), writes=[('const', tl.name)])
        P.op('pool', lambda tl=tl: g.affine_select(out=tl[:], in_=tl[:], pattern=[[1, 128]],
                                                   compare_op=ALU.is_ge, fill=0.0, base=0,
                                                   channel_multiplier=-1), writes=[('const', tl.name)])
    P.op('pool', lambda: g.memset(self.tri_gt_f[:], 1.0), writes=[('const', 'tri_gt_f')])
    P.op('pool', lambda: g.affine_select(out=self.tri_gt_f[:], in_=self.tri_gt_f[:], pattern=[[-1, 128]],
                                         compare_op=ALU.is_gt, fill=0.0, base=0,
                                         channel_multiplier=1), writes=[('const', 'tri_gt_f')])
    self.wkey = 0
    P.barrier()


def _load_w(self, dst, src, tok, nk, q='pool'):
    P = self.P
    for kc in range(nk):
        key = f"w{self.wkey % 8}"
        self.wkey += 1
        P.dma(q, out=dst(kc), in_=src(kc), key=key, writes=[(tok, kc)])


def _toks(tok, nk):
    return [(tok, kc) for kc in range(nk)]


Net.setup_globals = _setup_globals
Net.load_w = _load_w


def _prologue(self):
    P, nc = self.P, self.nc
    with ExitStack() as es:
        sb = lambda name, shape, dt: es.enter_context(nc.sbuf_tensor(_uid(name), shape, dt))
        posi = sb("posi", [64, T], I32)
        ang = sb("ang", [64, T], F32)
        red = sb("red", [64, T], F32)
        tab = sb("tab", [64, T], F32)
        invf = sb("invf", [64, 1], F32)
        negpi = sb("negpi", [64, 1], F32)
        P.dma('sp', out=posi[:], in_=self.pos_in[0, :].partition_broadcast(64), key="ld0", writes=['posi'])
        P.dma('sp', out=invf[:], in_=self.inv_freq[:, :], key="ld1", writes=['invf'])
        P.op('dve', lambda: nc.vector.memset(negpi[:], -math.pi), writes=['negpi'])
        P.op('dve', lambda: nc.vector.tensor_copy(ang[:], posi[:]), reads=['posi'], writes=['ang'])
        P.op('dve', lambda: nc.vector.tensor_scalar(ang[:], ang[:], invf[:, 0:1], None, op0=ALU.mult),
             reads=['invf'], writes=['ang'])
        ki = sb("ki", [64, T], I32)
        kf = sb("kf", [64, T], F32)
        C1 = 6.28125
        C2 = 2 * math.pi - C1
        P.op('dve', lambda: nc.vector.tensor_scalar(red[:], ang[:], 1.0 / (2 * math.pi), None, op0=ALU.mult),
             reads=['ang'], writes=['red'])
        P.op('dve', lambda: nc.vector.tensor_copy(ki[:], red[:]), reads=['red'], writes=['ki'])
        P.op('dve', lambda: nc.vector.tensor_copy(kf[:], ki[:]), reads=['ki'], writes=['kf'])
        P.op('dve', lambda: nc.vector.scalar_tensor_tensor(out=red[:], in0=kf[:], scalar=-C1, in1=ang[:],
                                                           op0=ALU.mult, op1=ALU.add), reads=['kf', 'ang'], writes=['red'])
        P.op('dve', lambda: nc.vector.scalar_tensor_tensor(out=red[:], in0=kf[:], scalar=-C2, in1=red[:],
                                                           op0=ALU.mult, op1=ALU.add), reads=['kf', 'red'], writes=['red'])

        def wrap_sin(shift):
            P.op('dve', lambda: nc.vector.tensor_scalar(ang[:], red[:], shift, None, op0=ALU.add),
                 reads=['red'], writes=['ang'])
            P.op('dve', lambda: nc.vector.tensor_scalar(kf[:], ang[:], math.pi, 2 * math.pi, op0=ALU.is_gt, op1=ALU.mult),
                 reads=['ang'], writes=['kf'])
            P.op('dve', lambda: nc.vector.tensor_tensor(ang[:], ang[:], kf[:], op=ALU.subtract),
                 reads=['ang', 'kf'], writes=['ang'])
            P.op('dve', lambda: nc.vector.tensor_scalar(ang[:], ang[:], -math.pi, math.pi, op0=ALU.max, op1=ALU.min),
                 reads=['ang'], writes=['ang'])
            P.op('act', lambda: nc.scalar.activation(out=tab[:], in_=ang[:], func=AF.Sin),
                 reads=['ang'], writes=['tab'])
        wrap_sin(0.0)
        P.op('dve', lambda: nc.vector.tensor_scalar(tab[0:32, :], tab[0:32, :], -1.0, None, op0=ALU.mult),
             reads=['tab'], writes=['tab'])
        P.dma('sp', out=self.sin_d[:, :], in_=tab[:], key="st0", reads=['tab'], writes=['sin_d'])
        wrap_sin(0.5 * math.pi)
        P.dma('sp', out=self.cos_d[:, :], in_=tab[:], key="st1", reads=['tab'], writes=['cos_d'])
        xin = [sb(f"xin{i}", [128, D], F32) for i in range(2)]
        xbf = [sb(f"xbf{i}", [128, D], BF16) for i in range(2)]
        stg = [sb(f"stg{i}", [128, 8, TB], BF16) for i in range(2)]
        for t in range(NT):
            i = t % 2
            b, tt = divmod(t, 4)
            P.dma('sp', out=xin[i][:], in_=self.x_in[t * 128:(t + 1) * 128, :], key=f"ld{2 + i}", writes=[('xin', i)])
            P.op('dve', lambda i=i: nc.vector.tensor_copy(xbf[i][:], xin[i][:]), reads=[('xin', i)], writes=[('xbf', i)])
            for kc in range(8):
                P.op('pe', lambda i=i, kc=kc: nc.tensor.transpose(self.psb[:, kc * 128:(kc + 1) * 128],
                                                                   xbf[i][:, kc * 128:(kc + 1) * 128], self.ident_bf[:]),
                     reads=[('xbf', i)], writes=['psb'], inc=(kc == 7))
            P.op('act', lambda b=b, tt=tt: nc.scalar.copy(stg[b % 2][:, :, tt * 128:(tt + 1) * 128],
                                                         self.psb[:].rearrange("p (k n) -> p k n", k=8)),
                 reads=['psb'], writes=[('stg', b % 2)])
            if tt == 3:
                P.dma('sp', out=self.xT_d[:, :, b * TB:(b + 1) * TB], in_=stg[b % 2][:], key=f"st{2 + b % 2}",
                      reads=[('stg', b % 2)], writes=[('xT_d', b)])
    P.barrier()


Net.prologue = _prologue


def _pass_mla(self, l):
    P, nc = self.P, self.nc
    ps = self.ps
    with ExitStack() as es:
        sb = lambda name, shape, dt: es.enter_context(nc.sbuf_tensor(_uid(name), shape, dt))
        Wmla = sb("Wmla", [128, 8, 640], BF16)
        Wkr = sb("Wkr", [128, 8, 128], BF16)
        WgA = sb("WgA", [128, 8, D], BF16)
        wuq = sb("wuq", [128, 3, H * 192], BF16)
        wuqsw = sb("wuqsw", [128, 3, H, 64], BF16)
        wukv = sb("wukv", [128, 2, H, 256], BF16)
        wukT = sb("wukT", [128, H, 256], BF16)
        wpa = sb("wpa", [128, 8, D], BF16)
        gq = sb("gq", [128, QR], F32)
        gkv = sb("gkv", [128, KVR], F32)
        w_in = self.w_in
        rows = lambda kc: slice(kc * 128, (kc + 1) * 128)
        self.load_w(lambda kc: Wmla[:, kc, :], lambda kc: w_in[l, rows(kc), 0:640], 'Wmla', 8)
        self.load_w(lambda kc: Wkr[:, kc, 0:64], lambda kc: w_in[l, rows(kc), C_KR:C_KR + 64], 'Wkr', 8)
        self.load_w(lambda kc: wuq[:, kc, :], lambda kc: self.w_uq[l, rows(kc), :], 'wuq', 3)
        self.load_w(lambda kc: wukv[:, kc, :, :], lambda kc: self.w_ukv[l, rows(kc), :, :], 'wukv', 2)
        P.dma('sp', out=gq[:], in_=self.q_norm[l, :].partition_broadcast(128), key="ld0", writes=['gq'])
        P.dma('sp', out=gkv[:], in_=self.kv_norm[l, :].partition_broadcast(128), key="ld1", writes=['gkv'])
        self.load_w(lambda kc: wpa[:, kc, :], lambda kc: self.w_proj_a[l, rows(kc), :], 'wpa', 8)
        self.load_w(lambda kc: WgA[:, kc, :], lambda kc: w_in[l, rows(kc), C_G:C_G + D], 'WgA', 8)
        for kc in range(8):
            P.op('dve', lambda kc=kc: nc.vector.tensor_copy(Wkr[:, kc, 64:96], Wkr[:, kc, 32:64]),
                 reads=[('Wkr', kc)], writes=[('Wkrs', kc)])
            P.op('dve', lambda kc=kc: nc.vector.tensor_copy(Wkr[:, kc, 96:128], Wkr[:, kc, 0:32]),
                 reads=[('Wkr', kc)], writes=[('Wkrs', kc)])
        for kc in range(3):
            v = wuq[:, kc, :].rearrange("p (h c) -> p h c", h=H)
            P.op('dve', lambda kc=kc, v=v: nc.vector.tensor_copy(wuqsw[:, kc, :, 0:32], v[:, :, 160:192]),
                 reads=[('wuq', kc)], writes=[('wuqsw', kc)])
            P.op('dve', lambda kc=kc, v=v: nc.vector.tensor_copy(wuqsw[:, kc, :, 32:64], v[:, :, 128:160]),
                 reads=[('wuq', kc)], writes=[('wuqsw', kc)])
        for h in range(H):
            for cc in range(2):
                P.op('pe', lambda h=h, cc=cc: nc.tensor.transpose(self.psb[:, cc * 128:(cc + 1) * 128],
                                                                   wukv[:, cc, h, 0:128], self.ident_bf[:]),
                     reads=[('wukv', cc)], writes=['psb'], inc=(cc == 1))
            P.op('act', lambda h=h: nc.scalar.copy(wukT[:, h, :], self.psb[:, 0:256]), reads=['psb'], writes=[('wukT', h)])

        ckvT = sb("ckvT", [128, 2, T], BF16)
        krT = sb("krT", [64, T], BF16)
        ckvTM = sb("ckvTM", [128, NT, KVR], BF16)
        xblk = [sb(f"xblk{i}", [128, 8, TB], BF16) for i in range(2)]
        cos1 = sb("cosb", [64, TB], F32)
        sin1 = sb("sinb", [64, TB], F32)
        cosb = [cos1, cos1]
        sinb = [sin1, sin1]
        cqTM2 = [sb(f"cqTM{i}", [128, QR], BF16) for i in range(2)]
        cqT = sb("cqT", [128, 3, TB], BF16)
        junk = sb("junk", [128, 512], F32)
        ssA = [sb(f"ss{i}", [128, 8], F32) for i in range(2)]
        qn = [sb(f"qn{i}", [128, TB], BF16) for i in range(2)]
        qlat = sb("qlat", [128, H, 2, TB], BF16)
        qrope = sb("qrope", [64, H, TB], BF16)
        rtmp = sb("rtmp", [64, TB], F32)
        rtmp2 = sb("rtmp2", [64, TB], F32)
        PT = [sb(f"PT{i}", [128, TB], BF16) for i in range(3)]
        rs = sb("rs", [128, TB], F32)
        olat = [sb(f"olat{i}", [128, 2, TB], BF16) for i in range(2)]
        oT2 = [sb(f"oT{i}", [128, H, TB], BF16) for i in range(2)]
        sig2 = [sb(f"sig{i}", [128, TB], F32) for i in range(2)]
        mT1 = sb("mT", [128, 8, TB], BF16)
        mT2 = [mT1, mT1]
        W = lambda name, n: _toks(name, n)
        rot = [0]

        def nb():
            i = rot[0] % 7
            rot[0] += 1
            return ps[i], f'ps{i}'


        def outproj(b, i2, xb, t0):
            oT, mT = oT2[i2], mT2[i2]
            for dc in range(8):
                dd = slice(dc * 128, (dc + 1) * 128)
                sg = sig2[dc % 2]
                P.mm(ps[5][:, :], [(WgA[:, kc, dd], xb[:, kc, :]) for kc in range(8)], 'ps5',
                     [('xblk', i2)] + W('WgA', 8))
                P.op('act', lambda: nc.scalar.activation(out=sg[:], in_=ps[5][:, :], func=AF.Sigmoid),
                     reads=['ps5'], writes=[('sig', dc % 2)])
                P.mm(ps[6][:, :], [(wpa[:, hh, dd], oT[:, hh, :]) for hh in range(H)], 'ps6',
                     [('oT', i2, hh) for hh in range(H)] + W('wpa', 8))
                P.op('dve', lambda: nc.vector.tensor_tensor(mT[:, dc, :], ps[6][:, :], sg[:], op=ALU.mult),
                     reads=['ps6', ('sig', dc % 2)], writes=[('mT', dc)])
                yield
            P.dma('sp', out=self.mA_d[:, :, t0:t0 + TB], in_=mT[:], key=f"st{i2}",
                  reads=[('mT', dc) for dc in range(8)], writes=[('mA_d', b)])
            yield

        bg = None
        for b in range(NB):
            i2 = b % 2
            t0 = b * TB
            P.dma('sp', out=xblk[i2][:], in_=self.xT_d[:, :, t0:t0 + TB], key=f"ld{2 + i2}", writes=[('xblk', i2)])
            P.dma('sp', out=cosb[i2][:], in_=self.cos_d[:, t0:t0 + TB], key=f"ld{4 + i2}", writes=['cosb'])
            P.dma('sp', out=sinb[i2][:], in_=self.sin_d[:, t0:t0 + TB], key=f"ld{6 + i2}", writes=['sinb'])
            xb = xblk[i2]
            tmbank = {}

            def tmA(tt):
                cols = slice(tt * 128, (tt + 1) * 128)
                pA, tA = nb()
                pB, tB = nb()
                tmbank[tt] = (pA, tA, pB, tB)
                P.mm(pA[:, 0:512], [(xb[:, kc, cols], Wmla[:, kc, 0:512]) for kc in range(8)], tA,
                     [('xblk', i2)] + W('Wmla', 8))
                P.mm(pB[:, 0:128], [(xb[:, kc, cols], Wmla[:, kc, 512:640]) for kc in range(8)], tB,
                     [('xblk', i2)] + W('Wmla', 8))

            def tmB(tt):
                t = b * 4 + tt
                p = tt % 2
                pA, tA, pB, tB = tmbank[tt]
                s_ = ssA[p]
                cq = cqTM2[p]
                K = lambda k: ('ss', p, k)
                P.op('act', lambda: nc.scalar.activation(out=junk[:, 0:384], in_=pA[:, 0:384], func=AF.Square,
                                                         accum_out=s_[:, 0:1]), reads=[tA], writes=['junk', K(0)])
                P.op('act', lambda: nc.scalar.activation(out=junk[:, 384:512], in_=pA[:, 384:512], func=AF.Square,
                                                         accum_out=s_[:, 1:2]), reads=[tA], writes=['junk', K(1)])
                P.op('act', lambda: nc.scalar.activation(out=junk[:, 0:128], in_=pB[:, 0:128], func=AF.Square,
                                                         accum_out=s_[:, 2:3]), reads=[tB], writes=['junk', K(2)])
                P.op('dve', lambda: nc.vector.tensor_scalar(s_[:, 4:5], s_[:, 0:1], 1.0 / QR, RMS_EPS, op0=ALU.mult, op1=ALU.add),
                     reads=[K(0)], writes=[K(4)])
                P.op('dve', lambda: nc.vector.tensor_tensor(s_[:, 5:6], s_[:, 1:2], s_[:, 2:3], op=ALU.add),
                     reads=[K(1), K(2)], writes=[K(5)])
                P.op('dve', lambda: nc.vector.tensor_scalar(s_[:, 5:6], s_[:, 5:6], 1.0 / KVR, RMS_EPS, op0=ALU.mult, op1=ALU.add),
                     reads=[K(5)], writes=[K(5)])
                P.op('act', lambda: nc.scalar.activation(out=s_[:, 6:8], in_=s_[:, 4:6], func=AF.Sqrt), reads=[K(4), K(5)], writes=[K(6)])
                P.op('dve', lambda: nc.vector.reciprocal(s_[:, 4:6], s_[:, 6:8]), reads=[K(6)], writes=[K(4), K(5)])
                P.op('dve', lambda: nc.vector.scalar_tensor_tensor(out=cq[:], in0=pA[:, 0:384], scalar=s_[:, 4:5],
                                                                   in1=gq[:], op0=ALU.mult, op1=ALU.mult),
                     reads=[tA, K(4), 'gq'], writes=[('cqTM', p)])
                P.op('dve', lambda: nc.vector.scalar_tensor_tensor(out=ckvTM[:, t, 0:128], in0=pA[:, 384:512],
                                                                   scalar=s_[:, 5:6], in1=gkv[:, 0:128],
                                                                   op0=ALU.mult, op1=ALU.mult),
                     reads=[tA, K(5), 'gkv'], writes=[('ckvTM', t)])
                P.op('dve', lambda: nc.vector.scalar_tensor_tensor(out=ckvTM[:, t, 128:256], in0=pB[:, 0:128],
                                                                   scalar=s_[:, 5:6], in1=gkv[:, 128:256],
                                                                   op0=ALU.mult, op1=ALU.mult),
                     reads=[tB, K(5), 'gkv'], writes=[('ckvTM', t)])

            def tmC(tt):
                t = b * 4 + tt
                p = tt % 2
                cols = slice(tt * 128, (tt + 1) * 128)
                cq = cqTM2[p]
                for j in range(3):
                    P.op('pe', lambda j=j: nc.tensor.transpose(self.psb[:, j * 128:(j + 1) * 128],
                                                               cq[:, j * 128:(j + 1) * 128], self.ident_bf[:]),
                         reads=[('cqTM', p)], writes=['psb'], inc=False)
                for j in range(2):
                    P.op('pe', lambda j=j: nc.tensor.transpose(self.psb[:, (3 + j) * 128:(4 + j) * 128],
                                                               ckvTM[:, t, j * 128:(j + 1) * 128], self.ident_bf[:]),
                         reads=[('ckvTM', t)], writes=['psb'], inc=(j == 1))
                P.op('act', lambda: nc.scalar.copy(cqT[:, :, cols], self.psb[:, 0:384].rearrange("p (k n) -> p k n", k=3)),
                     reads=['psb'], writes=['cqT'])
                P.op('act', lambda: nc.scalar.copy(ckvT[:, :, t * 128:(t + 1) * 128],
                                                   self.psb[:, 384:640].rearrange("p (k n) -> p k n", k=2)),
                     reads=['psb'], writes=[('ckvT', t)])

            for step in range(6):
                if step < 4:
                    tmA(step)
                if 0 <= step - 1 < 4:
                    tmB(step - 1)
                if 0 <= step - 2 < 4:
                    tmC(step - 2)
            pA, tA = nb()
            pB, tB = nb()
            P.mm(pA[0:64, :], [(Wkr[:, kc, 0:64], xb[:, kc, :]) for kc in range(8)], tA,
                 [('xblk', i2)] + W('Wkr', 8))
            P.mm(pB[0:64, :], [(Wkr[:, kc, 64:128], xb[:, kc, :]) for kc in range(8)], tB,
                 [('xblk', i2)] + W('Wkrs', 8))

            def rope(psA, psB, outap, rA, rB, wtok):
                P.op('dve', lambda: nc.vector.tensor_tensor(rtmp[:], psB, sinb[i2][:], op=ALU.mult),
                     reads=[rB, 'sinb'], writes=['rtmp'])
                P.op('dve', lambda: nc.vector.tensor_tensor(rtmp2[:], psA, cosb[i2][:], op=ALU.mult),
                     reads=[rA, 'cosb'], writes=['rtmp2'])
                P.op('dve', lambda: nc.vector.tensor_tensor(outap, rtmp[:], rtmp2[:], op=ALU.add),
                     reads=['rtmp', 'rtmp2'], writes=[wtok])
            rope(pA[0:64, :], pB[0:64, :], krT[:, t0:t0 + TB], tA, tB, ('krT', b))

            for h in range(H):
                c0 = h * 192
                pq, tq_ = nb()
                P.mm(pq[:, :], [(wuq[:, kc, c0:c0 + 128], cqT[:, kc, :]) for kc in range(3)], tq_,
                     ['cqT'] + W('wuq', 3))
                P.op('act', lambda pq=pq, h=h: nc.scalar.copy(qn[h % 2][:], pq[:, :]), reads=[tq_], writes=[('qn', h % 2)])
                pa_, ta_ = nb()
                pb_, tb_ = nb()
                P.mm(pa_[0:64, :], [(wuq[:, kc, c0 + 128:c0 + 192], cqT[:, kc, :]) for kc in range(3)], ta_,
                     ['cqT'] + W('wuq', 3))
                P.mm(pb_[0:64, :], [(wuqsw[:, kc, h, :], cqT[:, kc, :]) for kc in range(3)], tb_,
                     ['cqT'] + W('wuqsw', 3))
                for cc in range(2):
                    pl, tl_ = nb()
                    P.mm(pl[:, :], [(wukT[:, h, cc * 128:(cc + 1) * 128], qn[h % 2][:])], tl_, [('qn', h % 2), ('wukT', h)])
                    P.op('dve' if cc == 0 else 'act',
                         (lambda h=h, cc=cc, pl=pl: nc.vector.tensor_copy(qlat[:, h, cc, :], pl[:, :])) if cc == 0 else
                         (lambda h=h, cc=cc, pl=pl: nc.scalar.copy(qlat[:, h, cc, :], pl[:, :])),
                         reads=[tl_], writes=[('qlat', h)])
                rope(pa_[0:64, :], pb_[0:64, :], qrope[:, h, :], ta_, tb_, ('qrope', h))

            nkt = 4 * b + 4
            items = [(h, kt) for h in range(H) for kt in range(nkt)]

            def stage_S(i):
                h, kt = items[i]
                d = kt - 4 * b
                q0 = max(d, 0) * 128
                qs = slice(q0, TB)
                si, pi = i % 2, i % 3
                st = ps[si]
                kk = slice(kt * 128, (kt + 1) * 128)
                P.mm(st[:, qs], [(ckvT[:, 0, kk], qlat[:, h, 0, qs]), (ckvT[:, 1, kk], qlat[:, h, 1, qs]),
                                 (krT[:, kk], qrope[:, h, qs])], ('ST', si),
                     [('ckvT', kt), ('krT', kt // 4), ('qlat', h), ('qrope', h)])
                P.op('act', lambda: nc.scalar.activation(out=PT[pi][:, qs], in_=st[:, qs], func=AF.Exp, scale=SCALE_A),
                     reads=[('ST', si)], writes=[('PT', pi)])
                if d >= 0:
                    dq = slice(q0, q0 + 128)
                    P.op('pool', lambda: nc.gpsimd.tensor_tensor(PT[pi][:, dq], PT[pi][:, dq], self.mask_le[:], op=ALU.mult),
                         reads=[('PT', pi)], writes=[('PT', pi)])

            def stage_V(i):
                h, kt = items[i]
                d = kt - 4 * b
                q0 = max(d, 0) * 128
                qs = slice(q0, TB)
                pi = i % 3
                first, last = (kt == 0), (kt == nkt - 1)
                for cc in range(2):
                    P.op('pe', lambda cc=cc: nc.tensor.matmul(
                        ps[2 + cc][:, qs], ckvTM[:, kt, cc * 128:(cc + 1) * 128], PT[pi][:, qs], start=first, stop=last),
                         reads=[('PT', pi), ('ckvTM', kt)], writes=[('oacc', cc)], inc=False)
                P.op('pe', lambda: nc.tensor.matmul(ps[4][:, qs], self.ones_bf[:], PT[pi][:, qs], start=first, stop=last),
                     reads=[('PT', pi)], writes=['osum'], inc=True)
                P._record(('e', 'pe', P.cnt['pe']), [('PT', pi), ('ckvTM', kt)], [('oacc', 0), ('oacc', 1)])
                if last:
                    ol = olat[h % 2]
                    P.op('dve', lambda: nc.vector.reciprocal(rs[:], ps[4][:, :]), reads=['osum'], writes=['rs'])
                    for cc in range(2):
                        P.op('dve', lambda cc=cc: nc.vector.tensor_tensor(ol[:, cc, :], ps[2 + cc][:, :], rs[:], op=ALU.mult),
                             reads=[('oacc', cc), 'rs'], writes=[('olat', h % 2)])

            def proj_o(h):
                ol = olat[h % 2]
                P.mm(ps[5][:, :], [(wukv[:, cc, h, 128:256], ol[:, cc, :]) for cc in range(2)], 'ps5',
                     [('olat', h % 2)] + W('wukv', 2))
                P.op('act', lambda: nc.scalar.copy(oT2[i2][:, h, :], ps[5][:, :]), reads=['ps5'], writes=[('oT', i2, h)])

            pending = []
            if os.environ.get('MLA_ABL'):
                items = items[:int(os.environ['MLA_ABL'])]
            every = max(len(items) // 9, 1)
            stage_S(0)
            for i in range(len(items)):
                if i + 1 < len(items):
                    stage_S(i + 1)
                stage_V(i)
                h, kt = items[i]
                if kt == nkt - 1:
                    pending.append((i + 2, h))
                while pending and pending[0][0] <= i:
                    proj_o(pending.pop(0)[1])
                if bg is not None and i % every == every - 1:
                    next(bg, None)
            for _, h in pending:
                proj_o(h)
            if bg is not None:
                for _ in bg:
                    pass
            bg = outproj(b, i2, xb, t0)
        for _ in bg:
            pass

    P.barrier()


Net.pass_mla = _pass_mla


def _pass_mem(self, l):
    P, nc = self.P, self.nc
    ps = self.ps
    SC = 256 ** -0.5
    with ExitStack() as es:
        sb = lambda name, shape, dt: es.enter_context(nc.sbuf_tensor(_uid(name), shape, dt))
        Wqm = sb("Wqm", [128, 8, D], BF16)
        WgC = sb("WgC", [128, 8, D], BF16)
        wpc = sb("wpc", [128, 8, D], BF16)
        KT = sb("KT", [128, 4, 2, 256], BF16)
        V = sb("V", [128, 2, D], BF16)
        rows = lambda kc: slice(kc * 128, (kc + 1) * 128)
        W = lambda name, n: _toks(name, n)
        self.load_w(lambda kc: Wqm[:, kc, :], lambda kc: self.w_in[l, rows(kc), C_QM:C_QM + D], 'Wqm', 8)
        self.load_w(lambda kc: WgC[:, kc, :], lambda kc: self.w_in[l, rows(kc), C_G + 2 * D:C_G + 3 * D], 'WgC', 8)
        self.load_w(lambda kc: wpc[:, kc, :], lambda kc: self.w_proj_c[l, rows(kc), :], 'wpc', 8)
        with ExitStack() as es2:
            sb2 = lambda name, shape, dt: es2.enter_context(nc.sbuf_tensor(_uid(name), shape, dt))
            wmkv = sb2("wmkv", [128, 8, 2048], BF16)
            memT = sb2("memT", [128, 8, 256], BF16)
            mtile = sb2("mtile", [128, D], BF16)
            self.load_w(lambda kc: wmkv[:, kc, :], lambda kc: self.w_mem_kv[l, rows(kc), :], 'wmkv', 8)
            for mt in range(2):
                P.dma('pool', out=mtile[:], in_=self.mem_in[mt * 128:(mt + 1) * 128, :], key="ld0", writes=['mtile'])
                for kc in range(8):
                    P.op('pe', lambda kc=kc: nc.tensor.transpose(self.psb[:, kc * 128:(kc + 1) * 128],
                                                                 mtile[:, kc * 128:(kc + 1) * 128], self.ident_bf[:]),
                         reads=['mtile'], writes=['psb'], inc=(kc == 7))
                P.op('act', lambda mt=mt: nc.scalar.copy(memT[:, :, mt * 128:(mt + 1) * 128],
                                                         self.psb[:].rearrange("p (k n) -> p k n", k=8)),
                     reads=['psb'], writes=['memT'])
            for h in range(4):
                for dc in range(2):
                    c0 = h * 256 + dc * 128
                    P.mm(ps[5][:, 0:256], [(wmkv[:, kc, c0:c0 + 128], memT[:, kc, :]) for kc in range(8)], 'ps5',
                         ['memT'] + W('wmkv', 8))
                    P.op('act', lambda h=h, dc=dc: nc.scalar.copy(KT[:, h, dc, :], ps[5][:, 0:256]), reads=['ps5'], writes=['KT'])
            for mt in range(2):
                for half in range(2):
                    c0 = 1024 + half * 512
                    P.mm(ps[6][:, :], [(memT[:, kc, mt * 128:(mt + 1) * 128], wmkv[:, kc, c0:c0 + 512]) for kc in range(8)],
                         'ps6', ['memT'] + W('wmkv', 8))
                    P.op('dve', lambda mt=mt, half=half: nc.vector.tensor_copy(V[:, mt, half * 512:(half + 1) * 512], ps[6][:, :]),
                         reads=['ps6'], writes=['V'])
            P.barrier()
        xblk = [sb(f"xblk{i}", [128, 8, TB], BF16) for i in range(2)]
        qm = sb("qm", [128, 2, TB], BF16)
        PT = [sb(f"PT{i}", [128, 2, TB], BF16) for i in range(2)]
        rs = sb("rs", [128, TB], F32)
        ocT = sb("ocT", [128, 8, TB], BF16)
        sig = sb("sig", [128, TB], F32)
        mT = sb("mT", [128, 8, TB], BF16)
        for b in range(NB):
            i2 = b % 2
            t0 = b * TB
            P.dma('sp', out=xblk[i2][:], in_=self.xT_d[:, :, t0:t0 + TB], key=f"ld{2 + i2}", writes=[('xblk', i2)])
            xb = xblk[i2]
            for h in range(4):
                pi = h % 2
                for dc in range(2):
                    c0 = h * 256 + dc * 128
                    pb, pt = (ps[5], 'ps5') if dc == 0 else (ps[6], 'ps6')
                    P.mm(pb[:, :], [(Wqm[:, kc, c0:c0 + 128], xb[:, kc, :]) for kc in range(8)], pt,
                         [('xblk', i2)] + W('Wqm', 8))
                    if dc == 0:
                        P.op('act', lambda pb=pb: nc.scalar.copy(qm[:, 0, :], pb[:, :]), reads=[pt], writes=[('qm', 0)])
                    else:
                        P.op('dve', lambda pb=pb: nc.vector.tensor_copy(qm[:, 1, :], pb[:, :]), reads=[pt], writes=[('qm', 1)])
                for mt in range(2):
                    P.mm(ps[mt][:, :], [(KT[:, h, dc, mt * 128:(mt + 1) * 128], qm[:, dc, :]) for dc in range(2)], ('ST', mt),
                         ['KT', ('qm', 0), ('qm', 1)])
                    P.op('act', lambda mt=mt, pi=pi: nc.scalar.activation(out=PT[pi][:, mt, :], in_=ps[mt][:, :], func=AF.Exp, scale=SC),
                         reads=[('ST', mt)], writes=[('PT', pi, mt)])
                for vc in range(2):
                    c0 = h * 256 + vc * 128
                    P.mm(ps[2 + vc][:, :], [(V[:, mt, c0:c0 + 128], PT[pi][:, mt, :]) for mt in range(2)], ('oacc', vc),
                         ['V', ('PT', pi, 0), ('PT', pi, 1)])
                P.mm(ps[4][:, :], [(self.ones_bf[:], PT[pi][:, mt, :]) for mt in range(2)], 'osum',
                     [('PT', pi, 0), ('PT', pi, 1)])
                P.op('dve', lambda: nc.vector.reciprocal(rs[:], ps[4][:, :]), reads=['osum'], writes=['rs'])
                for vc in range(2):
                    P.op('dve', lambda h=h, vc=vc: nc.vector.tensor_tensor(ocT[:, h * 2 + vc, :], ps[2 + vc][:, :], rs[:], op=ALU.mult),
                         reads=[('oacc', vc), 'rs'], writes=[('ocT', h * 2 + vc)])
            for dc in range(8):
                dd = slice(dc * 128, (dc + 1) * 128)
                P.mm(ps[5][:, :], [(WgC[:, kc, dd], xb[:, kc, :]) for kc in range(8)], 'ps5',
                     [('xblk', i2)] + W('WgC', 8))
                P.op('act', lambda: nc.scalar.activation(out=sig[:], in_=ps[5][:, :], func=AF.Sigmoid),
                     reads=['ps5'], writes=['sig'])
                P.mm(ps[6][:, :], [(wpc[:, j, dd], ocT[:, j, :]) for j in range(8)], 'ps6',
                     [('ocT', j) for j in range(8)] + W('wpc', 8))
                P.op('dve', lambda dc=dc: nc.vector.tensor_tensor(mT[:, dc, :], ps[6][:, :], sig[:], op=ALU.mult),
                     reads=['ps6', 'sig'], writes=[('mT', dc)])
            P.dma('sp', out=self.mC_d[:, :, t0:t0 + TB], in_=mT[:], key="st0",
                  reads=[('mT', dc) for dc in range(8)], writes=[('mC_d', b)])
    P.barrier()


Net.pass_mem = _pass_mem


def _pass_ssd_a(self, l):
    P, nc = self.P, self.nc
    ps = self.ps
    with ExitStack() as es:
        sb = lambda name, shape, dt: es.enter_context(nc.sbuf_tensor(_uid(name), shape, dt))
        Wx = sb("Wx", [128, 8, 4096], BF16)
        cw5 = sb("cw5", [5, 4096], F32)
        cwb = sb("cwb", [128, 32, 5], F32)
        rows = lambda kc: slice(kc * 128, (kc + 1) * 128)
        W = lambda name, n: _toks(name, n)
        self.load_w(lambda kc: Wx[:, kc, :], lambda kc: self.w_in[l, rows(kc), C_XBC:C_XBC + 4096], 'Wx', 8)
        P.dma('sp', out=cw5[0:4, :], in_=self.conv_w[l, :, :], key="ld0", writes=['cw5a'])
        P.dma('sp', out=cw5[4:5, :], in_=self.conv_b[l:l + 1, :], key="ld1", writes=['cw5b'])
        for cc in range(32):
            P.op('pe', lambda cc=cc: nc.tensor.transpose(ps[5][:, cc * 5:(cc + 1) * 5], cw5[0:5, cc * 128:(cc + 1) * 128],
                                                         self.ident_f[0:5, 0:5]),
                 reads=['cw5a', 'cw5b'], writes=['ps5'], inc=(cc == 31))
        P.op('act', lambda: nc.scalar.copy(cwb[:].rearrange("p c k -> p (c k)"), ps[5][:, 0:160]), reads=['ps5'], writes=['cwb'])
        xblk = [sb(f"xblk{i}", [128, 8, TB], BF16) for i in range(2)]
        pre = [sb(f"pre{i}", [128, TB + 3], BF16) for i in range(3)]
        carry = sb("carry", [128, 32, 3], BF16)
        xbcT = sb("xbcT", [128, 32, TB], BF16)
        tmst = [sb(f"tmst{i}", [128, 3072], BF16) for i in range(2)]
        dg = sb("dg", [128, 32, 4, 128], BF16)
        for cc in range(32):
            for k in range(4):
                P.op('dve', lambda cc=cc, k=k: nc.vector.tensor_scalar(dg[:, cc, k, :], self.ident_bf[:], cwb[:, cc, k:k + 1], None, op0=ALU.mult),
                     reads=['cwb'], writes=[('dg', cc)])
        P.op('dve', lambda: nc.vector.memset(carry[:], 0.0), writes=[('carry', cc) for cc in range(32)])
        n = 0
        rot = [0]

        def nb():
            i = rot[0] % 7
            rot[0] += 1
            return ps[i], f'ps{i}'

        for b in range(NB):
            i2 = b % 2
            t0 = b * TB
            P.dma('sp', out=xblk[i2][:], in_=self.xT_d[:, :, t0:t0 + TB], key=f"ld{2 + i2}", writes=[('xblk', i2)])
            xb = xblk[i2]
            for cc in range(32):
                j = n % 3
                n += 1
                pb, pt = nb()
                pr = pre[j]
                P.mm(pb[:, :], [(Wx[:, kc, cc * 128:(cc + 1) * 128], xb[:, kc, :]) for kc in range(8)], pt,
                     [('xblk', i2)] + W('Wx', 8))
                P.op('act', lambda pr=pr, cc=cc: nc.scalar.copy(pr[:, 0:3], carry[:, cc, :]),
                     reads=[('carry', cc)], writes=[('pre', j)])
                P.op('act', lambda pr=pr, pb=pb: nc.scalar.copy(pr[:, 3:TB + 3], pb[:, :]), reads=[pt], writes=[('pre', j)])
                P.op('act', lambda pr=pr, cc=cc: nc.scalar.copy(carry[:, cc, :], pr[:, TB:TB + 3]),
                     reads=[('pre', j)], writes=[('carry', cc)])
                pc, pct = nb()
                P.mm(pc[:, :], [(dg[:, cc, k, :], pr[:, k:k + TB]) for k in range(4)], pct, [('pre', j), ('dg', cc)])
                P.op('act', lambda pc=pc, cc=cc: nc.scalar.activation(out=xbcT[:, cc, :], in_=pc[:, :], func=AF.Silu, bias=cwb[:, cc, 4:5]),
                     reads=[pct, 'cwb'], writes=[('xbcT', cc)])
            P.dma('sp', out=self.BCT_d[:, :, t0:t0 + TB], in_=xbcT[:, 16:32, :], key="st0",
                  reads=[('xbcT', cc) for cc in range(16, 32)], writes=[('BCT_d', b)])
            for tt in range(4):
                t = b * 4 + tt
                ti = t % 2
                for r in range(3):
                    for q in range(8):
                        cc = r * 8 + q
                        P.op('pe', lambda cc=cc, q=q, tt=tt: nc.tensor.transpose(self.psb[:, q * 128:(q + 1) * 128],
                                                                                 xbcT[:, cc, tt * 128:(tt + 1) * 128], self.ident_bf[:]),
                             reads=[('xbcT', cc)], writes=['psb'], inc=(q == 7))
                    P.op('dve', lambda ti=ti, r=r: nc.vector.tensor_copy(tmst[ti][:, r * 1024:(r + 1) * 1024], self.psb[:, :]),
                         reads=['psb'], writes=[('tmst', ti)])
                P.dma('sp', out=self.xsB_d[t * 128:(t + 1) * 128, :], in_=tmst[ti][:], key=f"st{1 + ti}",
                      reads=[('tmst', ti)], writes=[('xsB_d', t)])
    P.barrier()


def _pass_ssd_b(self, l):
    P, nc = self.P, self.nc
    ps = self.ps
    with ExitStack() as es:
        sb = lambda name, shape, dt: es.enter_context(nc.sbuf_tensor(_uid(name), shape, dt))
        rows = lambda kc: slice(kc * 128, (kc + 1) * 128)
        W = lambda name, n: _toks(name, n)
        Wz = sb("Wz", [128, 8, 2048], BF16)
        Wdt = sb("Wdt", [128, 8, 32], BF16)
        dtb = sb("dtb", [128, 32], F32)
        abc = sb("abc", [128, 32], F32)
        dsk = sb("dsk", [128, 32], F32)
        ngb = sb("ngb", [128, 2048], F32)
        self.load_w(lambda kc: Wz[:, kc, :], lambda kc: self.w_in[l, rows(kc), C_Z:C_Z + 2048], 'Wz', 8)
        self.load_w(lambda kc: Wdt[:, kc, :], lambda kc: self.w_in[l, rows(kc), C_DT:C_DT + 32], 'Wdt', 8)
        P.dma('sp', out=dtb[:], in_=self.dt_bias[l, :].partition_broadcast(128), key="ld0", writes=['dtb'])
        P.dma('sp', out=abc[:], in_=self.a_log[l, :].partition_broadcast(128), key="ld1", writes=['abc'])
        P.dma('sp', out=dsk[:], in_=self.d_skip[l, :].partition_broadcast(128), key="ld2", writes=['dsk'])
        P.dma('sp', out=ngb[:], in_=self.ssm_norm[l, :].partition_broadcast(128), key="ld3", writes=['ngb'])
        P.op('act', lambda: nc.scalar.activation(out=abc[:], in_=abc[:], func=AF.Exp), reads=['abc'], writes=['abc'])
        P.op('dve', lambda: nc.vector.tensor_scalar(abc[:], abc[:], -1.0, None, op0=ALU.mult), reads=['abc'], writes=['abc'])
        ST = sb("ST", [128, 8, 256], F32)
        STb = sb("STb", [128, 8, 256], BF16)
        P.op('dve', lambda: nc.vector.memset(ST[:], 0.0), writes=[('STATE', g) for g in range(8)])
        P.op('pool', lambda: nc.gpsimd.memset(STb[:], 0.0), writes=[('STb', g) for g in range(8)])
        xblk = [sb(f"xblk{i}", [128, 8, TB], BF16) for i in range(2)]
        bct = [sb(f"bct{i}", [128, 16, TB], BF16) for i in range(2)]
        xsB = [sb(f"xsB{i}", [128, 3072], BF16) for i in range(2)]
        sm = sb("sm", [128, 8, 32], F32)
        xdt = sb("xdt", [128, 2048], BF16)
        xdt2 = sb("xdt2", [128, 2048], BF16)
        dam = [sb(f"dam{i}", [128, 4, 128], BF16) for i in range(2)]
        dec = [sb(f"dec{i}", [128, 4, 128], F32) for i in range(2)]
        cbm = [sb(f"cbm{i}", [128, 128], F32) for i in range(2)]
        G = [sb(f"G{i}", [128, 4, 128], BF16) for i in range(2)]
        tmp = [sb(f"tmp{i}", [128, 256], F32) for i in range(2)]
        y = sb("y", [128, 2048], F32)
        zs = [sb(f"zs{i}", [128, 512], F32) for i in range(2)]
        junk = sb("junk", [128, 256], F32)
        ssq = sb("ssq", [128, 16], F32)
        yn = sb("yn", [128, 2048], BF16)
        ynT = [sb(f"ynT{i}", [128, 16, TB], BF16) for i in range(2)]
        v3 = lambda ap, h: ap.rearrange("p (h c) -> p h c", h=h)
        bc3 = lambda ap, n: ap.unsqueeze(2).to_broadcast([128, ap.shape[1], n])
        sm2 = [sm, sb("smB", [128, 8, 32], F32)]
        xdtA = [xdt, sb("xdtB", [128, 2048], BF16)]
        xdt2A = [xdt2, sb("xdt2B", [128, 2048], BF16)]
        yA = [y, sb("yB", [128, 2048], F32)]

        def load_blk(b):
            i2 = b % 2
            t0 = b * TB
            P.dma('sp', out=xblk[i2][:], in_=self.xT_d[:, :, t0:t0 + TB], key=f"ld{4 + i2}", writes=[('xblk', i2)])
            P.dma('sp', out=bct[i2][:], in_=self.BCT_d[:, :, t0:t0 + TB], key=f"ld{6 + i2}", writes=[('bct', i2)])

        def pre(t):
            b, tt = divmod(t, 4)
            i2 = b % 2
            ti = t % 2
            if tt == 0:
                load_blk(b)
            xb = xblk[i2]
            cols = slice(tt * 128, (tt + 1) * 128)
            xs = xsB[ti]
            sm = sm2[ti]
            S = lambda k: ('sm', ti, k)
            P.dma('sp', out=xs[:], in_=self.xsB_d[t * 128:(t + 1) * 128, :], key=f"ld{8 + ti}", writes=[('xsB', ti)])
            P.mm(ps[5][:, 0:32], [(xb[:, kc, cols], Wdt[:, kc, :]) for kc in range(8)], 'ps5', [('xblk', i2)] + W('Wdt', 8))
            P.op('dve', lambda: nc.vector.tensor_tensor(sm[:, 0, :], ps[5][:, 0:32], dtb[:], op=ALU.add),
                 reads=['ps5', 'dtb'], writes=[S(0)])
            P.op('dve', lambda: nc.vector.tensor_scalar(sm[:, 1, :], sm[:, 0, :], -1.0, None, op0=ALU.mult),
                 reads=[S(0)], writes=[S(1)])
            P.op('dve', lambda: nc.vector.tensor_tensor(sm[:, 1, :], sm[:, 1, :], sm[:, 0, :], op=ALU.min),
                 reads=[S(0), S(1)], writes=[S(1)])
            P.op('act', lambda: nc.scalar.activation(out=sm[:, 2, :], in_=sm[:, 1, :], func=AF.Exp),
                 reads=[S(1)], writes=[S(2)])
            P.op('act', lambda: nc.scalar.activation(out=sm[:, 2, :], in_=sm[:, 2, :], func=AF.Ln, bias=1.0),
                 reads=[S(2)], writes=[S(2)])
            P.op('dve', lambda: nc.vector.scalar_tensor_tensor(out=sm[:, 3, :], in0=sm[:, 0, :], scalar=0.0, in1=sm[:, 2, :],
                                                               op0=ALU.max, op1=ALU.add), reads=[S(0), S(2)], writes=[S(3)])
            P.op('dve', lambda: nc.vector.tensor_tensor(sm[:, 4, :], sm[:, 3, :], abc[:], op=ALU.mult),
                 reads=[S(3), 'abc'], writes=[S(4)])
            P.mm(ps[5][:, 64:96], [(self.tri_le_f[:], sm[:, 4, :])], 'ps5', [S(4)])
            P.mm(ps[5][:, 128:160], [(self.tri_gt_f[:], sm[:, 4, :])], 'ps5', [S(4)])
            P.mm(ps[5][:, 192:224], [(self.ones_f[:], sm[:, 4, :])], 'ps5', [S(4)])
            P.op('act', lambda: nc.scalar.activation(out=sm[:, 5, :], in_=ps[5][:, 64:96], func=AF.Exp), reads=['ps5'], writes=[S(5)])
            P.op('act', lambda: nc.scalar.activation(out=sm[:, 6, :], in_=ps[5][:, 128:160], func=AF.Exp), reads=['ps5'], writes=[S(6)])
            P.op('act', lambda: nc.scalar.activation(out=sm[:, 7, :], in_=ps[5][:, 192:224], func=AF.Exp), reads=['ps5'], writes=[S(7)])
            P.op('dve', lambda: nc.vector.tensor_tensor(v3(xdtA[ti][:], 32), v3(xs[:, 0:2048], 32), bc3(sm[:, 3, :], 64), op=ALU.mult),
                 reads=[('xsB', ti), S(3)], writes=[('xdt', ti)])
            P.op('pool', lambda: nc.gpsimd.tensor_tensor(v3(xdt2A[ti][:], 32), v3(xdtA[ti][:], 32), bc3(sm[:, 6, :], 64), op=ALU.mult),
                 reads=[('xdt', ti), S(6)], writes=[('xdt2', ti)])

        def grp(t):
            b, tt = divmod(t, 4)
            i2 = b % 2
            ti = t % 2
            cols = slice(tt * 128, (tt + 1) * 128)
            xs = xsB[ti]
            sm = sm2[ti]
            S = lambda k: ('sm', ti, k)
            xdt_, xdt2_, y_ = xdtA[ti], xdt2A[ti], yA[ti]

            def stage1(g):
                gi = g % 2
                hs = slice(g * 4, (g + 1) * 4)
                P.op('pool', lambda: nc.gpsimd.tensor_tensor(
                    dam[gi][:], self.tri_gt_f[:, :].unsqueeze(1).to_broadcast([128, 4, 128]), bc3(sm[:, 4, hs], 128), op=ALU.mult),
                     reads=[S(4)], writes=[('dam', gi)])
                sp_, spt = (ps[0], ('SEG', 0)) if gi == 0 else (ps[1], ('SEG', 1))
                for r in range(4):
                    P.mm(sp_[:, r * 128:(r + 1) * 128], [(dam[gi][:, r, :], self.mask_le[:])], spt, [('dam', gi)])
                P.op('act', lambda: nc.scalar.activation(out=dec[gi][:].rearrange("p r s -> p (r s)"), in_=sp_[:, :], func=AF.Exp),
                     reads=[spt], writes=[('dec', gi)])
                cp_, cpt = (ps[2], ('CB', 0)) if gi == 0 else (ps[3], ('CB', 1))
                P.mm(cp_[:, 0:128], [(bct[i2][:, g, cols], bct[i2][:, 8 + g, cols])], cpt, [('bct', i2)])
                P.op('dve', lambda: nc.vector.tensor_tensor(cbm[gi][:], cp_[:, 0:128], self.tri_le_f[:], op=ALU.mult),
                     reads=[cpt], writes=[('cbm', gi)])
                P.op('dve', lambda: nc.vector.tensor_tensor(G[gi][:], dec[gi][:], cbm[gi][:, :].unsqueeze(1).to_broadcast([128, 4, 128]),
                                                            op=ALU.mult),
                     reads=[('dec', gi), ('cbm', gi)], writes=[('G', gi)])

            def stage2(g):
                gi = g % 2
                hs = slice(g * 4, (g + 1) * 4)
                cp_ = ps[2] if gi == 0 else ps[3]
                bp_ = ps[4] if gi == 0 else ps[6]
                for r in range(4):
                    hh = g * 4 + r
                    P.mm(bp_[:, r * 64:(r + 1) * 64],
                         [(G[gi][:, r, :], xdt_[:, hh * 64:(hh + 1) * 64])], ('YI', gi), [('G', gi), ('xdt', ti)])
                P.mm(bp_[:, 256:512], [(xs[:, 2048 + g * 128:2048 + (g + 1) * 128], xdt2_[:, g * 256:(g + 1) * 256])],
                     ('SU', gi), [('xsB', ti), ('xdt2', ti)])
                P.mm(cp_[:, 256:512], [(bct[i2][:, 8 + g, cols], STb[:, g, :])], ('YS', gi), [('bct', i2), ('STb', g)])
                P.op('dve', lambda: nc.vector.tensor_tensor(v3(tmp[gi][:], 4), v3(cp_[:, 256:512], 4), bc3(sm[:, 5, hs], 64), op=ALU.mult),
                     reads=[('YS', gi), S(5)], writes=[('tmp', gi)])
                P.op('dve', lambda: nc.vector.tensor_tensor(y_[:, g * 256:(g + 1) * 256], tmp[gi][:], bp_[:, 0:256], op=ALU.add),
                     reads=[('tmp', gi), ('YI', gi)], writes=[('y', ti, g)])
                P.op('pool', lambda: nc.gpsimd.tensor_tensor(v3(ST[:, g, :], 4), v3(ST[:, g, :], 4), bc3(sm[:, 7, hs], 64), op=ALU.mult),
                     reads=[S(7)], writes=[('STATE', g)])
                P.op('dve', lambda: nc.vector.tensor_tensor(ST[:, g, :], ST[:, g, :], bp_[:, 256:512], op=ALU.add),
                     reads=[('SU', gi)], writes=[('STATE', g)])
                P.op('act', lambda: nc.scalar.copy(STb[:, g, :], ST[:, g, :]), reads=[('STATE', g)], writes=[('STb', g)])

            if SSD_PIPE:
                stage1(0)
                for g in range(8):
                    if g + 1 < 8:
                        stage1(g + 1)
                    stage2(g)
            else:
                for g in range(8):
                    stage1(g)
                    stage2(g)

        def post(t):
            b, tt = divmod(t, 4)
            i2 = b % 2
            ti = t % 2
            t0 = b * TB
            cols = slice(tt * 128, (tt + 1) * 128)
            xb = xblk[i2]
            xs = xsB[ti]
            xdt2_, y_ = xdt2A[ti], yA[ti]
            ally = [('y', ti, g) for g in range(8)]
            P.op('pool', lambda: nc.gpsimd.tensor_tensor(v3(xdt2_[:], 32), v3(xs[:, 0:2048], 32), bc3(dsk[:, :], 64), op=ALU.mult),
                 reads=[('xsB', ti), 'dsk'], writes=[('xdt2', ti)])
            P.op('dve', lambda: nc.vector.tensor_tensor(y_[:], y_[:], xdt2_[:], op=ALU.add), reads=ally + [('xdt2', ti)], writes=ally)
            for q in range(4):
                zi = q % 2
                qs = slice(q * 512, (q + 1) * 512)
                zp, zpt = (ps[0], ('SEG', 0)) if zi == 0 else (ps[1], ('SEG', 1))
                P.mm(zp[:, :], [(xb[:, kc, cols], Wz[:, kc, qs]) for kc in range(8)], zpt, [('xblk', i2)] + W('Wz', 8))
                P.op('act', lambda zi=zi, zp=zp: nc.scalar.activation(out=zs[zi][:], in_=zp[:, :], func=AF.Silu), reads=[zpt], writes=[('zs', zi)])
                P.op('dve', lambda zi=zi, qs=qs: nc.vector.tensor_tensor(y_[:, qs], y_[:, qs], zs[zi][:], op=ALU.mult),
                     reads=[('zs', zi)] + ally, writes=ally)
            for g in range(8):
                P.op('act', lambda g=g: nc.scalar.activation(out=junk[:], in_=y_[:, g * 256:(g + 1) * 256], func=AF.Square, accum_out=ssq[:, g:g + 1]),
                     reads=ally, writes=['junk', ('ssq', g)])
            allq = [('ssq', g) for g in range(8)]
            P.op('dve', lambda: nc.vector.tensor_scalar(ssq[:, 8:16], ssq[:, 0:8], 1.0 / 256, RMS_EPS, op0=ALU.mult, op1=ALU.add),
                 reads=allq, writes=['ssq8'])
            P.op('act', lambda: nc.scalar.activation(out=ssq[:, 8:16], in_=ssq[:, 8:16], func=AF.Sqrt), reads=['ssq8'], writes=['ssq8'])
            P.op('dve', lambda: nc.vector.reciprocal(ssq[:, 8:16], ssq[:, 8:16]), reads=['ssq8'], writes=['ssq8'])
            for g in range(8):
                gs = slice(g * 256, (g + 1) * 256)
                P.op('dve', lambda g=g, gs=gs: nc.vector.scalar_tensor_tensor(
                    out=yn[:, gs], in0=y_[:, gs], scalar=ssq[:, 8 + g:9 + g], in1=ngb[:, gs], op0=ALU.mult, op1=ALU.mult),
                     reads=ally + ['ssq8', 'ngb'], writes=[('yn', g)])
            for r in range(2):
                for q in range(8):
                    P.op('pe', lambda r=r, q=q: nc.tensor.transpose(self.psb[:, q * 128:(q + 1) * 128],
                                                                    yn[:, (r * 8 + q) * 128:(r * 8 + q + 1) * 128], self.ident_bf[:]),
                         reads=[('yn', g) for g in range(8)], writes=['psb'], inc=(q == 7))
                P.op('act', lambda r=r: nc.scalar.copy(ynT[i2][:, r * 8:(r + 1) * 8, cols],
                                                       self.psb[:].rearrange("p (k n) -> p k n", k=8)),
                     reads=['psb'], writes=[('ynT', i2)])
            if tt == 3:
                P.dma('sp', out=self.ynT_d[:, :, t0:t0 + TB], in_=ynT[i2][:], key=f"st{i2}", reads=[('ynT', i2)], writes=[('ynT_d', b)])

        if SSD_HOIST:
            pre(0)
            for t in range(NT):
                if t + 1 < NT:
                    pre(t + 1)
                grp(t)
                post(t)
        else:
            for t in range(NT):
                pre(t)
                grp(t)
                post(t)
    P.barrier()


def _pass_ssd_c(self, l):
    P, nc = self.P, self.nc
    ps = self.ps
    with ExitStack() as es:
        sb = lambda name, shape, dt: es.enter_context(nc.sbuf_tensor(_uid(name), shape, dt))
        rows = lambda kc: slice(kc * 128, (kc + 1) * 128)
        W = lambda name, n: _toks(name, n)
        WgB = sb("WgB", [128, 8, D], BF16)
        wpb = sb("wpb", [128, 16, D], BF16)
        self.load_w(lambda kc: WgB[:, kc, :], lambda kc: self.w_in[l, rows(kc), C_G + D:C_G + 2 * D], 'WgB', 8)
        self.load_w(lambda kc: wpb[:, kc, :], lambda kc: self.w_proj_b[l, rows(kc), :], 'wpb', 16)
        xblk = [sb(f"xblk{i}", [128, 8, TB], BF16) for i in range(2)]
        ynb = [sb(f"ynb{i}", [128, 16, TB], BF16) for i in range(2)]
        sig = [sb(f"sig{i}", [128, TB], F32) for i in range(2)]
        mT = [sb(f"mT{i}", [128, 8, TB], BF16) for i in range(2)]
        for b in range(NB):
            i2 = b % 2
            t0 = b * TB
            P.dma('sp', out=xblk[i2][:], in_=self.xT_d[:, :, t0:t0 + TB], key=f"ld{2 + i2}", writes=[('xblk', i2)])
            P.dma('sp', out=ynb[i2][:], in_=self.ynT_d[:, :, t0:t0 + TB], key=f"ld{4 + i2}", writes=[('ynb', i2)])
            xb = xblk[i2]
            for dc in range(8):
                dd = slice(dc * 128, (dc + 1) * 128)
                si = dc % 2
                pa, pat = (ps[5], 'ps5') if si == 0 else (ps[3], 'ps3')
                pb, pbt = (ps[6], 'ps6') if si == 0 else (ps[4], 'ps4')
                P.mm(pa[:, :], [(WgB[:, kc, dd], xb[:, kc, :]) for kc in range(8)], pat, [('xblk', i2)] + W('WgB', 8))
                P.op('act', lambda si=si, pa=pa: nc.scalar.activation(out=sig[si][:], in_=pa[:, :], func=AF.Sigmoid),
                     reads=[pat], writes=[('sig', si)])
                P.mm(pb[:, :], [(wpb[:, j, dd], ynb[i2][:, j, :]) for j in range(16)], pbt, [('ynb', i2)] + W('wpb', 16))
                P.op('dve', lambda dc=dc, si=si, pb=pb: nc.vector.tensor_tensor(mT[i2][:, dc, :], pb[:, :], sig[si][:], op=ALU.mult),
                     reads=[pbt, ('sig', si)], writes=[('mT', i2)])
            P.dma('sp', out=self.mB_d[:, :, t0:t0 + TB], in_=mT[i2][:], key=f"st{i2}", reads=[('mT', i2)], writes=[('mB_d', b)])
    P.barrier()


Net.pass_ssd_a = _pass_ssd_a
Net.pass_ssd_b = _pass_ssd_b
Net.pass_ssd_c = _pass_ssd_c


def _layer_norm_tile(self, u, gbt, bbt, out, st, tag):
    P, nc = self.P, self.nc
    junk = self.ln_junk
    P.op('act', lambda: nc.scalar.activation(out=junk[:], in_=u, func=AF.Identity, accum_out=st[:, 0:1]),
         reads=[tag + 'u'], writes=['lnjunk', tag + 's0'])
    P.op('act', lambda: nc.scalar.activation(out=junk[:], in_=u, func=AF.Square, accum_out=st[:, 1:2]),
         reads=[tag + 'u'], writes=['lnjunk', tag + 's1'])
    P.op('dve', lambda: nc.vector.tensor_scalar(st[:, 2:4], st[:, 0:2], 1.0 / D, None, op0=ALU.mult),
         reads=[tag + 's0', tag + 's1'], writes=[tag + 's2'])
    P.op('dve', lambda: nc.vector.tensor_tensor(st[:, 4:5], st[:, 2:3], st[:, 2:3], op=ALU.mult), reads=[tag + 's2'], writes=[tag + 's4'])
    P.op('dve', lambda: nc.vector.tensor_tensor(st[:, 5:6], st[:, 3:4], st[:, 4:5], op=ALU.subtract), reads=[tag + 's2', tag + 's4'], writes=[tag + 's5'])
    P.op('dve', lambda: nc.vector.tensor_scalar(st[:, 5:6], st[:, 5:6], NORM_EPS, None, op0=ALU.add), reads=[tag + 's5'], writes=[tag + 's5'])
    P.op('act', lambda: nc.scalar.activation(out=st[:, 6:7], in_=st[:, 5:6], func=AF.Sqrt), reads=[tag + 's5'], writes=[tag + 's6'])
    P.op('dve', lambda: nc.vector.reciprocal(st[:, 7:8], st[:, 6:7]), reads=[tag + 's6'], writes=[tag + 's7'])
    P.op('dve', lambda: nc.vector.tensor_scalar(out, u, st[:, 2:3], st[:, 7:8], op0=ALU.subtract, op1=ALU.mult),
         reads=[tag + 'u', tag + 's2', tag + 's7'], writes=[tag + 'o'])
    P.op('pool', lambda: nc.gpsimd.tensor_tensor(out, out, gbt[:], op=ALU.mult), reads=[tag + 'g'], writes=[tag + 'o'])
    P.op('pool', lambda: nc.gpsimd.tensor_tensor(out, out, bbt[:], op=ALU.add), reads=[tag + 'g'], writes=[tag + 'o'])


def _pass_ln1(self, l):
    P, nc = self.P, self.nc
    ps = self.ps
    xa_src = self.x_in if l == 0 else self.xa_d
    with ExitStack() as es:
        sb = lambda name, shape, dt: es.enter_context(nc.sbuf_tensor(_uid(name), shape, dt))
        rows = lambda kc: slice(kc * 128, (kc + 1) * 128)
        W = lambda name, n: _toks(name, n)
        wout = sb("wout", [128, 8, D], BF16)
        rw = sb("rw", [128, 8, 16], F32)
        rb = sb("rb", [128, 16], F32)
        gbt = sb("gbt", [128, D], F32)
        bbt = sb("bbt", [128, D], F32)
        self.ln_junk = sb("lnjunk", [128, D], F32)
        self.load_w(lambda kc: wout[:, kc, :], lambda kc: self.w_out[l, rows(kc), :], 'wout', 8)
        for kc in range(8):
            P.dma('sp', out=rw[:, kc, :], in_=self.router_w[rows(kc), :], key="ld0", writes=[('rw', kc)])
        P.dma('sp', out=rb[:], in_=self.router_bias[0, :].partition_broadcast(128), key="ld1", writes=['rb'])
        P.dma('sp', out=gbt[:], in_=self.ln1_g[l, :].partition_broadcast(128), key="ld2", writes=['lg'])
        P.dma('sp', out=bbt[:], in_=self.ln1_b[l, :].partition_broadcast(128), key="ld3", writes=['lg'])
        mblk = [[sb(f"m{n}{i}", [128, 8, TB], BF16) for n in "ABC"] for i in range(2)]
        xa = [sb(f"xa{i}", [128, D], F32) for i in range(2)]
        u = [sb(f"u{i}", [128, D], F32) for i in range(2)]
        x1 = [sb(f"x1{i}", [128, D], F32) for i in range(2)]
        st = sb("st", [128, 8], F32)
        x1Tf = sb("x1Tf", [128, 8, 128], F32)
        stg = [sb(f"stg{i}", [128, 8, TB], BF16) for i in range(2)]
        r = sb("r", [128, 12, 16], F32)
        gts = [sb(f"gts{i}", [16, TB], F32) for i in range(2)]
        srcs = [self.mA_d, self.mB_d, self.mC_d]
        st2 = [st, sb("stB", [128, 8], F32)]

        def stA(t):
            b, tt = divmod(t, 4)
            i2, ti = b % 2, t % 2
            t0 = b * TB
            cols = slice(tt * 128, (tt + 1) * 128)
            tag = f'L1{ti}'
            if tt == 0:
                for n in range(3):
                    P.dma('sp', out=mblk[i2][n][:], in_=srcs[n][:, :, t0:t0 + TB], key=f"ld{4 + 3 * i2 + n}", writes=[('mblk', i2, n)])
            P.dma('sp', out=xa[ti][:], in_=xa_src[t * 128:(t + 1) * 128, :], key=f"ld{10 + ti}", writes=[('xa', ti)])
            for half in range(2):
                pb, pt = (ps[5], 'ps5') if half == 0 else (ps[6], 'ps6')
                P.mm(pb[:, :], [(mblk[i2][n][:, dc, cols], wout[:, dc, half * 512:(half + 1) * 512]) for n in range(3) for dc in range(8)],
                     pt, [('mblk', i2, n) for n in range(3)] + W('wout', 8))
                P.op('dve', lambda half=half, pb=pb: nc.vector.scalar_tensor_tensor(
                    out=u[ti][:, half * 512:(half + 1) * 512], in0=xa[ti][:, half * 512:(half + 1) * 512], scalar=DN_ALPHA, in1=pb[:, :],
                    op0=ALU.mult, op1=ALU.add), reads=[('xa', ti), pt], writes=[tag + 'u'])

        def stB(t):
            ti = t % 2
            tag = f'L1{ti}'
            self.layer_norm_tile(u[ti][:], gbt, bbt, x1[ti][:], st2[ti], tag)
            P._record(('e', 'pool', P.cnt['pool']), ['lg'], [])
            P.dma('sp', out=self.x1_d[t * 128:(t + 1) * 128, :], in_=x1[ti][:], key=f"st{ti}", reads=[tag + 'o'], writes=[('x1_d', t)])

        def stC(t):
            b, tt = divmod(t, 4)
            i2, ti = b % 2, t % 2
            t0 = b * TB
            cols = slice(tt * 128, (tt + 1) * 128)
            tag = f'L1{ti}'
            for kc in range(8):
                pb = ps[0] if kc < 4 else ps[1]
                P.op('pe', lambda kc=kc, pb=pb: nc.tensor.matmul(pb[:, (kc % 4) * 128:(kc % 4 + 1) * 128],
                                                                 x1[ti][:, kc * 128:(kc + 1) * 128], self.ident_f[:],
                                                                 start=True, stop=True),
                     reads=[tag + 'o'], writes=[('TP', kc // 4)], inc=(kc % 4 == 3))
            for hf in range(2):
                P.op('act', lambda hf=hf: nc.scalar.copy(x1Tf[:, hf * 4:(hf + 1) * 4, :], ps[hf][:].rearrange("p (k n) -> p k n", k=4)),
                     reads=[('TP', hf)], writes=['x1Tf'])
                P.op('dve', lambda hf=hf: nc.vector.tensor_copy(stg[i2][:, hf * 4:(hf + 1) * 4, cols], x1Tf[:, hf * 4:(hf + 1) * 4, :]),
                     reads=['x1Tf'], writes=[('stg', i2)])
            P.mm(ps[2][:, 0:16], [(x1Tf[:, kc, :], rw[:, kc, :]) for kc in range(8)], 'ps2', ['x1Tf'] + W('rw', 8))
            R = lambda i: r[:, i, :]
            R4 = lambda i: r[:, i, :].rearrange("p (g e) -> p g e", g=4)
            P.op('act', lambda: nc.scalar.activation(out=R(0), in_=ps[2][:, 0:16], func=AF.Sigmoid), reads=['ps2'], writes=['r0'])
            P.op('dve', lambda: nc.vector.tensor_tensor(R(1), R(0), rb[:], op=ALU.add), reads=['r0', 'rb'], writes=['r1'])
            pairs = [(0, 1), (0, 2), (0, 3), (1, 2), (1, 3), (2, 3)]
            for pi_, (a_, b_) in enumerate(pairs):
                P.op('dve', lambda pi_=pi_, a_=a_, b_=b_: nc.vector.tensor_tensor(r[:, 2 + pi_ // 4, (pi_ % 4) * 4:(pi_ % 4) * 4 + 4],
                                                                                R4(1)[:, :, a_], R4(1)[:, :, b_], op=ALU.add),
                     reads=['r1'], writes=['r2'])
            P.op('dve', lambda: nc.vector.tensor_tensor(r[:, 4, 0:4], r[:, 2, 0:4], r[:, 2, 4:8], op=ALU.max), reads=['r2'], writes=['r4'])
            P.op('dve', lambda: nc.vector.tensor_tensor(r[:, 4, 4:8], r[:, 2, 8:12], r[:, 2, 12:16], op=ALU.max), reads=['r2'], writes=['r4'])
            P.op('dve', lambda: nc.vector.tensor_tensor(r[:, 4, 8:12], r[:, 3, 0:4], r[:, 3, 4:8], op=ALU.max), reads=['r2'], writes=['r4'])
            P.op('dve', lambda: nc.vector.tensor_tensor(r[:, 4, 0:4], r[:, 4, 0:4], r[:, 4, 4:8], op=ALU.max), reads=['r4'], writes=['r4'])
            P.op('dve', lambda: nc.vector.tensor_tensor(r[:, 4, 0:4], r[:, 4, 0:4], r[:, 4, 8:12], op=ALU.max), reads=['r4'], writes=['r4'])
            P.op('dve', lambda: nc.vector.tensor_reduce(out=r[:, 5, 0:1], in_=r[:, 4, 0:4], axis=AX.X, op=ALU.max), reads=['r4'], writes=['r5'])
            P.op('dve', lambda: nc.vector.tensor_scalar(r[:, 5, 4:8], r[:, 4, 0:4], r[:, 5, 0:1], None, op0=ALU.is_equal), reads=['r4', 'r5'], writes=['r5m'])
            P.op('dve', lambda: nc.vector.tensor_scalar(r[:, 5, 8:12], r[:, 5, 4:8], 1.0, 1e30, op0=ALU.subtract, op1=ALU.mult), reads=['r5m'], writes=['r5p'])
            P.op('dve', lambda: nc.vector.tensor_tensor(R4(6), R4(1), r[:, 5, 4:8].unsqueeze(2).to_broadcast([128, 4, 4]), op=ALU.mult),
                 reads=['r1', 'r5m'], writes=['r6'])
            P.op('dve', lambda: nc.vector.tensor_tensor(R4(6), R4(6), r[:, 5, 8:12].unsqueeze(2).to_broadcast([128, 4, 4]), op=ALU.add),
                 reads=['r6', 'r5p'], writes=['r6'])
            P.op('dve', lambda: nc.vector.tensor_reduce(out=r[:, 7, 0:1], in_=R(6), axis=AX.X, op=ALU.max), reads=['r6'], writes=['r7'])
            P.op('dve', lambda: nc.vector.tensor_scalar(R(8), R(6), r[:, 7, 0:1], None, op0=ALU.is_equal), reads=['r6', 'r7'], writes=['r8'])
            P.op('dve', lambda: nc.vector.scalar_tensor_tensor(out=R(9), in0=R(8), scalar=-1e30, in1=R(6), op0=ALU.mult, op1=ALU.add),
                 reads=['r8', 'r6'], writes=['r9'])
            P.op('dve', lambda: nc.vector.tensor_reduce(out=r[:, 7, 1:2], in_=R(9), axis=AX.X, op=ALU.max), reads=['r9'], writes=['r7b'])
            P.op('dve', lambda: nc.vector.tensor_scalar(R(10), R(9), r[:, 7, 1:2], None, op0=ALU.is_equal), reads=['r9', 'r7b'], writes=['r10'])
            P.op('dve', lambda: nc.vector.tensor_tensor(R(10), R(10), R(8), op=ALU.add), reads=['r10', 'r8'], writes=['r10'])
            P.op('dve', lambda: nc.vector.tensor_tensor(R(11), R(10), R(0), op=ALU.mult), reads=['r10', 'r0'], writes=['r11'])
            P.op('dve', lambda: nc.vector.tensor_reduce(out=r[:, 7, 2:3], in_=R(11), axis=AX.X, op=ALU.add), reads=['r11'], writes=['r7c'])
            P.op('dve', lambda: nc.vector.reciprocal(r[:, 7, 3:4], r[:, 7, 2:3]), reads=['r7c'], writes=['r7d'])
            P.op('dve', lambda: nc.vector.tensor_scalar(R(11), R(11), r[:, 7, 3:4], None, op0=ALU.mult), reads=['r11', 'r7d'], writes=['r11'])
            P.op('pe', lambda: nc.tensor.matmul(ps[3][0:16, 0:128], R(11), self.ident_f[:], start=True, stop=True), reads=['r11'], writes=['ps3'])
            P.op('act', lambda: nc.scalar.copy(gts[i2][:, cols], ps[3][0:16, 0:128]), reads=['ps3'], writes=[('gts', i2)])
            if tt == 3:
                P.dma('sp', out=self.x1T_d[:, :, t0:t0 + TB], in_=stg[i2][:], key=f"st{2 + i2}", reads=[('stg', i2)], writes=[('x1T_d', b)])
                P.dma('sp', out=self.gT_d[:, t0:t0 + TB], in_=gts[i2][:], key=f"st{4 + i2}", reads=[('gts', i2)], writes=[('gT_d', b)])

        for step in range(NT + 2):
            if step < NT:
                stA(step)
            if 0 <= step - 1 < NT:
                stB(step - 1)
            if 0 <= step - 2 < NT:
                stC(step - 2)
    P.barrier()


def _pass_moe(self, l, last):
    P, nc = self.P, self.nc
    ps = self.ps
    BB = 1024
    with ExitStack() as es:
        sb = lambda name, shape, dt: es.enter_context(nc.sbuf_tensor(_uid(name), shape, dt))
        rows = lambda kc: slice(kc * 128, (kc + 1) * 128)
        W = lambda name, n: _toks(name, n)
        gbt = sb("gbt", [128, D], F32)
        bbt = sb("bbt", [128, D], F32)
        self.ln_junk = sb("lnjunk", [128, D], F32)
        E16 = sb("E16", [16, 16, 128], F32)
        P.dma('sp', out=gbt[:], in_=self.ln2_g[l, :].partition_broadcast(128), key="ld2", writes=['lg'])
        P.dma('sp', out=bbt[:], in_=self.ln2_b[l, :].partition_broadcast(128), key="ld3", writes=['lg'])
        P.op('pool', lambda: nc.gpsimd.memset(E16[:], 0.0), writes=['E16'])
        P.op('pool', lambda: nc.gpsimd.affine_select(out=E16[:], in_=E16[:], pattern=[[-1, 16], [0, 128]], compare_op=ALU.not_equal,
                                                     fill=1.0, base=0, channel_multiplier=1), writes=['E16'])
        x1T = sb("x1T", [128, 8, BB], BF16)
        gT = sb("gT", [16, BB], F32)
        acc = sb("acc", [128, 8, D], F32)
        w1 = [sb(f"w1{i}", [128, 8, 512], BF16) for i in range(2)]
        w3 = [sb(f"w3{i}", [128, 8, 512], BF16) for i in range(2)]
        w2 = [sb(f"w2{i}", [128, 4, D], BF16) for i in range(2)]
        gb = [sb(f"gb{i}", [128, 512], F32) for i in range(2)]
        s1 = [sb(f"s1{i}", [128, 512], F32) for i in range(2)]
        tq = [sb(f"tq{i}", [128, 512], F32) for i in range(2)]
        hT = [sb(f"hT{i}", [128, 4, 512], BF16) for i in range(2)]
        x1t = [sb(f"x1t{i}", [128, D], F32) for i in range(2)]
        u = [sb(f"u{i}", [128, D], F32) for i in range(2)]
        x2 = [sb(f"x2{i}", [128, D], F32) for i in range(2)]
        x2b = [sb(f"x2b{i}", [128, D], BF16) for i in range(2)]
        st = sb("st", [128, 8], F32)
        st2 = [st, sb("stB", [128, 8], F32)]
        stg = sb("stg", [128, 8, BB], BF16)
        dst = self.y_out if last else self.xa_d
        for bb in range(T // BB):
            t0 = bb * BB
            P.dma('sp', out=x1T[:], in_=self.x1T_d[:, :, t0:t0 + BB], key="ld4", writes=['x1T'])
            P.dma('sp', out=gT[:], in_=self.gT_d[:, t0:t0 + BB], key="ld5", writes=['gT'])
            units = [(e, sub) for e in range(16) for sub in range(BB // 512)]

            def stage_F(ui):
                e, sub = units[ui]
                ei = (ebase + e) % 2
                hi = (ubase + ui) % 2
                if sub == 0:
                    self.load_w(lambda kc: w1[ei][:, kc, :], lambda kc: self.exp_w1[l, e, rows(kc), :], ('w1', ei), 8)
                    self.load_w(lambda kc: w3[ei][:, kc, :], lambda kc: self.exp_w3[l, e, rows(kc), :], ('w3', ei), 8)
                    self.load_w(lambda kc: w2[ei][:, kc, :], lambda kc: self.exp_w2[l, e, rows(kc), :], ('w2', ei), 4)
                sc = slice(sub * 512, (sub + 1) * 512)
                P.mm(ps[4][:, :], [(E16[:, e, :], gT[:, sc])], 'ps4', ['E16', 'gT'])
                P.op('act', lambda: nc.scalar.copy(gb[hi][:], ps[4][:, :]), reads=['ps4'], writes=[('gb', hi)])
                for fc in range(4):
                    fi = fc % 2
                    fs = slice(fc * 128, (fc + 1) * 128)
                    pa, pat = (ps[0], 'ps0') if fi == 0 else (ps[2], 'ps2')
                    pb, pbt = (ps[1], 'ps1') if fi == 0 else (ps[3], 'ps3')
                    P.mm(pa[:, :], [(w1[ei][:, kc, fs], x1T[:, kc, sc]) for kc in range(8)], pat, ['x1T'] + W(('w1', ei), 8))
                    P.mm(pb[:, :], [(w3[ei][:, kc, fs], x1T[:, kc, sc]) for kc in range(8)], pbt, ['x1T'] + W(('w3', ei), 8))
                    P.op('act', lambda fi=fi, pa=pa: nc.scalar.activation(out=s1[fi][:], in_=pa[:, :], func=AF.Silu), reads=[pat], writes=[('s1', fi)])
                    P.op('dve', lambda fi=fi, pb=pb: nc.vector.tensor_tensor(tq[fi][:], pb[:, :], gb[hi][:], op=ALU.mult),
                         reads=[pbt, ('gb', hi)], writes=[('tq', fi)])
                    P.op('dve', lambda fi=fi, fc=fc: nc.vector.tensor_tensor(hT[hi][:, fc, :], tq[fi][:], s1[fi][:], op=ALU.mult),
                         reads=[('tq', fi), ('s1', fi)], writes=[('hT', hi, fc)])

            def stage_S(ui):
                e, sub = units[ui]
                ei = (ebase + e) % 2
                hi = (ubase + ui) % 2
                for tt in range(4):
                    tl = sub * 4 + tt
                    cols = slice(tt * 128, (tt + 1) * 128)
                    for half in range(2):
                        pb, pt = (ps[5], 'ps5') if half == 0 else (ps[6], 'ps6')
                        hs = slice(half * 512, (half + 1) * 512)
                        P.mm(pb[:, :], [(hT[hi][:, fc, cols], w2[ei][:, fc, hs]) for fc in range(4)], pt,
                             [('hT', hi, fc) for fc in range(4)] + W(('w2', ei), 4))
                        if e == 0:
                            P.op('dve', lambda tl=tl, hs=hs, pb=pb: nc.vector.tensor_copy(acc[:, tl, hs], pb[:, :]),
                                 reads=[pt], writes=[('acc', tl, half)])
                        else:
                            P.op('dve', lambda tl=tl, hs=hs, pb=pb: nc.vector.tensor_tensor(acc[:, tl, hs], acc[:, tl, hs], pb[:, :], op=ALU.add),
                                 reads=[pt], writes=[('acc', tl, half)])

            ebase = bb * 16
            ubase = bb * len(units)
            stage_F(0)
            for ui in range(len(units)):
                if ui + 1 < len(units):
                    stage_F(ui + 1)
                stage_S(ui)
            ntl = BB // 128

            def eA(tl):
                t = bb * ntl + tl
                ti = t % 2
                tag = f'L2{ti}'
                P.dma('sp', out=x1t[ti][:], in_=self.x1_d[t * 128:(t + 1) * 128, :], key=f"ld{6 + ti}", writes=[('x1t', ti)])
                P.op('dve', lambda: nc.vector.scalar_tensor_tensor(
                    out=u[ti][:], in0=x1t[ti][:], scalar=DN_ALPHA, in1=acc[:, tl, :], op0=ALU.mult, op1=ALU.add),
                     reads=[('x1t', ti), ('acc', tl, 0), ('acc', tl, 1)], writes=[tag + 'u'])

            def eB(tl):
                t = bb * ntl + tl
                ti = t % 2
                tag = f'L2{ti}'
                self.layer_norm_tile(u[ti][:], gbt, bbt, x2[ti][:], st2[ti], tag)
                P._record(('e', 'pool', P.cnt['pool']), ['lg'], [])
                P.dma('sp', out=dst[t * 128:(t + 1) * 128, :], in_=x2[ti][:], key=f"st{ti}", reads=[tag + 'o'], writes=[('dst', t)])

            def eC(tl):
                t = bb * ntl + tl
                ti = t % 2
                tag = f'L2{ti}'
                if last:
                    return
                P.op('act', lambda: nc.scalar.copy(x2b[ti][:], x2[ti][:]), reads=[tag + 'o'], writes=[('x2b', ti)])
                for kc in range(8):
                    P.op('pe', lambda kc=kc: nc.tensor.transpose(self.psb[:, kc * 128:(kc + 1) * 128],
                                                                 x2b[ti][:, kc * 128:(kc + 1) * 128], self.ident_bf[:]),
                         reads=[('x2b', ti)], writes=['psb'], inc=(kc == 7))
                P.op('act', lambda: nc.scalar.copy(stg[:, :, tl * 128:(tl + 1) * 128],
                                                   self.psb[:].rearrange("p (k n) -> p k n", k=8)),
                     reads=['psb'], writes=['stg'])

            for step in range(ntl + 2):
                if step < ntl:
                    eA(step)
                if 0 <= step - 1 < ntl:
                    eB(step - 1)
                if 0 <= step - 2 < ntl:
                    eC(step - 2)
            if not last:
                P.dma('sp', out=self.xT_d[:, :, t0:t0 + BB], in_=stg[:], key="st2", reads=['stg'], writes=[('xT_d', bb)])
    P.barrier()


Net.layer_norm_tile = _layer_norm_tile
Net.pass_ln1 = _pass_ln1
Net.pass_moe = _pass_moe


def build(n_layers=DEPTH, stages=("mla", "ssd", "mem", "moe"), dbg=()):
    net = Net(n_layers)
    net.declare()
    P = net.P
    net.setup_globals()
    net.prologue()
    for l in range(n_layers):
        if "mla" in stages:
            net.pass_mla(l)
        if "mem" in stages:
            net.pass_mem(l)
        if "ssd" in stages or "ssda" in stages:
            net.pass_ssd_a(l)
        if "ssd" in stages or "ssdb" in stages:
            net.pass_ssd_b(l)
        if "ssd" in stages or "ssdc" in stages:
            net.pass_ssd_c(l)
        if "moe" in stages or "ln1" in stages:
            net.pass_ln1(l)
        if "moe" in stages:
            net.pass_moe(l, last=(l == n_layers - 1))
    for name in dbg:
        src = getattr(net, name)
        dst = net.dbg("dbg_" + name, src.shape, src.dtype)
        P.dma('sp', out=dst, in_=src, key="st0", reads=[], writes=[('dbg', name)])
    P.barrier()
    print("instructions:", P.ninst, "sems:", P.nsem, {e: P.cnt[e] for e in P.cnt}, "dmas:", getattr(P, 'ndma', 0), "descs~", getattr(P, 'ndesc', 0))
    return net


def make_inputs(inputs, b):
    m = {}
    for k, v in inputs.items():
        v = np.asarray(v)
        if k in ("x", "mem"):
            m[k] = np.ascontiguousarray(v[b])
        elif k == "positions":
            m[k] = np.ascontiguousarray(v[b:b + 1]).astype(np.int32)
        elif k == "router_bias":
            m[k] = np.ascontiguousarray(v.reshape(1, 16))
        else:
            m[k] = np.ascontiguousarray(v)
    inv = (10000.0 ** (-np.arange(0, 64, 2, dtype=np.float32) / 64)).astype(np.float32)
    m["inv_freq"] = np.concatenate([inv, inv]).reshape(64, 1).astype(np.float32)
    return m


def kernel(**inputs):
    net = build()
    in_maps = [make_inputs(inputs, b) for b in range(8)]
    res = run_bass_kernel_spmd(net.nc, in_maps, core_ids=list(range(8)))
    return np.stack([np.asarray(res.results[b]["y"]) for b in range(8)], axis=0).astype(np.float32)
```

```python
import math
from contextlib import ExitStack

import numpy as np
import concourse.bass as bass
import concourse.mybir as mybir
from concourse.bass_utils import run_bass_kernel_spmd

F32 = mybir.dt.float32
BF16 = mybir.dt.bfloat16
I32 = mybir.dt.int32
AF = mybir.ActivationFunctionType
ALU = mybir.AluOpType
AX = mybir.AxisListType

D = 1024
T = 4096
DEPTH = 4
NT = T // 128
TB = 512
NB = T // TB
H = 8
QR = 384
KVR = 256
NOPE = 128
ROPE = 64
VD = 128
N_IN = 10976
C_DQ = 0
C_DKV = 384
C_KR = 640
C_Z = 704
C_XBC = 2752
C_DT = 6848
C_QM = 6880
C_G = 7904
DN_ALPHA = (2 * DEPTH) ** 0.25
NORM_EPS = 1e-5
RMS_EPS = 1e-6
SCALE_A = (NOPE + ROPE) ** -0.5

EPOCH = 30000
import os
SSD_PIPE = int(os.environ.get('SSD_PIPE', '1'))
SSD_HOIST = int(os.environ.get('SSD_HOIST', '1'))
_UID = [0]


def _uid(name):
    _UID[0] += 1
    return f"{name}_{_UID[0]}"


def _runs(ap):
    dims = [(int(st), int(n)) for st, n in ap.ap]
    total = 1
    for st, n in dims:
        total *= n
    run = 1
    exp = 1
    for st, n in reversed(dims[1:] if len(dims) > 1 else dims):
        if st == exp:
            run *= n
            exp *= n
        else:
            break
    return max(total // max(run, 1), 1)


_BANK_OF = {'ST': lambda i: i, 'SEG': lambda i: i, 'TP': lambda i: i, 'oacc': lambda i: 2 + i, 'CB': lambda i: 2 + i,
            'YS': lambda i: 2 + i, 'YI': lambda i: 4 if i == 0 else 6, 'SU': lambda i: 4 if i == 0 else 6}
_BANK_NAMES = {'ps0': 0, 'ps1': 1, 'ps2': 2, 'ps3': 3, 'ps4': 4, 'ps5': 5, 'ps6': 6, 'psb': 7, 'osum': 4}


def _banks(tokens):
    out = []
    for t in tokens:
        if isinstance(t, str):
            b = _BANK_NAMES.get(t)
        elif isinstance(t, tuple) and t and t[0] in _BANK_OF and len(t) == 2:
            b = _BANK_OF[t[0]](t[1])
        else:
            b = None
        if b is not None:
            out.append(('bank', b))
    return out


class Prog:
    def __init__(self):
        self.nc = bass.Bass("TRN2", target_bir_lowering=False)
        nc = self.nc
        self.es = ExitStack()
        self.eng = dict(pe=nc.tensor, act=nc.scalar, dve=nc.vector, pool=nc.gpsimd, sp=nc.sync)
        self.cnt = {e: 0 for e in self.eng}
        self.sems = {e: [] for e in self.eng}
        self.waited = {e: {} for e in self.eng}
        self.lastw = {}
        self.readers = {}
        self.dsem = {}
        self.dcnt = {}
        self.nsem = 0
        self.ninst = 0

    def _new_sem(self, name):
        self.nsem += 1
        return self.es.enter_context(self.nc.semaphore(name))

    def _esem(self, e, n):
        ep = (n - 1) // EPOCH
        while len(self.sems[e]) <= ep:
            self.sems[e].append(self._new_sem(f"s_{e}_{len(self.sems[e])}"))
        return self.sems[e][ep], (n - 1) % EPOCH + 1

    def _wait(self, c, ev):
        if ev is None:
            return
        kind, p, n = ev
        if kind == 'e':
            if p == c:
                if c == 'pe' or n > self.cnt[c] or n < self.cnt[c] - 1:
                    return
            if self.waited[c].get(p, 0) >= n:
                return
            sem, val = self._esem(p, n)
            self.eng[c].wait_ge(sem, val)
            self.waited[c][p] = n
        else:
            k = ('d', p)
            if self.waited[c].get(k, 0) >= n:
                return
            self.eng[c].wait_ge(self.dsem[p], 16 * n)
            self.waited[c][k] = n

    def _deps(self, reads, writes):
        deps = []
        for r in reads:
            ev = self.lastw.get(r)
            if ev is not None:
                deps.append(ev)
        for w in writes:
            ev = self.lastw.get(w)
            if ev is not None:
                deps.append(ev)
            rd = self.readers.get(w)
            if rd:
                deps.extend(rd.values())
        return deps

    def _record(self, ev, reads, writes):
        key = (ev[0], ev[1])
        for r in reads:
            self.readers.setdefault(r, {})[key] = ev
        for w in writes:
            self.lastw[w] = ev
            self.readers[w] = {}

    def op(self, e, fn, reads=(), writes=(), inc=True):
        bk = _banks(reads) + _banks(writes)
        if bk:
            writes = list(writes) + bk
        for ev in self._deps(reads, writes):
            self._wait(e, ev)
        inst = fn()
        self.ninst += 1
        if inc:
            self.cnt[e] += 1
            n = self.cnt[e]
            sem, _ = self._esem(e, n)
            inst.then_inc(sem, 1)
            ev = ('e', e, n)
        else:
            ev = ('e', e, self.cnt[e] + 1)
        self._record(ev, reads, writes)
        return ev

    def dma(self, q, out, in_, key, reads=(), writes=()):
        if key not in self.dsem:
            self.dsem[key] = self._new_sem(f"d_{key}")
            self.dcnt[key] = 0
        kres = ('dmakey', key)
        for ev in self._deps(reads, list(writes) + [kres]):
            self._wait(q, ev)
        inst = self.eng[q].dma_start(out=out, in_=in_)
        self.ninst += 1
        self.ndma = getattr(self, 'ndma', 0) + 1
        self.ndesc = getattr(self, 'ndesc', 0) + max(_runs(out), _runs(in_))
        self.dcnt[key] += 1
        inst.then_inc(self.dsem[key], 16)
        ev = ('d', key, self.dcnt[key])
        self._record(ev, reads, list(writes) + [kres])
        return ev

    def barrier(self):
        for c in self.eng:
            for p in self.eng:
                if p != c and self.cnt[p] > 0:
                    self._wait(c, ('e', p, self.cnt[p]))
            for k, m in self.dcnt.items():
                if m > 0:
                    self._wait(c, ('d', k, m))
        self.lastw.clear()
        self.readers.clear()

    def mm(self, out, pairs, wres, reads):
        nc = self.nc
        n = len(pairs)
        for i, (l, r) in enumerate(pairs):
            self.op('pe', lambda l=l, r=r, i=i: nc.tensor.matmul(out, l, r, start=(i == 0), stop=(i == n - 1)),
                    reads=reads if i == 0 else (), writes=[wres], inc=(i == n - 1))
        ev = ('e', 'pe', self.cnt['pe'])
        self._record(ev, reads, ())


class Net:
    def __init__(self, n_layers=DEPTH, debug=None):
        self.P = Prog()
        self.nc = self.P.nc
        self.n_layers = n_layers
        self.debug = debug or {}
        self.dbg_out = {}

    def declare(self):
        nc = self.nc
        L = DEPTH
        def inp(name, shape, dt=F32):
            return nc.dram_tensor(name, list(shape), dt, kind="ExternalInput").ap()
        self.x_in = inp("x", [T, D])
        self.mem_in = inp("mem", [256, D])
        self.pos_in = inp("positions", [1, T], I32)
        self.w_in = inp("w_in", [L, D, N_IN])
        self.q_norm = inp("q_norm", [L, QR])
        self.w_uq = inp("w_uq", [L, QR, H * 192])
        self.kv_norm = inp("kv_norm", [L, KVR])
        self.w_ukv = inp("w_ukv", [L, KVR, H, 256])
        self.w_proj_a = inp("w_proj_a", [L, D, D])
        self.conv_w = inp("conv_w", [L, 4, 4096])
        self.conv_b = inp("conv_b", [L, 4096])
        self.dt_bias = inp("dt_bias", [L, 32])
        self.a_log = inp("a_log", [L, 32])
        self.d_skip = inp("d_skip", [L, 32])
        self.ssm_norm = inp("ssm_norm", [L, 2048])
        self.w_proj_b = inp("w_proj_b", [L, 2048, D])
        self.w_mem_kv = inp("w_mem_kv", [L, D, 2048])
        self.w_proj_c = inp("w_proj_c", [L, D, D])
        self.w_out = inp("w_out", [L, D, D])
        self.ln1_g = inp("ln1_g", [L, D])
        self.ln1_b = inp("ln1_b", [L, D])
        self.router_w = inp("router_w", [D, 16])
        self.router_bias = inp("router_bias", [1, 16])
        self.exp_w1 = inp("exp_w1", [L, 16, D, 512])
        self.exp_w3 = inp("exp_w3", [L, 16, D, 512])
        self.exp_w2 = inp("exp_w2", [L, 16, 512, D])
        self.ln2_g = inp("ln2_g", [L, D])
        self.ln2_b = inp("ln2_b", [L, D])
        self.inv_freq = inp("inv_freq", [64, 1])
        self.y_out = nc.dram_tensor("y", [T, D], F32, kind="ExternalOutput").ap()

        def scr(name, shape, dt):
            return nc.dram_tensor(name, list(shape), dt, kind="Internal").ap()
        self.xT_d = scr("xT_d", [128, 8, T], BF16)
        self.x1T_d = scr("x1T_d", [128, 8, T], BF16)
        self.xa_d = scr("xa_d", [T, D], F32)
        self.x1_d = scr("x1_d", [T, D], F32)
        self.mA_d = scr("mA_d", [128, 8, T], BF16)
        self.mB_d = scr("mB_d", [128, 8, T], BF16)
        self.mC_d = scr("mC_d", [128, 8, T], BF16)
        self.xsB_d = scr("xsB_d", [T, 3072], BF16)
        self.BCT_d = scr("BCT_d", [128, 16, T], BF16)
        self.ynT_d = scr("ynT_d", [128, 16, T], BF16)
        self.gT_d = scr("gT_d", [16, T], F32)
        self.cos_d = scr("cos_d", [64, T], F32)
        self.sin_d = scr("sin_d", [64, T], F32)

    def dbg(self, name, shape, dt=F32):
        ap = self.nc.dram_tensor(name, list(shape), dt, kind="ExternalOutput").ap()
        self.dbg_out[name] = ap
        return ap


def _setup_globals(self):
    P, nc = self.P, self.nc
    es = P.es
    sb = lambda name, shape, dt: es.enter_context(nc.sbuf_tensor(_uid(name), shape, dt))
    self.ps = [es.enter_context(nc.psum_tensor(f"ps{i}", [128, 512], F32)) for i in range(7)]
    self.psb = es.enter_context(nc.psum_tensor("psb", [128, 1024], BF16))
    self.ident_bf = sb("ident_bf", [128, 128], BF16)
    self.ident_f = sb("ident_f", [128, 128], F32)
    self.ones_bf = sb("ones_bf", [128, 128], BF16)
    self.ones_f = sb("ones_f", [128, 128], F32)
    self.mask_le = sb("mask_le", [128, 128], BF16)
    self.tri_le_f = sb("tri_le_f", [128, 128], F32)
    self.tri_gt_f = sb("tri_gt_f", [128, 128], F32)
    g = nc.gpsimd
    for tl, val in ((self.ident_bf, 0.0), (self.ident_f, 0.0)):
        P.op('pool', lambda tl=tl: g.memset(tl[:], 0.0), writes=[('const', tl.name)])
        P.op('pool', lambda tl=tl: g.affine_select(out=tl[:], in_=tl[:], pattern=[[-1, 128]],
                                                   compare_op=ALU.not_equal, fill=1.0, base=0,
                                                   channel_multiplier=1), writes=[('const', tl.name)])
    P.op('pool', lambda: g.memset(self.ones_bf[:], 1.0), writes=[('const', 'ones_bf')])
    P.op('pool', lambda: g.memset(self.ones_f[:], 1.0), writes=[('const', 'ones_f')])
    for tl in (self.mask_le, self.tri_le_f):
        P.op('pool', lambda tl=tl: g.memset(tl[:], 1.0), writes=[('const', tl.name)])
        P.op('pool', lambda tl=tl: g.affine_select(out=tl[:], in_=tl[:], pattern=[[1, 128]],
                                                   compare_op=ALU.is_ge, fill=0.0, base=0,
                                                   channel_multiplier=-1), writes=[('const', tl.name)])
    P.op('pool', lambda: g.memset(self.tri_gt_f[:], 1.0), writes=[('const', 'tri_gt_f')])
    P.op('pool', lambda: g.affine_select(out=self.tri_gt_f[:], in_=self.tri_gt_f[:], pattern=[[-1, 128]],
                                         compare_op=ALU.is_gt, fill=0.0, base=0,
                                         channel_multiplier=1), writes=[('const', 'tri_gt_f')])
    self.wkey = 0
    P.barrier()


def _load_w(self, dst, src, tok, nk, q='pool'):
    P = self.P
    for kc in range(nk):
        key = f"w{self.wkey % 8}"
        self.wkey += 1
        P.dma(q, out=dst(kc), in_=src(kc), key=key, writes=[(tok, kc)])


def _toks(tok, nk):
    return [(tok, kc) for kc in range(nk)]


Net.setup_globals = _setup_globals
Net.load_w = _load_w


def _prologue(self):
    P, nc = self.P, self.nc
    with ExitStack() as es:
        sb = lambda name, shape, dt: es.enter_context(nc.sbuf_tensor(_uid(name), shape, dt))
        posi = sb("posi", [64, T], I32)
        ang = sb("ang", [64, T], F32)
        red = sb("red", [64, T], F32)
        tab = sb("tab", [64, T], F32)
        invf = sb("invf", [64, 1], F32)
        negpi = sb("negpi", [64, 1], F32)
        P.dma('sp', out=posi[:], in_=self.pos_in[0, :].partition_broadcast(64), key="ld0", writes=['posi'])
        P.dma('sp', out=invf[:], in_=self.inv_freq[:, :], key="ld1", writes=['invf'])
        P.op('dve', lambda: nc.vector.memset(negpi[:], -math.pi), writes=['negpi'])
        P.op('dve', lambda: nc.vector.tensor_copy(ang[:], posi[:]), reads=['posi'], writes=['ang'])
        P.op('dve', lambda: nc.vector.tensor_scalar(ang[:], ang[:], invf[:, 0:1], None, op0=ALU.mult),
             reads=['invf'], writes=['ang'])
        ki = sb("ki", [64, T], I32)
        kf = sb("kf", [64, T], F32)
        C1 = 6.28125
        C2 = 2 * math.pi - C1
        P.op('dve', lambda: nc.vector.tensor_scalar(red[:], ang[:], 1.0 / (2 * math.pi), None, op0=ALU.mult),
             reads=['ang'], writes=['red'])
        P.op('dve', lambda: nc.vector.tensor_copy(ki[:], red[:]), reads=['red'], writes=['ki'])
        P.op('dve', lambda: nc.vector.tensor_copy(kf[:], ki[:]), reads=['ki'], writes=['kf'])
        P.op('dve', lambda: nc.vector.scalar_tensor_tensor(out=red[:], in0=kf[:], scalar=-C1, in1=ang[:],
                                                           op0=ALU.mult, op1=ALU.add), reads=['kf', 'ang'], writes=['red'])
        P.op('dve', lambda: nc.vector.scalar_tensor_tensor(out=red[:], in0=kf[:], scalar=-C2, in1=red[:],
                                                           op0=ALU.mult, op1=ALU.add), reads=['kf', 'red'], writes=['red'])

        def wrap_sin(shift):
            P.op('dve', lambda: nc.vector.tensor_scalar(ang[:], red[:], shift, None, op0=ALU.add),
                 reads=['red'], writes=['ang'])
            P.op('dve', lambda: nc.vector.tensor_scalar(kf[:], ang[:], math.pi, 2 * math.pi, op0=ALU.is_gt, op1=ALU.mult),
                 reads=['ang'], writes=['kf'])
            P.op('dve', lambda: nc.vector.tensor_tensor(ang[:], ang[:], kf[:], op=ALU.subtract),
                 reads=['ang', 'kf'], writes=['ang'])
            P.op('dve', lambda: nc.vector.tensor_scalar(ang[:], ang[:], -math.pi, math.pi, op0=ALU.max, op1=ALU.min),
                 reads=['ang'], writes=['ang'])
            P.op('act', lambda: nc.scalar.activation(out=tab[:], in_=ang[:], func=AF.Sin),
                 reads=['ang'], writes=['tab'])
        wrap_sin(0.0)
        P.op('dve', lambda: nc.vector.tensor_scalar(tab[0:32, :], tab[0:32, :], -1.0, None, op0=ALU.mult),
             reads=['tab'], writes=['tab'])
        P.dma('sp', out=self.sin_d[:, :], in_=tab[:], key="st0", reads=['tab'], writes=['sin_d'])
        wrap_sin(0.5 * math.pi)
        P.dma('sp', out=self.cos_d[:, :], in_=tab[:], key="st1", reads=['tab'], writes=['cos_d'])
        xin = [sb(f"xin{i}", [128, D], F32) for i in range(2)]
        xbf = [sb(f"xbf{i}", [128, D], BF16) for i in range(2)]
        stg = [sb(f"stg{i}", [128, 8, TB], BF16) for i in range(2)]
        for t in range(NT):
            i = t % 2
            b, tt = divmod(t, 4)
            P.dma('sp', out=xin[i][:], in_=self.x_in[t * 128:(t + 1) * 128, :], key=f"ld{2 + i}", writes=[('xin', i)])
            P.op('dve', lambda i=i: nc.vector.tensor_copy(xbf[i][:], xin[i][:]), reads=[('xin', i)], writes=[('xbf', i)])
            for kc in range(8):
                P.op('pe', lambda i=i, kc=kc: nc.tensor.transpose(self.psb[:, kc * 128:(kc + 1) * 128],
                                                                   xbf[i][:, kc * 128:(kc + 1) * 128], self.ident_bf[:]),
                     reads=[('xbf', i)], writes=['psb'], inc=(kc == 7))
            P.op('act', lambda b=b, tt=tt: nc.scalar.copy(stg[b % 2][:, :, tt * 128:(tt + 1) * 128],
                                                         self.psb[:].rearrange("p (k n) -> p k n", k=8)),
                 reads=['psb'], writes=[('stg', b % 2)])
            if tt == 3:
                P.dma('sp', out=self.xT_d[:, :, b * TB:(b + 1) * TB], in_=stg[b % 2][:], key=f"st{2 + b % 2}",
                      reads=[('stg', b % 2)], writes=[('xT_d', b)])
    P.barrier()


Net.prologue = _prologue


def _pass_mla(self, l):
    P, nc = self.P, self.nc
    ps = self.ps
    with ExitStack() as es:
        sb = lambda name, shape, dt: es.enter_context(nc.sbuf_tensor(_uid(name), shape, dt))
        Wmla = sb("Wmla", [128, 8, 640], BF16)
        Wkr = sb("Wkr", [128, 8, 128], BF16)
        WgA = sb("WgA", [128, 8, D], BF16)
        wuq = sb("wuq", [128, 3, H * 192], BF16)
        wuqsw = sb("wuqsw", [128, 3, H, 64], BF16)
        wukv = sb("wukv", [128, 2, H, 256], BF16)
        wukT = sb("wukT", [128, H, 256], BF16)
        wpa = sb("wpa", [128, 8, D], BF16)
        gq = sb("gq", [128, QR], F32)
        gkv = sb("gkv", [128, KVR], F32)
        w_in = self.w_in
        rows = lambda kc: slice(kc * 128, (kc + 1) * 128)
        self.load_w(lambda kc: Wmla[:, kc, :], lambda kc: w_in[l, rows(kc), 0:640], 'Wmla', 8)
        self.load_w(lambda kc: Wkr[:, kc, 0:64], lambda kc: w_in[l, rows(kc), C_KR:C_KR + 64], 'Wkr', 8)
        self.load_w(lambda kc: wuq[:, kc, :], lambda kc: self.w_uq[l, rows(kc), :], 'wuq', 3)
        self.load_w(lambda kc: wukv[:, kc, :, :], lambda kc: self.w_ukv[l, rows(kc), :, :], 'wukv', 2)
        P.dma('sp', out=gq[:], in_=self.q_norm[l, :].partition_broadcast(128), key="ld0", writes=['gq'])
        P.dma('sp', out=gkv[:], in_=self.kv_norm[l, :].partition_broadcast(128), key="ld1", writes=['gkv'])
        self.load_w(lambda kc: wpa[:, kc, :], lambda kc: self.w_proj_a[l, rows(kc), :], 'wpa', 8)
        self.load_w(lambda kc: WgA[:, kc, :], lambda kc: w_in[l, rows(kc), C_G:C_G + D], 'WgA', 8)
        for kc in range(8):
            P.op('dve', lambda kc=kc: nc.vector.tensor_copy(Wkr[:, kc, 64:96], Wkr[:, kc, 32:64]),
                 reads=[('Wkr', kc)], writes=[('Wkrs', kc)])
            P.op('dve', lambda kc=kc: nc.vector.tensor_copy(Wkr[:, kc, 96:128], Wkr[:, kc, 0:32]),
                 reads=[('Wkr', kc)], writes=[('Wkrs', kc)])
        for kc in range(3):
            v = wuq[:, kc, :].rearrange("p (h c) -> p h c", h=H)
            P.op('dve', lambda kc=kc, v=v: nc.vector.tensor_copy(wuqsw[:, kc, :, 0:32], v[:, :, 160:192]),
                 reads=[('wuq', kc)], writes=[('wuqsw', kc)])
            P.op('dve', lambda kc=kc, v=v: nc.vector.tensor_copy(wuqsw[:, kc, :, 32:64], v[:, :, 128:160]),
                 reads=[('wuq', kc)], writes=[('wuqsw', kc)])
        for h in range(H):
            for cc in range(2):
                P.op('pe', lambda h=h, cc=cc: nc.tensor.transpose(self.psb[:, cc * 128:(cc + 1) * 128],
                                                                   wukv[:, cc, h, 0:128], self.ident_bf[:]),
                     reads=[('wukv', cc)], writes=['psb'], inc=(cc == 1))
            P.op('act', lambda h=h: nc.scalar.copy(wukT[:, h, :], self.psb[:, 0:256]), reads=['psb'], writes=[('wukT', h)])

        ckvT = sb("ckvT", [128, 2, T], BF16)
        krT = sb("krT", [64, T], BF16)
        ckvTM = sb("ckvTM", [128, NT, KVR], BF16)
        xblk = [sb(f"xblk{i}", [128, 8, TB], BF16) for i in range(2)]
        cos1 = sb("cosb", [64, TB], F32)
        sin1 = sb("sinb", [64, TB], F32)
        cosb = [cos1, cos1]
        sinb = [sin1, sin1]
        cqTM2 = [sb(f"cqTM{i}", [128, QR], BF16) for i in range(2)]
        cqT = sb("cqT", [128, 3, TB], BF16)
        junk = sb("junk", [128, 512], F32)
        ssA = [sb(f"ss{i}", [128, 8], F32) for i in range(2)]
        qn = [sb(f"qn{i}", [128, TB], BF16) for i in range(2)]
        qlat = sb("qlat", [128, H, 2, TB], BF16)
        qrope = sb("qrope", [64, H, TB], BF16)
        rtmp = sb("rtmp", [64, TB], F32)
        rtmp2 = sb("rtmp2", [64, TB], F32)
        PT = [sb(f"PT{i}", [128, TB], BF16) for i in range(3)]
        rs = sb("rs", [128, TB], F32)
        olat = [sb(f"olat{i}", [128, 2, TB], BF16) for i in range(2)]
        oT2 = [sb(f"oT{i}", [128, H, TB], BF16) for i in range(2)]
        sig2 = [sb(f"sig{i}", [128, TB], F32) for i in range(2)]
        mT1 = sb("mT", [128, 8, TB], BF16)
        mT2 = [mT1, mT1]
        W = lambda name, n: _toks(name, n)
        rot = [0]

        def nb():
            i = rot[0] % 7
            rot[0] += 1
            return ps[i], f'ps{i}'


        def outproj(b, i2, xb, t0):
            oT, mT = oT2[i2], mT2[i2]
            for dc in range(8):
                dd = slice(dc * 128, (dc + 1) * 128)
                sg = sig2[dc % 2]
                P.mm(ps[5][:, :], [(WgA[:, kc, dd], xb[:, kc, :]) for kc in range(8)], 'ps5',
                     [('xblk', i2)] + W('WgA', 8))
                P.op('act', lambda: nc.scalar.activation(out=sg[:], in_=ps[5][:, :], func=AF.Sigmoid),
                     reads=['ps5'], writes=[('sig', dc % 2)])
                P.mm(ps[6][:, :], [(wpa[:, hh, dd], oT[:, hh, :]) for hh in range(H)], 'ps6',
                     [('oT', i2, hh) for hh in range(H)] + W('wpa', 8))
                P.op('dve', lambda: nc.vector.tensor_tensor(mT[:, dc, :], ps[6][:, :], sg[:], op=ALU.mult),
                     reads=['ps6', ('sig', dc % 2)], writes=[('mT', dc)])
                yield
            P.dma('sp', out=self.mA_d[:, :, t0:t0 + TB], in_=mT[:], key=f"st{i2}",
                  reads=[('mT', dc) for dc in range(8)], writes=[('mA_d', b)])
            yield

        bg = None
        for b in range(NB):
            i2 = b % 2
            t0 = b * TB
            P.dma('sp', out=xblk[i2][:], in_=self.xT_d[:, :, t0:t0 + TB], key=f"ld{2 + i2}", writes=[('xblk', i2)])
            P.dma('sp', out=cosb[i2][:], in_=self.cos_d[:, t0:t0 + TB], key=f"ld{4 + i2}", writes=['cosb'])
            P.dma('sp', out=sinb[i2][:], in_=self.sin_d[:, t0:t0 + TB], key=f"ld{6 + i2}", writes=['sinb'])
            xb = xblk[i2]
            tmbank = {}

            def tmA(tt):
                cols = slice(tt * 128, (tt + 1) * 128)
                pA, tA = nb()
                pB, tB = nb()
                tmbank[tt] = (pA, tA, pB, tB)
                P.mm(pA[:, 0:512], [(xb[:, kc, cols], Wmla[:, kc, 0:512]) for kc in range(8)], tA,
                     [('xblk', i2)] + W('Wmla', 8))
                P.mm(pB[:, 0:128], [(xb[:, kc, cols], Wmla[:, kc, 512:640]) for kc in range(8)], tB,
                     [('xblk', i2)] + W('Wmla', 8))

            def tmB(tt):
                t = b * 4 + tt
                p = tt % 2
                pA, tA, pB, tB = tmbank[tt]
                s_ = ssA[p]
                cq = cqTM2[p]
                K = lambda k: ('ss', p, k)
                P.op('act', lambda: nc.scalar.activation(out=junk[:, 0:384], in_=pA[:, 0:384], func=AF.Square,
                                                         accum_out=s_[:, 0:1]), reads=[tA], writes=['junk', K(0)])
                P.op('act', lambda: nc.scalar.activation(out=junk[:, 384:512], in_=pA[:, 384:512], func=AF.Square,
                                                         accum_out=s_[:, 1:2]), reads=[tA], writes=['junk', K(1)])
                P.op('act', lambda: nc.scalar.activation(out=junk[:, 0:128], in_=pB[:, 0:128], func=AF.Square,
                                                         accum_out=s_[:, 2:3]), reads=[tB], writes=['junk', K(2)])
                P.op('dve', lambda: nc.vector.tensor_scalar(s_[:, 4:5], s_[:, 0:1], 1.0 / QR, RMS_EPS, op0=ALU.mult, op1=ALU.add),
                     reads=[K(0)], writes=[K(4)])
                P.op('dve', lambda: nc.vector.tensor_tensor(s_[:, 5:6], s_[:, 1:2], s_[:, 2:3], op=ALU.add),
                     reads=[K(1), K(2)], writes=[K(5)])
                P.op('dve', lambda: nc.vector.tensor_scalar(s_[:, 5:6], s_[:, 5:6], 1.0 / KVR, RMS_EPS, op0=ALU.mult, op1=ALU.add),
                     reads=[K(5)], writes=[K(5)])
                P.op('act', lambda: nc.scalar.activation(out=s_[:, 6:8], in_=s_[:, 4:6], func=AF.Sqrt), reads=[K(4), K(5)], writes=[K(6)])
                P.op('dve', lambda: nc.vector.reciprocal(s_[:, 4:6], s_[:, 6:8]), reads=[K(6)], writes=[K(4), K(5)])
                P.op('dve', lambda: nc.vector.scalar_tensor_tensor(out=cq[:], in0=pA[:, 0:384], scalar=s_[:, 4:5],
                                                                   in1=gq[:], op0=ALU.mult, op1=ALU.mult),
                     reads=[tA, K(4), 'gq'], writes=[('cqTM', p)])
                P.op('dve', lambda: nc.vector.scalar_tensor_tensor(out=ckvTM[:, t, 0:128], in0=pA[:, 384:512],
                                                                   scalar=s_[:, 5:6], in1=gkv[:, 0:128],
                                                                   op0=ALU.mult, op1=ALU.mult),
                     reads=[tA, K(5), 'gkv'], writes=[('ckvTM', t)])
                P.op('dve', lambda: nc.vector.scalar_tensor_tensor(out=ckvTM[:, t, 128:256], in0=pB[:, 0:128],
                                                                   scalar=s_[:, 5:6], in1=gkv[:, 128:256],
                                                                   op0=ALU.mult, op1=ALU.mult),
                     reads=[tB, K(5), 'gkv'], writes=[('ckvTM', t)])

            def tmC(tt):
                t = b * 4 + tt
                p = tt % 2
                cols = slice(tt * 128, (tt + 1) * 128)
                cq = cqTM2[p]
                for j in range(3):
                    P.op('pe', lambda j=j: nc.tensor.transpose(self.psb[:, j * 128:(j + 1) * 128],
                                                               cq[:, j * 128:(j + 1) * 128], self.ident_bf[:]),
                         reads=[('cqTM', p)], writes=['psb'], inc=False)
                for j in range(2):
                    P.op('pe', lambda j=j: nc.tensor.transpose(self.psb[:, (3 + j) * 128:(4 + j) * 128],
                                                               ckvTM[:, t, j * 128:(j + 1) * 128], self.ident_bf[:]),
                         reads=[('ckvTM', t)], writes=['psb'], inc=(j == 1))
                P.op('act', lambda: nc.scalar.copy(cqT[:, :, cols], self.psb[:, 0:384].rearrange("p (k n) -> p k n", k=3)),
                     reads=['psb'], writes=['cqT'])
                P.op('act', lambda: nc.scalar.copy(ckvT[:, :, t * 128:(t + 1) * 128],
                                                   self.psb[:, 384:640].rearrange("p (k n) -> p k n", k=2)),
                     reads=['psb'], writes=[('ckvT', t)])

            for step in range(6):
                if step < 4:
                    tmA(step)
                if 0 <= step - 1 < 4:
                    tmB(step - 1)
                if 0 <= step - 2 < 4:
                    tmC(step - 2)
            pA, tA = nb()
            pB, tB = nb()
            P.mm(pA[0:64, :], [(Wkr[:, kc, 0:64], xb[:, kc, :]) for kc in range(8)], tA,
                 [('xblk', i2)] + W('Wkr', 8))
            P.mm(pB[0:64, :], [(Wkr[:, kc, 64:128], xb[:, kc, :]) for kc in range(8)], tB,
                 [('xblk', i2)] + W('Wkrs', 8))

            def rope(psA, psB, outap, rA, rB, wtok):
                P.op('dve', lambda: nc.vector.tensor_tensor(rtmp[:], psB, sinb[i2][:], op=ALU.mult),
                     reads=[rB, 'sinb'], writes=['rtmp'])
                P.op('dve', lambda: nc.vector.tensor_tensor(rtmp2[:], psA, cosb[i2][:], op=ALU.mult),
                     reads=[rA, 'cosb'], writes=['rtmp2'])
                P.op('dve', lambda: nc.vector.tensor_tensor(outap, rtmp[:], rtmp2[:], op=ALU.add),
                     reads=['rtmp', 'rtmp2'], writes=[wtok])
            rope(pA[0:64, :], pB[0:64, :], krT[:, t0:t0 + TB], tA, tB, ('krT', b))

            for h in range(H):
                c0 = h * 192
                pq, tq_ = nb()
                P.mm(pq[:, :], [(wuq[:, kc, c0:c0 + 128], cqT[:, kc, :]) for kc in range(3)], tq_,
                     ['cqT'] + W('wuq', 3))
                P.op('act', lambda pq=pq, h=h: nc.scalar.copy(qn[h % 2][:], pq[:, :]), reads=[tq_], writes=[('qn', h % 2)])
                pa_, ta_ = nb()
                pb_, tb_ = nb()
                P.mm(pa_[0:64, :], [(wuq[:, kc, c0 + 128:c0 + 192], cqT[:, kc, :]) for kc in range(3)], ta_,
                     ['cqT'] + W('wuq', 3))
                P.mm(pb_[0:64, :], [(wuqsw[:, kc, h, :], cqT[:, kc, :]) for kc in range(3)], tb_,
                     ['cqT'] + W('wuqsw', 3))
                for cc in range(2):
                    pl, tl_ = nb()
                    P.mm(pl[:, :], [(wukT[:, h, cc * 128:(cc + 1) * 128], qn[h % 2][:])], tl_, [('qn', h % 2), ('wukT', h)])
                    P.op('dve' if cc == 0 else 'act',
                         (lambda h=h, cc=cc, pl=pl: nc.vector.tensor_copy(qlat[:, h, cc, :], pl[:, :])) if cc == 0 else
                         (lambda h=h, cc=cc, pl=pl: nc.scalar.copy(qlat[:, h, cc, :], pl[:, :])),
                         reads=[tl_], writes=[('qlat', h)])
                rope(pa_[0:64, :], pb_[0:64, :], qrope[:, h, :], ta_, tb_, ('qrope', h))

            nkt = 4 * b + 4
            items = [(h, kt) for h in range(H) for kt in range(nkt)]

            def stage_S(i):
                h, kt = items[i]
                d = kt - 4 * b
                q0 = max(d, 0) * 128
                qs = slice(q0, TB)
                si, pi = i % 2, i % 3
                st = ps[si]
                kk = slice(kt * 128, (kt + 1) * 128)
                P.mm(st[:, qs], [(ckvT[:, 0, kk], qlat[:, h, 0, qs]), (ckvT[:, 1, kk], qlat[:, h, 1, qs]),
                                 (krT[:, kk], qrope[:, h, qs])], ('ST', si),
                     [('ckvT', kt), ('krT', kt // 4), ('qlat', h), ('qrope', h)])
                P.op('act', lambda: nc.scalar.activation(out=PT[pi][:, qs], in_=st[:, qs], func=AF.Exp, scale=SCALE_A),
                     reads=[('ST', si)], writes=[('PT', pi)])
                if d >= 0:
                    dq = slice(q0, q0 + 128)
                    P.op('pool', lambda: nc.gpsimd.tensor_tensor(PT[pi][:, dq], PT[pi][:, dq], self.mask_le[:], op=ALU.mult),
                         reads=[('PT', pi)], writes=[('PT', pi)])

            def stage_V(i):
                h, kt = items[i]
                d = kt - 4 * b
                q0 = max(d, 0) * 128
                qs = slice(q0, TB)
                pi = i % 3
                first, last = (kt == 0), (kt == nkt - 1)
                for cc in range(2):
                    P.op('pe', lambda cc=cc: nc.tensor.matmul(
                        ps[2 + cc][:, qs], ckvTM[:, kt, cc * 128:(cc + 1) * 128], PT[pi][:, qs], start=first, stop=last),
                         reads=[('PT', pi), ('ckvTM', kt)], writes=[('oacc', cc)], inc=False)
                P.op('pe', lambda: nc.tensor.matmul(ps[4][:, qs], self.ones_bf[:], PT[pi][:, qs], start=first, stop=last),
                     reads=[('PT', pi)], writes=['osum'], inc=True)
                P._record(('e', 'pe', P.cnt['pe']), [('PT', pi), ('ckvTM', kt)], [('oacc', 0), ('oacc', 1)])
                if last:
                    ol = olat[h % 2]
                    P.op('dve', lambda: nc.vector.reciprocal(rs[:], ps[4][:, :]), reads=['osum'], writes=['rs'])
                    for cc in range(2):
                        P.op('dve', lambda cc=cc: nc.vector.tensor_tensor(ol[:, cc, :], ps[2 + cc][:, :], rs[:], op=ALU.mult),
                             reads=[('oacc', cc), 'rs'], writes=[('olat', h % 2)])

            def proj_o(h):
                ol = olat[h % 2]
                P.mm(ps[5][:, :], [(wukv[:, cc, h, 128:256], ol[:, cc, :]) for cc in range(2)], 'ps5',
                     [('olat', h % 2)] + W('wukv', 2))
                P.op('act', lambda: nc.scalar.copy(oT2[i2][:, h, :], ps[5][:, :]), reads=['ps5'], writes=[('oT', i2, h)])

            pending = []
            if os.environ.get('MLA_ABL'):
                items = items[:int(os.environ['MLA_ABL'])]
            every = max(len(items) // 9, 1)
            stage_S(0)
            for i in range(len(items)):
                if i + 1 < len(items):
                    stage_S(i + 1)
                stage_V(i)
                h, kt = items[i]
                if kt == nkt - 1:
                    pending.append((i + 2, h))
                while pending and pending[0][0] <= i:
                    proj_o(pending.pop(0)[1])
                if bg is not None and i % every == every - 1:
                    next(bg, None)
            for _, h in pending:
                proj_o(h)
            if bg is not None:
                for _ in bg:
                    pass
            bg = outproj(b, i2, xb, t0)
        for _ in bg:
            pass

    P.barrier()


Net.pass_mla = _pass_mla


def _pass_mem(self, l):
    P, nc = self.P, self.nc
    ps = self.ps
    SC = 256 ** -0.5
    with ExitStack() as es:
        sb = lambda name, shape, dt: es.enter_context(nc.sbuf_tensor(_uid(name), shape, dt))
        Wqm = sb("Wqm", [128, 8, D], BF16)
        WgC = sb("WgC", [128, 8, D], BF16)
        wpc = sb("wpc", [128, 8, D], BF16)
        KT = sb("KT", [128, 4, 2, 256], BF16)
        V = sb("V", [128, 2, D], BF16)
        rows = lambda kc: slice(kc * 128, (kc + 1) * 128)
        W = lambda name, n: _toks(name, n)
        self.load_w(lambda kc: Wqm[:, kc, :], lambda kc: self.w_in[l, rows(kc), C_QM:C_QM + D], 'Wqm', 8)
        self.load_w(lambda kc: WgC[:, kc, :], lambda kc: self.w_in[l, rows(kc), C_G + 2 * D:C_G + 3 * D], 'WgC', 8)
        self.load_w(lambda kc: wpc[:, kc, :], lambda kc: self.w_proj_c[l, rows(kc), :], 'wpc', 8)
        with ExitStack() as es2:
            sb2 = lambda name, shape, dt: es2.enter_context(nc.sbuf_tensor(_uid(name), shape, dt))
            wmkv = sb2("wmkv", [128, 8, 2048], BF16)
            memT = sb2("memT", [128, 8, 256], BF16)
            mtile = sb2("mtile", [128, D], BF16)
            self.load_w(lambda kc: wmkv[:, kc, :], lambda kc: self.w_mem_kv[l, rows(kc), :], 'wmkv', 8)
            for mt in range(2):
                P.dma('pool', out=mtile[:], in_=self.mem_in[mt * 128:(mt + 1) * 128, :], key="ld0", writes=['mtile'])
                for kc in range(8):
                    P.op('pe', lambda kc=kc: nc.tensor.transpose(self.psb[:, kc * 128:(kc + 1) * 128],
                                                                 mtile[:, kc * 128:(kc + 1) * 128], self.ident_bf[:]),
                         reads=['mtile'], writes=['psb'], inc=(kc == 7))
                P.op('act', lambda mt=mt: nc.scalar.copy(memT[:, :, mt * 128:(mt + 1) * 128],
                                                         self.psb[:].rearrange("p (k n) -> p k n", k=8)),
                     reads=['psb'], writes=['memT'])
            for h in range(4):
                for dc in range(2):
                    c0 = h * 256 + dc * 128
                    P.mm(ps[5][:, 0:256], [(wmkv[:, kc, c0:c0 + 128], memT[:, kc, :]) for kc in range(8)], 'ps5',
                         ['memT'] + W('wmkv', 8))
                    P.op('act', lambda h=h, dc=dc: nc.scalar.copy(KT[:, h, dc, :], ps[5][:, 0:256]), reads=['ps5'], writes=['KT'])
            for mt in range(2):
                for half in range(2):
                    c0 = 1024 + half * 512
                    P.mm(ps[6][:, :], [(memT[:, kc, mt * 128:(mt + 1) * 128], wmkv[:, kc, c0:c0 + 512]) for kc in range(8)],
                         'ps6', ['memT'] + W('wmkv', 8))
                    P.op('dve', lambda mt=mt, half=half: nc.vector.tensor_copy(V[:, mt, half * 512:(half + 1) * 512], ps[6][:, :]),
                         reads=['ps6'], writes=['V'])
            P.barrier()
        xblk = [sb(f"xblk{i}", [128, 8, TB], BF16) for i in range(2)]
        qm = sb("qm", [128, 2, TB], BF16)
        PT = [sb(f"PT{i}", [128, 2, TB], BF16) for i in range(2)]
        rs = sb("rs", [128, TB], F32)
        ocT = sb("ocT", [128, 8, TB], BF16)
        sig = sb("sig", [128, TB], F32)
        mT = sb("mT", [128, 8, TB], BF16)
        qm2 = [qm, sb("qmB", [128, 2, TB], BF16)]
        sig2 = [sig, sb("sigB", [128, TB], F32)]
        rot = [0]

        def nb():
            i = rot[0] % 7
            rot[0] += 1
            return ps[i], f'ps{i}'

        for b in range(NB):
            i2 = b % 2
            t0 = b * TB
            P.dma('sp', out=xblk[i2][:], in_=self.xT_d[:, :, t0:t0 + TB], key=f"ld{2 + i2}", writes=[('xblk', i2)])
            xb = xblk[i2]

            def stage1(h):
                pi = h % 2
                qq = qm2[pi]
                for dc in range(2):
                    c0 = h * 256 + dc * 128
                    pb, pt = (ps[5], 'ps5') if dc == 0 else (ps[6], 'ps6')
                    P.mm(pb[:, :], [(Wqm[:, kc, c0:c0 + 128], xb[:, kc, :]) for kc in range(8)], pt,
                         [('xblk', i2)] + W('Wqm', 8))
                    if dc == 0:
                        P.op('act', lambda pb=pb: nc.scalar.copy(qq[:, 0, :], pb[:, :]), reads=[pt], writes=[('qm', pi, 0)])
                    else:
                        P.op('dve', lambda pb=pb: nc.vector.tensor_copy(qq[:, 1, :], pb[:, :]), reads=[pt], writes=[('qm', pi, 1)])
                for mt in range(2):
                    P.mm(ps[mt][:, :], [(KT[:, h, dc, mt * 128:(mt + 1) * 128], qq[:, dc, :]) for dc in range(2)], ('ST', mt),
                         ['KT', ('qm', pi, 0), ('qm', pi, 1)])
                    P.op('act', lambda mt=mt: nc.scalar.activation(out=PT[pi][:, mt, :], in_=ps[mt][:, :], func=AF.Exp, scale=SC),
                         reads=[('ST', mt)], writes=[('PT', pi, mt)])

            def stage2(h):
                pi = h % 2
                for vc in range(2):
                    c0 = h * 256 + vc * 128
                    P.mm(ps[2 + vc][:, :], [(V[:, mt, c0:c0 + 128], PT[pi][:, mt, :]) for mt in range(2)], ('oacc', vc),
                         ['V', ('PT', pi, 0), ('PT', pi, 1)])
                P.mm(ps[4][:, :], [(self.ones_bf[:], PT[pi][:, mt, :]) for mt in range(2)], 'osum',
                     [('PT', pi, 0), ('PT', pi, 1)])
                P.op('dve', lambda: nc.vector.reciprocal(rs[:], ps[4][:, :]), reads=['osum'], writes=['rs'])
                for vc in range(2):
                    P.op('dve', lambda vc=vc: nc.vector.tensor_tensor(ocT[:, h * 2 + vc, :], ps[2 + vc][:, :], rs[:], op=ALU.mult),
                         reads=[('oacc', vc), 'rs'], writes=[('ocT', h * 2 + vc)])

            stage1(0)
            for h in range(4):
                if h + 1 < 4:
                    stage1(h + 1)
                stage2(h)
            for dc in range(8):
                dd = slice(dc * 128, (dc + 1) * 128)
                sg = sig2[dc % 2]
                pA, tA = nb()
                pB, tB = nb()
                P.mm(pA[:, :], [(WgC[:, kc, dd], xb[:, kc, :]) for kc in range(8)], tA,
                     [('xblk', i2)] + W('WgC', 8))
                P.op('act', lambda pA=pA, sg=sg: nc.scalar.activation(out=sg[:], in_=pA[:, :], func=AF.Sigmoid),
                     reads=[tA], writes=[('sig', dc % 2)])
                P.mm(pB[:, :], [(wpc[:, j, dd], ocT[:, j, :]) for j in range(8)], tB,
                     [('ocT', j) for j in range(8)] + W('wpc', 8))
                P.op('dve', lambda dc=dc, pB=pB, sg=sg: nc.vector.tensor_tensor(mT[:, dc, :], pB[:, :], sg[:], op=ALU.mult),
                     reads=[tB, ('sig', dc % 2)], writes=[('mT', dc)])
            P.dma('sp', out=self.mC_d[:, :, t0:t0 + TB], in_=mT[:], key="st0",
                  reads=[('mT', dc) for dc in range(8)], writes=[('mC_d', b)])
    P.barrier()


Net.pass_mem = _pass_mem


def _pass_ssd_a(self, l):
    P, nc = self.P, self.nc
    ps = self.ps
    with ExitStack() as es:
        sb = lambda name, shape, dt: es.enter_context(nc.sbuf_tensor(_uid(name), shape, dt))
        Wx = sb("Wx", [128, 8, 4096], BF16)
        cw5 = sb("cw5", [5, 4096], F32)
        cwb = sb("cwb", [128, 32, 5], F32)
        rows = lambda kc: slice(kc * 128, (kc + 1) * 128)
        W = lambda name, n: _toks(name, n)
        self.load_w(lambda kc: Wx[:, kc, :], lambda kc: self.w_in[l, rows(kc), C_XBC:C_XBC + 4096], 'Wx', 8)
        P.dma('sp', out=cw5[0:4, :], in_=self.conv_w[l, :, :], key="ld0", writes=['cw5a'])
        P.dma('sp', out=cw5[4:5, :], in_=self.conv_b[l:l + 1, :], key="ld1", writes=['cw5b'])
        for cc in range(32):
            P.op('pe', lambda cc=cc: nc.tensor.transpose(ps[5][:, cc * 5:(cc + 1) * 5], cw5[0:5, cc * 128:(cc + 1) * 128],
                                                         self.ident_f[0:5, 0:5]),
                 reads=['cw5a', 'cw5b'], writes=['ps5'], inc=(cc == 31))
        P.op('act', lambda: nc.scalar.copy(cwb[:].rearrange("p c k -> p (c k)"), ps[5][:, 0:160]), reads=['ps5'], writes=['cwb'])
        xblk = [sb(f"xblk{i}", [128, 8, TB], BF16) for i in range(2)]
        pre = [sb(f"pre{i}", [128, TB + 3], BF16) for i in range(3)]
        carry = sb("carry", [128, 32, 3], BF16)
        xbcT = sb("xbcT", [128, 32, TB], BF16)
        tmst = [sb(f"tmst{i}", [128, 3072], BF16) for i in range(2)]
        dg = sb("dg", [128, 32, 4, 128], BF16)
        for cc in range(32):
            for k in range(4):
                P.op('dve', lambda cc=cc, k=k: nc.vector.tensor_scalar(dg[:, cc, k, :], self.ident_bf[:], cwb[:, cc, k:k + 1], None, op0=ALU.mult),
                     reads=['cwb'], writes=[('dg', cc)])
        P.op('dve', lambda: nc.vector.memset(carry[:], 0.0), writes=[('carry', cc) for cc in range(32)])
        n = 0
        rot = [0]

        def nb():
            i = rot[0] % 7
            rot[0] += 1
            return ps[i], f'ps{i}'

        for b in range(NB):
            i2 = b % 2
            t0 = b * TB
            P.dma('sp', out=xblk[i2][:], in_=self.xT_d[:, :, t0:t0 + TB], key=f"ld{2 + i2}", writes=[('xblk', i2)])
            xb = xblk[i2]
            for cc in range(32):
                j = n % 3
                n += 1
                pb, pt = nb()
                pr = pre[j]
                P.mm(pb[:, :], [(Wx[:, kc, cc * 128:(cc + 1) * 128], xb[:, kc, :]) for kc in range(8)], pt,
                     [('xblk', i2)] + W('Wx', 8))
                P.op('act', lambda pr=pr, cc=cc: nc.scalar.copy(pr[:, 0:3], carry[:, cc, :]),
                     reads=[('carry', cc)], writes=[('pre', j)])
                P.op('act', lambda pr=pr, pb=pb: nc.scalar.copy(pr[:, 3:TB + 3], pb[:, :]), reads=[pt], writes=[('pre', j)])
                P.op('act', lambda pr=pr, cc=cc: nc.scalar.copy(carry[:, cc, :], pr[:, TB:TB + 3]),
                     reads=[('pre', j)], writes=[('carry', cc)])
                pc, pct = nb()
                P.mm(pc[:, :], [(dg[:, cc, k, :], pr[:, k:k + TB]) for k in range(4)], pct, [('pre', j), ('dg', cc)])
                P.op('act', lambda pc=pc, cc=cc: nc.scalar.activation(out=xbcT[:, cc, :], in_=pc[:, :], func=AF.Silu, bias=cwb[:, cc, 4:5]),
                     reads=[pct, 'cwb'], writes=[('xbcT', cc)])
            P.dma('sp', out=self.BCT_d[:, :, t0:t0 + TB], in_=xbcT[:, 16:32, :], key="st0",
                  reads=[('xbcT', cc) for cc in range(16, 32)], writes=[('BCT_d', b)])
            for tt in range(4):
                t = b * 4 + tt
                ti = t % 2
                for r in range(3):
                    for q in range(8):
                        cc = r * 8 + q
                        P.op('pe', lambda cc=cc, q=q, tt=tt: nc.tensor.transpose(self.psb[:, q * 128:(q + 1) * 128],
                                                                                 xbcT[:, cc, tt * 128:(tt + 1) * 128], self.ident_bf[:]),
                             reads=[('xbcT', cc)], writes=['psb'], inc=(q == 7))
                    P.op('dve', lambda ti=ti, r=r: nc.vector.tensor_copy(tmst[ti][:, r * 1024:(r + 1) * 1024], self.psb[:, :]),
                         reads=['psb'], writes=[('tmst', ti)])
                P.dma('sp', out=self.xsB_d[t * 128:(t + 1) * 128, :], in_=tmst[ti][:], key=f"st{1 + ti}",
                      reads=[('tmst', ti)], writes=[('xsB_d', t)])
    P.barrier()


def _pass_ssd_b(self, l):
    P, nc = self.P, self.nc
    ps = self.ps
    with ExitStack() as es:
        sb = lambda name, shape, dt: es.enter_context(nc.sbuf_tensor(_uid(name), shape, dt))
        rows = lambda kc: slice(kc * 128, (kc + 1) * 128)
        W = lambda name, n: _toks(name, n)
        Wz = sb("Wz", [128, 8, 2048], BF16)
        Wdt = sb("Wdt", [128, 8, 32], BF16)
        dtb = sb("dtb", [128, 32], F32)
        abc = sb("abc", [128, 32], F32)
        dsk = sb("dsk", [128, 32], F32)
        ngb = sb("ngb", [128, 2048], F32)
        self.load_w(lambda kc: Wz[:, kc, :], lambda kc: self.w_in[l, rows(kc), C_Z:C_Z + 2048], 'Wz', 8)
        self.load_w(lambda kc: Wdt[:, kc, :], lambda kc: self.w_in[l, rows(kc), C_DT:C_DT + 32], 'Wdt', 8)
        P.dma('sp', out=dtb[:], in_=self.dt_bias[l, :].partition_broadcast(128), key="ld0", writes=['dtb'])
        P.dma('sp', out=abc[:], in_=self.a_log[l, :].partition_broadcast(128), key="ld1", writes=['abc'])
        P.dma('sp', out=dsk[:], in_=self.d_skip[l, :].partition_broadcast(128), key="ld2", writes=['dsk'])
        P.dma('sp', out=ngb[:], in_=self.ssm_norm[l, :].partition_broadcast(128), key="ld3", writes=['ngb'])
        P.op('act', lambda: nc.scalar.activation(out=abc[:], in_=abc[:], func=AF.Exp), reads=['abc'], writes=['abc'])
        P.op('dve', lambda: nc.vector.tensor_scalar(abc[:], abc[:], -1.0, None, op0=ALU.mult), reads=['abc'], writes=['abc'])
        ST = sb("ST", [128, 8, 256], F32)
        STb = sb("STb", [128, 8, 256], BF16)
        P.op('dve', lambda: nc.vector.memset(ST[:], 0.0), writes=[('STATE', g) for g in range(8)])
        P.op('pool', lambda: nc.gpsimd.memset(STb[:], 0.0), writes=[('STb', g) for g in range(8)])
        xblk = [sb(f"xblk{i}", [128, 8, TB], BF16) for i in range(2)]
        bct = [sb(f"bct{i}", [128, 16, TB], BF16) for i in range(2)]
        xsB = [sb(f"xsB{i}", [128, 3072], BF16) for i in range(2)]
        sm = sb("sm", [128, 8, 32], F32)
        xdt = sb("xdt", [128, 2048], BF16)
        xdt2 = sb("xdt2", [128, 2048], BF16)
        dam = [sb(f"dam{i}", [128, 4, 128], BF16) for i in range(2)]
        dec = [sb(f"dec{i}", [128, 4, 128], F32) for i in range(2)]
        cbm = [sb(f"cbm{i}", [128, 128], F32) for i in range(2)]
        G = [sb(f"G{i}", [128, 4, 128], BF16) for i in range(2)]
        tmp = [sb(f"tmp{i}", [128, 256], F32) for i in range(2)]
        y = sb("y", [128, 2048], F32)
        zs = [sb(f"zs{i}", [128, 512], F32) for i in range(2)]
        junk = sb("junk", [128, 256], F32)
        ssq = sb("ssq", [128, 16], F32)
        yn = sb("yn", [128, 2048], BF16)
        ynT = [sb(f"ynT{i}", [128, 16, TB], BF16) for i in range(2)]
        v3 = lambda ap, h: ap.rearrange("p (h c) -> p h c", h=h)
        bc3 = lambda ap, n: ap.unsqueeze(2).to_broadcast([128, ap.shape[1], n])
        sm2 = [sm, sb("smB", [128, 8, 32], F32)]
        xdtA = [xdt, sb("xdtB", [128, 2048], BF16)]
        xdt2A = [xdt2, sb("xdt2B", [128, 2048], BF16)]
        yA = [y, sb("yB", [128, 2048], F32)]

        def load_blk(b):
            i2 = b % 2
            t0 = b * TB
            P.dma('sp', out=xblk[i2][:], in_=self.xT_d[:, :, t0:t0 + TB], key=f"ld{4 + i2}", writes=[('xblk', i2)])
            P.dma('sp', out=bct[i2][:], in_=self.BCT_d[:, :, t0:t0 + TB], key=f"ld{6 + i2}", writes=[('bct', i2)])

        def pre(t):
            b, tt = divmod(t, 4)
            i2 = b % 2
            ti = t % 2
            if tt == 0:
                load_blk(b)
            xb = xblk[i2]
            cols = slice(tt * 128, (tt + 1) * 128)
            xs = xsB[ti]
            sm = sm2[ti]
            S = lambda k: ('sm', ti, k)
            P.dma('sp', out=xs[:], in_=self.xsB_d[t * 128:(t + 1) * 128, :], key=f"ld{8 + ti}", writes=[('xsB', ti)])
            P.mm(ps[5][:, 0:32], [(xb[:, kc, cols], Wdt[:, kc, :]) for kc in range(8)], 'ps5', [('xblk', i2)] + W('Wdt', 8))
            P.op('dve', lambda: nc.vector.tensor_tensor(sm[:, 0, :], ps[5][:, 0:32], dtb[:], op=ALU.add),
                 reads=['ps5', 'dtb'], writes=[S(0)])
            P.op('dve', lambda: nc.vector.tensor_scalar(sm[:, 1, :], sm[:, 0, :], -1.0, None, op0=ALU.mult),
                 reads=[S(0)], writes=[S(1)])
            P.op('dve', lambda: nc.vector.tensor_tensor(sm[:, 1, :], sm[:, 1, :], sm[:, 0, :], op=ALU.min),
                 reads=[S(0), S(1)], writes=[S(1)])
            P.op('act', lambda: nc.scalar.activation(out=sm[:, 2, :], in_=sm[:, 1, :], func=AF.Exp),
                 reads=[S(1)], writes=[S(2)])
            P.op('act', lambda: nc.scalar.activation(out=sm[:, 2, :], in_=sm[:, 2, :], func=AF.Ln, bias=1.0),
                 reads=[S(2)], writes=[S(2)])
            P.op('dve', lambda: nc.vector.scalar_tensor_tensor(out=sm[:, 3, :], in0=sm[:, 0, :], scalar=0.0, in1=sm[:, 2, :],
                                                               op0=ALU.max, op1=ALU.add), reads=[S(0), S(2)], writes=[S(3)])
            P.op('dve', lambda: nc.vector.tensor_tensor(sm[:, 4, :], sm[:, 3, :], abc[:], op=ALU.mult),
                 reads=[S(3), 'abc'], writes=[S(4)])
            P.mm(ps[5][:, 64:96], [(self.tri_le_f[:], sm[:, 4, :])], 'ps5', [S(4)])
            P.mm(ps[5][:, 128:160], [(self.tri_gt_f[:], sm[:, 4, :])], 'ps5', [S(4)])
            P.mm(ps[5][:, 192:224], [(self.ones_f[:], sm[:, 4, :])], 'ps5', [S(4)])
            P.op('act', lambda: nc.scalar.activation(out=sm[:, 5, :], in_=ps[5][:, 64:96], func=AF.Exp), reads=['ps5'], writes=[S(5)])
            P.op('act', lambda: nc.scalar.activation(out=sm[:, 6, :], in_=ps[5][:, 128:160], func=AF.Exp), reads=['ps5'], writes=[S(6)])
            P.op('act', lambda: nc.scalar.activation(out=sm[:, 7, :], in_=ps[5][:, 192:224], func=AF.Exp), reads=['ps5'], writes=[S(7)])
            P.op('dve', lambda: nc.vector.tensor_tensor(v3(xdtA[ti][:], 32), v3(xs[:, 0:2048], 32), bc3(sm[:, 3, :], 64), op=ALU.mult),
                 reads=[('xsB', ti), S(3)], writes=[('xdt', ti)])
            P.op('pool', lambda: nc.gpsimd.tensor_tensor(v3(xdt2A[ti][:], 32), v3(xdtA[ti][:], 32), bc3(sm[:, 6, :], 64), op=ALU.mult),
                 reads=[('xdt', ti), S(6)], writes=[('xdt2', ti)])

        def grp(t):
            b, tt = divmod(t, 4)
            i2 = b % 2
            ti = t % 2
            cols = slice(tt * 128, (tt + 1) * 128)
            xs = xsB[ti]
            sm = sm2[ti]
            S = lambda k: ('sm', ti, k)
            xdt_, xdt2_, y_ = xdtA[ti], xdt2A[ti], yA[ti]

            def stage1(g):
                gi = g % 2
                hs = slice(g * 4, (g + 1) * 4)
                P.op('pool', lambda: nc.gpsimd.tensor_tensor(
                    dam[gi][:], self.tri_gt_f[:, :].unsqueeze(1).to_broadcast([128, 4, 128]), bc3(sm[:, 4, hs], 128), op=ALU.mult),
                     reads=[S(4)], writes=[('dam', gi)])
                sp_, spt = (ps[0], ('SEG', 0)) if gi == 0 else (ps[1], ('SEG', 1))
                for r in range(4):
                    P.mm(sp_[:, r * 128:(r + 1) * 128], [(dam[gi][:, r, :], self.mask_le[:])], spt, [('dam', gi)])
                P.op('act', lambda: nc.scalar.activation(out=dec[gi][:].rearrange("p r s -> p (r s)"), in_=sp_[:, :], func=AF.Exp),
                     reads=[spt], writes=[('dec', gi)])
                cp_, cpt = (ps[2], ('CB', 0)) if gi == 0 else (ps[3], ('CB', 1))
                P.mm(cp_[:, 0:128], [(bct[i2][:, g, cols], bct[i2][:, 8 + g, cols])], cpt, [('bct', i2)])
                P.op('dve', lambda: nc.vector.tensor_tensor(cbm[gi][:], cp_[:, 0:128], self.tri_le_f[:], op=ALU.mult),
                     reads=[cpt], writes=[('cbm', gi)])
                P.op('dve', lambda: nc.vector.tensor_tensor(G[gi][:], dec[gi][:], cbm[gi][:, :].unsqueeze(1).to_broadcast([128, 4, 128]),
                                                            op=ALU.mult),
                     reads=[('dec', gi), ('cbm', gi)], writes=[('G', gi)])

            def stage2(g):
                gi = g % 2
                hs = slice(g * 4, (g + 1) * 4)
                cp_ = ps[2] if gi == 0 else ps[3]
                bp_ = ps[4] if gi == 0 else ps[6]
                for r in range(4):
                    hh = g * 4 + r
                    P.mm(bp_[:, r * 64:(r + 1) * 64],
                         [(G[gi][:, r, :], xdt_[:, hh * 64:(hh + 1) * 64])], ('YI', gi), [('G', gi), ('xdt', ti)])
                P.mm(bp_[:, 256:512], [(xs[:, 2048 + g * 128:2048 + (g + 1) * 128], xdt2_[:, g * 256:(g + 1) * 256])],
                     ('SU', gi), [('xsB', ti), ('xdt2', ti)])
                P.mm(cp_[:, 256:512], [(bct[i2][:, 8 + g, cols], STb[:, g, :])], ('YS', gi), [('bct', i2), ('STb', g)])
                P.op('dve', lambda: nc.vector.tensor_tensor(v3(tmp[gi][:], 4), v3(cp_[:, 256:512], 4), bc3(sm[:, 5, hs], 64), op=ALU.mult),
                     reads=[('YS', gi), S(5)], writes=[('tmp', gi)])
                P.op('dve', lambda: nc.vector.tensor_tensor(y_[:, g * 256:(g + 1) * 256], tmp[gi][:], bp_[:, 0:256], op=ALU.add),
                     reads=[('tmp', gi), ('YI', gi)], writes=[('y', ti, g)])
                P.op('pool', lambda: nc.gpsimd.tensor_tensor(v3(ST[:, g, :], 4), v3(ST[:, g, :], 4), bc3(sm[:, 7, hs], 64), op=ALU.mult),
                     reads=[S(7)], writes=[('STATE', g)])
                P.op('dve', lambda: nc.vector.tensor_tensor(ST[:, g, :], ST[:, g, :], bp_[:, 256:512], op=ALU.add),
                     reads=[('SU', gi)], writes=[('STATE', g)])
                P.op('act', lambda: nc.scalar.copy(STb[:, g, :], ST[:, g, :]), reads=[('STATE', g)], writes=[('STb', g)])

            if SSD_PIPE:
                stage1(0)
                for g in range(8):
                    if g + 1 < 8:
                        stage1(g + 1)
                    stage2(g)
            else:
                for g in range(8):
                    stage1(g)
                    stage2(g)

        def post(t):
            b, tt = divmod(t, 4)
            i2 = b % 2
            ti = t % 2
            t0 = b * TB
            cols = slice(tt * 128, (tt + 1) * 128)
            xb = xblk[i2]
            xs = xsB[ti]
            xdt2_, y_ = xdt2A[ti], yA[ti]
            ally = [('y', ti, g) for g in range(8)]
            P.op('pool', lambda: nc.gpsimd.tensor_tensor(v3(xdt2_[:], 32), v3(xs[:, 0:2048], 32), bc3(dsk[:, :], 64), op=ALU.mult),
                 reads=[('xsB', ti), 'dsk'], writes=[('xdt2', ti)])
            P.op('dve', lambda: nc.vector.tensor_tensor(y_[:], y_[:], xdt2_[:], op=ALU.add), reads=ally + [('xdt2', ti)], writes=ally)
            for q in range(4):
                zi = q % 2
                qs = slice(q * 512, (q + 1) * 512)
                zp, zpt = (ps[0], ('SEG', 0)) if zi == 0 else (ps[1], ('SEG', 1))
                P.mm(zp[:, :], [(xb[:, kc, cols], Wz[:, kc, qs]) for kc in range(8)], zpt, [('xblk', i2)] + W('Wz', 8))
                P.op('act', lambda zi=zi, zp=zp: nc.scalar.activation(out=zs[zi][:], in_=zp[:, :], func=AF.Silu), reads=[zpt], writes=[('zs', zi)])
                P.op('dve', lambda zi=zi, qs=qs: nc.vector.tensor_tensor(y_[:, qs], y_[:, qs], zs[zi][:], op=ALU.mult),
                     reads=[('zs', zi)] + ally, writes=ally)
            for g in range(8):
                P.op('act', lambda g=g: nc.scalar.activation(out=junk[:], in_=y_[:, g * 256:(g + 1) * 256], func=AF.Square, accum_out=ssq[:, g:g + 1]),
                     reads=ally, writes=['junk', ('ssq', g)])
            allq = [('ssq', g) for g in range(8)]
            P.op('dve', lambda: nc.vector.tensor_scalar(ssq[:, 8:16], ssq[:, 0:8], 1.0 / 256, RMS_EPS, op0=ALU.mult, op1=ALU.add),
                 reads=allq, writes=['ssq8'])
            P.op('act', lambda: nc.scalar.activation(out=ssq[:, 8:16], in_=ssq[:, 8:16], func=AF.Sqrt), reads=['ssq8'], writes=['ssq8'])
            P.op('dve', lambda: nc.vector.reciprocal(ssq[:, 8:16], ssq[:, 8:16]), reads=['ssq8'], writes=['ssq8'])
            for g in range(8):
                gs = slice(g * 256, (g + 1) * 256)
                P.op('dve', lambda g=g, gs=gs: nc.vector.scalar_tensor_tensor(
                    out=yn[:, gs], in0=y_[:, gs], scalar=ssq[:, 8 + g:9 + g], in1=ngb[:, gs], op0=ALU.mult, op1=ALU.mult),
                     reads=ally + ['ssq8', 'ngb'], writes=[('yn', g)])
            for r in range(2):
                for q in range(8):
                    P.op('pe', lambda r=r, q=q: nc.tensor.transpose(self.psb[:, q * 128:(q + 1) * 128],
                                                                    yn[:, (r * 8 + q) * 128:(r * 8 + q + 1) * 128], self.ident_bf[:]),
                         reads=[('yn', g) for g in range(8)], writes=['psb'], inc=(q == 7))
                P.op('act', lambda r=r: nc.scalar.copy(ynT[i2][:, r * 8:(r + 1) * 8, cols],
                                                       self.psb[:].rearrange("p (k n) -> p k n", k=8)),
                     reads=['psb'], writes=[('ynT', i2)])
            if tt == 3:
                P.dma('sp', out=self.ynT_d[:, :, t0:t0 + TB], in_=ynT[i2][:], key=f"st{i2}", reads=[('ynT', i2)], writes=[('ynT_d', b)])

        if SSD_HOIST:
            pre(0)
            for t in range(NT):
                if t + 1 < NT:
                    pre(t + 1)
                grp(t)
                post(t)
        else:
            for t in range(NT):
                pre(t)
                grp(t)
                post(t)
    P.barrier()


def _pass_ssd_c(self, l):
    P, nc = self.P, self.nc
    ps = self.ps
    with ExitStack() as es:
        sb = lambda name, shape, dt: es.enter_context(nc.sbuf_tensor(_uid(name), shape, dt))
        rows = lambda kc: slice(kc * 128, (kc + 1) * 128)
        W = lambda name, n: _toks(name, n)
        WgB = sb("WgB", [128, 8, D], BF16)
        wpb = sb("wpb", [128, 16, D], BF16)
        self.load_w(lambda kc: WgB[:, kc, :], lambda kc: self.w_in[l, rows(kc), C_G + D:C_G + 2 * D], 'WgB', 8)
        self.load_w(lambda kc: wpb[:, kc, :], lambda kc: self.w_proj_b[l, rows(kc), :], 'wpb', 16)
        xblk = [sb(f"xblk{i}", [128, 8, TB], BF16) for i in range(2)]
        ynb = [sb(f"ynb{i}", [128, 16, TB], BF16) for i in range(2)]
        sig = [sb(f"sig{i}", [128, TB], F32) for i in range(2)]
        mT = [sb(f"mT{i}", [128, 8, TB], BF16) for i in range(2)]
        for b in range(NB):
            i2 = b % 2
            t0 = b * TB
            P.dma('sp', out=xblk[i2][:], in_=self.xT_d[:, :, t0:t0 + TB], key=f"ld{2 + i2}", writes=[('xblk', i2)])
            P.dma('sp', out=ynb[i2][:], in_=self.ynT_d[:, :, t0:t0 + TB], key=f"ld{4 + i2}", writes=[('ynb', i2)])
            xb = xblk[i2]
            for dc in range(8):
                dd = slice(dc * 128, (dc + 1) * 128)
                si = dc % 2
                pa, pat = ps[(2 * dc) % 7], f'ps{(2 * dc) % 7}'
                pb, pbt = ps[(2 * dc + 1) % 7], f'ps{(2 * dc + 1) % 7}'
                P.mm(pa[:, :], [(WgB[:, kc, dd], xb[:, kc, :]) for kc in range(8)], pat, [('xblk', i2)] + W('WgB', 8))
                P.op('act', lambda si=si, pa=pa: nc.scalar.activation(out=sig[si][:], in_=pa[:, :], func=AF.Sigmoid),
                     reads=[pat], writes=[('sig', si)])
                P.mm(pb[:, :], [(wpb[:, j, dd], ynb[i2][:, j, :]) for j in range(16)], pbt, [('ynb', i2)] + W('wpb', 16))
                P.op('dve', lambda dc=dc, si=si, pb=pb: nc.vector.tensor_tensor(mT[i2][:, dc, :], pb[:, :], sig[si][:], op=ALU.mult),
                     reads=[pbt, ('sig', si)], writes=[('mT', i2)])
            P.dma('sp', out=self.mB_d[:, :, t0:t0 + TB], in_=mT[i2][:], key=f"st{i2}", reads=[('mT', i2)], writes=[('mB_d', b)])
    P.barrier()


Net.pass_ssd_a = _pass_ssd_a
Net.pass_ssd_b = _pass_ssd_b
Net.pass_ssd_c = _pass_ssd_c


def _layer_norm_tile(self, u, gbt, bbt, out, st, tag):
    P, nc = self.P, self.nc
    junk = self.ln_junk
    P.op('act', lambda: nc.scalar.activation(out=junk[:], in_=u, func=AF.Identity, accum_out=st[:, 0:1]),
         reads=[tag + 'u'], writes=['lnjunk', tag + 's0'])
    P.op('act', lambda: nc.scalar.activation(out=junk[:], in_=u, func=AF.Square, accum_out=st[:, 1:2]),
         reads=[tag + 'u'], writes=['lnjunk', tag + 's1'])
    P.op('dve', lambda: nc.vector.tensor_scalar(st[:, 2:4], st[:, 0:2], 1.0 / D, None, op0=ALU.mult),
         reads=[tag + 's0', tag + 's1'], writes=[tag + 's2'])
    P.op('dve', lambda: nc.vector.tensor_tensor(st[:, 4:5], st[:, 2:3], st[:, 2:3], op=ALU.mult), reads=[tag + 's2'], writes=[tag + 's4'])
    P.op('dve', lambda: nc.vector.tensor_tensor(st[:, 5:6], st[:, 3:4], st[:, 4:5], op=ALU.subtract), reads=[tag + 's2', tag + 's4'], writes=[tag + 's5'])
    P.op('dve', lambda: nc.vector.tensor_scalar(st[:, 5:6], st[:, 5:6], NORM_EPS, None, op0=ALU.add), reads=[tag + 's5'], writes=[tag + 's5'])
    P.op('act', lambda: nc.scalar.activation(out=st[:, 6:7], in_=st[:, 5:6], func=AF.Sqrt), reads=[tag + 's5'], writes=[tag + 's6'])
    P.op('dve', lambda: nc.vector.reciprocal(st[:, 7:8], st[:, 6:7]), reads=[tag + 's6'], writes=[tag + 's7'])
    P.op('dve', lambda: nc.vector.tensor_scalar(out, u, st[:, 2:3], st[:, 7:8], op0=ALU.subtract, op1=ALU.mult),
         reads=[tag + 'u', tag + 's2', tag + 's7'], writes=[tag + 'o'])
    P.op('pool', lambda: nc.gpsimd.tensor_tensor(out, out, gbt[:], op=ALU.mult), reads=[tag + 'g'], writes=[tag + 'o'])
    P.op('pool', lambda: nc.gpsimd.tensor_tensor(out, out, bbt[:], op=ALU.add), reads=[tag + 'g'], writes=[tag + 'o'])


def _pass_ln1(self, l):
    P, nc = self.P, self.nc
    ps = self.ps
    xa_src = self.x_in if l == 0 else self.xa_d
    with ExitStack() as es:
        sb = lambda name, shape, dt: es.enter_context(nc.sbuf_tensor(_uid(name), shape, dt))
        rows = lambda kc: slice(kc * 128, (kc + 1) * 128)
        W = lambda name, n: _toks(name, n)
        wout = sb("wout", [128, 8, D], BF16)
        rw = sb("rw", [128, 8, 16], F32)
        rb = sb("rb", [128, 16], F32)
        gbt = sb("gbt", [128, D], F32)
        bbt = sb("bbt", [128, D], F32)
        self.ln_junk = sb("lnjunk", [128, D], F32)
        self.load_w(lambda kc: wout[:, kc, :], lambda kc: self.w_out[l, rows(kc), :], 'wout', 8)
        for kc in range(8):
            P.dma('sp', out=rw[:, kc, :], in_=self.router_w[rows(kc), :], key="ld0", writes=[('rw', kc)])
        P.dma('sp', out=rb[:], in_=self.router_bias[0, :].partition_broadcast(128), key="ld1", writes=['rb'])
        P.dma('sp', out=gbt[:], in_=self.ln1_g[l, :].partition_broadcast(128), key="ld2", writes=['lg'])
        P.dma('sp', out=bbt[:], in_=self.ln1_b[l, :].partition_broadcast(128), key="ld3", writes=['lg'])
        mblk = [[sb(f"m{n}{i}", [128, 8, TB], BF16) for n in "ABC"] for i in range(2)]
        xa = [sb(f"xa{i}", [128, D], F32) for i in range(2)]
        u = [sb(f"u{i}", [128, D], F32) for i in range(2)]
        x1 = [sb(f"x1{i}", [128, D], F32) for i in range(2)]
        st = sb("st", [128, 8], F32)
        x1Tf = sb("x1Tf", [128, 8, 128], F32)
        stg = [sb(f"stg{i}", [128, 8, TB], BF16) for i in range(2)]
        r = sb("r", [128, 12, 16], F32)
        gts = [sb(f"gts{i}", [16, TB], F32) for i in range(2)]
        srcs = [self.mA_d, self.mB_d, self.mC_d]
        st2 = [st, sb("stB", [128, 8], F32)]

        def stA(t):
            b, tt = divmod(t, 4)
            i2, ti = b % 2, t % 2
            t0 = b * TB
            cols = slice(tt * 128, (tt + 1) * 128)
            tag = f'L1{ti}'
            if tt == 0:
                for n in range(3):
                    P.dma('sp', out=mblk[i2][n][:], in_=srcs[n][:, :, t0:t0 + TB], key=f"ld{4 + 3 * i2 + n}", writes=[('mblk', i2, n)])
            P.dma('sp', out=xa[ti][:], in_=xa_src[t * 128:(t + 1) * 128, :], key=f"ld{10 + ti}", writes=[('xa', ti)])
            for half in range(2):
                pb, pt = (ps[5], 'ps5') if half == 0 else (ps[6], 'ps6')
                P.mm(pb[:, :], [(mblk[i2][n][:, dc, cols], wout[:, dc, half * 512:(half + 1) * 512]) for n in range(3) for dc in range(8)],
                     pt, [('mblk', i2, n) for n in range(3)] + W('wout', 8))
                P.op('dve', lambda half=half, pb=pb: nc.vector.scalar_tensor_tensor(
                    out=u[ti][:, half * 512:(half + 1) * 512], in0=xa[ti][:, half * 512:(half + 1) * 512], scalar=DN_ALPHA, in1=pb[:, :],
                    op0=ALU.mult, op1=ALU.add), reads=[('xa', ti), pt], writes=[tag + 'u'])

        def stB(t):
            ti = t % 2
            tag = f'L1{ti}'
            self.layer_norm_tile(u[ti][:], gbt, bbt, x1[ti][:], st2[ti], tag)
            P._record(('e', 'pool', P.cnt['pool']), ['lg'], [])
            P.dma('sp', out=self.x1_d[t * 128:(t + 1) * 128, :], in_=x1[ti][:], key=f"st{ti}", reads=[tag + 'o'], writes=[('x1_d', t)])

        def stC(t):
            b, tt = divmod(t, 4)
            i2, ti = b % 2, t % 2
            t0 = b * TB
            cols = slice(tt * 128, (tt + 1) * 128)
            tag = f'L1{ti}'
            for kc in range(8):
                pb = ps[0] if kc < 4 else ps[1]
                P.op('pe', lambda kc=kc, pb=pb: nc.tensor.matmul(pb[:, (kc % 4) * 128:(kc % 4 + 1) * 128],
                                                                 x1[ti][:, kc * 128:(kc + 1) * 128], self.ident_f[:],
                                                                 start=True, stop=True),
                     reads=[tag + 'o'], writes=[('TP', kc // 4)], inc=(kc % 4 == 3))
            for hf in range(2):
                P.op('act', lambda hf=hf: nc.scalar.copy(x1Tf[:, hf * 4:(hf + 1) * 4, :], ps[hf][:].rearrange("p (k n) -> p k n", k=4)),
                     reads=[('TP', hf)], writes=['x1Tf'])
                P.op('dve', lambda hf=hf: nc.vector.tensor_copy(stg[i2][:, hf * 4:(hf + 1) * 4, cols], x1Tf[:, hf * 4:(hf + 1) * 4, :]),
                     reads=['x1Tf'], writes=[('stg', i2)])
            P.mm(ps[2][:, 0:16], [(x1Tf[:, kc, :], rw[:, kc, :]) for kc in range(8)], 'ps2', ['x1Tf'] + W('rw', 8))
            R = lambda i: r[:, i, :]
            R4 = lambda i: r[:, i, :].rearrange("p (g e) -> p g e", g=4)
            P.op('act', lambda: nc.scalar.activation(out=R(0), in_=ps[2][:, 0:16], func=AF.Sigmoid), reads=['ps2'], writes=['r0'])
            P.op('dve', lambda: nc.vector.tensor_tensor(R(1), R(0), rb[:], op=ALU.add), reads=['r0', 'rb'], writes=['r1'])
            pairs = [(0, 1), (0, 2), (0, 3), (1, 2), (1, 3), (2, 3)]
            for pi_, (a_, b_) in enumerate(pairs):
                P.op('dve', lambda pi_=pi_, a_=a_, b_=b_: nc.vector.tensor_tensor(r[:, 2 + pi_ // 4, (pi_ % 4) * 4:(pi_ % 4) * 4 + 4],
                                                                                R4(1)[:, :, a_], R4(1)[:, :, b_], op=ALU.add),
                     reads=['r1'], writes=['r2'])
            P.op('dve', lambda: nc.vector.tensor_tensor(r[:, 4, 0:4], r[:, 2, 0:4], r[:, 2, 4:8], op=ALU.max), reads=['r2'], writes=['r4'])
            P.op('dve', lambda: nc.vector.tensor_tensor(r[:, 4, 4:8], r[:, 2, 8:12], r[:, 2, 12:16], op=ALU.max), reads=['r2'], writes=['r4'])
            P.op('dve', lambda: nc.vector.tensor_tensor(r[:, 4, 8:12], r[:, 3, 0:4], r[:, 3, 4:8], op=ALU.max), reads=['r2'], writes=['r4'])
            P.op('dve', lambda: nc.vector.tensor_tensor(r[:, 4, 0:4], r[:, 4, 0:4], r[:, 4, 4:8], op=ALU.max), reads=['r4'], writes=['r4'])
            P.op('dve', lambda: nc.vector.tensor_tensor(r[:, 4, 0:4], r[:, 4, 0:4], r[:, 4, 8:12], op=ALU.max), reads=['r4'], writes=['r4'])
            P.op('dve', lambda: nc.vector.tensor_reduce(out=r[:, 5, 0:1], in_=r[:, 4, 0:4], axis=AX.X, op=ALU.max), reads=['r4'], writes=['r5'])
            P.op('dve', lambda: nc.vector.tensor_scalar(r[:, 5, 4:8], r[:, 4, 0:4], r[:, 5, 0:1], None, op0=ALU.is_equal), reads=['r4', 'r5'], writes=['r5m'])
            P.op('dve', lambda: nc.vector.tensor_scalar(r[:, 5, 8:12], r[:, 5, 4:8], 1.0, 1e30, op0=ALU.subtract, op1=ALU.mult), reads=['r5m'], writes=['r5p'])
            P.op('dve', lambda: nc.vector.tensor_tensor(R4(6), R4(1), r[:, 5, 4:8].unsqueeze(2).to_broadcast([128, 4, 4]), op=ALU.mult),
                 reads=['r1', 'r5m'], writes=['r6'])
            P.op('dve', lambda: nc.vector.tensor_tensor(R4(6), R4(6), r[:, 5, 8:12].unsqueeze(2).to_broadcast([128, 4, 4]), op=ALU.add),
                 reads=['r6', 'r5p'], writes=['r6'])
            P.op('dve', lambda: nc.vector.tensor_reduce(out=r[:, 7, 0:1], in_=R(6), axis=AX.X, op=ALU.max), reads=['r6'], writes=['r7'])
            P.op('dve', lambda: nc.vector.tensor_scalar(R(8), R(6), r[:, 7, 0:1], None, op0=ALU.is_equal), reads=['r6', 'r7'], writes=['r8'])
            P.op('dve', lambda: nc.vector.scalar_tensor_tensor(out=R(9), in0=R(8), scalar=-1e30, in1=R(6), op0=ALU.mult, op1=ALU.add),
                 reads=['r8', 'r6'], writes=['r9'])
            P.op('dve', lambda: nc.vector.tensor_reduce(out=r[:, 7, 1:2], in_=R(9), axis=AX.X, op=ALU.max), reads=['r9'], writes=['r7b'])
            P.op('dve', lambda: nc.vector.tensor_scalar(R(10), R(9), r[:, 7, 1:2], None, op0=ALU.is_equal), reads=['r9', 'r7b'], writes=['r10'])
            P.op('dve', lambda: nc.vector.tensor_tensor(R(10), R(10), R(8), op=ALU.add), reads=['r10', 'r8'], writes=['r10'])
            P.op('dve', lambda: nc.vector.tensor_tensor(R(11), R(10), R(0), op=ALU.mult), reads=['r10', 'r0'], writes=['r11'])
            P.op('dve', lambda: nc.vector.tensor_reduce(out=r[:, 7, 2:3], in_=R(11), axis=AX.X, op=ALU.add), reads=['r11'], writes=['r7c'])
            P.op('dve', lambda: nc.vector.reciprocal(r[:, 7, 3:4], r[:, 7, 2:3]), reads=['r7c'], writes=['r7d'])
            P.op('dve', lambda: nc.vector.tensor_scalar(R(11), R(11), r[:, 7, 3:4], None, op0=ALU.mult), reads=['r11', 'r7d'], writes=['r11'])
            P.op('pe', lambda: nc.tensor.matmul(ps[3][0:16, 0:128], R(11), self.ident_f[:], start=True, stop=True), reads=['r11'], writes=['ps3'])
            P.op('act', lambda: nc.scalar.copy(gts[i2][:, cols], ps[3][0:16, 0:128]), reads=['ps3'], writes=[('gts', i2)])
            if tt == 3:
                P.dma('sp', out=self.x1T_d[:, :, t0:t0 + TB], in_=stg[i2][:], key=f"st{2 + i2}", reads=[('stg', i2)], writes=[('x1T_d', b)])
                P.dma('sp', out=self.gT_d[:, t0:t0 + TB], in_=gts[i2][:], key=f"st{4 + i2}", reads=[('gts', i2)], writes=[('gT_d', b)])

        for step in range(NT + 2):
            if step < NT:
                stA(step)
            if 0 <= step - 1 < NT:
                stB(step - 1)
            if 0 <= step - 2 < NT:
                stC(step - 2)
    P.barrier()


def _pass_moe(self, l, last):
    P, nc = self.P, self.nc
    ps = self.ps
    BB = 1024
    with ExitStack() as es:
        sb = lambda name, shape, dt: es.enter_context(nc.sbuf_tensor(_uid(name), shape, dt))
        rows = lambda kc: slice(kc * 128, (kc + 1) * 128)
        W = lambda name, n: _toks(name, n)
        gbt = sb("gbt", [128, D], F32)
        bbt = sb("bbt", [128, D], F32)
        self.ln_junk = sb("lnjunk", [128, D], F32)
        E16 = sb("E16", [16, 16, 128], F32)
        P.dma('sp', out=gbt[:], in_=self.ln2_g[l, :].partition_broadcast(128), key="ld2", writes=['lg'])
        P.dma('sp', out=bbt[:], in_=self.ln2_b[l, :].partition_broadcast(128), key="ld3", writes=['lg'])
        P.op('pool', lambda: nc.gpsimd.memset(E16[:], 0.0), writes=['E16'])
        P.op('pool', lambda: nc.gpsimd.affine_select(out=E16[:], in_=E16[:], pattern=[[-1, 16], [0, 128]], compare_op=ALU.not_equal,
                                                     fill=1.0, base=0, channel_multiplier=1), writes=['E16'])
        x1T = sb("x1T", [128, 8, BB], BF16)
        gT = sb("gT", [16, BB], F32)
        acc = sb("acc", [128, 8, D], F32)
        w1 = [sb(f"w1{i}", [128, 8, 512], BF16) for i in range(2)]
        w3 = [sb(f"w3{i}", [128, 8, 512], BF16) for i in range(2)]
        w2 = [sb(f"w2{i}", [128, 4, D], BF16) for i in range(2)]
        gb = [sb(f"gb{i}", [128, 512], F32) for i in range(2)]
        s1 = [sb(f"s1{i}", [128, 512], F32) for i in range(2)]
        tq = [sb(f"tq{i}", [128, 512], F32) for i in range(2)]
        hT = [sb(f"hT{i}", [128, 4, 512], BF16) for i in range(2)]
        x1t = [sb(f"x1t{i}", [128, D], F32) for i in range(2)]
        u = [sb(f"u{i}", [128, D], F32) for i in range(2)]
        x2 = [sb(f"x2{i}", [128, D], F32) for i in range(2)]
        x2b = [sb(f"x2b{i}", [128, D], BF16) for i in range(2)]
        st = sb("st", [128, 8], F32)
        st2 = [st, sb("stB", [128, 8], F32)]
        stg = sb("stg", [128, 8, BB], BF16)
        dst = self.y_out if last else self.xa_d
        for bb in range(T // BB):
            t0 = bb * BB
            P.dma('sp', out=x1T[:], in_=self.x1T_d[:, :, t0:t0 + BB], key="ld4", writes=['x1T'])
            P.dma('sp', out=gT[:], in_=self.gT_d[:, t0:t0 + BB], key="ld5", writes=['gT'])
            units = [(e, sub) for e in range(16) for sub in range(BB // 512)]

            def stage_F(ui):
                e, sub = units[ui]
                ei = (ebase + e) % 2
                hi = (ubase + ui) % 2
                if sub == 0:
                    self.load_w(lambda kc: w1[ei][:, kc, :], lambda kc: self.exp_w1[l, e, rows(kc), :], ('w1', ei), 8)
                    self.load_w(lambda kc: w3[ei][:, kc, :], lambda kc: self.exp_w3[l, e, rows(kc), :], ('w3', ei), 8)
                    self.load_w(lambda kc: w2[ei][:, kc, :], lambda kc: self.exp_w2[l, e, rows(kc), :], ('w2', ei), 4)
                sc = slice(sub * 512, (sub + 1) * 512)
                P.mm(ps[4][:, :], [(E16[:, e, :], gT[:, sc])], 'ps4', ['E16', 'gT'])
                P.op('act', lambda: nc.scalar.copy(gb[hi][:], ps[4][:, :]), reads=['ps4'], writes=[('gb', hi)])
                for fc in range(4):
                    fi = fc % 2
                    fs = slice(fc * 128, (fc + 1) * 128)
                    pa, pat = (ps[0], 'ps0') if fi == 0 else (ps[2], 'ps2')
                    pb, pbt = (ps[1], 'ps1') if fi == 0 else (ps[3], 'ps3')
                    P.mm(pa[:, :], [(w1[ei][:, kc, fs], x1T[:, kc, sc]) for kc in range(8)], pat, ['x1T'] + W(('w1', ei), 8))
                    P.mm(pb[:, :], [(w3[ei][:, kc, fs], x1T[:, kc, sc]) for kc in range(8)], pbt, ['x1T'] + W(('w3', ei), 8))
                    P.op('act', lambda fi=fi, pa=pa: nc.scalar.activation(out=s1[fi][:], in_=pa[:, :], func=AF.Silu), reads=[pat], writes=[('s1', fi)])
                    P.op('dve', lambda fi=fi, pb=pb: nc.vector.tensor_tensor(tq[fi][:], pb[:, :], gb[hi][:], op=ALU.mult),
                         reads=[pbt, ('gb', hi)], writes=[('tq', fi)])
                    P.op('dve', lambda fi=fi, fc=fc: nc.vector.tensor_tensor(hT[hi][:, fc, :], tq[fi][:], s1[fi][:], op=ALU.mult),
                         reads=[('tq', fi), ('s1', fi)], writes=[('hT', hi, fc)])

            def stage_S(ui):
                e, sub = units[ui]
                ei = (ebase + e) % 2
                hi = (ubase + ui) % 2
                for tt in range(4):
                    tl = sub * 4 + tt
                    cols = slice(tt * 128, (tt + 1) * 128)
                    for half in range(2):
                        pb, pt = (ps[5], 'ps5') if half == 0 else (ps[6], 'ps6')
                        hs = slice(half * 512, (half + 1) * 512)
                        P.mm(pb[:, :], [(hT[hi][:, fc, cols], w2[ei][:, fc, hs]) for fc in range(4)], pt,
                             [('hT', hi, fc) for fc in range(4)] + W(('w2', ei), 4))
                        if e == 0:
                            P.op('dve', lambda tl=tl, hs=hs, pb=pb: nc.vector.tensor_copy(acc[:, tl, hs], pb[:, :]),
                                 reads=[pt], writes=[('acc', tl, half)])
                        else:
                            P.op('dve', lambda tl=tl, hs=hs, pb=pb: nc.vector.tensor_tensor(acc[:, tl, hs], acc[:, tl, hs], pb[:, :], op=ALU.add),
                                 reads=[pt], writes=[('acc', tl, half)])

            ebase = bb * 16
            ubase = bb * len(units)
            stage_F(0)
            for ui in range(len(units)):
                if ui + 1 < len(units):
                    stage_F(ui + 1)
                stage_S(ui)
            ntl = BB // 128

            def eA(tl):
                t = bb * ntl + tl
                ti = t % 2
                tag = f'L2{ti}'
                P.dma('sp', out=x1t[ti][:], in_=self.x1_d[t * 128:(t + 1) * 128, :], key=f"ld{6 + ti}", writes=[('x1t', ti)])
                P.op('dve', lambda: nc.vector.scalar_tensor_tensor(
                    out=u[ti][:], in0=x1t[ti][:], scalar=DN_ALPHA, in1=acc[:, tl, :], op0=ALU.mult, op1=ALU.add),
                     reads=[('x1t', ti), ('acc', tl, 0), ('acc', tl, 1)], writes=[tag + 'u'])

            def eB(tl):
                t = bb * ntl + tl
                ti = t % 2
                tag = f'L2{ti}'
                self.layer_norm_tile(u[ti][:], gbt, bbt, x2[ti][:], st2[ti], tag)
                P._record(('e', 'pool', P.cnt['pool']), ['lg'], [])
                P.dma('sp', out=dst[t * 128:(t + 1) * 128, :], in_=x2[ti][:], key=f"st{ti}", reads=[tag + 'o'], writes=[('dst', t)])

            def eC(tl):
                t = bb * ntl + tl
                ti = t % 2
                tag = f'L2{ti}'
                if last:
                    return
                P.op('act', lambda: nc.scalar.copy(x2b[ti][:], x2[ti][:]), reads=[tag + 'o'], writes=[('x2b', ti)])
                for kc in range(8):
                    P.op('pe', lambda kc=kc: nc.tensor.transpose(self.psb[:, kc * 128:(kc + 1) * 128],
                                                                 x2b[ti][:, kc * 128:(kc + 1) * 128], self.ident_bf[:]),
                         reads=[('x2b', ti)], writes=['psb'], inc=(kc == 7))
                P.op('act', lambda: nc.scalar.copy(stg[:, :, tl * 128:(tl + 1) * 128],
                                                   self.psb[:].rearrange("p (k n) -> p k n", k=8)),
                     reads=['psb'], writes=['stg'])

            for step in range(ntl + 2):
                if step < ntl:
                    eA(step)
                if 0 <= step - 1 < ntl:
                    eB(step - 1)
                if 0 <= step - 2 < ntl:
                    eC(step - 2)
            if not last:
                P.dma('sp', out=self.xT_d[:, :, t0:t0 + BB], in_=stg[:], key="st2", reads=['stg'], writes=[('xT_d', bb)])
    P.barrier()


Net.layer_norm_tile = _layer_norm_tile
Net.pass_ln1 = _pass_ln1
Net.pass_moe = _pass_moe


def build(n_layers=DEPTH, stages=("mla", "ssd", "mem", "moe"), dbg=()):
    net = Net(n_layers)
    net.declare()
    P = net.P
    net.setup_globals()
    net.prologue()
    for l in range(n_layers):
        if "mla" in stages:
            net.pass_mla(l)
        if "mem" in stages:
            net.pass_mem(l)
        if "ssd" in stages or "ssda" in stages:
            net.pass_ssd_a(l)
        if "ssd" in stages or "ssdb" in stages:
            net.pass_ssd_b(l)
        if "ssd" in stages or "ssdc" in stages:
            net.pass_ssd_c(l)
        if "moe" in stages or "ln1" in stages:
            net.pass_ln1(l)
        if "moe" in stages:
            net.pass_moe(l, last=(l == n_layers - 1))
    for name in dbg:
        src = getattr(net, name)
        dst = net.dbg("dbg_" + name, src.shape, src.dtype)
        P.dma('sp', out=dst, in_=src, key="st0", reads=[], writes=[('dbg', name)])
    P.barrier()
    print("instructions:", P.ninst, "sems:", P.nsem, {e: P.cnt[e] for e in P.cnt}, "dmas:", getattr(P, 'ndma', 0), "descs~", getattr(P, 'ndesc', 0))
    return net


def make_inputs(inputs, b):
    m = {}
    for k, v in inputs.items():
        v = np.asarray(v)
        if k in ("x", "mem"):
            m[k] = np.ascontiguousarray(v[b])
        elif k == "positions":
            m[k] = np.ascontiguousarray(v[b:b + 1]).astype(np.int32)
        elif k == "router_bias":
            m[k] = np.ascontiguousarray(v.reshape(1, 16))
        else:
            m[k] = np.ascontiguousarray(v)
    inv = (10000.0 ** (-np.arange(0, 64, 2, dtype=np.float32) / 64)).astype(np.float32)
    m["inv_freq"] = np.concatenate([inv, inv]).reshape(64, 1).astype(np.float32)
    return m


def kernel(**inputs):
    net = build()
    in_maps = [make_inputs(inputs, b) for b in range(8)]
    res = run_bass_kernel_spmd(net.nc, in_maps, core_ids=list(range(8)))
    return np.stack([np.asarray(res.results[b]["y"]) for b in range(8)], axis=0).astype(np.float32)
```

```python
import math
from contextlib import ExitStack

import numpy as np
import concourse.bass as bass
import concourse.mybir as mybir
from concourse.bass_utils import run_bass_kernel_spmd

F32 = mybir.dt.float32
BF16 = mybir.dt.bfloat16
I32 = mybir.dt.int32
AF = mybir.ActivationFunctionType
ALU = mybir.AluOpType
AX = mybir.AxisListType

D = 1024
T = 4096
DEPTH = 4
NT = T // 128
TB = 512
NB = T // TB
H = 8
QR = 384
KVR = 256
NOPE = 128
ROPE = 64
VD = 128
N_IN = 10976
C_DQ = 0
C_DKV = 384
C_KR = 640
C_Z = 704
C_XBC = 2752
C_DT = 6848
C_QM = 6880
C_G = 7904
DN_ALPHA = (2 * DEPTH) ** 0.25
NORM_EPS = 1e-5
RMS_EPS = 1e-6
SCALE_A = (NOPE + ROPE) ** -0.5

EPOCH = 30000
import os
SSD_PIPE = int(os.environ.get('SSD_PIPE', '1'))
SSD_HOIST = int(os.environ.get('SSD_HOIST', '1'))
_UID = [0]


def _uid(name):
    _UID[0] += 1
    return f"{name}_{_UID[0]}"


def _runs(ap):
    dims = [(int(st), int(n)) for st, n in ap.ap]
    total = 1
    for st, n in dims:
        total *= n
    run = 1
    exp = 1
    for st, n in reversed(dims[1:] if len(dims) > 1 else dims):
        if st == exp:
            run *= n
            exp *= n
        else:
            break
    return max(total // max(run, 1), 1)


_BANK_OF = {'ST': lambda i: i, 'SEG': lambda i: i, 'TP': lambda i: i, 'oacc': lambda i: 2 + i, 'CB': lambda i: 2 + i,
            'YS': lambda i: 2 + i, 'YI': lambda i: 4 if i == 0 else 6, 'SU': lambda i: 4 if i == 0 else 6}
_BANK_NAMES = {'ps0': 0, 'ps1': 1, 'ps2': 2, 'ps3': 3, 'ps4': 4, 'ps5': 5, 'ps6': 6, 'psb': 7, 'osum': 4}


def _banks(tokens):
    out = []
    for t in tokens:
        if isinstance(t, str):
            b = _BANK_NAMES.get(t)
        elif isinstance(t, tuple) and t and t[0] in _BANK_OF and len(t) == 2:
            b = _BANK_OF[t[0]](t[1])
        else:
            b = None
        if b is not None:
            out.append(('bank', b))
    return out


class Prog:
    def __init__(self):
        self.nc = bass.Bass("TRN2", target_bir_lowering=False)
        nc = self.nc
        self.es = ExitStack()
        self.eng = dict(pe=nc.tensor, act=nc.scalar, dve=nc.vector, pool=nc.gpsimd, sp=nc.sync)
        self.cnt = {e: 0 for e in self.eng}
        self.sems = {e: [] for e in self.eng}
        self.waited = {e: {} for e in self.eng}
        self.lastw = {}
        self.readers = {}
        self.dsem = {}
        self.dcnt = {}
        self.nsem = 0
        self.ninst = 0

    def _new_sem(self, name):
        self.nsem += 1
        return self.es.enter_context(self.nc.semaphore(name))

    def _esem(self, e, n):
        ep = (n - 1) // EPOCH
        while len(self.sems[e]) <= ep:
            self.sems[e].append(self._new_sem(f"s_{e}_{len(self.sems[e])}"))
        return self.sems[e][ep], (n - 1) % EPOCH + 1

    def _wait(self, c, ev):
        if ev is None:
            return
        kind, p, n = ev
        if kind == 'e':
            if p == c:
                if c == 'pe' or n > self.cnt[c] or n < self.cnt[c] - 1:
                    return
            if self.waited[c].get(p, 0) >= n:
                return
            sem, val = self._esem(p, n)
            self.eng[c].wait_ge(sem, val)
            self.waited[c][p] = n
        else:
            k = ('d', p)
            if self.waited[c].get(k, 0) >= n:
                return
            self.eng[c].wait_ge(self.dsem[p], 16 * n)
            self.waited[c][k] = n

    def _deps(self, reads, writes):
        deps = []
        for r in reads:
            ev = self.lastw.get(r)
            if ev is not None:
                deps.append(ev)
        for w in writes:
            ev = self.lastw.get(w)
            if ev is not None:
                deps.append(ev)
            rd = self.readers.get(w)
            if rd:
                deps.extend(rd.values())
        return deps

    def _record(self, ev, reads, writes):
        key = (ev[0], ev[1])
        for r in reads:
            self.readers.setdefault(r, {})[key] = ev
        for w in writes:
            self.lastw[w] = ev
            self.readers[w] = {}

    def op(self, e, fn, reads=(), writes=(), inc=True):
        bk = _banks(reads) + _banks(writes)
        if bk:
            writes = list(writes) + bk
        for ev in self._deps(reads, writes):
            self._wait(e, ev)
        inst = fn()
        self.ninst += 1
        if inc:
            self.cnt[e] += 1
            n = self.cnt[e]
            sem, _ = self._esem(e, n)
            inst.then_inc(sem, 1)
            ev = ('e', e, n)
        else:
            ev = ('e', e, self.cnt[e] + 1)
        self._record(ev, reads, writes)
        return ev

    def dma(self, q, out, in_, key, reads=(), writes=()):
        if key not in self.dsem:
            self.dsem[key] = self._new_sem(f"d_{key}")
            self.dcnt[key] = 0
        kres = ('dmakey', key)
        for ev in self._deps(reads, list(writes) + [kres]):
            self._wait(q, ev)
        inst = self.eng[q].dma_start(out=out, in_=in_)
        self.ninst += 1
        self.ndma = getattr(self, 'ndma', 0) + 1
        self.ndesc = getattr(self, 'ndesc', 0) + max(_runs(out), _runs(in_))
        self.dcnt[key] += 1
        inst.then_inc(self.dsem[key], 16)
        ev = ('d', key, self.dcnt[key])
        self._record(ev, reads, list(writes) + [kres])
        return ev

    def barrier(self):
        for c in self.eng:
            for p in self.eng:
                if p != c and self.cnt[p] > 0:
                    self._wait(c, ('e', p, self.cnt[p]))
            for k, m in self.dcnt.items():
                if m > 0:
                    self._wait(c, ('d', k, m))
        self.lastw.clear()
        self.readers.clear()

    def mm(self, out, pairs, wres, reads):
        nc = self.nc
        n = len(pairs)
        for i, (l, r) in enumerate(pairs):
            self.op('pe', lambda l=l, r=r, i=i: nc.tensor.matmul(out, l, r, start=(i == 0), stop=(i == n - 1)),
                    reads=reads if i == 0 else (), writes=[wres], inc=(i == n - 1))
        ev = ('e', 'pe', self.cnt['pe'])
        self._record(ev, reads, ())


class Net:
    def __init__(self, n_layers=DEPTH, debug=None):
        self.P = Prog()
        self.nc = self.P.nc
        self.n_layers = n_layers
        self.debug = debug or {}
        self.dbg_out = {}

    def declare(self):
        nc = self.nc
        L = DEPTH
        def inp(name, shape, dt=F32):
            return nc.dram_tensor(name, list(shape), dt, kind="ExternalInput").ap()
        self.x_in = inp("x", [T, D])
        self.mem_in = inp("mem", [256, D])
        self.pos_in = inp("positions", [1, T], I32)
        self.w_in = inp("w_in", [L, D, N_IN])
        self.q_norm = inp("q_norm", [L, QR])
        self.w_uq = inp("w_uq", [L, QR, H * 192])
        self.kv_norm = inp("kv_norm", [L, KVR])
        self.w_ukv = inp("w_ukv", [L, KVR, H, 256])
        self.w_proj_a = inp("w_proj_a", [L, D, D])
        self.conv_w = inp("conv_w", [L, 4, 4096])
        self.conv_b = inp("conv_b", [L, 4096])
        self.dt_bias = inp("dt_bias", [L, 32])
        self.a_log = inp("a_log", [L, 32])
        self.d_skip = inp("d_skip", [L, 32])
        self.ssm_norm = inp("ssm_norm", [L, 2048])
        self.w_proj_b = inp("w_proj_b", [L, 2048, D])
        self.w_mem_kv = inp("w_mem_kv", [L, D, 2048])
        self.w_proj_c = inp("w_proj_c", [L, D, D])
        self.w_out = inp("w_out", [L, D, D])
        self.ln1_g = inp("ln1_g", [L, D])
        self.ln1_b = inp("ln1_b", [L, D])
        self.router_w = inp("router_w", [D, 16])
        self.router_bias = inp("router_bias", [1, 16])
        self.exp_w1 = inp("exp_w1", [L, 16, D, 512])
        self.exp_w3 = inp("exp_w3", [L, 16, D, 512])
        self.exp_w2 = inp("exp_w2", [L, 16, 512, D])
        self.ln2_g = inp("ln2_g", [L, D])
        self.ln2_b = inp("ln2_b", [L, D])
        self.inv_freq = inp("inv_freq", [64, 1])
        self.y_out = nc.dram_tensor("y", [T, D], F32, kind="ExternalOutput").ap()

        def scr(name, shape, dt):
            return nc.dram_tensor(name, list(shape), dt, kind="Internal").ap()
        self.xT_d = scr("xT_d", [128, 8, T], BF16)
        self.x1T_d = scr("x1T_d", [128, 8, T], BF16)
        self.xa_d = scr("xa_d", [T, D], F32)
        self.x1_d = scr("x1_d", [T, D], F32)
        self.mA_d = scr("mA_d", [128, 8, T], BF16)
        self.mB_d = scr("mB_d", [128, 8, T], BF16)
        self.mC_d = scr("mC_d", [128, 8, T], BF16)
        self.xsB_d = scr("xsB_d", [T, 3072], BF16)
        self.BCT_d = scr("BCT_d", [128, 16, T], BF16)
        self.ynT_d = scr("ynT_d", [128, 16, T], BF16)
        self.gT_d = scr("gT_d", [16, T], F32)
        self.cos_d = scr("cos_d", [64, T], F32)
        self.sin_d = scr("sin_d", [64, T], F32)

    def dbg(self, name, shape, dt=F32):
        ap = self.nc.dram_tensor(name, list(shape), dt, kind="ExternalOutput").ap()
        self.dbg_out[name] = ap
        return ap


def _setup_globals(self):
    P, nc = self.P, self.nc
    es = P.es
    sb = lambda name, shape, dt: es.enter_context(nc.sbuf_tensor(_uid(name), shape, dt))
    self.ps = [es.enter_context(nc.psum_tensor(f"ps{i}", [128, 512], F32)) for i in range(7)]
    self.psb = es.enter_context(nc.psum_tensor("psb", [128, 1024], BF16))
    self.ident_bf = sb("ident_bf", [128, 128], BF16)
    self.ident_f = sb("ident_f", [128, 128], F32)
    self.ones_bf = sb("ones_bf", [128, 128], BF16)
    self.ones_f = sb("ones_f", [128, 128], F32)
    self.mask_le = sb("mask_le", [128, 128], BF16)
    self.tri_le_f = sb("tri_le_f", [128, 128], F32)
    self.tri_gt_f = sb("tri_gt_f", [128, 128], F32)
    g = nc.gpsimd
    for tl, val in ((self.ident_bf, 0.0), (self.ident_f, 0.0)):
        P.op('pool', lambda tl=tl: g.memset(tl[:], 0.0), writes=[('const', tl.name)])
        P.op('pool', lambda tl=tl: g.affine_select(out=tl[:], in_=tl[:], pattern=[[-1, 128]],
                                                   compare_op=ALU.not_equal, fill=1.0, base=0,
                                                   channel_multiplier=1), writes=[('const', tl.name)])
    P.op('pool', lambda: g.memset(self.ones_bf[:], 1.0), writes=[('const', 'ones_bf')])
    P.op('pool', lambda: g.memset(self.ones_f[:], 1.0), writes=[('const', 'ones_f')])
    for tl in (self.mask_le, self.tri_le_f):
        P.op('pool', lambda tl=tl: g.memset(tl[:], 1.0), writes=[('const', tl.name)])
        P.op('pool', lambda tl=tl: g.affine_select(out=tl[:], in_=tl[:], pattern=[[1, 128]],
                                                   compare_op=ALU.is_ge, fill=0.0, base=0,
                                                   channel_multiplier=-1), writes=[('const', tl.name)])
    P.op('pool', lambda: g.memset(self.tri_gt_f[:], 1.0), writes=[('const', 'tri_gt_f')])
    P.op('pool', lambda: g.affine_select(out=self.tri_gt_f[:], in_=self.tri_gt_f[:], pattern=[[-1, 128]],
                                         compare_op=ALU.is_gt, fill=0.0, base=0,
                                         channel_multiplier=1), writes=[('const', 'tri_gt_f')])
    self.wkey = 0
    P.barrier()


def _load_w(self, dst, src, tok, nk, q='pool'):
    P = self.P
    for kc in range(nk):
        key = f"w{self.wkey % 8}"
        self.wkey += 1
        P.dma(q, out=dst(kc), in_=src(kc), key=key, writes=[(tok, kc)])


def _toks(tok, nk):
    return [(tok, kc) for kc in range(nk)]


Net.setup_globals = _setup_globals
Net.load_w = _load_w


def _prologue(self):
    P, nc = self.P, self.nc
    with ExitStack() as es:
        sb = lambda name, shape, dt: es.enter_context(nc.sbuf_tensor(_uid(name), shape, dt))
        posi = sb("posi", [64, T], I32)
        ang = sb("ang", [64, T], F32)
        red = sb("red", [64, T], F32)
        tab = sb("tab", [64, T], F32)
        invf = sb("invf", [64, 1], F32)
        negpi = sb("negpi", [64, 1], F32)
        P.dma('sp', out=posi[:], in_=self.pos_in[0, :].partition_broadcast(64), key="ld0", writes=['posi'])
        P.dma('sp', out=invf[:], in_=self.inv_freq[:, :], key="ld1", writes=['invf'])
        P.op('dve', lambda: nc.vector.memset(negpi[:], -math.pi), writes=['negpi'])
        P.op('dve', lambda: nc.vector.tensor_copy(ang[:], posi[:]), reads=['posi'], writes=['ang'])
        P.op('dve', lambda: nc.vector.tensor_scalar(ang[:], ang[:], invf[:, 0:1], None, op0=ALU.mult),
             reads=['invf'], writes=['ang'])
        ki = sb("ki", [64, T], I32)
        kf = sb("kf", [64, T], F32)
        C1 = 6.28125
        C2 = 2 * math.pi - C1
        P.op('dve', lambda: nc.vector.tensor_scalar(red[:], ang[:], 1.0 / (2 * math.pi), None, op0=ALU.mult),
             reads=['ang'], writes=['red'])
        P.op('dve', lambda: nc.vector.tensor_copy(ki[:], red[:]), reads=['red'], writes=['ki'])
        P.op('dve', lambda: nc.vector.tensor_copy(kf[:], ki[:]), reads=['ki'], writes=['kf'])
        P.op('dve', lambda: nc.vector.scalar_tensor_tensor(out=red[:], in0=kf[:], scalar=-C1, in1=ang[:],
                                                           op0=ALU.mult, op1=ALU.add), reads=['kf', 'ang'], writes=['red'])
        P.op('dve', lambda: nc.vector.scalar_tensor_tensor(out=red[:], in0=kf[:], scalar=-C2, in1=red[:],
                                                           op0=ALU.mult, op1=ALU.add), reads=['kf', 'red'], writes=['red'])

        def wrap_sin(shift):
            P.op('dve', lambda: nc.vector.tensor_scalar(ang[:], red[:], shift, None, op0=ALU.add),
                 reads=['red'], writes=['ang'])
            P.op('dve', lambda: nc.vector.tensor_scalar(kf[:], ang[:], math.pi, 2 * math.pi, op0=ALU.is_gt, op1=ALU.mult),
                 reads=['ang'], writes=['kf'])
            P.op('dve', lambda: nc.vector.tensor_tensor(ang[:], ang[:], kf[:], op=ALU.subtract),
                 reads=['ang', 'kf'], writes=['ang'])
            P.op('dve', lambda: nc.vector.tensor_scalar(ang[:], ang[:], -math.pi, math.pi, op0=ALU.max, op1=ALU.min),
                 reads=['ang'], writes=['ang'])
            P.op('act', lambda: nc.scalar.activation(out=tab[:], in_=ang[:], func=AF.Sin),
                 reads=['ang'], writes=['tab'])
        wrap_sin(0.0)
        P.op('dve', lambda: nc.vector.tensor_scalar(tab[0:32, :], tab[0:32, :], -1.0, None, op0=ALU.mult),
             reads=['tab'], writes=['tab'])
        P.dma('sp', out=self.sin_d[:, :], in_=tab[:], key="st0", reads=['tab'], writes=['sin_d'])
        wrap_sin(0.5 * math.pi)
        P.dma('sp', out=self.cos_d[:, :], in_=tab[:], key="st1", reads=['tab'], writes=['cos_d'])
        xin = [sb(f"xin{i}", [128, D], F32) for i in range(2)]
        xbf = [sb(f"xbf{i}", [128, D], BF16) for i in range(2)]
        stg = [sb(f"stg{i}", [128, 8, TB], BF16) for i in range(2)]
        for t in range(NT):
            i = t % 2
            b, tt = divmod(t, 4)
            P.dma('sp', out=xin[i][:], in_=self.x_in[t * 128:(t + 1) * 128, :], key=f"ld{2 + i}", writes=[('xin', i)])
            P.op('dve', lambda i=i: nc.vector.tensor_copy(xbf[i][:], xin[i][:]), reads=[('xin', i)], writes=[('xbf', i)])
            for kc in range(8):
                P.op('pe', lambda i=i, kc=kc: nc.tensor.transpose(self.psb[:, kc * 128:(kc + 1) * 128],
                                                                   xbf[i][:, kc * 128:(kc + 1) * 128], self.ident_bf[:]),
                     reads=[('xbf', i)], writes=['psb'], inc=(kc == 7))
            P.op('act', lambda b=b, tt=tt: nc.scalar.copy(stg[b % 2][:, :, tt * 128:(tt + 1) * 128],
                                                         self.psb[:].rearrange("p (k n) -> p k n", k=8)),
                 reads=['psb'], writes=[('stg', b % 2)])
            if tt == 3:
                P.dma('sp', out=self.xT_d[:, :, b * TB:(b + 1) * TB], in_=stg[b % 2][:], key=f"st{2 + b % 2}",
                      reads=[('stg', b % 2)], writes=[('xT_d', b)])
    P.barrier()


Net.prologue = _prologue


def _pass_mla(self, l):
    P, nc = self.P, self.nc
    ps = self.ps
    with ExitStack() as es:
        sb = lambda name, shape, dt: es.enter_context(nc.sbuf_tensor(_uid(name), shape, dt))
        Wmla = sb("Wmla", [128, 8, 640], BF16)
        Wkr = sb("Wkr", [128, 8, 128], BF16)
        WgA = sb("WgA", [128, 8, D], BF16)
        wuq = sb("wuq", [128, 3, H * 192], BF16)
        wuqsw = sb("wuqsw", [128, 3, H, 64], BF16)
        wukv = sb("wukv", [128, 2, H, 256], BF16)
        wukT = sb("wukT", [128, H, 256], BF16)
        wpa = sb("wpa", [128, 8, D], BF16)
        gq = sb("gq", [128, QR], F32)
        gkv = sb("gkv", [128, KVR], F32)
        w_in = self.w_in
        rows = lambda kc: slice(kc * 128, (kc + 1) * 128)
        self.load_w(lambda kc: Wmla[:, kc, :], lambda kc: w_in[l, rows(kc), 0:640], 'Wmla', 8)
        self.load_w(lambda kc: Wkr[:, kc, 0:64], lambda kc: w_in[l, rows(kc), C_KR:C_KR + 64], 'Wkr', 8)
        self.load_w(lambda kc: wuq[:, kc, :], lambda kc: self.w_uq[l, rows(kc), :], 'wuq', 3)
        self.load_w(lambda kc: wukv[:, kc, :, :], lambda kc: self.w_ukv[l, rows(kc), :, :], 'wukv', 2)
        P.dma('sp', out=gq[:], in_=self.q_norm[l, :].partition_broadcast(128), key="ld0", writes=['gq'])
        P.dma('sp', out=gkv[:], in_=self.kv_norm[l, :].partition_broadcast(128), key="ld1", writes=['gkv'])
        self.load_w(lambda kc: wpa[:, kc, :], lambda kc: self.w_proj_a[l, rows(kc), :], 'wpa', 8)
        self.load_w(lambda kc: WgA[:, kc, :], lambda kc: w_in[l, rows(kc), C_G:C_G + D], 'WgA', 8)
        for kc in range(8):
            P.op('dve', lambda kc=kc: nc.vector.tensor_copy(Wkr[:, kc, 64:96], Wkr[:, kc, 32:64]),
                 reads=[('Wkr', kc)], writes=[('Wkrs', kc)])
            P.op('dve', lambda kc=kc: nc.vector.tensor_copy(Wkr[:, kc, 96:128], Wkr[:, kc, 0:32]),
                 reads=[('Wkr', kc)], writes=[('Wkrs', kc)])
        for kc in range(3):
            v = wuq[:, kc, :].rearrange("p (h c) -> p h c", h=H)
            P.op('dve', lambda kc=kc, v=v: nc.vector.tensor_copy(wuqsw[:, kc, :, 0:32], v[:, :, 160:192]),
                 reads=[('wuq', kc)], writes=[('wuqsw', kc)])
            P.op('dve', lambda kc=kc, v=v: nc.vector.tensor_copy(wuqsw[:, kc, :, 32:64], v[:, :, 128:160]),
                 reads=[('wuq', kc)], writes=[('wuqsw', kc)])
        for h in range(H):
            for cc in range(2):
                P.op('pe', lambda h=h, cc=cc: nc.tensor.transpose(self.psb[:, cc * 128:(cc + 1) * 128],
                                                                   wukv[:, cc, h, 0:128], self.ident_bf[:]),
                     reads=[('wukv', cc)], writes=['psb'], inc=(cc == 1))
            P.op('act', lambda h=h: nc.scalar.copy(wukT[:, h, :], self.psb[:, 0:256]), reads=['psb'], writes=[('wukT', h)])

        ckvT = sb("ckvT", [128, 2, T], BF16)
        krT = sb("krT", [64, T], BF16)
        ckvTM = sb("ckvTM", [128, NT, KVR], BF16)
        xblk = [sb(f"xblk{i}", [128, 8, TB], BF16) for i in range(2)]
        cos1 = sb("cosb", [64, TB], F32)
        sin1 = sb("sinb", [64, TB], F32)
        cosb = [cos1, cos1]
        sinb = [sin1, sin1]
        cqTM2 = [sb(f"cqTM{i}", [128, QR], BF16) for i in range(2)]
        cqT = sb("cqT", [128, 3, TB], BF16)
        junk = sb("junk", [128, 512], F32)
        ssA = [sb(f"ss{i}", [128, 8], F32) for i in range(2)]
        qn = [sb(f"qn{i}", [128, TB], BF16) for i in range(2)]
        qlat = sb("qlat", [128, H, 2, TB], BF16)
        qrope = sb("qrope", [64, H, TB], BF16)
        rtmp = sb("rtmp", [64, TB], F32)
        rtmp2 = sb("rtmp2", [64, TB], F32)
        PT = [sb(f"PT{i}", [128, TB], BF16) for i in range(3)]
        rs = sb("rs", [128, TB], F32)
        olat = [sb(f"olat{i}", [128, 2, TB], BF16) for i in range(2)]
        oT2 = [sb(f"oT{i}", [128, H, TB], BF16) for i in range(2)]
        sig2 = [sb(f"sig{i}", [128, TB], F32) for i in range(2)]
        mT1 = sb("mT", [128, 8, TB], BF16)
        mT2 = [mT1, mT1]
        W = lambda name, n: _toks(name, n)
        rot = [0]

        def nb():
            i = rot[0] % 7
            rot[0] += 1
            return ps[i], f'ps{i}'


        def outproj(b, i2, xb, t0):
            oT, mT = oT2[i2], mT2[i2]
            for dc in range(8):
                dd = slice(dc * 128, (dc + 1) * 128)
                sg = sig2[dc % 2]
                P.mm(ps[5][:, :], [(WgA[:, kc, dd], xb[:, kc, :]) for kc in range(8)], 'ps5',
                     [('xblk', i2)] + W('WgA', 8))
                P.op('act', lambda: nc.scalar.activation(out=sg[:], in_=ps[5][:, :], func=AF.Sigmoid),
                     reads=['ps5'], writes=[('sig', dc % 2)])
                P.mm(ps[6][:, :], [(wpa[:, hh, dd], oT[:, hh, :]) for hh in range(H)], 'ps6',
                     [('oT', i2, hh) for hh in range(H)] + W('wpa', 8))
                P.op('dve', lambda: nc.vector.tensor_tensor(mT[:, dc, :], ps[6][:, :], sg[:], op=ALU.mult),
                     reads=['ps6', ('sig', dc % 2)], writes=[('mT', dc)])
                yield
            P.dma('sp', out=self.mA_d[:, :, t0:t0 + TB], in_=mT[:], key=f"st{i2}",
                  reads=[('mT', dc) for dc in range(8)], writes=[('mA_d', b)])
            yield

        bg = None
        for b in range(NB):
            i2 = b % 2
            t0 = b * TB
            P.dma('sp', out=xblk[i2][:], in_=self.xT_d[:, :, t0:t0 + TB], key=f"ld{2 + i2}", writes=[('xblk', i2)])
            P.dma('sp', out=cosb[i2][:], in_=self.cos_d[:, t0:t0 + TB], key=f"ld{4 + i2}", writes=['cosb'])
            P.dma('sp', out=sinb[i2][:], in_=self.sin_d[:, t0:t0 + TB], key=f"ld{6 + i2}", writes=['sinb'])
            xb = xblk[i2]
            tmbank = {}

            def tmA(tt):
                cols = slice(tt * 128, (tt + 1) * 128)
                pA, tA = nb()
                pB, tB = nb()
                tmbank[tt] = (pA, tA, pB, tB)
                P.mm(pA[:, 0:512], [(xb[:, kc, cols], Wmla[:, kc, 0:512]) for kc in range(8)], tA,
                     [('xblk', i2)] + W('Wmla', 8))
                P.mm(pB[:, 0:128], [(xb[:, kc, cols], Wmla[:, kc, 512:640]) for kc in range(8)], tB,
                     [('xblk', i2)] + W('Wmla', 8))

            def tmB(tt):
                t = b * 4 + tt
                p = tt % 2
                pA, tA, pB, tB = tmbank[tt]
                s_ = ssA[p]
                cq = cqTM2[p]
                K = lambda k: ('ss', p, k)
                P.op('act', lambda: nc.scalar.activation(out=junk[:, 0:384], in_=pA[:, 0:384], func=AF.Square,
                                                         accum_out=s_[:, 0:1]), reads=[tA], writes=['junk', K(0)])
                P.op('act', lambda: nc.scalar.activation(out=junk[:, 384:512], in_=pA[:, 384:512], func=AF.Square,
                                                         accum_out=s_[:, 1:2]), reads=[tA], writes=['junk', K(1)])
                P.op('act', lambda: nc.scalar.activation(out=junk[:, 0:128], in_=pB[:, 0:128], func=AF.Square,
                                                         accum_out=s_[:, 2:3]), reads=[tB], writes=['junk', K(2)])
                P.op('dve', lambda: nc.vector.tensor_scalar(s_[:, 4:5], s_[:, 0:1], 1.0 / QR, RMS_EPS, op0=ALU.mult, op1=ALU.add),
                     reads=[K(0)], writes=[K(4)])
                P.op('dve', lambda: nc.vector.tensor_tensor(s_[:, 5:6], s_[:, 1:2], s_[:, 2:3], op=ALU.add),
                     reads=[K(1), K(2)], writes=[K(5)])
                P.op('dve', lambda: nc.vector.tensor_scalar(s_[:, 5:6], s_[:, 5:6], 1.0 / KVR, RMS_EPS, op0=ALU.mult, op1=ALU.add),
                     reads=[K(5)], writes=[K(5)])
                P.op('act', lambda: nc.scalar.activation(out=s_[:, 6:8], in_=s_[:, 4:6], func=AF.Sqrt), reads=[K(4), K(5)], writes=[K(6)])
                P.op('dve', lambda: nc.vector.reciprocal(s_[:, 4:6], s_[:, 6:8]), reads=[K(6)], writes=[K(4), K(5)])
                P.op('dve', lambda: nc.vector.scalar_tensor_tensor(out=cq[:], in0=pA[:, 0:384], scalar=s_[:, 4:5],
                                                                   in1=gq[:], op0=ALU.mult, op1=ALU.mult),
                     reads=[tA, K(4), 'gq'], writes=[('cqTM', p)])
                P.op('dve', lambda: nc.vector.scalar_tensor_tensor(out=ckvTM[:, t, 0:128], in0=pA[:, 384:512],
                                                                   scalar=s_[:, 5:6], in1=gkv[:, 0:128],
                                                                   op0=ALU.mult, op1=ALU.mult),
                     reads=[tA, K(5), 'gkv'], writes=[('ckvTM', t)])
                P.op('dve', lambda: nc.vector.scalar_tensor_tensor(out=ckvTM[:, t, 128:256], in0=pB[:, 0:128],
                                                                   scalar=s_[:, 5:6], in1=gkv[:, 128:256],
                                                                   op0=ALU.mult, op1=ALU.mult),
                     reads=[tB, K(5), 'gkv'], writes=[('ckvTM', t)])

            def tmC(tt):
                t = b * 4 + tt
                p = tt % 2
                cols = slice(tt * 128, (tt + 1) * 128)
                cq = cqTM2[p]
                for j in range(3):
                    P.op('pe', lambda j=j: nc.tensor.transpose(self.psb[:, j * 128:(j + 1) * 128],
                                                               cq[:, j * 128:(j + 1) * 128], self.ident_bf[:]),
                         reads=[('cqTM', p)], writes=['psb'], inc=False)
                for j in range(2):
                    P.op('pe', lambda j=j: nc.tensor.transpose(self.psb[:, (3 + j) * 128:(4 + j) * 128],
                                                               ckvTM[:, t, j * 128:(j + 1) * 128], self.ident_bf[:]),
                         reads=[('ckvTM', t)], writes=['psb'], inc=(j == 1))
                P.op('act', lambda: nc.scalar.copy(cqT[:, :, cols], self.psb[:, 0:384].rearrange("p (k n) -> p k n", k=3)),
                     reads=['psb'], writes=['cqT'])
                P.op('act', lambda: nc.scalar.copy(ckvT[:, :, t * 128:(t + 1) * 128],
                                                   self.psb[:, 384:640].rearrange("p (k n) -> p k n", k=2)),
                     reads=['psb'], writes=[('ckvT', t)])

            for step in range(6):
                if step < 4:
                    tmA(step)
                if 0 <= step - 1 < 4:
                    tmB(step - 1)
                if 0 <= step - 2 < 4:
                    tmC(step - 2)
            pA, tA = nb()
            pB, tB = nb()
            P.mm(pA[0:64, :], [(Wkr[:, kc, 0:64], xb[:, kc, :]) for kc in range(8)], tA,
                 [('xblk', i2)] + W('Wkr', 8))
            P.mm(pB[0:64, :], [(Wkr[:, kc, 64:128], xb[:, kc, :]) for kc in range(8)], tB,
                 [('xblk', i2)] + W('Wkrs', 8))

            def rope(psA, psB, outap, rA, rB, wtok):
                P.op('dve', lambda: nc.vector.tensor_tensor(rtmp[:], psB, sinb[i2][:], op=ALU.mult),
                     reads=[rB, 'sinb'], writes=['rtmp'])
                P.op('dve', lambda: nc.vector.tensor_tensor(rtmp2[:], psA, cosb[i2][:], op=ALU.mult),
                     reads=[rA, 'cosb'], writes=['rtmp2'])
                P.op('dve', lambda: nc.vector.tensor_tensor(outap, rtmp[:], rtmp2[:], op=ALU.add),
                     reads=['rtmp', 'rtmp2'], writes=[wtok])
            rope(pA[0:64, :], pB[0:64, :], krT[:, t0:t0 + TB], tA, tB, ('krT', b))

            for h in range(H):
                c0 = h * 192
                pq, tq_ = nb()
                P.mm(pq[:, :], [(wuq[:, kc, c0:c0 + 128], cqT[:, kc, :]) for kc in range(3)], tq_,
                     ['cqT'] + W('wuq', 3))
                P.op('act', lambda pq=pq, h=h: nc.scalar.copy(qn[h % 2][:], pq[:, :]), reads=[tq_], writes=[('qn', h % 2)])
                pa_, ta_ = nb()
                pb_, tb_ = nb()
                P.mm(pa_[0:64, :], [(wuq[:, kc, c0 + 128:c0 + 192], cqT[:, kc, :]) for kc in range(3)], ta_,
                     ['cqT'] + W('wuq', 3))
                P.mm(pb_[0:64, :], [(wuqsw[:, kc, h, :], cqT[:, kc, :]) for kc in range(3)], tb_,
                     ['cqT'] + W('wuqsw', 3))
                for cc in range(2):
                    pl, tl_ = nb()
                    P.mm(pl[:, :], [(wukT[:, h, cc * 128:(cc + 1) * 128], qn[h % 2][:])], tl_, [('qn', h % 2), ('wukT', h)])
                    P.op('dve' if cc == 0 else 'act',
                         (lambda h=h, cc=cc, pl=pl: nc.vector.tensor_copy(qlat[:, h, cc, :], pl[:, :])) if cc == 0 else
                         (lambda h=h, cc=cc, pl=pl: nc.scalar.copy(qlat[:, h, cc, :], pl[:, :])),
                         reads=[tl_], writes=[('qlat', h)])
                rope(pa_[0:64, :], pb_[0:64, :], qrope[:, h, :], ta_, tb_, ('qrope', h))

            nkt = 4 * b + 4
            items = [(h, kt) for h in range(H) for kt in range(nkt)]

            def stage_S(i):
                h, kt = items[i]
                d = kt - 4 * b
                q0 = max(d, 0) * 128
                qs = slice(q0, TB)
                si, pi = i % 2, i % 3
                st = ps[si]
                kk = slice(kt * 128, (kt + 1) * 128)
                P.mm(st[:, qs], [(ckvT[:, 0, kk], qlat[:, h, 0, qs]), (ckvT[:, 1, kk], qlat[:, h, 1, qs]),
                                 (krT[:, kk], qrope[:, h, qs])], ('ST', si),
                     [('ckvT', kt), ('krT', kt // 4), ('qlat', h), ('qrope', h)])
                P.op('act', lambda: nc.scalar.activation(out=PT[pi][:, qs], in_=st[:, qs], func=AF.Exp, scale=SCALE_A),
                     reads=[('ST', si)], writes=[('PT', pi)])
                if d >= 0:
                    dq = slice(q0, q0 + 128)
                    P.op('pool', lambda: nc.gpsimd.tensor_tensor(PT[pi][:, dq], PT[pi][:, dq], self.mask_le[:], op=ALU.mult),
                         reads=[('PT', pi)], writes=[('PT', pi)])

            def stage_V(i):
                h, kt = items[i]
                d = kt - 4 * b
                q0 = max(d, 0) * 128
                qs = slice(q0, TB)
                pi = i % 3
                first, last = (kt == 0), (kt == nkt - 1)
                for cc in range(2):
                    P.op('pe', lambda cc=cc: nc.tensor.matmul(
                        ps[2 + cc][:, qs], ckvTM[:, kt, cc * 128:(cc + 1) * 128], PT[pi][:, qs], start=first, stop=last),
                         reads=[('PT', pi), ('ckvTM', kt)], writes=[('oacc', cc)], inc=False)
                P.op('pe', lambda: nc.tensor.matmul(ps[4][:, qs], self.ones_bf[:], PT[pi][:, qs], start=first, stop=last),
                     reads=[('PT', pi)], writes=['osum'], inc=True)
                P._record(('e', 'pe', P.cnt['pe']), [('PT', pi), ('ckvTM', kt)], [('oacc', 0), ('oacc', 1)])
                if last:
                    ol = olat[h % 2]
                    P.op('dve', lambda: nc.vector.reciprocal(rs[:], ps[4][:, :]), reads=['osum'], writes=['rs'])
                    for cc in range(2):
                        P.op('dve', lambda cc=cc: nc.vector.tensor_tensor(ol[:, cc, :], ps[2 + cc][:, :], rs[:], op=ALU.mult),
                             reads=[('oacc', cc), 'rs'], writes=[('olat', h % 2)])

            def proj_o(h):
                ol = olat[h % 2]
                P.mm(ps[5][:, :], [(wukv[:, cc, h, 128:256], ol[:, cc, :]) for cc in range(2)], 'ps5',
                     [('olat', h % 2)] + W('wukv', 2))
                P.op('act', lambda: nc.scalar.copy(oT2[i2][:, h, :], ps[5][:, :]), reads=['ps5'], writes=[('oT', i2, h)])

            pending = []
            if os.environ.get('MLA_ABL'):
                items = items[:int(os.environ['MLA_ABL'])]
            every = max(len(items) // 9, 1)
            stage_S(0)
            for i in range(len(items)):
                if i + 1 < len(items):
                    stage_S(i + 1)
                stage_V(i)
                h, kt = items[i]
                if kt == nkt - 1:
                    pending.append((i + 2, h))
                while pending and pending[0][0] <= i:
                    proj_o(pending.pop(0)[1])
                if bg is not None and i % every == every - 1:
                    next(bg, None)
            for _, h in pending:
                proj_o(h)
            if bg is not None:
                for _ in bg:
                    pass
            bg = outproj(b, i2, xb, t0)
        for _ in bg:
            pass

    P.barrier()


Net.pass_mla = _pass_mla


def _pass_mem(self, l):
    P, nc = self.P, self.nc
    ps = self.ps
    SC = 256 ** -0.5
    with ExitStack() as es:
        sb = lambda name, shape, dt: es.enter_context(nc.sbuf_tensor(_uid(name), shape, dt))
        Wqm = sb("Wqm", [128, 8, D], BF16)
        WgC = sb("WgC", [128, 8, D], BF16)
        wpc = sb("wpc", [128, 8, D], BF16)
        KT = sb("KT", [128, 4, 2, 256], BF16)
        V = sb("V", [128, 2, D], BF16)
        rows = lambda kc: slice(kc * 128, (kc + 1) * 128)
        W = lambda name, n: _toks(name, n)
        self.load_w(lambda kc: Wqm[:, kc, :], lambda kc: self.w_in[l, rows(kc), C_QM:C_QM + D], 'Wqm', 8)
        self.load_w(lambda kc: WgC[:, kc, :], lambda kc: self.w_in[l, rows(kc), C_G + 2 * D:C_G + 3 * D], 'WgC', 8)
        self.load_w(lambda kc: wpc[:, kc, :], lambda kc: self.w_proj_c[l, rows(kc), :], 'wpc', 8)
        with ExitStack() as es2:
            sb2 = lambda name, shape, dt: es2.enter_context(nc.sbuf_tensor(_uid(name), shape, dt))
            wmkv = sb2("wmkv", [128, 8, 2048], BF16)
            memT = sb2("memT", [128, 8, 256], BF16)
            mtile = sb2("mtile", [128, D], BF16)
            self.load_w(lambda kc: wmkv[:, kc, :], lambda kc: self.w_mem_kv[l, rows(kc), :], 'wmkv', 8)
            for mt in range(2):
                P.dma('pool', out=mtile[:], in_=self.mem_in[mt * 128:(mt + 1) * 128, :], key="ld0", writes=['mtile'])
                for kc in range(8):
                    P.op('pe', lambda kc=kc: nc.tensor.transpose(self.psb[:, kc * 128:(kc + 1) * 128],
                                                                 mtile[:, kc * 128:(kc + 1) * 128], self.ident_bf[:]),
                         reads=['mtile'], writes=['psb'], inc=(kc == 7))
                P.op('act', lambda mt=mt: nc.scalar.copy(memT[:, :, mt * 128:(mt + 1) * 128],
                                                         self.psb[:].rearrange("p (k n) -> p k n", k=8)),
                     reads=['psb'], writes=['memT'])
            for h in range(4):
                for dc in range(2):
                    c0 = h * 256 + dc * 128
                    P.mm(ps[5][:, 0:256], [(wmkv[:, kc, c0:c0 + 128], memT[:, kc, :]) for kc in range(8)], 'ps5',
                         ['memT'] + W('wmkv', 8))
                    P.op('act', lambda h=h, dc=dc: nc.scalar.copy(KT[:, h, dc, :], ps[5][:, 0:256]), reads=['ps5'], writes=['KT'])
            for mt in range(2):
                for half in range(2):
                    c0 = 1024 + half * 512
                    P.mm(ps[6][:, :], [(memT[:, kc, mt * 128:(mt + 1) * 128], wmkv[:, kc, c0:c0 + 512]) for kc in range(8)],
                         'ps6', ['memT'] + W('wmkv', 8))
                    P.op('dve', lambda mt=mt, half=half: nc.vector.tensor_copy(V[:, mt, half * 512:(half + 1) * 512], ps[6][:, :]),
                         reads=['ps6'], writes=['V'])
            P.barrier()
        xblk = [sb(f"xblk{i}", [128, 8, TB], BF16) for i in range(2)]
        qm = sb("qm", [128, 2, TB], BF16)
        PT = [sb(f"PT{i}", [128, 2, TB], BF16) for i in range(2)]
        rs = sb("rs", [128, TB], F32)
        ocT = sb("ocT", [128, 8, TB], BF16)
        sig = sb("sig", [128, TB], F32)
        mT = sb("mT", [128, 8, TB], BF16)
        for b in range(NB):
            i2 = b % 2
            t0 = b * TB
            P.dma('sp', out=xblk[i2][:], in_=self.xT_d[:, :, t0:t0 + TB], key=f"ld{2 + i2}", writes=[('xblk', i2)])
            xb = xblk[i2]
            for h in range(4):
                pi = h % 2
                for dc in range(2):
                    c0 = h * 256 + dc * 128
                    pb, pt = (ps[5], 'ps5') if dc == 0 else (ps[6], 'ps6')
                    P.mm(pb[:, :], [(Wqm[:, kc, c0:c0 + 128], xb[:, kc, :]) for kc in range(8)], pt,
                         [('xblk', i2)] + W('Wqm', 8))
                    if dc == 0:
                        P.op('act', lambda pb=pb: nc.scalar.copy(qm[:, 0, :], pb[:, :]), reads=[pt], writes=[('qm', 0)])
                    else:
                        P.op('dve', lambda pb=pb: nc.vector.tensor_copy(qm[:, 1, :], pb[:, :]), reads=[pt], writes=[('qm', 1)])
                for mt in range(2):
                    P.mm(ps[mt][:, :], [(KT[:, h, dc, mt * 128:(mt + 1) * 128], qm[:, dc, :]) for dc in range(2)], ('ST', mt),
                         ['KT', ('qm', 0), ('qm', 1)])
                    P.op('act', lambda mt=mt, pi=pi: nc.scalar.activation(out=PT[pi][:, mt, :], in_=ps[mt][:, :], func=AF.Exp, scale=SC),
                         reads=[('ST', mt)], writes=[('PT', pi, mt)])
                for vc in range(2):
                    c0 = h * 256 + vc * 128
                    P.mm(ps[2 + vc][:, :], [(V[:, mt, c0:c0 + 128], PT[pi][:, mt, :]) for mt in range(2)], ('oacc', vc),
                         ['V', ('PT', pi, 0), ('PT', pi, 1)])
                P.mm(ps[4][:, :], [(self.ones_bf[:], PT[pi][:, mt, :]) for mt in range(2)], 'osum',
                     [('PT', pi, 0), ('PT', pi, 1)])
                P.op('dve', lambda: nc.vector.reciprocal(rs[:], ps[4][:, :]), reads=['osum'], writes=['rs'])
                for vc in range(2):
                    P.op('dve', lambda h=h, vc=vc: nc.vector.tensor_tensor(ocT[:, h * 2 + vc, :], ps[2 + vc][:, :], rs[:], op=ALU.mult),
                         reads=[('oacc', vc), 'rs'], writes=[('ocT', h * 2 + vc)])
            for dc in range(8):
                dd = slice(dc * 128, (dc + 1) * 128)
                P.mm(ps[5][:, :], [(WgC[:, kc, dd], xb[:, kc, :]) for kc in range(8)], 'ps5',
                     [('xblk', i2)] + W('WgC', 8))
                P.op('act', lambda: nc.scalar.activation(out=sig[:], in_=ps[5][:, :], func=AF.Sigmoid),
                     reads=['ps5'], writes=['sig'])
                P.mm(ps[6][:, :], [(wpc[:, j, dd], ocT[:, j, :]) for j in range(8)], 'ps6',
                     [('ocT', j) for j in range(8)] + W('wpc', 8))
                P.op('dve', lambda dc=dc: nc.vector.tensor_tensor(mT[:, dc, :], ps[6][:, :], sig[:], op=ALU.mult),
                     reads=['ps6', 'sig'], writes=[('mT', dc)])
            P.dma('sp', out=self.mC_d[:, :, t0:t0 + TB], in_=mT[:], key="st0",
                  reads=[('mT', dc) for dc in range(8)], writes=[('mC_d', b)])
    P.barrier()


Net.pass_mem = _pass_mem


def _pass_ssd_a(self, l):
    P, nc = self.P, self.nc
    ps = self.ps
    with ExitStack() as es:
        sb = lambda name, shape, dt: es.enter_context(nc.sbuf_tensor(_uid(name), shape, dt))
        Wx = sb("Wx", [128, 8, 4096], BF16)
        cw5 = sb("cw5", [5, 4096], F32)
        cwb = sb("cwb", [128, 32, 5], F32)
        rows = lambda kc: slice(kc * 128, (kc + 1) * 128)
        W = lambda name, n: _toks(name, n)
        self.load_w(lambda kc: Wx[:, kc, :], lambda kc: self.w_in[l, rows(kc), C_XBC:C_XBC + 4096], 'Wx', 8)
        P.dma('sp', out=cw5[0:4, :], in_=self.conv_w[l, :, :], key="ld0", writes=['cw5a'])
        P.dma('sp', out=cw5[4:5, :], in_=self.conv_b[l:l + 1, :], key="ld1", writes=['cw5b'])
        for cc in range(32):
            P.op('pe', lambda cc=cc: nc.tensor.transpose(ps[5][:, cc * 5:(cc + 1) * 5], cw5[0:5, cc * 128:(cc + 1) * 128],
                                                         self.ident_f[0:5, 0:5]),
                 reads=['cw5a', 'cw5b'], writes=['ps5'], inc=(cc == 31))
        P.op('act', lambda: nc.scalar.copy(cwb[:].rearrange("p c k -> p (c k)"), ps[5][:, 0:160]), reads=['ps5'], writes=['cwb'])
        xblk = [sb(f"xblk{i}", [128, 8, TB], BF16) for i in range(2)]
        pre = [sb(f"pre{i}", [128, TB + 3], BF16) for i in range(3)]
        carry = sb("carry", [128, 32, 3], BF16)
        xbcT = sb("xbcT", [128, 32, TB], BF16)
        tmst = [sb(f"tmst{i}", [128, 3072], BF16) for i in range(2)]
        dg = sb("dg", [128, 32, 4, 128], BF16)
        for cc in range(32):
            for k in range(4):
                P.op('dve', lambda cc=cc, k=k: nc.vector.tensor_scalar(dg[:, cc, k, :], self.ident_bf[:], cwb[:, cc, k:k + 1], None, op0=ALU.mult),
                     reads=['cwb'], writes=[('dg', cc)])
        P.op('dve', lambda: nc.vector.memset(carry[:], 0.0), writes=[('carry', cc) for cc in range(32)])
        n = 0
        rot = [0]

        def nb():
            i = rot[0] % 7
            rot[0] += 1
            return ps[i], f'ps{i}'

        for b in range(NB):
            i2 = b % 2
            t0 = b * TB
            P.dma('sp', out=xblk[i2][:], in_=self.xT_d[:, :, t0:t0 + TB], key=f"ld{2 + i2}", writes=[('xblk', i2)])
            xb = xblk[i2]
            for cc in range(32):
                j = n % 3
                n += 1
                pb, pt = nb()
                pr = pre[j]
                P.mm(pb[:, :], [(Wx[:, kc, cc * 128:(cc + 1) * 128], xb[:, kc, :]) for kc in range(8)], pt,
                     [('xblk', i2)] + W('Wx', 8))
                P.op('act', lambda pr=pr, cc=cc: nc.scalar.copy(pr[:, 0:3], carry[:, cc, :]),
                     reads=[('carry', cc)], writes=[('pre', j)])
                P.op('act', lambda pr=pr, pb=pb: nc.scalar.copy(pr[:, 3:TB + 3], pb[:, :]), reads=[pt], writes=[('pre', j)])
                P.op('act', lambda pr=pr, cc=cc: nc.scalar.copy(carry[:, cc, :], pr[:, TB:TB + 3]),
                     reads=[('pre', j)], writes=[('carry', cc)])
                pc, pct = nb()
                P.mm(pc[:, :], [(dg[:, cc, k, :], pr[:, k:k + TB]) for k in range(4)], pct, [('pre', j), ('dg', cc)])
                P.op('act', lambda pc=pc, cc=cc: nc.scalar.activation(out=xbcT[:, cc, :], in_=pc[:, :], func=AF.Silu, bias=cwb[:, cc, 4:5]),
                     reads=[pct, 'cwb'], writes=[('xbcT', cc)])
            P.dma('sp', out=self.BCT_d[:, :, t0:t0 + TB], in_=xbcT[:, 16:32, :], key="st0",
                  reads=[('xbcT', cc) for cc in range(16, 32)], writes=[('BCT_d', b)])
            for tt in range(4):
                t = b * 4 + tt
                ti = t % 2
                for r in range(3):
                    for q in range(8):
                        cc = r * 8 + q
                        P.op('pe', lambda cc=cc, q=q, tt=tt: nc.tensor.transpose(self.psb[:, q * 128:(q + 1) * 128],
                                                                                 xbcT[:, cc, tt * 128:(tt + 1) * 128], self.ident_bf[:]),
                             reads=[('xbcT', cc)], writes=['psb'], inc=(q == 7))
                    P.op('dve', lambda ti=ti, r=r: nc.vector.tensor_copy(tmst[ti][:, r * 1024:(r + 1) * 1024], self.psb[:, :]),
                         reads=['psb'], writes=[('tmst', ti)])
                P.dma('sp', out=self.xsB_d[t * 128:(t + 1) * 128, :], in_=tmst[ti][:], key=f"st{1 + ti}",
                      reads=[('tmst', ti)], writes=[('xsB_d', t)])
    P.barrier()


def _pass_ssd_b(self, l):
    P, nc = self.P, self.nc
    ps = self.ps
    with ExitStack() as es:
        sb = lambda name, shape, dt: es.enter_context(nc.sbuf_tensor(_uid(name), shape, dt))
        rows = lambda kc: slice(kc * 128, (kc + 1) * 128)
        W = lambda name, n: _toks(name, n)
        Wz = sb("Wz", [128, 8, 2048], BF16)
        Wdt = sb("Wdt", [128, 8, 32], BF16)
        dtb = sb("dtb", [128, 32], F32)
        abc = sb("abc", [128, 32], F32)
        dsk = sb("dsk", [128, 32], F32)
        ngb = sb("ngb", [128, 2048], F32)
        self.load_w(lambda kc: Wz[:, kc, :], lambda kc: self.w_in[l, rows(kc), C_Z:C_Z + 2048], 'Wz', 8)
        self.load_w(lambda kc: Wdt[:, kc, :], lambda kc: self.w_in[l, rows(kc), C_DT:C_DT + 32], 'Wdt', 8)
        P.dma('sp', out=dtb[:], in_=self.dt_bias[l, :].partition_broadcast(128), key="ld0", writes=['dtb'])
        P.dma('sp', out=abc[:], in_=self.a_log[l, :].partition_broadcast(128), key="ld1", writes=['abc'])
        P.dma('sp', out=dsk[:], in_=self.d_skip[l, :].partition_broadcast(128), key="ld2", writes=['dsk'])
        P.dma('sp', out=ngb[:], in_=self.ssm_norm[l, :].partition_broadcast(128), key="ld3", writes=['ngb'])
        P.op('act', lambda: nc.scalar.activation(out=abc[:], in_=abc[:], func=AF.Exp), reads=['abc'], writes=['abc'])
        P.op('dve', lambda: nc.vector.tensor_scalar(abc[:], abc[:], -1.0, None, op0=ALU.mult), reads=['abc'], writes=['abc'])
        ST = sb("ST", [128, 8, 256], F32)
        STb = sb("STb", [128, 8, 256], BF16)
        P.op('dve', lambda: nc.vector.memset(ST[:], 0.0), writes=[('STATE', g) for g in range(8)])
        P.op('pool', lambda: nc.gpsimd.memset(STb[:], 0.0), writes=[('STb', g) for g in range(8)])
        xblk = [sb(f"xblk{i}", [128, 8, TB], BF16) for i in range(2)]
        bct = [sb(f"bct{i}", [128, 16, TB], BF16) for i in range(2)]
        xsB = [sb(f"xsB{i}", [128, 3072], BF16) for i in range(2)]
        sm = sb("sm", [128, 8, 32], F32)
        xdt = sb("xdt", [128, 2048], BF16)
        xdt2 = sb("xdt2", [128, 2048], BF16)
        dam = [sb(f"dam{i}", [128, 4, 128], BF16) for i in range(2)]
        dec = [sb(f"dec{i}", [128, 4, 128], F32) for i in range(2)]
        cbm = [sb(f"cbm{i}", [128, 128], F32) for i in range(2)]
        G = [sb(f"G{i}", [128, 4, 128], BF16) for i in range(2)]
        tmp = [sb(f"tmp{i}", [128, 256], F32) for i in range(2)]
        y = sb("y", [128, 2048], F32)
        zs = [sb(f"zs{i}", [128, 512], F32) for i in range(2)]
        junk = sb("junk", [128, 256], F32)
        ssq = sb("ssq", [128, 16], F32)
        yn = sb("yn", [128, 2048], BF16)
        ynT = [sb(f"ynT{i}", [128, 16, TB], BF16) for i in range(2)]
        v3 = lambda ap, h: ap.rearrange("p (h c) -> p h c", h=h)
        bc3 = lambda ap, n: ap.unsqueeze(2).to_broadcast([128, ap.shape[1], n])
        sm2 = [sm, sb("smB", [128, 8, 32], F32)]
        xdtA = [xdt, sb("xdtB", [128, 2048], BF16)]
        xdt2A = [xdt2, sb("xdt2B", [128, 2048], BF16)]
        yA = [y, sb("yB", [128, 2048], F32)]

        def load_blk(b):
            i2 = b % 2
            t0 = b * TB
            P.dma('sp', out=xblk[i2][:], in_=self.xT_d[:, :, t0:t0 + TB], key=f"ld{4 + i2}", writes=[('xblk', i2)])
            P.dma('sp', out=bct[i2][:], in_=self.BCT_d[:, :, t0:t0 + TB], key=f"ld{6 + i2}", writes=[('bct', i2)])

        def pre(t):
            b, tt = divmod(t, 4)
            i2 = b % 2
            ti = t % 2
            if tt == 0:
                load_blk(b)
            xb = xblk[i2]
            cols = slice(tt * 128, (tt + 1) * 128)
            xs = xsB[ti]
            sm = sm2[ti]
            S = lambda k: ('sm', ti, k)
            P.dma('sp', out=xs[:], in_=self.xsB_d[t * 128:(t + 1) * 128, :], key=f"ld{8 + ti}", writes=[('xsB', ti)])
            P.mm(ps[5][:, 0:32], [(xb[:, kc, cols], Wdt[:, kc, :]) for kc in range(8)], 'ps5', [('xblk', i2)] + W('Wdt', 8))
            yield
            P.op('dve', lambda: nc.vector.tensor_tensor(sm[:, 0, :], ps[5][:, 0:32], dtb[:], op=ALU.add),
                 reads=['ps5', 'dtb'], writes=[S(0)])
            P.op('dve', lambda: nc.vector.tensor_scalar(sm[:, 1, :], sm[:, 0, :], -1.0, None, op0=ALU.mult),
                 reads=[S(0)], writes=[S(1)])
            P.op('dve', lambda: nc.vector.tensor_tensor(sm[:, 1, :], sm[:, 1, :], sm[:, 0, :], op=ALU.min),
                 reads=[S(0), S(1)], writes=[S(1)])
            yield
            P.op('act', lambda: nc.scalar.activation(out=sm[:, 2, :], in_=sm[:, 1, :], func=AF.Exp),
                 reads=[S(1)], writes=[S(2)])
            P.op('act', lambda: nc.scalar.activation(out=sm[:, 2, :], in_=sm[:, 2, :], func=AF.Ln, bias=1.0),
                 reads=[S(2)], writes=[S(2)])
            yield
            P.op('dve', lambda: nc.vector.scalar_tensor_tensor(out=sm[:, 3, :], in0=sm[:, 0, :], scalar=0.0, in1=sm[:, 2, :],
                                                               op0=ALU.max, op1=ALU.add), reads=[S(0), S(2)], writes=[S(3)])
            P.op('dve', lambda: nc.vector.tensor_tensor(sm[:, 4, :], sm[:, 3, :], abc[:], op=ALU.mult),
                 reads=[S(3), 'abc'], writes=[S(4)])
            yield
            P.mm(ps[5][:, 64:96], [(self.tri_le_f[:], sm[:, 4, :])], 'ps5', [S(4)])
            P.mm(ps[5][:, 128:160], [(self.tri_gt_f[:], sm[:, 4, :])], 'ps5', [S(4)])
            P.mm(ps[5][:, 192:224], [(self.ones_f[:], sm[:, 4, :])], 'ps5', [S(4)])
            yield
            P.op('act', lambda: nc.scalar.activation(out=sm[:, 5, :], in_=ps[5][:, 64:96], func=AF.Exp), reads=['ps5'], writes=[S(5)])
            P.op('act', lambda: nc.scalar.activation(out=sm[:, 6, :], in_=ps[5][:, 128:160], func=AF.Exp), reads=['ps5'], writes=[S(6)])
            P.op('act', lambda: nc.scalar.activation(out=sm[:, 7, :], in_=ps[5][:, 192:224], func=AF.Exp), reads=['ps5'], writes=[S(7)])
            yield
            P.op('dve', lambda: nc.vector.tensor_tensor(v3(xdtA[ti][:], 32), v3(xs[:, 0:2048], 32), bc3(sm[:, 3, :], 64), op=ALU.mult),
                 reads=[('xsB', ti), S(3)], writes=[('xdt', ti)])
            P.op('pool', lambda: nc.gpsimd.tensor_tensor(v3(xdt2A[ti][:], 32), v3(xdtA[ti][:], 32), bc3(sm[:, 6, :], 64), op=ALU.mult),
                 reads=[('xdt', ti), S(6)], writes=[('xdt2', ti)])

        def grp(t, gens=()):
            b, tt = divmod(t, 4)
            i2 = b % 2
            ti = t % 2
            cols = slice(tt * 128, (tt + 1) * 128)
            xs = xsB[ti]
            sm = sm2[ti]
            S = lambda k: ('sm', ti, k)
            xdt_, xdt2_, y_ = xdtA[ti], xdt2A[ti], yA[ti]

            def stage1(g):
                gi = g % 2
                hs = slice(g * 4, (g + 1) * 4)
                P.op('pool', lambda: nc.gpsimd.tensor_tensor(
                    dam[gi][:], self.tri_gt_f[:, :].unsqueeze(1).to_broadcast([128, 4, 128]), bc3(sm[:, 4, hs], 128), op=ALU.mult),
                     reads=[S(4)], writes=[('dam', gi)])
                sp_, spt = (ps[0], ('SEG', 0)) if gi == 0 else (ps[1], ('SEG', 1))
                for r in range(4):
                    P.mm(sp_[:, r * 128:(r + 1) * 128], [(dam[gi][:, r, :], self.mask_le[:])], spt, [('dam', gi)])
                P.op('act', lambda: nc.scalar.activation(out=dec[gi][:].rearrange("p r s -> p (r s)"), in_=sp_[:, :], func=AF.Exp),
                     reads=[spt], writes=[('dec', gi)])
                cp_, cpt = (ps[2], ('CB', 0)) if gi == 0 else (ps[3], ('CB', 1))
                P.mm(cp_[:, 0:128], [(bct[i2][:, g, cols], bct[i2][:, 8 + g, cols])], cpt, [('bct', i2)])
                P.op('dve', lambda: nc.vector.tensor_tensor(cbm[gi][:], cp_[:, 0:128], self.tri_le_f[:], op=ALU.mult),
                     reads=[cpt], writes=[('cbm', gi)])
                P.op('dve', lambda: nc.vector.tensor_tensor(G[gi][:], dec[gi][:], cbm[gi][:, :].unsqueeze(1).to_broadcast([128, 4, 128]),
                                                            op=ALU.mult),
                     reads=[('dec', gi), ('cbm', gi)], writes=[('G', gi)])

            def stage2(g):
                gi = g % 2
                hs = slice(g * 4, (g + 1) * 4)
                cp_ = ps[2] if gi == 0 else ps[3]
                bp_ = ps[4] if gi == 0 else ps[6]
                for r in range(4):
                    hh = g * 4 + r
                    P.mm(bp_[:, r * 64:(r + 1) * 64],
                         [(G[gi][:, r, :], xdt_[:, hh * 64:(hh + 1) * 64])], ('YI', gi), [('G', gi), ('xdt', ti)])
                P.mm(bp_[:, 256:512], [(xs[:, 2048 + g * 128:2048 + (g + 1) * 128], xdt2_[:, g * 256:(g + 1) * 256])],
                     ('SU', gi), [('xsB', ti), ('xdt2', ti)])
                P.mm(cp_[:, 256:512], [(bct[i2][:, 8 + g, cols], STb[:, g, :])], ('YS', gi), [('bct', i2), ('STb', g)])
                P.op('dve', lambda: nc.vector.tensor_tensor(v3(tmp[gi][:], 4), v3(cp_[:, 256:512], 4), bc3(sm[:, 5, hs], 64), op=ALU.mult),
                     reads=[('YS', gi), S(5)], writes=[('tmp', gi)])
                P.op('dve', lambda: nc.vector.tensor_tensor(y_[:, g * 256:(g + 1) * 256], tmp[gi][:], bp_[:, 0:256], op=ALU.add),
                     reads=[('tmp', gi), ('YI', gi)], writes=[('y', ti, g)])
                P.op('pool', lambda: nc.gpsimd.tensor_tensor(v3(ST[:, g, :], 4), v3(ST[:, g, :], 4), bc3(sm[:, 7, hs], 64), op=ALU.mult),
                     reads=[S(7)], writes=[('STATE', g)])
                P.op('dve', lambda: nc.vector.tensor_tensor(ST[:, g, :], ST[:, g, :], bp_[:, 256:512], op=ALU.add),
                     reads=[('SU', gi)], writes=[('STATE', g)])
                P.op('act', lambda: nc.scalar.copy(STb[:, g, :], ST[:, g, :]), reads=[('STATE', g)], writes=[('STb', g)])

            if SSD_PIPE:
                stage1(0)
                for g in range(8):
                    if g + 1 < 8:
                        stage1(g + 1)
                    stage2(g)
                    for gen, k in gens:
                        for _ in range(k):
                            next(gen, None)
            else:
                for g in range(8):
                    stage1(g)
                    stage2(g)

        def post(t):
            b, tt = divmod(t, 4)
            i2 = b % 2
            ti = t % 2
            t0 = b * TB
            cols = slice(tt * 128, (tt + 1) * 128)
            xb = xblk[i2]
            xs = xsB[ti]
            xdt2_, y_ = xdt2A[ti], yA[ti]
            ally = [('y', ti, g) for g in range(8)]
            P.op('pool', lambda: nc.gpsimd.tensor_tensor(v3(xdt2_[:], 32), v3(xs[:, 0:2048], 32), bc3(dsk[:, :], 64), op=ALU.mult),
                 reads=[('xsB', ti), 'dsk'], writes=[('xdt2', ti)])
            P.op('dve', lambda: nc.vector.tensor_tensor(y_[:], y_[:], xdt2_[:], op=ALU.add), reads=ally + [('xdt2', ti)], writes=ally)
            return post_tail(t)

        def post_tail(t):
            b, tt = divmod(t, 4)
            i2 = b % 2
            ti = t % 2
            t0 = b * TB
            cols = slice(tt * 128, (tt + 1) * 128)
            xb = xblk[i2]
            y_ = yA[ti]
            ally = [('y', ti, g) for g in range(8)]
            for q in range(4):
                zi = q % 2
                qs = slice(q * 512, (q + 1) * 512)
                zp, zpt = (ps[0], ('SEG', 0)) if zi == 0 else (ps[1], ('SEG', 1))
                P.mm(zp[:, :], [(xb[:, kc, cols], Wz[:, kc, qs]) for kc in range(8)], zpt, [('xblk', i2)] + W('Wz', 8))
                P.op('act', lambda zi=zi, zp=zp: nc.scalar.activation(out=zs[zi][:], in_=zp[:, :], func=AF.Silu), reads=[zpt], writes=[('zs', zi)])
                P.op('dve', lambda zi=zi, qs=qs: nc.vector.tensor_tensor(y_[:, qs], y_[:, qs], zs[zi][:], op=ALU.mult),
                     reads=[('zs', zi)] + ally, writes=ally)
                yield
            for g in range(8):
                P.op('act', lambda g=g: nc.scalar.activation(out=junk[:], in_=y_[:, g * 256:(g + 1) * 256], func=AF.Square, accum_out=ssq[:, g:g + 1]),
                     reads=ally, writes=['junk', ('ssq', g)])
            allq = [('ssq', g) for g in range(8)]
            P.op('dve', lambda: nc.vector.tensor_scalar(ssq[:, 8:16], ssq[:, 0:8], 1.0 / 256, RMS_EPS, op0=ALU.mult, op1=ALU.add),
                 reads=allq, writes=['ssq8'])
            P.op('act', lambda: nc.scalar.activation(out=ssq[:, 8:16], in_=ssq[:, 8:16], func=AF.Sqrt), reads=['ssq8'], writes=['ssq8'])
            P.op('dve', lambda: nc.vector.reciprocal(ssq[:, 8:16], ssq[:, 8:16]), reads=['ssq8'], writes=['ssq8'])
            yield
            for g in range(8):
                gs = slice(g * 256, (g + 1) * 256)
                P.op('dve', lambda g=g, gs=gs: nc.vector.scalar_tensor_tensor(
                    out=yn[:, gs], in0=y_[:, gs], scalar=ssq[:, 8 + g:9 + g], in1=ngb[:, gs], op0=ALU.mult, op1=ALU.mult),
                     reads=ally + ['ssq8', 'ngb'], writes=[('yn', g)])
                if g % 4 == 3:
                    yield
            for r in range(2):
                for q in range(8):
                    P.op('pe', lambda r=r, q=q: nc.tensor.transpose(self.psb[:, q * 128:(q + 1) * 128],
                                                                    yn[:, (r * 8 + q) * 128:(r * 8 + q + 1) * 128], self.ident_bf[:]),
                         reads=[('yn', g) for g in range(8)], writes=['psb'], inc=(q == 7))
                P.op('act', lambda r=r: nc.scalar.copy(ynT[i2][:, r * 8:(r + 1) * 8, cols],
                                                       self.psb[:].rearrange("p (k n) -> p k n", k=8)),
                     reads=['psb'], writes=[('ynT', i2)])
                yield
            if tt == 3:
                P.dma('sp', out=self.ynT_d[:, :, t0:t0 + TB], in_=ynT[i2][:], key=f"st{i2}", reads=[('ynT', i2)], writes=[('ynT_d', b)])

        def drain(gen):
            if gen is not None:
                for _ in gen:
                    pass

        drain(pre(0))
        bgpost = None
        for t in range(NT):
            gpre = pre(t + 1) if t + 1 < NT else None
            gens = [(g_, k_) for g_, k_ in ((gpre, 1), (bgpost, 2)) if g_ is not None]
            grp(t, gens)
            drain(gpre)
            drain(bgpost)
            bgpost = post(t)
        drain(bgpost)
    P.barrier()


def _pass_ssd_c(self, l):
    P, nc = self.P, self.nc
    ps = self.ps
    with ExitStack() as es:
        sb = lambda name, shape, dt: es.enter_context(nc.sbuf_tensor(_uid(name), shape, dt))
        rows = lambda kc: slice(kc * 128, (kc + 1) * 128)
        W = lambda name, n: _toks(name, n)
        WgB = sb("WgB", [128, 8, D], BF16)
        wpb = sb("wpb", [128, 16, D], BF16)
        self.load_w(lambda kc: WgB[:, kc, :], lambda kc: self.w_in[l, rows(kc), C_G + D:C_G + 2 * D], 'WgB', 8)
        self.load_w(lambda kc: wpb[:, kc, :], lambda kc: self.w_proj_b[l, rows(kc), :], 'wpb', 16)
        xblk = [sb(f"xblk{i}", [128, 8, TB], BF16) for i in range(2)]
        ynb = [sb(f"ynb{i}", [128, 16, TB], BF16) for i in range(2)]
        sig = [sb(f"sig{i}", [128, TB], F32) for i in range(2)]
        mT = [sb(f"mT{i}", [128, 8, TB], BF16) for i in range(2)]
        for b in range(NB):
            i2 = b % 2
            t0 = b * TB
            P.dma('sp', out=xblk[i2][:], in_=self.xT_d[:, :, t0:t0 + TB], key=f"ld{2 + i2}", writes=[('xblk', i2)])
            P.dma('sp', out=ynb[i2][:], in_=self.ynT_d[:, :, t0:t0 + TB], key=f"ld{4 + i2}", writes=[('ynb', i2)])
            xb = xblk[i2]
            for dc in range(8):
                dd = slice(dc * 128, (dc + 1) * 128)
                si = dc % 2
                pa, pat = (ps[5], 'ps5') if si == 0 else (ps[3], 'ps3')
                pb, pbt = (ps[6], 'ps6') if si == 0 else (ps[4], 'ps4')
                P.mm(pa[:, :], [(WgB[:, kc, dd], xb[:, kc, :]) for kc in range(8)], pat, [('xblk', i2)] + W('WgB', 8))
                P.op('act', lambda si=si, pa=pa: nc.scalar.activation(out=sig[si][:], in_=pa[:, :], func=AF.Sigmoid),
                     reads=[pat], writes=[('sig', si)])
                P.mm(pb[:, :], [(wpb[:, j, dd], ynb[i2][:, j, :]) for j in range(16)], pbt, [('ynb', i2)] + W('wpb', 16))
                P.op('dve', lambda dc=dc, si=si, pb=pb: nc.vector.tensor_tensor(mT[i2][:, dc, :], pb[:, :], sig[si][:], op=ALU.mult),
                     reads=[pbt, ('sig', si)], writes=[('mT', i2)])
            P.dma('sp', out=self.mB_d[:, :, t0:t0 + TB], in_=mT[i2][:], key=f"st{i2}", reads=[('mT', i2)], writes=[('mB_d', b)])
    P.barrier()


Net.pass_ssd_a = _pass_ssd_a
Net.pass_ssd_b = _pass_ssd_b
Net.pass_ssd_c = _pass_ssd_c


def _layer_norm_tile(self, u, gbt, bbt, out, st, tag):
    P, nc = self.P, self.nc
    junk = self.ln_junk
    P.op('act', lambda: nc.scalar.activation(out=junk[:], in_=u, func=AF.Identity, accum_out=st[:, 0:1]),
         reads=[tag + 'u'], writes=['lnjunk', tag + 's0'])
    P.op('act', lambda: nc.scalar.activation(out=junk[:], in_=u, func=AF.Square, accum_out=st[:, 1:2]),
         reads=[tag + 'u'], writes=['lnjunk', tag + 's1'])
    P.op('dve', lambda: nc.vector.tensor_scalar(st[:, 2:4], st[:, 0:2], 1.0 / D, None, op0=ALU.mult),
         reads=[tag + 's0', tag + 's1'], writes=[tag + 's2'])
    P.op('dve', lambda: nc.vector.tensor_tensor(st[:, 4:5], st[:, 2:3], st[:, 2:3], op=ALU.mult), reads=[tag + 's2'], writes=[tag + 's4'])
    P.op('dve', lambda: nc.vector.tensor_tensor(st[:, 5:6], st[:, 3:4], st[:, 4:5], op=ALU.subtract), reads=[tag + 's2', tag + 's4'], writes=[tag + 's5'])
    P.op('dve', lambda: nc.vector.tensor_scalar(st[:, 5:6], st[:, 5:6], NORM_EPS, None, op0=ALU.add), reads=[tag + 's5'], writes=[tag + 's5'])
    P.op('act', lambda: nc.scalar.activation(out=st[:, 6:7], in_=st[:, 5:6], func=AF.Sqrt), reads=[tag + 's5'], writes=[tag + 's6'])
    P.op('dve', lambda: nc.vector.reciprocal(st[:, 7:8], st[:, 6:7]), reads=[tag + 's6'], writes=[tag + 's7'])
    P.op('dve', lambda: nc.vector.tensor_scalar(out, u, st[:, 2:3], st[:, 7:8], op0=ALU.subtract, op1=ALU.mult),
         reads=[tag + 'u', tag + 's2', tag + 's7'], writes=[tag + 'o'])
    P.op('pool', lambda: nc.gpsimd.tensor_tensor(out, out, gbt[:], op=ALU.mult), reads=[tag + 'g'], writes=[tag + 'o'])
    P.op('pool', lambda: nc.gpsimd.tensor_tensor(out, out, bbt[:], op=ALU.add), reads=[tag + 'g'], writes=[tag + 'o'])


def _pass_ln1(self, l):
    P, nc = self.P, self.nc
    ps = self.ps
    xa_src = self.x_in if l == 0 else self.xa_d
    with ExitStack() as es:
        sb = lambda name, shape, dt: es.enter_context(nc.sbuf_tensor(_uid(name), shape, dt))
        rows = lambda kc: slice(kc * 128, (kc + 1) * 128)
        W = lambda name, n: _toks(name, n)
        wout = sb("wout", [128, 8, D], BF16)
        rw = sb("rw", [128, 8, 16], F32)
        rb = sb("rb", [128, 16], F32)
        gbt = sb("gbt", [128, D], F32)
        bbt = sb("bbt", [128, D], F32)
        self.ln_junk = sb("lnjunk", [128, D], F32)
        self.load_w(lambda kc: wout[:, kc, :], lambda kc: self.w_out[l, rows(kc), :], 'wout', 8)
        for kc in range(8):
            P.dma('sp', out=rw[:, kc, :], in_=self.router_w[rows(kc), :], key="ld0", writes=[('rw', kc)])
        P.dma('sp', out=rb[:], in_=self.router_bias[0, :].partition_broadcast(128), key="ld1", writes=['rb'])
        P.dma('sp', out=gbt[:], in_=self.ln1_g[l, :].partition_broadcast(128), key="ld2", writes=['lg'])
        P.dma('sp', out=bbt[:], in_=self.ln1_b[l, :].partition_broadcast(128), key="ld3", writes=['lg'])
        mblk = [[sb(f"m{n}{i}", [128, 8, TB], BF16) for n in "ABC"] for i in range(2)]
        xa = [sb(f"xa{i}", [128, D], F32) for i in range(2)]
        u = [sb(f"u{i}", [128, D], F32) for i in range(2)]
        x1 = [sb(f"x1{i}", [128, D], F32) for i in range(2)]
        st = sb("st", [128, 8], F32)
        x1Tf = sb("x1Tf", [128, 8, 128], F32)
        stg = [sb(f"stg{i}", [128, 8, TB], BF16) for i in range(2)]
        r = sb("r", [128, 12, 16], F32)
        gts = [sb(f"gts{i}", [16, TB], F32) for i in range(2)]
        srcs = [self.mA_d, self.mB_d, self.mC_d]
        st2 = [st, sb("stB", [128, 8], F32)]

        def stA(t):
            b, tt = divmod(t, 4)
            i2, ti = b % 2, t % 2
            t0 = b * TB
            cols = slice(tt * 128, (tt + 1) * 128)
            tag = f'L1{ti}'
            if tt == 0:
                for n in range(3):
                    P.dma('sp', out=mblk[i2][n][:], in_=srcs[n][:, :, t0:t0 + TB], key=f"ld{4 + 3 * i2 + n}", writes=[('mblk', i2, n)])
            P.dma('sp', out=xa[ti][:], in_=xa_src[t * 128:(t + 1) * 128, :], key=f"ld{10 + ti}", writes=[('xa', ti)])
            for half in range(2):
                pb, pt = (ps[5], 'ps5') if half == 0 else (ps[6], 'ps6')
                P.mm(pb[:, :], [(mblk[i2][n][:, dc, cols], wout[:, dc, half * 512:(half + 1) * 512]) for n in range(3) for dc in range(8)],
                     pt, [('mblk', i2, n) for n in range(3)] + W('wout', 8))
                P.op('dve', lambda half=half, pb=pb: nc.vector.scalar_tensor_tensor(
                    out=u[ti][:, half * 512:(half + 1) * 512], in0=xa[ti][:, half * 512:(half + 1) * 512], scalar=DN_ALPHA, in1=pb[:, :],
                    op0=ALU.mult, op1=ALU.add), reads=[('xa', ti), pt], writes=[tag + 'u'])

        def stB(t):
            ti = t % 2
            tag = f'L1{ti}'
            self.layer_norm_tile(u[ti][:], gbt, bbt, x1[ti][:], st2[ti], tag)
            P._record(('e', 'pool', P.cnt['pool']), ['lg'], [])
            P.dma('sp', out=self.x1_d[t * 128:(t + 1) * 128, :], in_=x1[ti][:], key=f"st{ti}", reads=[tag + 'o'], writes=[('x1_d', t)])

        def stC(t):
            b, tt = divmod(t, 4)
            i2, ti = b % 2, t % 2
            t0 = b * TB
            cols = slice(tt * 128, (tt + 1) * 128)
            tag = f'L1{ti}'
            for kc in range(8):
                pb = ps[0] if kc < 4 else ps[1]
                P.op('pe', lambda kc=kc, pb=pb: nc.tensor.matmul(pb[:, (kc % 4) * 128:(kc % 4 + 1) * 128],
                                                                 x1[ti][:, kc * 128:(kc + 1) * 128], self.ident_f[:],
                                                                 start=True, stop=True),
                     reads=[tag + 'o'], writes=[('TP', kc // 4)], inc=(kc % 4 == 3))
            for hf in range(2):
                P.op('act', lambda hf=hf: nc.scalar.copy(x1Tf[:, hf * 4:(hf + 1) * 4, :], ps[hf][:].rearrange("p (k n) -> p k n", k=4)),
                     reads=[('TP', hf)], writes=['x1Tf'])
                P.op('dve', lambda hf=hf: nc.vector.tensor_copy(stg[i2][:, hf * 4:(hf + 1) * 4, cols], x1Tf[:, hf * 4:(hf + 1) * 4, :]),
                     reads=['x1Tf'], writes=[('stg', i2)])
            P.mm(ps[2][:, 0:16], [(x1Tf[:, kc, :], rw[:, kc, :]) for kc in range(8)], 'ps2', ['x1Tf'] + W('rw', 8))
            R = lambda i: r[:, i, :]
            R4 = lambda i: r[:, i, :].rearrange("p (g e) -> p g e", g=4)
            P.op('act', lambda: nc.scalar.activation(out=R(0), in_=ps[2][:, 0:16], func=AF.Sigmoid), reads=['ps2'], writes=['r0'])
            P.op('dve', lambda: nc.vector.tensor_tensor(R(1), R(0), rb[:], op=ALU.add), reads=['r0', 'rb'], writes=['r1'])
            pairs = [(0, 1), (0, 2), (0, 3), (1, 2), (1, 3), (2, 3)]
            for pi_, (a_, b_) in enumerate(pairs):
                P.op('dve', lambda pi_=pi_, a_=a_, b_=b_: nc.vector.tensor_tensor(r[:, 2 + pi_ // 4, (pi_ % 4) * 4:(pi_ % 4) * 4 + 4],
                                                                                R4(1)[:, :, a_], R4(1)[:, :, b_], op=ALU.add),
                     reads=['r1'], writes=['r2'])
            P.op('dve', lambda: nc.vector.tensor_tensor(r[:, 4, 0:4], r[:, 2, 0:4], r[:, 2, 4:8], op=ALU.max), reads=['r2'], writes=['r4'])
            P.op('dve', lambda: nc.vector.tensor_tensor(r[:, 4, 4:8], r[:, 2, 8:12], r[:, 2, 12:16], op=ALU.max), reads=['r2'], writes=['r4'])
            P.op('dve', lambda: nc.vector.tensor_tensor(r[:, 4, 8:12], r[:, 3, 0:4], r[:, 3, 4:8], op=ALU.max), reads=['r2'], writes=['r4'])
            P.op('dve', lambda: nc.vector.tensor_tensor(r[:, 4, 0:4], r[:, 4, 0:4], r[:, 4, 4:8], op=ALU.max), reads=['r4'], writes=['r4'])
            P.op('dve', lambda: nc.vector.tensor_tensor(r[:, 4, 0:4], r[:, 4, 0:4], r[:, 4, 8:12], op=ALU.max), reads=['r4'], writes=['r4'])
            P.op('dve', lambda: nc.vector.tensor_reduce(out=r[:, 5, 0:1], in_=r[:, 4, 0:4], axis=AX.X, op=ALU.max), reads=['r4'], writes=['r5'])
            P.op('dve', lambda: nc.vector.tensor_scalar(r[:, 5, 4:8], r[:, 4, 0:4], r[:, 5, 0:1], None, op0=ALU.is_equal), reads=['r4', 'r5'], writes=['r5m'])
            P.op('dve', lambda: nc.vector.tensor_scalar(r[:, 5, 8:12], r[:, 5, 4:8], 1.0, 1e30, op0=ALU.subtract, op1=ALU.mult), reads=['r5m'], writes=['r5p'])
            P.op('dve', lambda: nc.vector.tensor_tensor(R4(6), R4(1), r[:, 5, 4:8].unsqueeze(2).to_broadcast([128, 4, 4]), op=ALU.mult),
                 reads=['r1', 'r5m'], writes=['r6'])
            P.op('dve', lambda: nc.vector.tensor_tensor(R4(6), R4(6), r[:, 5, 8:12].unsqueeze(2).to_broadcast([128, 4, 4]), op=ALU.add),
                 reads=['r6', 'r5p'], writes=['r6'])
            P.op('dve', lambda: nc.vector.tensor_reduce(out=r[:, 7, 0:1], in_=R(6), axis=AX.X, op=ALU.max), reads=['r6'], writes=['r7'])
            P.op('dve', lambda: nc.vector.tensor_scalar(R(8), R(6), r[:, 7, 0:1], None, op0=ALU.is_equal), reads=['r6', 'r7'], writes=['r8'])
            P.op('dve', lambda: nc.vector.scalar_tensor_tensor(out=R(9), in0=R(8), scalar=-1e30, in1=R(6), op0=ALU.mult, op1=ALU.add),
                 reads=['r8', 'r6'], writes=['r9'])
            P.op('dve', lambda: nc.vector.tensor_reduce(out=r[:, 7, 1:2], in_=R(9), axis=AX.X, op=ALU.max), reads=['r9'], writes=['r7b'])
            P.op('dve', lambda: nc.vector.tensor_scalar(R(10), R(9), r[:, 7, 1:2], None, op0=ALU.is_equal), reads=['r9', 'r7b'], writes=['r10'])
            P.op('dve', lambda: nc.vector.tensor_tensor(R(10), R(10), R(8), op=ALU.add), reads=['r10', 'r8'], writes=['r10'])
            P.op('dve', lambda: nc.vector.tensor_tensor(R(11), R(10), R(0), op=ALU.mult), reads=['r10', 'r0'], writes=['r11'])
            P.op('dve', lambda: nc.vector.tensor_reduce(out=r[:, 7, 2:3], in_=R(11), axis=AX.X, op=ALU.add), reads=['r11'], writes=['r7c'])
            P.op('dve', lambda: nc.vector.reciprocal(r[:, 7, 3:4], r[:, 7, 2:3]), reads=['r7c'], writes=['r7d'])
            P.op('dve', lambda: nc.vector.tensor_scalar(R(11), R(11), r[:, 7, 3:4], None, op0=ALU.mult), reads=['r11', 'r7d'], writes=['r11'])
            P.op('pe', lambda: nc.tensor.matmul(ps[3][0:16, 0:128], R(11), self.ident_f[:], start=True, stop=True), reads=['r11'], writes=['ps3'])
            P.op('act', lambda: nc.scalar.copy(gts[i2][:, cols], ps[3][0:16, 0:128]), reads=['ps3'], writes=[('gts', i2)])
            if tt == 3:
                P.dma('sp', out=self.x1T_d[:, :, t0:t0 + TB], in_=stg[i2][:], key=f"st{2 + i2}", reads=[('stg', i2)], writes=[('x1T_d', b)])
                P.dma('sp', out=self.gT_d[:, t0:t0 + TB], in_=gts[i2][:], key=f"st{4 + i2}", reads=[('gts', i2)], writes=[('gT_d', b)])

        for step in range(NT + 2):
            if step < NT:
                stA(step)
            if 0 <= step - 1 < NT:
                stB(step - 1)
            if 0 <= step - 2 < NT:
                stC(step - 2)
    P.barrier()


def _pass_moe(self, l, last):
    P, nc = self.P, self.nc
    ps = self.ps
    BB = 1024
    with ExitStack() as es:
        sb = lambda name, shape, dt: es.enter_context(nc.sbuf_tensor(_uid(name), shape, dt))
        rows = lambda kc: slice(kc * 128, (kc + 1) * 128)
        W = lambda name, n: _toks(name, n)
        gbt = sb("gbt", [128, D], F32)
        bbt = sb("bbt", [128, D], F32)
        self.ln_junk = sb("lnjunk", [128, D], F32)
        E16 = sb("E16", [16, 16, 128], F32)
        P.dma('sp', out=gbt[:], in_=self.ln2_g[l, :].partition_broadcast(128), key="ld2", writes=['lg'])
        P.dma('sp', out=bbt[:], in_=self.ln2_b[l, :].partition_broadcast(128), key="ld3", writes=['lg'])
        P.op('pool', lambda: nc.gpsimd.memset(E16[:], 0.0), writes=['E16'])
        P.op('pool', lambda: nc.gpsimd.affine_select(out=E16[:], in_=E16[:], pattern=[[-1, 16], [0, 128]], compare_op=ALU.not_equal,
                                                     fill=1.0, base=0, channel_multiplier=1), writes=['E16'])
        x1T = sb("x1T", [128, 8, BB], BF16)
        gT = sb("gT", [16, BB], F32)
        acc = sb("acc", [128, 8, D], F32)
        w1 = [sb(f"w1{i}", [128, 8, 512], BF16) for i in range(2)]
        w3 = [sb(f"w3{i}", [128, 8, 512], BF16) for i in range(2)]
        w2 = [sb(f"w2{i}", [128, 4, D], BF16) for i in range(2)]
        gb = [sb(f"gb{i}", [128, 512], F32) for i in range(2)]
        s1 = [sb(f"s1{i}", [128, 512], F32) for i in range(2)]
        tq = [sb(f"tq{i}", [128, 512], F32) for i in range(2)]
        hT = [sb(f"hT{i}", [128, 4, 512], BF16) for i in range(2)]
        x1t = [sb(f"x1t{i}", [128, D], F32) for i in range(2)]
        u = [sb(f"u{i}", [128, D], F32) for i in range(2)]
        x2 = [sb(f"x2{i}", [128, D], F32) for i in range(2)]
        x2b = [sb(f"x2b{i}", [128, D], BF16) for i in range(2)]
        st = sb("st", [128, 8], F32)
        st2 = [st, sb("stB", [128, 8], F32)]
        stg = sb("stg", [128, 8, BB], BF16)
        dst = self.y_out if last else self.xa_d
        for bb in range(T // BB):
            t0 = bb * BB
            P.dma('sp', out=x1T[:], in_=self.x1T_d[:, :, t0:t0 + BB], key="ld4", writes=['x1T'])
            P.dma('sp', out=gT[:], in_=self.gT_d[:, t0:t0 + BB], key="ld5", writes=['gT'])
            units = [(e, sub) for e in range(16) for sub in range(BB // 512)]

            def stage_F(ui):
                e, sub = units[ui]
                ei = (ebase + e) % 2
                hi = (ubase + ui) % 2
                if sub == 0:
                    self.load_w(lambda kc: w1[ei][:, kc, :], lambda kc: self.exp_w1[l, e, rows(kc), :], ('w1', ei), 8)
                    self.load_w(lambda kc: w3[ei][:, kc, :], lambda kc: self.exp_w3[l, e, rows(kc), :], ('w3', ei), 8)
                    self.load_w(lambda kc: w2[ei][:, kc, :], lambda kc: self.exp_w2[l, e, rows(kc), :], ('w2', ei), 4)
                sc = slice(sub * 512, (sub + 1) * 512)
                P.mm(ps[4][:, :], [(E16[:, e, :], gT[:, sc])], 'ps4', ['E16', 'gT'])
                P.op('act', lambda: nc.scalar.copy(gb[hi][:], ps[4][:, :]), reads=['ps4'], writes=[('gb', hi)])
                for fc in range(4):
                    fi = fc % 2
                    fs = slice(fc * 128, (fc + 1) * 128)
                    pa, pat = (ps[0], 'ps0') if fi == 0 else (ps[2], 'ps2')
                    pb, pbt = (ps[1], 'ps1') if fi == 0 else (ps[3], 'ps3')
                    P.mm(pa[:, :], [(w1[ei][:, kc, fs], x1T[:, kc, sc]) for kc in range(8)], pat, ['x1T'] + W(('w1', ei), 8))
                    P.mm(pb[:, :], [(w3[ei][:, kc, fs], x1T[:, kc, sc]) for kc in range(8)], pbt, ['x1T'] + W(('w3', ei), 8))
                    P.op('act', lambda fi=fi, pa=pa: nc.scalar.activation(out=s1[fi][:], in_=pa[:, :], func=AF.Silu), reads=[pat], writes=[('s1', fi)])
                    P.op('dve', lambda fi=fi, pb=pb: nc.vector.tensor_tensor(tq[fi][:], pb[:, :], gb[hi][:], op=ALU.mult),
                         reads=[pbt, ('gb', hi)], writes=[('tq', fi)])
                    P.op('dve', lambda fi=fi, fc=fc: nc.vector.tensor_tensor(hT[hi][:, fc, :], tq[fi][:], s1[fi][:], op=ALU.mult),
                         reads=[('tq', fi), ('s1', fi)], writes=[('hT', hi, fc)])

            def stage_S(ui):
                e, sub = units[ui]
                ei = (ebase + e) % 2
                hi = (ubase + ui) % 2
                for tt in range(4):
                    tl = sub * 4 + tt
                    cols = slice(tt * 128, (tt + 1) * 128)
                    for half in range(2):
                        pb, pt = (ps[5], 'ps5') if half == 0 else (ps[6], 'ps6')
                        hs = slice(half * 512, (half + 1) * 512)
                        P.mm(pb[:, :], [(hT[hi][:, fc, cols], w2[ei][:, fc, hs]) for fc in range(4)], pt,
                             [('hT', hi, fc) for fc in range(4)] + W(('w2', ei), 4))
                        if e == 0:
                            P.op('dve', lambda tl=tl, hs=hs, pb=pb: nc.vector.tensor_copy(acc[:, tl, hs], pb[:, :]),
                                 reads=[pt], writes=[('acc', tl, half)])
                        else:
                            P.op('dve', lambda tl=tl, hs=hs, pb=pb: nc.vector.tensor_tensor(acc[:, tl, hs], acc[:, tl, hs], pb[:, :], op=ALU.add),
                                 reads=[pt], writes=[('acc', tl, half)])

            ebase = bb * 16
            ubase = bb * len(units)
            stage_F(0)
            for ui in range(len(units)):
                if ui + 1 < len(units):
                    stage_F(ui + 1)
                stage_S(ui)
            ntl = BB // 128

            def eA(tl):
                t = bb * ntl + tl
                ti = t % 2
                tag = f'L2{ti}'
                P.dma('sp', out=x1t[ti][:], in_=self.x1_d[t * 128:(t + 1) * 128, :], key=f"ld{6 + ti}", writes=[('x1t', ti)])
                P.op('dve', lambda: nc.vector.scalar_tensor_tensor(
                    out=u[ti][:], in0=x1t[ti][:], scalar=DN_ALPHA, in1=acc[:, tl, :], op0=ALU.mult, op1=ALU.add),
                     reads=[('x1t', ti), ('acc', tl, 0), ('acc', tl, 1)], writes=[tag + 'u'])

            def eB(tl):
                t = bb * ntl + tl
                ti = t % 2
                tag = f'L2{ti}'
                self.layer_norm_tile(u[ti][:], gbt, bbt, x2[ti][:], st2[ti], tag)
                P._record(('e', 'pool', P.cnt['pool']), ['lg'], [])
                P.dma('sp', out=dst[t * 128:(t + 1) * 128, :], in_=x2[ti][:], key=f"st{ti}", reads=[tag + 'o'], writes=[('dst', t)])

            def eC(tl):
                t = bb * ntl + tl
                ti = t % 2
                tag = f'L2{ti}'
                if last:
                    return
                P.op('act', lambda: nc.scalar.copy(x2b[ti][:], x2[ti][:]), reads=[tag + 'o'], writes=[('x2b', ti)])
                for kc in range(8):
                    P.op('pe', lambda kc=kc: nc.tensor.transpose(self.psb[:, kc * 128:(kc + 1) * 128],
                                                                 x2b[ti][:, kc * 128:(kc + 1) * 128], self.ident_bf[:]),
                         reads=[('x2b', ti)], writes=['psb'], inc=(kc == 7))
                P.op('act', lambda: nc.scalar.copy(stg[:, :, tl * 128:(tl + 1) * 128],
                                                   self.psb[:].rearrange("p (k n) -> p k n", k=8)),
                     reads=['psb'], writes=['stg'])

            for step in range(ntl + 2):
                if step < ntl:
                    eA(step)
                if 0 <= step - 1 < ntl:
                    eB(step - 1)
                if 0 <= step - 2 < ntl:
                    eC(step - 2)
            if not last:
                P.dma('sp', out=self.xT_d[:, :, t0:t0 + BB], in_=stg[:], key="st2", reads=['stg'], writes=[('xT_d', bb)])
    P.barrier()


Net.layer_norm_tile = _layer_norm_tile
Net.pass_ln1 = _pass_ln1
Net.pass_moe = _pass_moe


def build(n_layers=DEPTH, stages=("mla", "ssd", "mem", "moe"), dbg=()):
    net = Net(n_layers)
    net.declare()
    P = net.P
    net.setup_globals()
    net.prologue()
    for l in range(n_layers):
        if "mla" in stages:
            net.pass_mla(l)
        if "mem" in stages:
            net.pass_mem(l)
        if "ssd" in stages or "ssda" in stages:
            net.pass_ssd_a(l)
        if "ssd" in stages or "ssdb" in stages:
            net.pass_ssd_b(l)
        if "ssd" in stages or "ssdc" in stages:
            net.pass_ssd_c(l)
        if "moe" in stages or "ln1" in stages:
            net.pass_ln1(l)
        if "moe" in stages:
            net.pass_moe(l, last=(l == n_layers - 1))
    for name in dbg:
        src = getattr(net, name)
        dst = net.dbg("dbg_" + name, src.shape, src.dtype)
        P.dma('sp', out=dst, in_=src, key="st0", reads=[], writes=[('dbg', name)])
    P.barrier()
    print("instructions:", P.ninst, "sems:", P.nsem, {e: P.cnt[e] for e in P.cnt}, "dmas:", getattr(P, 'ndma', 0), "descs~", getattr(P, 'ndesc', 0))
    return net


def make_inputs(inputs, b):
    m = {}
    for k, v in inputs.items():
        v = np.asarray(v)
        if k in ("x", "mem"):
            m[k] = np.ascontiguousarray(v[b])
        elif k == "positions":
            m[k] = np.ascontiguousarray(v[b:b + 1]).astype(np.int32)
        elif k == "router_bias":
            m[k] = np.ascontiguousarray(v.reshape(1, 16))
        else:
            m[k] = np.ascontiguousarray(v)
    inv = (10000.0 ** (-np.arange(0, 64, 2, dtype=np.float32) / 64)).astype(np.float32)
    m["inv_freq"] = np.concatenate([inv, inv]).reshape(64, 1).astype(np.float32)
    return m


def kernel(**inputs):
    net = build()
    in_maps = [make_inputs(inputs, b) for b in range(8)]
    res = run_bass_kernel_spmd(net.nc, in_maps, core_ids=list(range(8)))
    return np.stack([np.asarray(res.results[b]["y"]) for b in range(8)], axis=0).astype(np.float32)
```
